# Optimizing a Trainium2 kernel written in Bass

```python
import jax, jax.numpy as jnp
from jax import lax
import numpy as np

D_MODEL = 1024
BATCH = 2
SEQ = 8192
DEPTH = 1

HEAD_DIM = 64
N_HEADS_DSA = 8
N_HEADS_FOX = 8
WIDTH_DSA = N_HEADS_DSA * HEAD_DIM
WIDTH_FOX = N_HEADS_FOX * HEAD_DIM
N_IDX_HEADS = 4
IDX_DIM = 64
TOPK_MAX = 256
ROPE_THETA = 500000.0
ROPE_DIM = HEAD_DIM // 4
BLOCK_Q = 128
RMS_EPS = 1e-6
NEG = -1e30

SPLIT_SIZES = (
    WIDTH_DSA, WIDTH_DSA, WIDTH_DSA, WIDTH_DSA,
    N_IDX_HEADS * IDX_DIM, IDX_DIM, N_IDX_HEADS,
    WIDTH_FOX, WIDTH_FOX, WIDTH_FOX, WIDTH_FOX,
    N_HEADS_FOX,
    2 * D_MODEL,
)
N_IN = int(sum(SPLIT_SIZES))
SPLIT_POINTS = tuple(int(v) for v in np.cumsum(SPLIT_SIZES)[:-1])

kernel_name = "hybrid_dsa_fox_gated_parallel"


def rmsnorm(x, gain):
    x32 = x.astype(jnp.float32)
    y = x32 * lax.rsqrt(jnp.mean(x32 * x32, axis=-1, keepdims=True) + RMS_EPS)
    return (y * gain.astype(jnp.float32)).astype(x.dtype)


def rope_partial(t, positions):
    half = ROPE_DIM // 2
    inv_freq = ROPE_THETA ** (-jnp.arange(half, dtype=jnp.float32) * 2.0 / ROPE_DIM)
    ang = positions.astype(jnp.float32)[..., None] * inv_freq
    if t.ndim == 4:
        ang = ang[:, :, None, :]
    cos, sin = jnp.cos(ang), jnp.sin(ang)
    t32 = t.astype(jnp.float32)
    x1, x2, rest = t32[..., :half], t32[..., half:ROPE_DIM], t32[..., ROPE_DIM:]
    out = jnp.concatenate([x1 * cos - x2 * sin, x2 * cos + x1 * sin, rest], axis=-1)
    return out.astype(t.dtype)


def dsa_attention(q, k, v, q_idx, k_idx, w_idx):
    B, S, H, Dh = q.shape
    top_k = min(TOPK_MAX, S // 4)
    n_blocks = S // BLOCK_Q
    key_pos = jnp.arange(S)
    scale = Dh ** -0.5

    def block(i):
        start = i * BLOCK_Q
        qb = lax.dynamic_slice_in_dim(q, start, BLOCK_Q, axis=1)
        qib = lax.dynamic_slice_in_dim(q_idx, start, BLOCK_Q, axis=1)
        wib = lax.dynamic_slice_in_dim(w_idx, start, BLOCK_Q, axis=1)
        q_pos = start + jnp.arange(BLOCK_Q)
        causal = key_pos[None, :] <= q_pos[:, None]
        rel = jax.nn.relu(jnp.einsum('bqhd,bsd->bqhs', qib, k_idx).astype(jnp.float32))
        score = jnp.einsum('bqh,bqhs->bqs', wib.astype(jnp.float32), rel)
        score = jnp.where(causal[None], score, -jnp.inf)
        _, idx = lax.top_k(score, top_k)
        gather = jax.vmap(lambda arr, ii: arr[ii])
        k_sel = gather(k, idx)
        v_sel = gather(v, idx)
        logits = jnp.einsum('bqhd,bqkhd->bhqk', qb, k_sel).astype(jnp.float32) * scale
        valid = idx <= q_pos[None, :, None]
        logits = jnp.where(valid[:, None], logits, NEG)
        p = jax.nn.softmax(logits, axis=-1).astype(v.dtype)
        return jnp.einsum('bhqk,bqkhd->bqhd', p, v_sel)

    out = lax.map(block, jnp.arange(n_blocks))
    return out.transpose(1, 0, 2, 3, 4).reshape(B, S, H, Dh)


def fox_attention(q, k, v, log_f):
    B, S, H, Dh = q.shape
    n_blocks = S // BLOCK_Q
    key_pos = jnp.arange(S)
    scale = Dh ** -0.5
    cum = jnp.cumsum(log_f, axis=1).transpose(0, 2, 1)

    def block(i):
        start = i * BLOCK_Q
        qb = lax.dynamic_slice_in_dim(q, start, BLOCK_Q, axis=1)
        cq = lax.dynamic_slice_in_dim(cum, start, BLOCK_Q, axis=2)
        q_pos = start + jnp.arange(BLOCK_Q)
        causal = key_pos[None, :] <= q_pos[:, None]
        logits = jnp.einsum('bqhd,bshd->bhqs', qb, k).astype(jnp.float32) * scale
        logits = logits + (cq[..., :, None] - cum[..., None, :])
        logits = jnp.where(causal[None, None], logits, NEG)
        p = jax.nn.softmax(logits, axis=-1).astype(v.dtype)
        return jnp.einsum('bhqs,bshd->bqhd', p, v)

    out = lax.map(block, jnp.arange(n_blocks))
    return out.transpose(1, 0, 2, 3, 4).reshape(B, S, H, Dh)


def setup_inputs(seed: int = 0) -> dict:
    key = jax.random.key(seed)
    ks = jax.random.split(key, 10)
    x = jax.random.normal(ks[0], (BATCH, SEQ, D_MODEL), jnp.float32)
    positions = jnp.broadcast_to(jnp.arange(SEQ, dtype=jnp.int32), (BATCH, SEQ))
    norm_gain = 1.0 + 0.02 * jax.random.normal(ks[1], (DEPTH, D_MODEL), jnp.float32)
    w_in = jax.random.normal(ks[2], (DEPTH, D_MODEL, N_IN), jnp.float32) * D_MODEL ** -0.5
    b_forget = 2.0 + 0.1 * jax.random.normal(ks[3], (DEPTH, N_HEADS_FOX), jnp.float32)
    b_merge = 0.01 * jax.random.normal(ks[4], (DEPTH, 2 * D_MODEL), jnp.float32)
    w_branch_dsa = jax.random.normal(ks[5], (DEPTH, WIDTH_DSA, D_MODEL), jnp.float32) * WIDTH_DSA ** -0.5
    w_branch_fox = jax.random.normal(ks[6], (DEPTH, WIDTH_FOX, D_MODEL), jnp.float32) * WIDTH_FOX ** -0.5
    w_out = jax.random.normal(ks[7], (DEPTH, D_MODEL, D_MODEL), jnp.float32) * D_MODEL ** -0.5
    final_gain = 1.0 + 0.02 * jax.random.normal(ks[8], (D_MODEL,), jnp.float32)
    return {"x": x, "positions": positions, "norm_gain": norm_gain, "w_in": w_in,
            "b_forget": b_forget, "b_merge": b_merge, "w_branch_dsa": w_branch_dsa,
            "w_branch_fox": w_branch_fox, "w_out": w_out, "final_gain": final_gain}


def reference(x, positions, norm_gain, w_in, b_forget, b_merge, w_branch_dsa,
              w_branch_fox, w_out, final_gain):
    B, S, _ = x.shape
    for l in range(DEPTH):
        h = rmsnorm(x, norm_gain[l])
        proj = jnp.einsum('bsd,dn->bsn', h, w_in[l])
        (a_q, a_k, a_v, a_gate, i_q, i_k, i_w,
         f_q, f_k, f_v, f_gate, f_logit, m_logit) = jnp.split(proj, SPLIT_POINTS, axis=-1)

        a_q = rope_partial(a_q.reshape(B, S, N_HEADS_DSA, HEAD_DIM), positions)
        a_k = rope_partial(a_k.reshape(B, S, N_HEADS_DSA, HEAD_DIM), positions)
        a_v = a_v.reshape(B, S, N_HEADS_DSA, HEAD_DIM)
        i_q = rope_partial(i_q.reshape(B, S, N_IDX_HEADS, IDX_DIM), positions) * (IDX_DIM ** -0.5)
        i_k = rope_partial(i_k, positions)
        i_w = i_w * (N_IDX_HEADS ** -0.5)
        y_a = dsa_attention(a_q, a_k, a_v, i_q, i_k, i_w).reshape(B, S, WIDTH_DSA)
        u_a = jnp.einsum('bsw,wd->bsd', y_a * jax.nn.silu(a_gate), w_branch_dsa[l])

        f_q = f_q.reshape(B, S, N_HEADS_FOX, HEAD_DIM)
        f_k = f_k.reshape(B, S, N_HEADS_FOX, HEAD_DIM)
        f_v = f_v.reshape(B, S, N_HEADS_FOX, HEAD_DIM)
        log_f = jax.nn.log_sigmoid((f_logit + b_forget[l]).astype(jnp.float32))
        y_b = fox_attention(f_q, f_k, f_v, log_f).reshape(B, S, WIDTH_FOX)
        u_b = jnp.einsum('bsw,wd->bsd', y_b * jax.nn.silu(f_gate), w_branch_fox[l])

        gates = jax.nn.sigmoid(m_logit + b_merge[l])
        g_a, g_b = gates[..., :D_MODEL], gates[..., D_MODEL:]
        merged = g_a * u_a + g_b * u_b
        x = x + jnp.einsum('bsd,de->bse', merged, w_out[l])
    return rmsnorm(x, final_gain)
```

```python
import math
import contextlib
import numpy as np
import concourse.bass as bass
import concourse.mybir as mybir
from concourse.bass_utils import run_bass_kernel_spmd

F32 = mybir.dt.float32
BF16 = mybir.dt.bfloat16
I32 = mybir.dt.int32
U8 = mybir.dt.uint8
AF = mybir.ActivationFunctionType
ALU = mybir.AluOpType

D = 1024
S_LEN = 8192
NQ = 2048
GQ = 512
NG = NQ // GQ
NKC = 2816
NQC = 5120
EPS = 1e-6
NIT = 16
RNG = 8.0
TOPK = 256.0
NEGB = -30000.0
MAGIC = 12582912.0
C1 = 6.28125
C2 = 2.0 * math.pi - 6.28125
ARENA = 164864
GPERS = 81920


class _Op:
    __slots__ = ("eng", "fn", "deps", "ticket", "has_dep", "dma", "sem", "target", "prev")

    def __init__(self, eng, fn, dma):
        self.eng = eng
        self.fn = fn
        self.deps = []
        self.ticket = None
        self.has_dep = False
        self.dma = dma
        self.sem = None
        self.target = None
        self.prev = None


class Sched:
    ENGS = ("pe", "act", "dve", "pool", "sp")

    def __init__(self, nc, n_dma_sems=14):
        self.nc = nc
        self.ops = {e: [] for e in self.ENGS}
        self.last_w = {}
        self.readers = {}
        self.nd = n_dma_sems
        self.rr = 0
        self.rr_sw = 0
        self.n_sw = 4
        self.dlast = [None] * n_dma_sems
        self.dcount = [0] * n_dma_sems
        self.bar_deps = []
        self.bar_pending = set()
        self.last_compute = {e: None for e in self.ENGS}

    def barrier(self):
        deps = [o for o in self.last_compute.values() if o is not None]
        deps += [o for o in self.dlast if o is not None]
        self.bar_deps = deps
        self.bar_pending = set(self.ENGS)
        self.last_w = {}
        self.readers = {}

    def op(self, eng, fn, reads=(), writes=(), dma=False):
        o = _Op(eng, fn, dma)
        deps = []
        if eng in self.bar_pending:
            deps.extend(self.bar_deps)
            self.bar_pending.discard(eng)
        for r in reads:
            w = self.last_w.get(r)
            if w is not None:
                deps.append(w)
        for w_ in writes:
            w = self.last_w.get(w_)
            if w is not None:
                deps.append(w)
            deps.extend(self.readers.get(w_, ()))
        seen = set()
        for d in deps:
            if d is o or id(d) in seen:
                continue
            seen.add(id(d))
            if d.eng == "pe" and eng == "pe" and not d.dma and not dma:
                continue
            o.deps.append(d)
            d.has_dep = True
        for r in reads:
            self.readers.setdefault(r, []).append(o)
        for w_ in writes:
            self.last_w[w_] = o
            self.readers[w_] = []
        if dma:
            if eng == "pool":
                k = self.rr_sw
                self.rr_sw = (self.rr_sw + 1) % self.n_sw
            else:
                k = self.n_sw + self.rr
                self.rr = (self.rr + 1) % (self.nd - self.n_sw)
            o.sem = k
            o.prev = self.dlast[k]
            self.dcount[k] += 16
            o.target = self.dcount[k]
            self.dlast[k] = o
        else:
            self.last_compute[eng] = o
        self.ops[eng].append(o)
        return o

    def alloc_sems(self, st):
        nc = self.nc
        self.esem = {e: st.enter_context(nc.semaphore("s_" + e)) for e in self.ENGS}
        self.dsem = [st.enter_context(nc.semaphore("d_%d" % i)) for i in range(self.nd)]

    def emit(self, block, final_waits=()):
        esem, dsem = self.esem, self.dsem
        for o in final_waits:
            o.has_dep = True
        for e in self.ENGS:
            c = 0
            for o in self.ops[e]:
                if (not o.dma) and o.has_dep:
                    c += 1
                    o.ticket = c

        def run(e, engobj, extra=None):
            waited = {}

            def wait_for(d):
                if d.dma:
                    key, val, sem = ("d", d.sem), d.target, dsem[d.sem]
                else:
                    key, val, sem = ("e", d.eng), d.ticket, esem[d.eng]
                if waited.get(key, 0) >= val:
                    return
                engobj.wait_ge(sem, val)
                waited[key] = val

            for o in self.ops[e]:
                for d in o.deps:
                    wait_for(d)
                if o.dma and o.prev is not None:
                    wait_for(o.prev)
                ins = o.fn(engobj)
                if o.dma:
                    ins.then_inc(dsem[o.sem], 16)
                elif o.has_dep:
                    ins.then_inc(esem[e], 1)
            if extra:
                for d in extra:
                    wait_for(d)

        @block.tensor
        def _(eng):
            run("pe", eng)

        @block.scalar
        def _(eng):
            run("act", eng)

        @block.vector
        def _(eng):
            run("dve", eng)

        @block.gpsimd
        def _(eng):
            run("pool", eng)

        @block.sync
        def _(eng):
            run("sp", eng, extra=list(final_waits))


class Buf:
    _n = 0

    def __init__(self, ap, name=None):
        Buf._n += 1
        self.ap = ap
        self.key = "%s#%d" % (name or "b", Buf._n)


def _keys(xs):
    out = []
    for x in xs:
        out.append(x.key if isinstance(x, Buf) else x)
    return out


_DT_SIZE = {F32: 4, BF16: 2, I32: 4, U8: 1}


class Arena:
    def __init__(self, t, nbytes):
        self.t = t
        self.n = nbytes
        self.off = 0

    def reset(self, off=0):
        self.off = off

    def alloc(self, shape, dt, name=None):
        n = 1
        for s in shape:
            n *= s
        nb = n * _DT_SIZE[dt]
        nb_al = (nb + 63) // 64 * 64
        assert self.off + nb_al <= self.n, ("arena overflow", name, self.off, nb_al, self.n)
        ap = self.t[:, self.off:self.off + nb].bitcast(dt)
        self.off += nb_al
        if len(shape) == 2:
            ap = ap.rearrange("p (a b) -> p a b", a=shape[0], b=shape[1])
        elif len(shape) == 3:
            ap = ap.rearrange("p (a b c) -> p a b c", a=shape[0], b=shape[1], c=shape[2])
        return Buf(ap, name)


def build(debug=False, stop_after=None):
    nc = bass.Bass("TRN2", target_bir_lowering=False)

    def din(name, shape, dt=F32):
        return nc.dram_tensor(name, list(shape), dt, kind="ExternalInput").ap()

    dbg_kind = "ExternalOutput" if debug else "Internal"

    def dscr(name, shape, dt):
        return nc.dram_tensor(name, list(shape), dt, kind=dbg_kind).ap()

    xT = din("xT", [128, 8, S_LEN])
    xTo = din("xTo", [128, 8, NQ])
    xo = din("xo", [NQ, D])
    pos_all = din("pos_all", [1, S_LEN], I32)
    pos_own = din("pos_own", [1, NQ], I32)
    wk = din("wk", [128, 8, NKC])
    wq = din("wq", [128, 8, NQC])
    wiw = din("wiw", [128, 8, 4])
    wbd = din("wbd", [64, 8, D])
    wbf = din("wbf", [64, 8, D])
    wo = din("wo", [128, 8, D])
    cvec = din("cvec", [128, 64])
    fgain = din("fgain", [1, D])
    identf = din("identf", [128, 128])
    bmaskf = din("bmaskf", [128, 32])
    cbiasf = din("cbiasf", [128, 512])
    y = nc.dram_tensor("y", [NQ, D], F32, kind="ExternalOutput").ap()

    kf_s = dscr("kf_s", [8, 68, S_LEN], BF16)
    ka_s = dscr("ka_s", [8, 64, S_LEN], BF16)
    ki_s = dscr("ki_s", [64, S_LEN], BF16)
    vf_s = dscr("vf_s", [8, 128, 64, 128], BF16)
    va_s = dscr("va_s", [8, 128, 64, 128], BF16)
    dbg = {}
    if debug:
        dbg["qfT"] = nc.dram_tensor("d_qfT", [128, 8, 512], BF16, kind="ExternalOutput").ap()
        dbg["qaT"] = nc.dram_tensor("d_qaT", [128, 8, 512], BF16, kind="ExternalOutput").ap()
        dbg["qiT"] = nc.dram_tensor("d_qiT", [128, 4, 512], BF16, kind="ExternalOutput").ap()
        dbg["sc"] = nc.dram_tensor("d_sc", [128, 8192], F32, kind="ExternalOutput").ap()
        dbg["lo"] = nc.dram_tensor("d_lo", [4, 128, 1], F32, kind="ExternalOutput").ap()
        dbg["mT"] = nc.dram_tensor("d_mT", [128, 64, 512], U8, kind="ExternalOutput").ap()
        dbg["yTa"] = nc.dram_tensor("d_yTa", [128, 8, 512], BF16, kind="ExternalOutput").ap()
        dbg["yTf"] = nc.dram_tensor("d_yTf", [128, 8, 512], BF16, kind="ExternalOutput").ap()

    st = contextlib.ExitStack()
    with st:
        def T(name, shape, dt):
            return Buf(st.enter_context(nc.sbuf_tensor(name, list(shape), dt))[:], name)

        arena_t = st.enter_context(nc.sbuf_tensor("arena", [128, ARENA], U8))
        AR = Arena(arena_t, ARENA)
        ps = [Buf(st.enter_context(nc.psum_tensor("ps%d" % i, [128, 512], F32))[:], "ps%d" % i) for i in range(8)]

        S = Sched(nc)
        S.alloc_sems(st)

        def DMA(q, out, in_, R=(), W=()):
            return S.op(q, lambda e: e.dma_start(out=out, in_=in_), _keys(R), _keys(W), dma=True)

        def TT(eng, out, in0, in1, op, R=(), W=()):
            return S.op(eng, lambda e: e.tensor_tensor(out=out, in0=in0, in1=in1, op=op), _keys(R), _keys(W))

        def TS(eng, out, in0, s1, op0, s2=None, op1=None, R=(), W=(), accum=None):
            def f(e):
                kw = dict(out=out, in0=in0, scalar1=s1, scalar2=s2, op0=op0)
                if op1 is not None:
                    kw["op1"] = op1
                if accum is not None:
                    kw["accum_out"] = accum
                return e.tensor_scalar(**kw)
            return S.op(eng, f, _keys(R), _keys(W))

        def STT(out, in0, scalar, in1, op0, op1, R=(), W=()):
            return S.op("dve", lambda e: e.scalar_tensor_tensor(out=out, in0=in0, scalar=scalar, in1=in1, op0=op0, op1=op1),
                        _keys(R), _keys(W))

        def ACT(out, in_, func, R=(), W=(), bias=None, scale=None, accum=None):
            def f(e):
                kw = dict(out=out, in_=in_, func=func)
                if bias is not None:
                    kw["bias"] = bias
                if scale is not None:
                    kw["scale"] = scale
                if accum is not None:
                    kw["accum_out"] = accum
                return e.activation(**kw)
            return S.op("act", f, _keys(R), _keys(W))

        def CP(eng, out, in_, R=(), W=()):
            return S.op(eng, lambda e: e.tensor_copy(out=out, in_=in_), _keys(R), _keys(W))

        def MS(eng, ap, val, W=()):
            return S.op(eng, lambda e: e.memset(ap, val), (), _keys(W))

        def MM(out, pairs, R=(), W=()):
            def f(e):
                ins = None
                n = len(pairs)
                for i, (l, r) in enumerate(pairs):
                    ins = e.matmul(out, lhsT=l, rhs=r, start=(i == 0), stop=(i == n - 1))
                return ins
            return S.op("pe", f, _keys(R), _keys(W))

        ones_bf = T("ones_bf", [128, 512], BF16)
        onesf = T("onesf", [128, 512], F32)
        ident = T("ident", [128, 128], BF16)
        bmask = T("bmask", [128, 32], BF16)
        cbias = T("cbias", [128, 512], F32)
        cv = T("cv", [128, 64], F32)
        nbf = T("nbf", [128, 1], F32)
        halfpi = T("halfpi", [128, 1], F32)
        wiwb = T("wiwb", [128, 8, 4], BF16)
        fg_bc = T("fg_bc", [128, D], F32)
        rcol_q = T("rcol_q", [128, 4], F32)
        absw = T("absw", [128, 16], F32)
        sgnw = T("sgnw", [128, 16], F32)
        w4 = T("w4", [128, 16], F32)
        rcol_as = [T("rcol_a0", [128, 4], F32), T("rcol_a1", [128, 4], F32)]
        bis_lo = T("bis_lo", [128, 1], F32)
        bis_mid = T("bis_mid", [128, 1], F32)
        bis_cnt = T("bis_cnt", [128, 1], F32)
        bis_ind = T("bis_ind", [128, 1], F32)
        fin_ss = T("fin_ss", [128, 1], F32)
        fin_r = T("fin_r", [128, 1], F32)

        GAIN = lambda c: cv.ap[:, c:c + 1]
        INVF = cv.ap[:, 8:9]
        SGNS = cv.ap[:, 9:10]
        BMRG = lambda c: cv.ap[:, 16 + c:17 + c]

        MS("dve", ones_bf.ap, 1.0, W=[ones_bf])
        MS("dve", onesf.ap, 1.0, W=[onesf])
        MS("dve", halfpi.ap, math.pi / 2, W=[halfpi])
        DMA("sp", cv.ap, cvec[:, :], W=[cv])
        DMA("sp", cbias.ap, cbiasf[:, :], W=[cbias])
        DMA("pool", ident.ap, identf[:, :], W=[ident])
        DMA("pool", bmask.ap, bmaskf[:, :], W=[bmask])
        DMA("pool", wiwb.ap, wiw[:, :, :], W=[wiwb])
        DMA("sp", fg_bc.ap, fgain[0:1, :].to_broadcast([128, D]), W=[fg_bc])
        TS("dve", nbf.ap, cv.ap[:, 10:11], -1.0, ALU.mult, R=[cv], W=[nbf])

        def load_x_dma(src, t0, xf):
            DMA("sp", xf.ap, src[:, :, t0:t0 + 512], W=[xf])

        def load_x_prep(xf, xb, xsq):
            ACT(xsq.ap, xf.ap, AF.Square, R=[xf], W=[xsq])
            for c in range(8):
                ACT(xb.ap[:, c, :], xf.ap[:, c, :], AF.Copy, R=[xf, cv], W=[xb], scale=GAIN(c))

        def load_x_group(src, t0, xf, xb, xsq):
            load_x_dma(src, t0, xf)
            load_x_prep(xf, xb, xsq)

        def rstd_gen(xsq, rbc, rcol, pA, pB):
            MM(pA.ap, [(ones_bf.ap[:, 0:128], xsq.ap[:, c, :]) for c in range(8)], R=[xsq, ones_bf], W=[pA])
            yield
            for tb in range(4):
                MM(pB.ap[:, tb:tb + 1], [(xsq.ap[:, c, tb * 128:(tb + 1) * 128], ones_bf.ap[:, 0:1]) for c in range(8)],
                   R=[xsq, ones_bf], W=[pB])
            yield
            TS("dve", rcol.ap, pB.ap[:, 0:4], 1.0 / D, ALU.mult, EPS, ALU.add, R=[pB], W=[rcol])
            TS("dve", rbc.ap, pA.ap, 1.0 / D, ALU.mult, EPS, ALU.add, R=[pA], W=[rbc])
            yield
            ACT(rcol.ap, rcol.ap, AF.Sqrt, R=[rcol], W=[rcol])
            ACT(rbc.ap, rbc.ap, AF.Sqrt, R=[rbc], W=[rbc])
            yield
            S.op("dve", lambda e: e.reciprocal(out=rcol.ap, in_=rcol.ap), _keys([rcol]), _keys([rcol]))
            yield
            S.op("dve", lambda e: e.reciprocal(out=rbc.ap, in_=rbc.ap), _keys([rbc]), _keys([rbc]))
            yield

        def rstd_from(xsq, xb_unused, rbc, rcol, pA, pB, scale_extra=1.0):
            for _ in rstd_gen(xsq, rbc, rcol, pA, pB):
                pass

        def rope_gen(possrc, t0, posi, ang, kk, sn, cs, rbc, scale_extra):
            DMA("sp", posi.ap, possrc[0:1, t0:t0 + 512].to_broadcast([128, 512]), W=[posi])
            CP("dve", ang.ap, posi.ap, R=[posi], W=[ang])
            yield
            TS("dve", ang.ap, ang.ap, INVF, ALU.mult, R=[ang, cv], W=[ang])
            yield
            TS("dve", kk.ap, ang.ap, 1.0 / (2.0 * math.pi), ALU.mult, MAGIC, ALU.add, R=[ang], W=[kk])
            yield
            TS("dve", kk.ap, kk.ap, -MAGIC, ALU.add, R=[kk], W=[kk])
            yield
            STT(ang.ap, kk.ap, -C1, ang.ap, ALU.mult, ALU.add, R=[kk, ang], W=[ang])
            yield
            STT(ang.ap, kk.ap, -C2, ang.ap, ALU.mult, ALU.add, R=[kk, ang], W=[ang])
            yield
            TS("dve", ang.ap, ang.ap, math.pi, ALU.min, -math.pi, ALU.max, R=[ang], W=[ang])
            yield
            STT(kk.ap, ang.ap, -1.0, ang.ap, ALU.mult, ALU.max, R=[ang], W=[kk])
            ACT(sn.ap, ang.ap, AF.Sin, R=[ang], W=[sn])
            ACT(cs.ap, kk.ap, AF.Sin, R=[kk, halfpi], W=[cs], bias=halfpi.ap[:, 0:1], scale=-1.0)
            yield
            STT(sn.ap, sn.ap, SGNS, rbc.ap, ALU.mult, ALU.mult, R=[sn, cv, rbc], W=[sn])
            yield
            if scale_extra != 1.0:
                TS("dve", sn.ap, sn.ap, scale_extra, ALU.mult, R=[sn], W=[sn])
                STT(cs.ap, cs.ap, scale_extra, rbc.ap, ALU.mult, ALU.mult, R=[cs, rbc], W=[cs])
            else:
                TT("dve", cs.ap, cs.ap, rbc.ap, ALU.mult, R=[cs, rbc], W=[cs])
            yield

        def rope_tables(possrc, t0, posi, ang, kk, sn, cs, rbc, scale_extra):
            for _ in rope_gen(possrc, t0, posi, ang, kk, sn, cs, rbc, scale_extra):
                pass

        AR.reset(0)
        wkb = AR.alloc([8, NKC], BF16, "wkb")
        xfs = [AR.alloc([8, 512], F32, "xf") for _ in range(2)]
        xbs = [AR.alloc([8, 512], BF16, "xb") for _ in range(2)]
        xsq1 = AR.alloc([8, 512], BF16, "xsq")
        posi = AR.alloc([512], I32, "posi")
        ang = AR.alloc([512], F32, "ang")
        kk = AR.alloc([512], F32, "kk")
        sns = [AR.alloc([512], F32, "sn") for _ in range(2)]
        css = [AR.alloc([512], F32, "cs") for _ in range(2)]
        rbcs = [AR.alloc([512], F32, "rbc") for _ in range(2)]
        t1s = [AR.alloc([512], F32, "t1") for _ in range(2)]
        t2s = [AR.alloc([512], F32, "t2") for _ in range(2)]
        ksts = [AR.alloc([512], BF16, "kst") for _ in range(2)]
        mf = AR.alloc([512], F32, "mf")
        kist = AR.alloc([512], BF16, "kist")
        ee = AR.alloc([512], F32, "ee")
        ncums = [AR.alloc([512], F32, "ncum") for _ in range(2)]
        r1s = AR.alloc([512], F32, "r1s")
        aug = AR.alloc([3, 512], BF16, "aug")
        vsts = [AR.alloc([8, 4, 128], BF16, "vst") for _ in range(2)]

        for i in range(4):
            DMA("pool", wkb.ap[:, :, i * 704:(i + 1) * 704], wk[:, :, i * 704:(i + 1) * 704], W=[wkb])
        for v in vsts:
            MS("pool", v.ap, 1.0, W=[v])

        n_tg = S_LEN // 512
        if stop_after == "A1":
            n_tg = 1
        if stop_after == "A0":
            n_tg = 0
        kst_i = 0
        def chain_gen(tgn):
            k = tgn % 2
            for _ in rstd_gen(xsq1, rbcs[k], rcol_as[k], ps[0], ps[1]):
                yield
            for _ in rope_gen(pos_all, tgn * 512, posi, ang, kk, sns[k], css[k], rbcs[k], 1.0):
                yield

        if n_tg > 0:
            load_x_dma(xT, 0, xfs[0])
            load_x_prep(xfs[0], xbs[0], xsq1)
            for _ in chain_gen(0):
                pass
        for tg in range(n_tg):
            t0 = tg * 512
            xb = xbs[tg % 2]
            rbc = rbcs[tg % 2]
            rcol_a = rcol_as[tg % 2]
            sn = sns[tg % 2]
            cs = css[tg % 2]
            if tg + 1 < n_tg:
                load_x_dma(xT, t0 + 512, xfs[(tg + 1) % 2])

            def proj_chunk(pbuf, n0):
                MM(pbuf.ap, [(wkb.ap[:, c, n0:n0 + 128], xb.ap[:, c, :]) for c in range(8)], R=[wkb, xb], W=[pbuf])

            for cc in range(4):
                pb = ps[2 + (cc % 2)]
                proj_chunk(pb, cc * 128)
                kst = ksts[kst_i % 2]
                kst_i += 1
                TT("dve", kst.ap, pb.ap, rbc.ap, ALU.mult, R=[pb, rbc], W=[kst])
                DMA("sp", kf_s[2 * cc, 0:64, t0:t0 + 512], kst.ap[0:64, :], R=[kst], W=["kf_s"])
                DMA("sp", kf_s[2 * cc + 1, 0:64, t0:t0 + 512], kst.ap[64:128, :], R=[kst], W=["kf_s"])
            DMA("sp", kf_s[:, 64, t0:t0 + 512], ones_bf.ap[0:8, :], R=[ones_bf], W=["kf_s"])
            for br in range(2):
                vst = vsts[br]
                dst = vf_s if br == 0 else va_s
                n0 = 1792 + br * 512
                for tb in range(4):
                    pv = ps[6 + (tb % 2)]
                    MM(pv.ap, [(xb.ap[:, c, tb * 128:(tb + 1) * 128], wkb.ap[:, c, n0:n0 + 512]) for c in range(8)],
                       R=[xb, wkb], W=[pv])
                    ACT(vst.ap[:, :, tb, 0:64], pv.ap.rearrange("p (h c) -> p h c", c=64), AF.Copy,
                        R=[pv, rcol_a], W=[vst], scale=rcol_a.ap[:, tb:tb + 1])
                for h in range(8):
                    DMA("sp", dst[h, :, tg * 4:(tg + 1) * 4, :], vst.ap[:, h, :, :], R=[vst], W=["v_s%d" % br])

            ahead = None
            if tg + 1 < n_tg:
                load_x_prep(xfs[(tg + 1) % 2], xbs[(tg + 1) % 2], xsq1)
                ahead = chain_gen(tg + 1)

            def adv(n):
                if ahead is not None:
                    for _ in range(n):
                        try:
                            next(ahead)
                        except StopIteration:
                            break

            for cc in range(5):
                pa, pb = (ps[4], ps[5]) if cc % 2 == 0 else (ps[2], ps[3])
                if cc < 4:
                    proj_chunk(pa, 512 + cc * 128)
                    proj_chunk(pb, 1024 + cc * 128)
                else:
                    proj_chunk(pa, 1536)
                    proj_chunk(pb, 1664)
                adv(2)
                t1 = t1s[cc % 2]
                t2 = t2s[cc % 2]
                TT("dve", t1.ap, pa.ap, cs.ap, ALU.mult, R=[pa, cs], W=[t1])
                adv(1)
                TT("dve", t2.ap, pb.ap, sn.ap, ALU.mult, R=[pb, sn], W=[t2])
                adv(1)
                if cc < 4:
                    kst = ksts[kst_i % 2]
                    kst_i += 1
                    TT("pool", kst.ap, t1.ap, t2.ap, ALU.add, R=[t1, t2], W=[kst])
                    DMA("sp", ka_s[2 * cc, :, t0:t0 + 512], kst.ap[0:64, :], R=[kst], W=["ka_s"])
                    DMA("sp", ka_s[2 * cc + 1, :, t0:t0 + 512], kst.ap[64:128, :], R=[kst], W=["ka_s"])
                else:
                    TT("pool", mf.ap, t1.ap, t2.ap, ALU.add, R=[t1, t2], W=[mf])
                    CP("pool", kist.ap[0:64, :], mf.ap[0:64, :], R=[mf], W=[kist])
                    DMA("sp", ki_s[:, t0:t0 + 512], kist.ap[0:64, :], R=[kist], W=["ki_s"])
                    ACT(ee.ap[96:104, :], mf.ap[96:104, :], AF.Exp, R=[mf, nbf], W=[ee], bias=nbf.ap[96:104, 0:1], scale=-1.0)
                    ACT(ee.ap[96:104, :], ee.ap[96:104, :], AF.Ln, R=[ee], W=[ee], bias=onesf.ap[96:104, 0:1], scale=1.0)
                    nc_cur = ncums[tg % 2]
                    nc_prev = ncums[(tg + 1) % 2]
                    init = 0.0 if tg == 0 else nc_prev.ap[96:104, 511:512]
                    S.op("dve", (lambda o_, d1_, i_: (lambda e: e.tensor_tensor_scan(
                        out=o_, data0=onesf.ap[96:104, :], data1=d1_, initial=i_, op0=ALU.mult, op1=ALU.add)))(
                        nc_cur.ap[96:104, :], ee.ap[96:104, :], init),
                        _keys([ee, onesf, nc_prev]), _keys([nc_cur]))
                    CP("dve", aug.ap[96:104, 0, :], nc_cur.ap[96:104, :], R=[nc_cur], W=[aug])
                    TT("dve", r1s.ap[96:104, :], nc_cur.ap[96:104, :], aug.ap[96:104, 0, :], ALU.subtract, R=[nc_cur, aug], W=[r1s])
                    CP("dve", aug.ap[96:104, 1, :], r1s.ap[96:104, :], R=[r1s], W=[aug])
                    TT("dve", r1s.ap[96:104, :], r1s.ap[96:104, :], aug.ap[96:104, 1, :], ALU.subtract, R=[r1s, aug], W=[r1s])
                    CP("dve", aug.ap[96:104, 2, :], r1s.ap[96:104, :], R=[r1s], W=[aug])
                    DMA("sp", kf_s[:, 65:68, t0:t0 + 512], aug.ap[96:104, :, :], R=[aug], W=["kf_s"])
            adv(1000)
        n_groups = NG
        if stop_after in ("A", "A1", "A0"):
            n_groups = 0
        elif stop_after is not None and stop_after.startswith("B"):
            n_groups = 1
        for g in range(n_groups):
            S.barrier()
            q0 = g * GQ
            win0 = 2048 * g
            L = 2048 * (g + 1)
            nkb = L // 128
            AR.reset(0)
            qfT = AR.alloc([8, 512], BF16, "qfT")
            qaT = AR.alloc([8, 512], BF16, "qaT")
            qiT = AR.alloc([4, 512], BF16, "qiT")
            xqb = AR.alloc([8, 512], BF16, "xqb")
            rbq = AR.alloc([512], F32, "rbq")
            mT = AR.alloc([64, 512], U8, "mT")
            yTa = AR.alloc([8, 512], BF16, "yTa")
            yTf = AR.alloc([8, 512], BF16, "yTf")
            assert AR.off <= GPERS, AR.off

            AR.reset(GPERS)
            xqf = AR.alloc([8, 512], F32, "xqf")
            xsqq = AR.alloc([8, 512], BF16, "xsqq")
            wqps = [AR.alloc([8, 512], BF16, "wqp") for _ in range(2)]
            posi = AR.alloc([512], I32, "posi")
            ang = AR.alloc([512], F32, "ang")
            kk = AR.alloc([512], F32, "kk")
            sn = AR.alloc([512], F32, "sn")
            cs = AR.alloc([512], F32, "cs")
            rb8 = AR.alloc([512], F32, "rb8")
            t1s = [AR.alloc([512], F32, "t1") for _ in range(2)]
            t2s = [AR.alloc([512], F32, "t2") for _ in range(2)]
            craws = [AR.alloc([2048], BF16, "craw") for _ in range(2)]

            load_x_group(xTo, q0, xqf, xqb, xsqq)
            rstd_from(xsqq, xqb, rbq, rcol_q, ps[0], ps[1])
            TS("dve", rb8.ap, rbq.ap, 0.125, ALU.mult, R=[rbq], W=[rb8])
            qchain = rope_gen(pos_own, q0, posi, ang, kk, sn, cs, rbq, 0.125)

            def qadv(n):
                for _ in range(n):
                    try:
                        next(qchain)
                    except StopIteration:
                        break

            wq_i = [0]

            def load_wq(piece):
                wb = wqps[wq_i[0] % 2]
                wq_i[0] += 1
                DMA("pool", wb.ap, wq[:, :, piece * 512:(piece + 1) * 512], W=[wb])
                return wb

            def qproj(pbuf, wb, n0):
                MM(pbuf.ap, [(wb.ap[:, c, n0:n0 + 128], xqb.ap[:, c, :]) for c in range(8)], R=[wb, xqb], W=[pbuf])

            MS("pool", qaT.ap[64:128, :, :], 0.0, W=[qaT])
            MS("pool", qfT.ap[64:68, :, :], 1.0, W=[qfT])
            wb = load_wq(0)
            for cc in range(4):
                pb = ps[2 + cc]
                qproj(pb, wb, cc * 128)
            for cc in range(4):
                pb = ps[2 + cc]
                TT("dve", qfT.ap[0:64, 2 * cc, :], pb.ap[0:64, :], rb8.ap[0:64, :], ALU.mult, R=[pb, rb8], W=[qfT])
                qadv(2)
                TT("dve", qfT.ap[0:64, 2 * cc + 1, :], pb.ap[64:128, :], rb8.ap[64:128, :], ALU.mult, R=[pb, rb8], W=[qfT])
                qadv(2)
            qadv(1000)
            for h in range(8):
                cr = craws[h % 2]
                DMA("sp", cr.ap[64:65, :], kf_s[h, 65:66, win0:win0 + 2048], R=["kf_s"], W=[cr])
                TS("dve", qfT.ap[64:65, h, :], cr.ap[64:65, :].rearrange("p (i r) -> p i r", r=4)[:, :, 0], -1.0, ALU.mult,
                   R=[cr], W=[qfT])
            wb1 = load_wq(1)
            wb2 = load_wq(2)
            for cc in range(4):
                pa, pb = ps[4 + 2 * (cc % 2)], ps[5 + 2 * (cc % 2)]
                qproj(pa, wb1, cc * 128)
                qproj(pb, wb2, cc * 128)
                t1 = t1s[cc % 2]
                t2 = t2s[cc % 2]
                TT("dve", t1.ap, pa.ap, cs.ap, ALU.mult, R=[pa, cs], W=[t1])
                TT("dve", t2.ap, pb.ap, sn.ap, ALU.mult, R=[pb, sn], W=[t2])
                TT("pool", qaT.ap[0:64, 2 * cc, :], t1.ap[0:64, :], t2.ap[0:64, :], ALU.add, R=[t1, t2], W=[qaT])
                TT("pool", qaT.ap[0:64, 2 * cc + 1, :], t1.ap[64:128, :], t2.ap[64:128, :], ALU.add, R=[t1, t2], W=[qaT])
            wb = load_wq(3)
            for cc in range(2):
                pa, pb = ps[4 + 2 * (cc % 2)], ps[5 + 2 * (cc % 2)]
                qproj(pa, wb, cc * 128)
                qproj(pb, wb, 256 + cc * 128)
                t1 = t1s[cc % 2]
                t2 = t2s[cc % 2]
                TT("dve", t1.ap, pa.ap, cs.ap, ALU.mult, R=[pa, cs], W=[t1])
                TT("dve", t2.ap, pb.ap, sn.ap, ALU.mult, R=[pb, sn], W=[t2])
                TT("pool", qiT.ap[0:64, 2 * cc, :], t1.ap[0:64, :], t2.ap[0:64, :], ALU.add, R=[t1, t2], W=[qiT])
                TT("pool", qiT.ap[0:64, 2 * cc + 1, :], t1.ap[64:128, :], t2.ap[64:128, :], ALU.add, R=[t1, t2], W=[qiT])
            for sb in range(4):
                MM(ps[1].ap[:, 8 + 4 * sb:12 + 4 * sb],
                   [(xqb.ap[:, c, sb * 128:(sb + 1) * 128], wiwb.ap[:, c, :]) for c in range(8)], R=[xqb, wiwb], W=[ps[1]])
                TS("dve", w4.ap[:, 4 * sb:4 * sb + 4], ps[1].ap[:, 8 + 4 * sb:12 + 4 * sb], rcol_q.ap[:, sb:sb + 1], ALU.mult,
                   0.5, ALU.mult, R=[ps[1], rcol_q], W=[w4])
            STT(absw.ap, w4.ap, -1.0, w4.ap, ALU.mult, ALU.max, R=[w4], W=[absw])
            TS("dve", sgnw.ap, w4.ap, 0.0, ALU.is_ge, 2.0, ALU.mult, R=[w4], W=[sgnw])
            TS("dve", sgnw.ap, sgnw.ap, -1.0, ALU.add, R=[sgnw], W=[sgnw])
            if debug and g == 0:
                DMA("sp", dbg["qfT"][:, :, :], qfT.ap, R=[qfT], W=["dbg1"])
                DMA("sp", dbg["qaT"][:, :, :], qaT.ap, R=[qaT], W=["dbg2"])
                DMA("sp", dbg["qiT"][:, :, :], qiT.ap, R=[qiT], W=["dbg3"])
            if stop_after == "B1":
                break

            S.barrier()
            AR.reset(GPERS)
            sc = AR.alloc([8192], F32, "sc")
            msk = AR.alloc([8192], BF16, "msk")
            rts = [AR.alloc([512], F32, "rt") for _ in range(2)]
            kits = [AR.alloc([2048], BF16, "kit") for _ in range(1)]
            f_kTps = [AR.alloc([2048], BF16, "kTp") for _ in range(2)]
            f_vps = [AR.alloc([16, 128], BF16, "vp") for _ in range(2)]
            f_Pts = [AR.alloc([512], BF16, "Pt") for _ in range(4)]
            f_recs = [AR.alloc([512], F32, "rec") for _ in range(1)]

            def attn_steps(br, kTps, vps, Pts, Pms, recs, psS, psO):
                tiles = [(h, pc, kb) for h in range(8) for pc in range(g + 1) for kb in range(16)]
                pend = []
                nS = len(psS)
                cur = {}
                piece_n = 0

                def issue_pv(item):
                    (h, pc, kb, pmat, vp, first, last) = item
                    po = psO[h % len(psO)]
                    S.op("pe", (lambda o_, l_, r_, f_, s_: (lambda e: e.matmul(o_, lhsT=l_, rhs=r_, start=f_, stop=s_)))(
                        po.ap, vp.ap[:, kb, :], pmat.ap, first, last), _keys([vp, pmat]), _keys([po]))
                    if last:
                        yT = yTa if br == 0 else yTf
                        rec = recs[h % len(recs)]
                        S.op("dve", (lambda o_, i_: (lambda e: e.reciprocal(out=o_, in_=i_)))(rec.ap[0:64, :], po.ap[64:128, :]),
                             _keys([po]), _keys([rec]))
                        TT("dve", yT.ap[0:64, h, :], po.ap[0:64, :], rec.ap[0:64, :], ALU.mult, R=[po, rec], W=[yT])

                for ti, (h, pc, kb) in enumerate(tiles):
                    if kb == 0:
                        kTp = kTps[piece_n % len(kTps)]
                        vp = vps[piece_n % len(vps)]
                        piece_n += 1
                        if br == 0:
                            DMA("sp", kTp.ap[0:64, :], ka_s[h, :, pc * 2048:(pc + 1) * 2048], R=["ka_s"], W=[kTp])
                            DMA("sp", vp.ap, va_s[h, :, pc * 16:(pc + 1) * 16, :], R=["v_s1"], W=[vp])
                        else:
                            DMA("sp", kTp.ap[0:68, :], kf_s[h, :, pc * 2048:(pc + 1) * 2048], R=["kf_s"], W=[kTp])
                            DMA("sp", vp.ap, vf_s[h, :, pc * 16:(pc + 1) * 16, :], R=["v_s0"], W=[vp])
                        cur["k"], cur["v"] = kTp, vp
                    kTp, vp = cur["k"], cur["v"]
                    pS = psS[ti % nS]
                    Pt = Pts[ti % len(Pts)]
                    diag = (br == 1 and pc == g)
                    c0 = 32 * kb if diag else 0
                    if br == 0:
                        MM(pS.ap[:, c0:512], [(kTp.ap[:, kb * 128:(kb + 1) * 128], qaT.ap[:, h, c0:512])], R=[kTp, qaT], W=[pS])
                    else:
                        MM(pS.ap[:, c0:512], [(kTp.ap[0:68, kb * 128:(kb + 1) * 128], qfT.ap[0:68, h, c0:512])], R=[kTp, qfT], W=[pS])
                    ACT(Pt.ap[:, c0:512], pS.ap[:, c0:512], AF.Exp, R=[pS], W=[Pt])
                    if br == 0:
                        Pm = Pms[ti % len(Pms)]
                        TT("pool" if ti % 2 == 1 else "dve", Pm.ap, Pt.ap, mT.ap[:, pc * 16 + kb, :], ALU.mult, R=[Pt, mT], W=[Pm])
                        pmat = Pm
                    else:
                        if diag:
                            if c0 > 0:
                                MS("pool", Pt.ap[:, 0:c0], 0.0, W=[Pt])
                            TT("pool", Pt.ap[:, c0:c0 + 32], Pt.ap[:, c0:c0 + 32], bmask.ap, ALU.mult, R=[Pt, bmask], W=[Pt])
                        pmat = Pt
                    pend.append((h, pc, kb, pmat, vp, (pc == 0 and kb == 0), (pc == g and kb == 15)))
                    if len(pend) > min(4, nS - 1):
                        issue_pv(pend.pop(0))
                    yield 1
                while pend:
                    issue_pv(pend.pop(0))

            def b2_units():
                for sb in range(4):
                    nch = 4 * g + sb + 1
                    Lsb = 512 * nch
                    kit = kits[0]
                    for ch in range(nch):
                        if ch % 4 == 0:
                            pc = ch // 4
                            DMA("sp", kit.ap[0:64, :], ki_s[:, pc * 2048:(pc + 1) * 2048], R=["ki_s"], W=[kit])
                        for h in range(4):
                            pb = ps[h]
                            MM(pb.ap, [(qiT.ap[0:64, h, sb * 128:(sb + 1) * 128], kit.ap[0:64, (ch % 4) * 512:(ch % 4 + 1) * 512])],
                               R=[qiT, kit], W=[pb])
                        for h in range(4):
                            pb = ps[h]
                            rt = rts[h % 2]
                            ACT(rt.ap, pb.ap, AF.Relu, R=[pb, absw], W=[rt], scale=absw.ap[:, 4 * sb + h:4 * sb + h + 1])
                            scs = sc.ap[:, ch * 512:(ch + 1) * 512]
                            sg = sgnw.ap[:, 4 * sb + h:4 * sb + h + 1]
                            if h == 0:
                                TS("dve", scs, rt.ap, sg, ALU.mult, R=[rt, sgnw], W=[sc])
                            else:
                                STT(scs, rt.ap, sg, scs, ALU.mult, ALU.add, R=[rt, sgnw, sc], W=[sc])
                        if ch == nch - 1:
                            TT("dve", sc.ap[:, ch * 512:(ch + 1) * 512], sc.ap[:, ch * 512:(ch + 1) * 512], cbias.ap, ALU.add,
                               R=[sc, cbias], W=[sc])
                        yield 3.2
                    if debug and g == 0 and sb == 3:
                        DMA("sp", dbg["sc"][:, 0:Lsb], sc.ap[:, 0:Lsb], R=[sc], W=["dbg4"])
                    MS("dve", bis_lo.ap, -RNG, W=[bis_lo])
                    for it in range(NIT):
                        step = RNG / (2.0 ** it)
                        TS("dve", bis_mid.ap, bis_lo.ap, step, ALU.add, R=[bis_lo], W=[bis_mid])
                        TS("dve", msk.ap[:, 0:Lsb], sc.ap[:, 0:Lsb], bis_mid.ap[:, 0:1], ALU.is_ge, None, ALU.add,
                           R=[sc, bis_mid], W=[msk, bis_cnt], accum=bis_cnt.ap[:, 0:1])
                        TS("dve", bis_ind.ap, bis_cnt.ap, TOPK, ALU.is_ge, step, ALU.mult, R=[bis_cnt], W=[bis_ind])
                        TT("dve", bis_lo.ap, bis_lo.ap, bis_ind.ap, ALU.add, R=[bis_lo, bis_ind], W=[bis_lo])
                        yield 0.5 + Lsb * 1.05e-3
                    TS("dve", bis_ind.ap, bis_lo.ap, -RNG, ALU.is_le, -1000.0, ALU.mult, R=[bis_lo], W=[bis_ind])
                    TT("dve", bis_lo.ap, bis_lo.ap, bis_ind.ap, ALU.add, R=[bis_lo, bis_ind], W=[bis_lo])
                    TS("dve", msk.ap[:, 0:Lsb], sc.ap[:, 0:Lsb], bis_lo.ap[:, 0:1], ALU.is_ge, R=[sc, bis_lo], W=[msk])
                    if debug and g == 0:
                        DMA("sp", dbg["lo"][sb], bis_lo.ap, R=[bis_lo], W=["dbg5"])
                    yield Lsb * 0.6e-3
                    for k4 in range(Lsb // 512):
                        pt = ps[k4 % 2]
                        ptb = pt.ap.bitcast(BF16)
                        for i in range(4):
                            kb = k4 * 4 + i
                            S.op("pe", (lambda o_, i_: (lambda e: e.transpose(out=o_, in_=i_, identity=ident.ap)))(
                                ptb[:, i * 128:(i + 1) * 128], msk.ap[:, kb * 128:(kb + 1) * 128]),
                                _keys([msk, ident]), _keys([pt]))
                        ACT(mT.ap[:, k4 * 4:(k4 + 1) * 4, sb * 128:(sb + 1) * 128],
                            ptb[:, 0:512].rearrange("p (a b) -> p a b", b=128), AF.Copy, R=[pt], W=[mT])
                        yield 0.7
                    if Lsb // 128 < nkb:
                        MS("pool", mT.ap[:, Lsb // 128:nkb, sb * 128:(sb + 1) * 128], 0, W=[mT])

            fox = attn_steps(1, f_kTps, f_vps, f_Pts, None, f_recs, [ps[4], ps[5]], [ps[6], ps[7]])
            n_fox = 128 * (g + 1)
            units = list()
            tot_est = 0.0
            for sb in range(4):
                nch = 4 * g + sb + 1
                Lsb = 512 * nch
                tot_est += nch * 3.2 + NIT * (0.5 + Lsb * 1.05e-3) + Lsb * 0.6e-3 + (Lsb // 512) * 0.7
            rate = n_fox / (0.9 * tot_est)
            acc = 0.0
            fox_done = False
            for wgt in b2_units():
                acc += wgt * rate
                while acc >= 1.0 and not fox_done:
                    acc -= 1.0
                    try:
                        next(fox)
                    except StopIteration:
                        fox_done = True
            if not fox_done:
                for _ in fox:
                    pass
            if debug and g == 0:
                DMA("sp", dbg["mT"][:, 0:nkb, :], mT.ap[:, 0:nkb, :], R=[mT], W=["dbg6"])
            if stop_after == "B2":
                break

            S.barrier()
            AR.reset(GPERS)
            kTps = [AR.alloc([2048], BF16, "kTp") for _ in range(3)]
            vps = [AR.alloc([16, 128], BF16, "vp") for _ in range(3)]
            Pts = [AR.alloc([512], BF16, "Pt") for _ in range(6)]
            Pms = [AR.alloc([512], BF16, "Pm") for _ in range(6)]
            recs = [AR.alloc([512], F32, "rec") for _ in range(2)]
            for kt_ in kTps:
                MS("pool", kt_.ap[64:128, :], 0.0, W=[kt_])
            for _ in attn_steps(0, kTps, vps, Pts, Pms, recs, [ps[0], ps[1], ps[2], ps[3], ps[6], ps[7]], [ps[4], ps[5]]):
                pass
            if debug and g == 0:
                DMA("sp", dbg["yTa"][:, :, :], yTa.ap, R=[yTa], W=["dbg7"])
                DMA("sp", dbg["yTf"][:, :, :], yTf.ap, R=[yTf], W=["dbg8"])
            if stop_after == "B3":
                break

            S.barrier()
            AR.reset(GPERS)
            wqps = [AR.alloc([8, 512], BF16, "wqp") for _ in range(2)]
            wbrs = [AR.alloc([8, 128], BF16, "wbr") for _ in range(2)]
            wobs = [AR.alloc([8, 512], BF16, "wob") for _ in range(2)]
            ygs = [AR.alloc([8, 512], BF16, "yg") for _ in range(2)]
            mrg = AR.alloc([8, 512], BF16, "mrg")
            gtmp = AR.alloc([512], F32, "gtmp")
            gbf = AR.alloc([512], BF16, "gbf")
            gms4 = [AR.alloc([512], F32, "gm") for _ in range(2)]
            e1s = [AR.alloc([512], F32, "e1") for _ in range(1)]
            e2s = [AR.alloc([512], F32, "e2") for _ in range(1)]
            xot = AR.alloc([D], F32, "xot")
            xnew = AR.alloc([D], F32, "xnew")
            wq_i = [0]
            for br in range(2):
                wb = load_wq(4 + br)
                yT = yTa if br == 0 else yTf
                for cc in range(4):
                    pb = ps[cc % 2]
                    qproj(pb, wb, cc * 128)
                    TT("dve", gtmp.ap, pb.ap, rbq.ap, ALU.mult, R=[pb, rbq], W=[gtmp])
                    for hh in range(2):
                        h = 2 * cc + hh
                        ACT(gbf.ap[0:64, :], gtmp.ap[64 * hh:64 * hh + 64, :], AF.Silu, R=[gtmp], W=[gbf])
                        TT("pool", ygs[br].ap[0:64, h, :], yT.ap[0:64, h, :], gbf.ap[0:64, :], ALU.mult, R=[yT, gbf], W=[ygs[br]])
            wbr_n = [0]
            wqm = {}
            for hf in range(2):
                DMA("pool", wobs[hf].ap, wo[:, :, hf * 512:(hf + 1) * 512], W=[wobs[hf]])
            for dc in range(8):
                pus = []
                par = dc % 2
                gms = [gms4[0], gms4[1]]
                e1, e2 = e1s[0], e2s[0]
                for br in range(2):
                    wsrc = wbd if br == 0 else wbf
                    wbr = wbrs[wbr_n[0] % 2]
                    wbr_n[0] += 1
                    DMA("pool", wbr.ap[0:64, :, :], wsrc[:, :, dc * 128:(dc + 1) * 128], W=[wbr])
                    pu = ps[4 * par + br]
                    MM(pu.ap, [(wbr.ap[0:64, h, :], ygs[br].ap[0:64, h, :]) for h in range(8)], R=[wbr, ygs[br]], W=[pu])
                    pus.append(pu)
                    mcol = br * 1024 + dc * 128
                    piece = 6 + mcol // 512
                    if wqm.get(br, (None, None))[0] != piece:
                        wqm[br] = (piece, load_wq(piece))
                    wbm = wqm[br][1]
                    pm = ps[4 * par + 2 + br]
                    qproj(pm, wbm, mcol % 512)
                    TT("dve", gms[br].ap, pm.ap, rbq.ap, ALU.mult, R=[pm, rbq], W=[gms[br]])
                    ACT(gms[br].ap, gms[br].ap, AF.Sigmoid, R=[gms[br], cv], W=[gms[br]], bias=BMRG(br * 8 + dc))
                TT("dve", e1.ap, pus[0].ap, gms[0].ap, ALU.mult, R=[pus[0], gms[0]], W=[e1])
                TT("dve", e2.ap, pus[1].ap, gms[1].ap, ALU.mult, R=[pus[1], gms[1]], W=[e2])
                TT("pool", mrg.ap[:, dc, :], e1.ap, e2.ap, ALU.add, R=[e1, e2], W=[mrg])
            for sb in range(4):
                r0 = q0 + sb * 128
                DMA("sp", xot.ap, xo[r0:r0 + 128, :], W=[xot])
                for hf in range(2):
                    wob = wobs[hf]
                    po = ps[(2 * sb + hf) % 8]
                    MM(po.ap, [(mrg.ap[:, dc, sb * 128:(sb + 1) * 128], wob.ap[:, dc, :]) for dc in range(8)], R=[mrg, wob], W=[po])
                    TT("dve", xnew.ap[:, hf * 512:(hf + 1) * 512], po.ap, xot.ap[:, hf * 512:(hf + 1) * 512], ALU.add,
                       R=[po, xot], W=[xnew])
                ACT(xot.ap, xnew.ap, AF.Square, R=[xnew, xot], W=[xot, fin_ss], accum=fin_ss.ap[:, 0:1])
                TS("dve", fin_r.ap, fin_ss.ap, 1.0 / D, ALU.mult, EPS, ALU.add, R=[fin_ss], W=[fin_r])
                ACT(fin_r.ap, fin_r.ap, AF.Sqrt, R=[fin_r], W=[fin_r])
                S.op("dve", lambda e: e.reciprocal(out=fin_r.ap, in_=fin_r.ap), _keys([fin_r]), _keys([fin_r]))
                STT(xnew.ap, xnew.ap, fin_r.ap[:, 0:1], fg_bc.ap, ALU.mult, ALU.mult, R=[xnew, fin_r, fg_bc], W=[xnew])
                DMA("sp", y[r0:r0 + 128, :], xnew.ap, R=[xnew], W=["y"])

        fw = [o for o in S.dlast if o is not None]
        with nc.Block() as block:
            S.emit(block, final_waits=fw)
    return nc


def _rot_perm():
    p = np.arange(64)
    p[0:8] = np.arange(8, 16)
    p[8:16] = np.arange(0, 8)
    return p


def _fm(w):
    n = w.shape[1]
    return np.ascontiguousarray(w.reshape(8, 128, n).transpose(1, 0, 2))


def prep_inputs(x, positions, norm_gain, w_in, b_forget, b_merge, w_branch_dsa, w_branch_fox, w_out, final_gain):
    x = np.asarray(x, np.float32)
    positions = np.asarray(positions, np.int32)
    W = np.asarray(w_in, np.float32)[0]
    o = 0
    cols = {}
    for name, n in (("aq", 512), ("ak", 512), ("av", 512), ("ag", 512), ("iq", 256), ("ik", 64), ("iw", 4),
                    ("fq", 512), ("fk", 512), ("fv", 512), ("fg", 512), ("fl", 8), ("mg", 2048)):
        cols[name] = W[:, o:o + n]
        o += n
    perm = _rot_perm()

    def rot(w, nh):
        idx = np.concatenate([h * 64 + perm for h in range(nh)])
        return w[:, idx]

    z32 = np.zeros((D, 32), np.float32)
    z24 = np.zeros((D, 24), np.float32)
    z64 = np.zeros((D, 64), np.float32)
    wk = np.concatenate([cols["fk"], cols["ak"], rot(cols["ak"], 8),
                         cols["ik"], z32, cols["fl"], z24, rot(cols["ik"], 1), z64,
                         cols["fv"], cols["av"]], axis=1)
    assert wk.shape[1] == NKC
    wq = np.concatenate([cols["fq"], cols["aq"], rot(cols["aq"], 8), cols["iq"], rot(cols["iq"], 4),
                         cols["ag"], cols["fg"], cols["mg"]], axis=1)
    assert wq.shape[1] == NQC
    wk_d, wq_d, wiw_d = _fm(wk), _fm(wq), _fm(np.ascontiguousarray(cols["iw"]))
    wbd = np.ascontiguousarray(np.asarray(w_branch_dsa, np.float32)[0].reshape(8, 64, D).transpose(1, 0, 2))
    wbf = np.ascontiguousarray(np.asarray(w_branch_fox, np.float32)[0].reshape(8, 64, D).transpose(1, 0, 2))
    wo = _fm(np.asarray(w_out, np.float32)[0])
    cvec = np.zeros((128, 64), np.float32)
    cvec[:, 0:8] = np.asarray(norm_gain, np.float32)[0].reshape(8, 128).T
    half = 8
    inv_freq = (500000.0 ** (-np.arange(half, dtype=np.float32) * 2.0 / 16.0)).astype(np.float32)
    for p in range(128):
        r = p % 64
        if r < 8:
            cvec[p, 8] = inv_freq[r]
            cvec[p, 9] = -1.0
        elif r < 16:
            cvec[p, 8] = inv_freq[r - 8]
            cvec[p, 9] = 1.0
    cvec[96:104, 10] = np.asarray(b_forget, np.float32)[0]
    cvec[:, 16:32] = np.asarray(b_merge, np.float32)[0].reshape(16, 128).T
    fgain = np.asarray(final_gain, np.float32).reshape(1, D)
    identf = np.eye(128, dtype=np.float32)
    in_maps = []
    xT_b = [np.ascontiguousarray(x[b].T.reshape(8, 128, S_LEN).transpose(1, 0, 2)) for b in range(x.shape[0])]
    for c in range(8):
        b, j = divmod(c, 4)
        p_idx = np.arange(128)[:, None]
        m_idx = np.arange(32)[None, :]
        bmaskf = (p_idx <= 4 * m_idx + j).astype(np.float32)
        s_idx = np.arange(512)[None, :]
        cbiasf = np.where(s_idx <= 4 * p_idx + j, 0.0, NEGB).astype(np.float32)
        in_maps.append({
            "xT": xT_b[b],
            "xTo": np.ascontiguousarray(xT_b[b][:, :, j::4]),
            "xo": np.ascontiguousarray(x[b, j::4, :]),
            "pos_all": np.ascontiguousarray(positions[b][None, :]),
            "pos_own": np.ascontiguousarray(positions[b][None, j::4]),
            "wk": wk_d, "wq": wq_d, "wiw": wiw_d, "wbd": wbd, "wbf": wbf, "wo": wo,
            "cvec": cvec, "fgain": fgain, "identf": identf, "bmaskf": bmaskf, "cbiasf": cbiasf,
        })
    return in_maps


_NC_CACHE = {}


def kernel(x, positions, norm_gain, w_in, b_forget, b_merge, w_branch_dsa, w_branch_fox, w_out, final_gain):
    in_maps = prep_inputs(x, positions, norm_gain, w_in, b_forget, b_merge, w_branch_dsa, w_branch_fox, w_out, final_gain)
    if "nc" not in _NC_CACHE:
        _NC_CACHE["nc"] = build()
    nc = _NC_CACHE["nc"]
    res = run_bass_kernel_spmd(nc, in_maps, core_ids=list(range(8)))
    out = np.empty((2, S_LEN, D), np.float32)
    for c in range(8):
        b, j = divmod(c, 4)
        out[b, j::4, :] = res.results[c]["y"]
    return out
```

```python
import math
import contextlib
import numpy as np
import concourse.bass as bass
import concourse.mybir as mybir
from concourse.bass_utils import run_bass_kernel_spmd

F32 = mybir.dt.float32
BF16 = mybir.dt.bfloat16
I32 = mybir.dt.int32
U8 = mybir.dt.uint8
AF = mybir.ActivationFunctionType
ALU = mybir.AluOpType

D = 1024
S_LEN = 8192
NQ = 2048
GQ = 512
NG = NQ // GQ
NKC = 2816
NQC = 5120
EPS = 1e-6
NIT = 16
RNG = 8.0
TOPK = 256.0
NEGB = -30000.0
MAGIC = 12582912.0
C1 = 6.28125
C2 = 2.0 * math.pi - 6.28125
ARENA = 164864
GPERS = 81920


class _Op:
    __slots__ = ("eng", "fn", "deps", "ticket", "has_dep", "dma", "sem", "target", "prev")

    def __init__(self, eng, fn, dma):
        self.eng = eng
        self.fn = fn
        self.deps = []
        self.ticket = None
        self.has_dep = False
        self.dma = dma
        self.sem = None
        self.target = None
        self.prev = None


class Sched:
    ENGS = ("pe", "act", "dve", "pool", "sp")

    def __init__(self, nc, n_dma_sems=14):
        self.nc = nc
        self.ops = {e: [] for e in self.ENGS}
        self.last_w = {}
        self.readers = {}
        self.nd = n_dma_sems
        self.rr = 0
        self.rr_sw = 0
        self.n_sw = 4
        self.dlast = [None] * n_dma_sems
        self.dcount = [0] * n_dma_sems
        self.bar_deps = []
        self.bar_pending = set()
        self.last_compute = {e: None for e in self.ENGS}

    def barrier(self):
        deps = [o for o in self.last_compute.values() if o is not None]
        deps += [o for o in self.dlast if o is not None]
        self.bar_deps = deps
        self.bar_pending = set(self.ENGS)
        self.last_w = {}
        self.readers = {}

    def op(self, eng, fn, reads=(), writes=(), dma=False):
        o = _Op(eng, fn, dma)
        deps = []
        if eng in self.bar_pending:
            deps.extend(self.bar_deps)
            self.bar_pending.discard(eng)
        for r in reads:
            w = self.last_w.get(r)
            if w is not None:
                deps.append(w)
        for w_ in writes:
            w = self.last_w.get(w_)
            if w is not None:
                deps.append(w)
            deps.extend(self.readers.get(w_, ()))
        seen = set()
        for d in deps:
            if d is o or id(d) in seen:
                continue
            seen.add(id(d))
            if d.eng == "pe" and eng == "pe" and not d.dma and not dma:
                continue
            o.deps.append(d)
            d.has_dep = True
        for r in reads:
            self.readers.setdefault(r, []).append(o)
        for w_ in writes:
            self.last_w[w_] = o
            self.readers[w_] = []
        if dma:
            if eng == "pool":
                k = self.rr_sw
                self.rr_sw = (self.rr_sw + 1) % self.n_sw
            else:
                k = self.n_sw + self.rr
                self.rr = (self.rr + 1) % (self.nd - self.n_sw)
            o.sem = k
            o.prev = self.dlast[k]
            self.dcount[k] += 16
            o.target = self.dcount[k]
            self.dlast[k] = o
        else:
            self.last_compute[eng] = o
        self.ops[eng].append(o)
        return o

    def alloc_sems(self, st):
        nc = self.nc
        self.esem = {e: st.enter_context(nc.semaphore("s_" + e)) for e in self.ENGS}
        self.dsem = [st.enter_context(nc.semaphore("d_%d" % i)) for i in range(self.nd)]

    def emit(self, block, final_waits=()):
        esem, dsem = self.esem, self.dsem
        for o in final_waits:
            o.has_dep = True
        for e in self.ENGS:
            c = 0
            for o in self.ops[e]:
                if (not o.dma) and o.has_dep:
                    c += 1
                    o.ticket = c

        def run(e, engobj, extra=None):
            waited = {}

            def wait_for(d):
                if d.dma:
                    key, val, sem = ("d", d.sem), d.target, dsem[d.sem]
                else:
                    key, val, sem = ("e", d.eng), d.ticket, esem[d.eng]
                if waited.get(key, 0) >= val:
                    return
                engobj.wait_ge(sem, val)
                waited[key] = val

            for o in self.ops[e]:
                for d in o.deps:
                    wait_for(d)
                if o.dma and o.prev is not None:
                    wait_for(o.prev)
                ins = o.fn(engobj)
                if o.dma:
                    ins.then_inc(dsem[o.sem], 16)
                elif o.has_dep:
                    ins.then_inc(esem[e], 1)
            if extra:
                for d in extra:
                    wait_for(d)

        @block.tensor
        def _(eng):
            run("pe", eng)

        @block.scalar
        def _(eng):
            run("act", eng)

        @block.vector
        def _(eng):
            run("dve", eng)

        @block.gpsimd
        def _(eng):
            run("pool", eng)

        @block.sync
        def _(eng):
            run("sp", eng, extra=list(final_waits))


class Buf:
    _n = 0

    def __init__(self, ap, name=None):
        Buf._n += 1
        self.ap = ap
        self.key = "%s#%d" % (name or "b", Buf._n)


def _keys(xs):
    out = []
    for x in xs:
        out.append(x.key if isinstance(x, Buf) else x)
    return out


_DT_SIZE = {F32: 4, BF16: 2, I32: 4, U8: 1}


class Arena:
    def __init__(self, t, nbytes):
        self.t = t
        self.n = nbytes
        self.off = 0

    def reset(self, off=0):
        self.off = off

    def alloc(self, shape, dt, name=None):
        n = 1
        for s in shape:
            n *= s
        nb = n * _DT_SIZE[dt]
        nb_al = (nb + 63) // 64 * 64
        assert self.off + nb_al <= self.n, ("arena overflow", name, self.off, nb_al, self.n)
        ap = self.t[:, self.off:self.off + nb].bitcast(dt)
        self.off += nb_al
        if len(shape) == 2:
            ap = ap.rearrange("p (a b) -> p a b", a=shape[0], b=shape[1])
        elif len(shape) == 3:
            ap = ap.rearrange("p (a b c) -> p a b c", a=shape[0], b=shape[1], c=shape[2])
        return Buf(ap, name)


def build(debug=False, stop_after=None):
    nc = bass.Bass("TRN2", target_bir_lowering=False)

    def din(name, shape, dt=F32):
        return nc.dram_tensor(name, list(shape), dt, kind="ExternalInput").ap()

    dbg_kind = "ExternalOutput" if debug else "Internal"

    def dscr(name, shape, dt):
        return nc.dram_tensor(name, list(shape), dt, kind=dbg_kind).ap()

    xT = din("xT", [128, 8, S_LEN])
    xTo = din("xTo", [128, 8, NQ])
    xo = din("xo", [NQ, D])
    pos_all = din("pos_all", [1, S_LEN], I32)
    pos_own = din("pos_own", [1, NQ], I32)
    wk = din("wk", [128, 8, NKC])
    wq = din("wq", [128, 8, NQC])
    wiw = din("wiw", [128, 8, 4])
    wbd = din("wbd", [64, 8, D])
    wbf = din("wbf", [64, 8, D])
    wo = din("wo", [128, 8, D])
    cvec = din("cvec", [128, 64])
    fgain = din("fgain", [1, D])
    identf = din("identf", [128, 128])
    bmaskf = din("bmaskf", [128, 32])
    cbiasf = din("cbiasf", [128, 512])
    y = nc.dram_tensor("y", [NQ, D], F32, kind="ExternalOutput").ap()

    kf_s = dscr("kf_s", [8, 68, S_LEN], BF16)
    ka_s = dscr("ka_s", [8, 64, S_LEN], BF16)
    ki_s = dscr("ki_s", [64, S_LEN], BF16)
    vf_s = dscr("vf_s", [8, 128, 64, 128], BF16)
    va_s = dscr("va_s", [8, 128, 64, 128], BF16)
    wq_s = nc.dram_tensor("wq_s", [128, 8, NQC], BF16, kind="Internal").ap()
    wbd_s = nc.dram_tensor("wbd_s", [64, 8, D], BF16, kind="Internal").ap()
    wbf_s = nc.dram_tensor("wbf_s", [64, 8, D], BF16, kind="Internal").ap()
    wo_s = nc.dram_tensor("wo_s", [128, 8, D], BF16, kind="Internal").ap()
    dbg = {}
    if debug:
        dbg["qfT"] = nc.dram_tensor("d_qfT", [128, 8, 512], BF16, kind="ExternalOutput").ap()
        dbg["qaT"] = nc.dram_tensor("d_qaT", [128, 8, 512], BF16, kind="ExternalOutput").ap()
        dbg["qiT"] = nc.dram_tensor("d_qiT", [128, 4, 512], BF16, kind="ExternalOutput").ap()
        dbg["sc"] = nc.dram_tensor("d_sc", [128, 8192], F32, kind="ExternalOutput").ap()
        dbg["lo"] = nc.dram_tensor("d_lo", [4, 128, 1], F32, kind="ExternalOutput").ap()
        dbg["mT"] = nc.dram_tensor("d_mT", [128, 64, 512], U8, kind="ExternalOutput").ap()
        dbg["yTa"] = nc.dram_tensor("d_yTa", [128, 8, 512], BF16, kind="ExternalOutput").ap()
        dbg["yTf"] = nc.dram_tensor("d_yTf", [128, 8, 512], BF16, kind="ExternalOutput").ap()

    st = contextlib.ExitStack()
    with st:
        def T(name, shape, dt):
            return Buf(st.enter_context(nc.sbuf_tensor(name, list(shape), dt))[:], name)

        arena_t = st.enter_context(nc.sbuf_tensor("arena", [128, ARENA], U8))
        AR = Arena(arena_t, ARENA)
        ps = [Buf(st.enter_context(nc.psum_tensor("ps%d" % i, [128, 512], F32))[:], "ps%d" % i) for i in range(8)]

        S = Sched(nc)
        S.alloc_sems(st)

        def DMA(q, out, in_, R=(), W=()):
            return S.op(q, lambda e: e.dma_start(out=out, in_=in_), _keys(R), _keys(W), dma=True)

        def TT(eng, out, in0, in1, op, R=(), W=()):
            return S.op(eng, lambda e: e.tensor_tensor(out=out, in0=in0, in1=in1, op=op), _keys(R), _keys(W))

        def TS(eng, out, in0, s1, op0, s2=None, op1=None, R=(), W=(), accum=None):
            def f(e):
                kw = dict(out=out, in0=in0, scalar1=s1, scalar2=s2, op0=op0)
                if op1 is not None:
                    kw["op1"] = op1
                if accum is not None:
                    kw["accum_out"] = accum
                return e.tensor_scalar(**kw)
            return S.op(eng, f, _keys(R), _keys(W))

        def STT(out, in0, scalar, in1, op0, op1, R=(), W=()):
            return S.op("dve", lambda e: e.scalar_tensor_tensor(out=out, in0=in0, scalar=scalar, in1=in1, op0=op0, op1=op1),
                        _keys(R), _keys(W))

        def ACT(out, in_, func, R=(), W=(), bias=None, scale=None, accum=None):
            def f(e):
                kw = dict(out=out, in_=in_, func=func)
                if bias is not None:
                    kw["bias"] = bias
                if scale is not None:
                    kw["scale"] = scale
                if accum is not None:
                    kw["accum_out"] = accum
                return e.activation(**kw)
            return S.op("act", f, _keys(R), _keys(W))

        def CP(eng, out, in_, R=(), W=()):
            return S.op(eng, lambda e: e.tensor_copy(out=out, in_=in_), _keys(R), _keys(W))

        def MS(eng, ap, val, W=()):
            return S.op(eng, lambda e: e.memset(ap, val), (), _keys(W))

        def MM(out, pairs, R=(), W=()):
            def f(e):
                ins = None
                n = len(pairs)
                for i, (l, r) in enumerate(pairs):
                    ins = e.matmul(out, lhsT=l, rhs=r, start=(i == 0), stop=(i == n - 1))
                return ins
            return S.op("pe", f, _keys(R), _keys(W))

        ones_bf = T("ones_bf", [128, 512], BF16)
        onesf = T("onesf", [128, 512], F32)
        ident = T("ident", [128, 128], BF16)
        bmask = T("bmask", [128, 32], BF16)
        cbias = T("cbias", [128, 512], F32)
        cv = T("cv", [128, 64], F32)
        nbf = T("nbf", [128, 1], F32)
        halfpi = T("halfpi", [128, 1], F32)
        wiwb = T("wiwb", [128, 8, 4], BF16)
        fg_bc = T("fg_bc", [128, D], F32)
        rcol_q = T("rcol_q", [128, 4], F32)
        absw = T("absw", [128, 16], F32)
        sgnw = T("sgnw", [128, 16], F32)
        w4 = T("w4", [128, 16], F32)
        rcol_as = [T("rcol_a0", [128, 4], F32), T("rcol_a1", [128, 4], F32)]
        bis_lo = T("bis_lo", [128, 1], F32)
        bis_mid = T("bis_mid", [128, 1], F32)
        bis_cnt = T("bis_cnt", [128, 1], F32)
        bis_ind = T("bis_ind", [128, 1], F32)
        fin_ss = T("fin_ss", [128, 1], F32)
        fin_r = T("fin_r", [128, 1], F32)

        GAIN = lambda c: cv.ap[:, c:c + 1]
        INVF = cv.ap[:, 8:9]
        SGNS = cv.ap[:, 9:10]
        BMRG = lambda c: cv.ap[:, 16 + c:17 + c]

        MS("dve", ones_bf.ap, 1.0, W=[ones_bf])
        MS("dve", onesf.ap, 1.0, W=[onesf])
        MS("dve", halfpi.ap, math.pi / 2, W=[halfpi])
        DMA("sp", cv.ap, cvec[:, :], W=[cv])
        DMA("sp", cbias.ap, cbiasf[:, :], W=[cbias])
        DMA("pool", ident.ap, identf[:, :], W=[ident])
        DMA("pool", bmask.ap, bmaskf[:, :], W=[bmask])
        DMA("pool", wiwb.ap, wiw[:, :, :], W=[wiwb])
        DMA("sp", fg_bc.ap, fgain[0:1, :].to_broadcast([128, D]), W=[fg_bc])
        TS("dve", nbf.ap, cv.ap[:, 10:11], -1.0, ALU.mult, R=[cv], W=[nbf])

        def load_x_dma(src, t0, xf):
            DMA("sp", xf.ap, src[:, :, t0:t0 + 512], W=[xf])

        def load_x_prep(xf, xb, xsq):
            ACT(xsq.ap, xf.ap, AF.Square, R=[xf], W=[xsq])
            for c in range(8):
                ACT(xb.ap[:, c, :], xf.ap[:, c, :], AF.Copy, R=[xf, cv], W=[xb], scale=GAIN(c))

        def load_x_group(src, t0, xf, xb, xsq):
            load_x_dma(src, t0, xf)
            load_x_prep(xf, xb, xsq)

        def rstd_gen(xsq, rbc, rcol, pA, pB):
            MM(pA.ap, [(ones_bf.ap[:, 0:128], xsq.ap[:, c, :]) for c in range(8)], R=[xsq, ones_bf], W=[pA])
            yield
            for tb in range(4):
                MM(pB.ap[:, tb:tb + 1], [(xsq.ap[:, c, tb * 128:(tb + 1) * 128], ones_bf.ap[:, 0:1]) for c in range(8)],
                   R=[xsq, ones_bf], W=[pB])
            yield
            TS("dve", rcol.ap, pB.ap[:, 0:4], 1.0 / D, ALU.mult, EPS, ALU.add, R=[pB], W=[rcol])
            TS("dve", rbc.ap, pA.ap, 1.0 / D, ALU.mult, EPS, ALU.add, R=[pA], W=[rbc])
            yield
            ACT(rcol.ap, rcol.ap, AF.Sqrt, R=[rcol], W=[rcol])
            ACT(rbc.ap, rbc.ap, AF.Sqrt, R=[rbc], W=[rbc])
            yield
            S.op("dve", lambda e: e.reciprocal(out=rcol.ap, in_=rcol.ap), _keys([rcol]), _keys([rcol]))
            yield
            S.op("dve", lambda e: e.reciprocal(out=rbc.ap, in_=rbc.ap), _keys([rbc]), _keys([rbc]))
            yield

        def rstd_from(xsq, xb_unused, rbc, rcol, pA, pB, scale_extra=1.0):
            for _ in rstd_gen(xsq, rbc, rcol, pA, pB):
                pass

        def rope_gen(possrc, t0, posi, ang, kk, sn, cs, rbc, scale_extra):
            DMA("sp", posi.ap, possrc[0:1, t0:t0 + 512].to_broadcast([128, 512]), W=[posi])
            CP("dve", ang.ap, posi.ap, R=[posi], W=[ang])
            yield
            TS("dve", ang.ap, ang.ap, INVF, ALU.mult, R=[ang, cv], W=[ang])
            yield
            TS("dve", kk.ap, ang.ap, 1.0 / (2.0 * math.pi), ALU.mult, MAGIC, ALU.add, R=[ang], W=[kk])
            yield
            TS("dve", kk.ap, kk.ap, -MAGIC, ALU.add, R=[kk], W=[kk])
            yield
            STT(ang.ap, kk.ap, -C1, ang.ap, ALU.mult, ALU.add, R=[kk, ang], W=[ang])
            yield
            STT(ang.ap, kk.ap, -C2, ang.ap, ALU.mult, ALU.add, R=[kk, ang], W=[ang])
            yield
            TS("dve", ang.ap, ang.ap, math.pi, ALU.min, -math.pi, ALU.max, R=[ang], W=[ang])
            yield
            STT(kk.ap, ang.ap, -1.0, ang.ap, ALU.mult, ALU.max, R=[ang], W=[kk])
            ACT(sn.ap, ang.ap, AF.Sin, R=[ang], W=[sn])
            ACT(cs.ap, kk.ap, AF.Sin, R=[kk, halfpi], W=[cs], bias=halfpi.ap[:, 0:1], scale=-1.0)
            yield
            STT(sn.ap, sn.ap, SGNS, rbc.ap, ALU.mult, ALU.mult, R=[sn, cv, rbc], W=[sn])
            yield
            if scale_extra != 1.0:
                TS("dve", sn.ap, sn.ap, scale_extra, ALU.mult, R=[sn], W=[sn])
                STT(cs.ap, cs.ap, scale_extra, rbc.ap, ALU.mult, ALU.mult, R=[cs, rbc], W=[cs])
            else:
                TT("dve", cs.ap, cs.ap, rbc.ap, ALU.mult, R=[cs, rbc], W=[cs])
            yield

        def rope_tables(possrc, t0, posi, ang, kk, sn, cs, rbc, scale_extra):
            for _ in rope_gen(possrc, t0, posi, ang, kk, sn, cs, rbc, scale_extra):
                pass

        AR.reset(0)
        wkb = AR.alloc([8, NKC], BF16, "wkb")
        xfs = [AR.alloc([8, 512], F32, "xf") for _ in range(2)]
        xbs = [AR.alloc([8, 512], BF16, "xb") for _ in range(2)]
        xsq1 = AR.alloc([8, 512], BF16, "xsq")
        posi = AR.alloc([512], I32, "posi")
        ang = AR.alloc([512], F32, "ang")
        kk = AR.alloc([512], F32, "kk")
        sns = [AR.alloc([512], F32, "sn") for _ in range(2)]
        css = [AR.alloc([512], F32, "cs") for _ in range(2)]
        rbcs = [AR.alloc([512], F32, "rbc") for _ in range(2)]
        t1s = [AR.alloc([512], F32, "t1") for _ in range(2)]
        t2s = [AR.alloc([512], F32, "t2") for _ in range(2)]
        ksts = [AR.alloc([512], BF16, "kst") for _ in range(2)]
        mf = AR.alloc([512], F32, "mf")
        kist = AR.alloc([512], BF16, "kist")
        ee = AR.alloc([512], F32, "ee")
        ncums = [AR.alloc([512], F32, "ncum") for _ in range(2)]
        r1s = AR.alloc([512], F32, "r1s")
        aug = AR.alloc([3, 512], BF16, "aug")
        vsts = [AR.alloc([8, 4, 128], BF16, "vst") for _ in range(2)]

        for i in range(4):
            DMA("pool", wkb.ap[:, :, i * 704:(i + 1) * 704], wk[:, :, i * 704:(i + 1) * 704], W=[wkb])
        for v in vsts:
            MS("pool", v.ap, 1.0, W=[v])
        for i in range(NQC // 512):
            DMA("pool", wq_s[:, :, i * 512:(i + 1) * 512], wq[:, :, i * 512:(i + 1) * 512], W=["wq_s"])
        for i in range(2):
            DMA("pool", wbd_s[:, :, i * 512:(i + 1) * 512], wbd[:, :, i * 512:(i + 1) * 512], W=["wbd_s"])
            DMA("pool", wbf_s[:, :, i * 512:(i + 1) * 512], wbf[:, :, i * 512:(i + 1) * 512], W=["wbf_s"])
            DMA("pool", wo_s[:, :, i * 512:(i + 1) * 512], wo[:, :, i * 512:(i + 1) * 512], W=["wo_s"])

        n_tg = S_LEN // 512
        if stop_after == "A1":
            n_tg = 1
        if stop_after == "A0":
            n_tg = 0
        kst_i = 0
        def chain_gen(tgn):
            k = tgn % 2
            for _ in rstd_gen(xsq1, rbcs[k], rcol_as[k], ps[0], ps[1]):
                yield
            for _ in rope_gen(pos_all, tgn * 512, posi, ang, kk, sns[k], css[k], rbcs[k], 1.0):
                yield

        if n_tg > 0:
            load_x_dma(xT, 0, xfs[0])
            load_x_prep(xfs[0], xbs[0], xsq1)
            for _ in chain_gen(0):
                pass
        for tg in range(n_tg):
            t0 = tg * 512
            xb = xbs[tg % 2]
            rbc = rbcs[tg % 2]
            rcol_a = rcol_as[tg % 2]
            sn = sns[tg % 2]
            cs = css[tg % 2]
            if tg + 1 < n_tg:
                load_x_dma(xT, t0 + 512, xfs[(tg + 1) % 2])

            def proj_chunk(pbuf, n0):
                MM(pbuf.ap, [(wkb.ap[:, c, n0:n0 + 128], xb.ap[:, c, :]) for c in range(8)], R=[wkb, xb], W=[pbuf])

            for cc in range(4):
                pb = ps[2 + (cc % 2)]
                proj_chunk(pb, cc * 128)
                kst = ksts[kst_i % 2]
                kst_i += 1
                TT("dve", kst.ap, pb.ap, rbc.ap, ALU.mult, R=[pb, rbc], W=[kst])
                DMA("sp", kf_s[2 * cc, 0:64, t0:t0 + 512], kst.ap[0:64, :], R=[kst], W=["kf_s"])
                DMA("sp", kf_s[2 * cc + 1, 0:64, t0:t0 + 512], kst.ap[64:128, :], R=[kst], W=["kf_s"])
            DMA("sp", kf_s[:, 64, t0:t0 + 512], ones_bf.ap[0:8, :], R=[ones_bf], W=["kf_s"])
            for br in range(2):
                vst = vsts[br]
                dst = vf_s if br == 0 else va_s
                n0 = 1792 + br * 512
                for tb in range(4):
                    pv = ps[6 + (tb % 2)]
                    MM(pv.ap, [(xb.ap[:, c, tb * 128:(tb + 1) * 128], wkb.ap[:, c, n0:n0 + 512]) for c in range(8)],
                       R=[xb, wkb], W=[pv])
                    ACT(vst.ap[:, :, tb, 0:64], pv.ap.rearrange("p (h c) -> p h c", c=64), AF.Copy,
                        R=[pv, rcol_a], W=[vst], scale=rcol_a.ap[:, tb:tb + 1])
                for h in range(8):
                    DMA("sp", dst[h, :, tg * 4:(tg + 1) * 4, :], vst.ap[:, h, :, :], R=[vst], W=["v_s%d" % br])

            ahead = None
            if tg + 1 < n_tg:
                load_x_prep(xfs[(tg + 1) % 2], xbs[(tg + 1) % 2], xsq1)
                ahead = chain_gen(tg + 1)

            def adv(n):
                if ahead is not None:
                    for _ in range(n):
                        try:
                            next(ahead)
                        except StopIteration:
                            break

            for cc in range(5):
                pa, pb = (ps[4], ps[5]) if cc % 2 == 0 else (ps[2], ps[3])
                if cc < 4:
                    proj_chunk(pa, 512 + cc * 128)
                    proj_chunk(pb, 1024 + cc * 128)
                else:
                    proj_chunk(pa, 1536)
                    proj_chunk(pb, 1664)
                adv(2)
                t1 = t1s[cc % 2]
                t2 = t2s[cc % 2]
                TT("dve", t1.ap, pa.ap, cs.ap, ALU.mult, R=[pa, cs], W=[t1])
                adv(1)
                TT("dve", t2.ap, pb.ap, sn.ap, ALU.mult, R=[pb, sn], W=[t2])
                adv(1)
                if cc < 4:
                    kst = ksts[kst_i % 2]
                    kst_i += 1
                    TT("pool", kst.ap, t1.ap, t2.ap, ALU.add, R=[t1, t2], W=[kst])
                    DMA("sp", ka_s[2 * cc, :, t0:t0 + 512], kst.ap[0:64, :], R=[kst], W=["ka_s"])
                    DMA("sp", ka_s[2 * cc + 1, :, t0:t0 + 512], kst.ap[64:128, :], R=[kst], W=["ka_s"])
                else:
                    TT("pool", mf.ap, t1.ap, t2.ap, ALU.add, R=[t1, t2], W=[mf])
                    CP("pool", kist.ap[0:64, :], mf.ap[0:64, :], R=[mf], W=[kist])
                    DMA("sp", ki_s[:, t0:t0 + 512], kist.ap[0:64, :], R=[kist], W=["ki_s"])
                    ACT(ee.ap[96:104, :], mf.ap[96:104, :], AF.Exp, R=[mf, nbf], W=[ee], bias=nbf.ap[96:104, 0:1], scale=-1.0)
                    ACT(ee.ap[96:104, :], ee.ap[96:104, :], AF.Ln, R=[ee], W=[ee], bias=onesf.ap[96:104, 0:1], scale=1.0)
                    nc_cur = ncums[tg % 2]
                    nc_prev = ncums[(tg + 1) % 2]
                    init = 0.0 if tg == 0 else nc_prev.ap[96:104, 511:512]
                    S.op("dve", (lambda o_, d1_, i_: (lambda e: e.tensor_tensor_scan(
                        out=o_, data0=onesf.ap[96:104, :], data1=d1_, initial=i_, op0=ALU.mult, op1=ALU.add)))(
                        nc_cur.ap[96:104, :], ee.ap[96:104, :], init),
                        _keys([ee, onesf, nc_prev]), _keys([nc_cur]))
                    CP("dve", aug.ap[96:104, 0, :], nc_cur.ap[96:104, :], R=[nc_cur], W=[aug])
                    TT("dve", r1s.ap[96:104, :], nc_cur.ap[96:104, :], aug.ap[96:104, 0, :], ALU.subtract, R=[nc_cur, aug], W=[r1s])
                    CP("dve", aug.ap[96:104, 1, :], r1s.ap[96:104, :], R=[r1s], W=[aug])
                    TT("dve", r1s.ap[96:104, :], r1s.ap[96:104, :], aug.ap[96:104, 1, :], ALU.subtract, R=[r1s, aug], W=[r1s])
                    CP("dve", aug.ap[96:104, 2, :], r1s.ap[96:104, :], R=[r1s], W=[aug])
                    DMA("sp", kf_s[:, 65:68, t0:t0 + 512], aug.ap[96:104, :, :], R=[aug], W=["kf_s"])
            adv(1000)
        n_groups = NG
        if stop_after in ("A", "A1", "A0"):
            n_groups = 0
        elif stop_after is not None and stop_after.startswith("B"):
            n_groups = 1
        for g in range(n_groups):
            S.barrier()
            q0 = g * GQ
            win0 = 2048 * g
            L = 2048 * (g + 1)
            nkb = L // 128
            AR.reset(0)
            qfT = AR.alloc([8, 512], BF16, "qfT")
            qaT = AR.alloc([8, 512], BF16, "qaT")
            qiT = AR.alloc([4, 512], BF16, "qiT")
            xqb = AR.alloc([8, 512], BF16, "xqb")
            rbq = AR.alloc([512], F32, "rbq")
            mT = AR.alloc([64, 512], U8, "mT")
            yTa = AR.alloc([8, 512], BF16, "yTa")
            yTf = AR.alloc([8, 512], BF16, "yTf")
            assert AR.off <= GPERS, AR.off

            AR.reset(GPERS)
            xqf = AR.alloc([8, 512], F32, "xqf")
            xsqq = AR.alloc([8, 512], BF16, "xsqq")
            wqps = [AR.alloc([8, 512], BF16, "wqp") for _ in range(2)]
            posi = AR.alloc([512], I32, "posi")
            ang = AR.alloc([512], F32, "ang")
            kk = AR.alloc([512], F32, "kk")
            sn = AR.alloc([512], F32, "sn")
            cs = AR.alloc([512], F32, "cs")
            rb8 = AR.alloc([512], F32, "rb8")
            t1s = [AR.alloc([512], F32, "t1") for _ in range(2)]
            t2s = [AR.alloc([512], F32, "t2") for _ in range(2)]
            craws = [AR.alloc([2048], BF16, "craw") for _ in range(2)]

            load_x_group(xTo, q0, xqf, xqb, xsqq)
            rstd_from(xsqq, xqb, rbq, rcol_q, ps[0], ps[1])
            TS("dve", rb8.ap, rbq.ap, 0.125, ALU.mult, R=[rbq], W=[rb8])
            qchain = rope_gen(pos_own, q0, posi, ang, kk, sn, cs, rbq, 0.125)

            def qadv(n):
                for _ in range(n):
                    try:
                        next(qchain)
                    except StopIteration:
                        break

            wq_i = [0]

            def load_wq(piece):
                wb = wqps[wq_i[0] % 2]
                wq_i[0] += 1
                DMA("sp", wb.ap, wq_s[:, :, piece * 512:(piece + 1) * 512], R=["wq_s"], W=[wb])
                return wb

            def qproj(pbuf, wb, n0):
                MM(pbuf.ap, [(wb.ap[:, c, n0:n0 + 128], xqb.ap[:, c, :]) for c in range(8)], R=[wb, xqb], W=[pbuf])

            MS("pool", qaT.ap[64:128, :, :], 0.0, W=[qaT])
            MS("pool", qfT.ap[64:68, :, :], 1.0, W=[qfT])
            wb = load_wq(0)
            for cc in range(4):
                pb = ps[2 + cc]
                qproj(pb, wb, cc * 128)
            for cc in range(4):
                pb = ps[2 + cc]
                TT("dve", qfT.ap[0:64, 2 * cc, :], pb.ap[0:64, :], rb8.ap[0:64, :], ALU.mult, R=[pb, rb8], W=[qfT])
                qadv(2)
                TT("dve", qfT.ap[0:64, 2 * cc + 1, :], pb.ap[64:128, :], rb8.ap[64:128, :], ALU.mult, R=[pb, rb8], W=[qfT])
                qadv(2)
            qadv(1000)
            for h in range(8):
                cr = craws[h % 2]
                DMA("sp", cr.ap[64:65, :], kf_s[h, 65:66, win0:win0 + 2048], R=["kf_s"], W=[cr])
                TS("dve", qfT.ap[64:65, h, :], cr.ap[64:65, :].rearrange("p (i r) -> p i r", r=4)[:, :, 0], -1.0, ALU.mult,
                   R=[cr], W=[qfT])
            wb1 = load_wq(1)
            wb2 = load_wq(2)
            for cc in range(4):
                pa, pb = ps[4 + 2 * (cc % 2)], ps[5 + 2 * (cc % 2)]
                qproj(pa, wb1, cc * 128)
                qproj(pb, wb2, cc * 128)
                t1 = t1s[cc % 2]
                t2 = t2s[cc % 2]
                TT("dve", t1.ap, pa.ap, cs.ap, ALU.mult, R=[pa, cs], W=[t1])
                TT("dve", t2.ap, pb.ap, sn.ap, ALU.mult, R=[pb, sn], W=[t2])
                TT("pool", qaT.ap[0:64, 2 * cc, :], t1.ap[0:64, :], t2.ap[0:64, :], ALU.add, R=[t1, t2], W=[qaT])
                TT("pool", qaT.ap[0:64, 2 * cc + 1, :], t1.ap[64:128, :], t2.ap[64:128, :], ALU.add, R=[t1, t2], W=[qaT])
            wb = load_wq(3)
            for cc in range(2):
                pa, pb = ps[4 + 2 * (cc % 2)], ps[5 + 2 * (cc % 2)]
                qproj(pa, wb, cc * 128)
                qproj(pb, wb, 256 + cc * 128)
                t1 = t1s[cc % 2]
                t2 = t2s[cc % 2]
                TT("dve", t1.ap, pa.ap, cs.ap, ALU.mult, R=[pa, cs], W=[t1])
                TT("dve", t2.ap, pb.ap, sn.ap, ALU.mult, R=[pb, sn], W=[t2])
                TT("pool", qiT.ap[0:64, 2 * cc, :], t1.ap[0:64, :], t2.ap[0:64, :], ALU.add, R=[t1, t2], W=[qiT])
                TT("pool", qiT.ap[0:64, 2 * cc + 1, :], t1.ap[64:128, :], t2.ap[64:128, :], ALU.add, R=[t1, t2], W=[qiT])
            for sb in range(4):
                MM(ps[1].ap[:, 8 + 4 * sb:12 + 4 * sb],
                   [(xqb.ap[:, c, sb * 128:(sb + 1) * 128], wiwb.ap[:, c, :]) for c in range(8)], R=[xqb, wiwb], W=[ps[1]])
                TS("dve", w4.ap[:, 4 * sb:4 * sb + 4], ps[1].ap[:, 8 + 4 * sb:12 + 4 * sb], rcol_q.ap[:, sb:sb + 1], ALU.mult,
                   0.5, ALU.mult, R=[ps[1], rcol_q], W=[w4])
            STT(absw.ap, w4.ap, -1.0, w4.ap, ALU.mult, ALU.max, R=[w4], W=[absw])
            TS("dve", sgnw.ap, w4.ap, 0.0, ALU.is_ge, 2.0, ALU.mult, R=[w4], W=[sgnw])
            TS("dve", sgnw.ap, sgnw.ap, -1.0, ALU.add, R=[sgnw], W=[sgnw])
            if debug and g == 0:
                DMA("sp", dbg["qfT"][:, :, :], qfT.ap, R=[qfT], W=["dbg1"])
                DMA("sp", dbg["qaT"][:, :, :], qaT.ap, R=[qaT], W=["dbg2"])
                DMA("sp", dbg["qiT"][:, :, :], qiT.ap, R=[qiT], W=["dbg3"])
            if stop_after == "B1":
                break

            S.barrier()
            AR.reset(GPERS)
            sc = AR.alloc([8192], F32, "sc")
            msk = AR.alloc([8192], BF16, "msk")
            rts = [AR.alloc([512], F32, "rt") for _ in range(2)]
            kits = [AR.alloc([2048], BF16, "kit") for _ in range(1)]
            f_kTps = [AR.alloc([2048], BF16, "kTp") for _ in range(2)]
            f_vps = [AR.alloc([16, 128], BF16, "vp") for _ in range(2)]
            f_Pts = [AR.alloc([512], BF16, "Pt") for _ in range(4)]
            f_recs = [AR.alloc([512], F32, "rec") for _ in range(1)]

            def attn_steps(br, kTps, vps, Pts, Pms, recs, psS, psO):
                tiles = [(h, pc, kb) for h in range(8) for pc in range(g + 1) for kb in range(16)]
                pend = []
                nS = len(psS)
                cur = {}
                piece_n = 0

                def issue_pv(item):
                    (h, pc, kb, pmat, vp, first, last) = item
                    po = psO[h % len(psO)]
                    S.op("pe", (lambda o_, l_, r_, f_, s_: (lambda e: e.matmul(o_, lhsT=l_, rhs=r_, start=f_, stop=s_)))(
                        po.ap, vp.ap[:, kb, :], pmat.ap, first, last), _keys([vp, pmat]), _keys([po]))
                    if last:
                        yT = yTa if br == 0 else yTf
                        rec = recs[h % len(recs)]
                        S.op("dve", (lambda o_, i_: (lambda e: e.reciprocal(out=o_, in_=i_)))(rec.ap[0:64, :], po.ap[64:128, :]),
                             _keys([po]), _keys([rec]))
                        TT("dve", yT.ap[0:64, h, :], po.ap[0:64, :], rec.ap[0:64, :], ALU.mult, R=[po, rec], W=[yT])

                for ti, (h, pc, kb) in enumerate(tiles):
                    if kb == 0:
                        kTp = kTps[piece_n % len(kTps)]
                        vp = vps[piece_n % len(vps)]
                        piece_n += 1
                        if br == 0:
                            DMA("sp", kTp.ap[0:64, :], ka_s[h, :, pc * 2048:(pc + 1) * 2048], R=["ka_s"], W=[kTp])
                            DMA("sp", vp.ap, va_s[h, :, pc * 16:(pc + 1) * 16, :], R=["v_s1"], W=[vp])
                        else:
                            DMA("sp", kTp.ap[0:68, :], kf_s[h, :, pc * 2048:(pc + 1) * 2048], R=["kf_s"], W=[kTp])
                            DMA("sp", vp.ap, vf_s[h, :, pc * 16:(pc + 1) * 16, :], R=["v_s0"], W=[vp])
                        cur["k"], cur["v"] = kTp, vp
                    kTp, vp = cur["k"], cur["v"]
                    pS = psS[ti % nS]
                    Pt = Pts[ti % len(Pts)]
                    diag = (br == 1 and pc == g)
                    c0 = 32 * kb if diag else 0
                    if br == 0:
                        MM(pS.ap[:, c0:512], [(kTp.ap[:, kb * 128:(kb + 1) * 128], qaT.ap[:, h, c0:512])], R=[kTp, qaT], W=[pS])
                    else:
                        MM(pS.ap[:, c0:512], [(kTp.ap[0:68, kb * 128:(kb + 1) * 128], qfT.ap[0:68, h, c0:512])], R=[kTp, qfT], W=[pS])
                    ACT(Pt.ap[:, c0:512], pS.ap[:, c0:512], AF.Exp, R=[pS], W=[Pt])
                    if br == 0:
                        Pm = Pms[ti % len(Pms)]
                        TT("pool" if ti % 2 == 1 else "dve", Pm.ap, Pt.ap, mT.ap[:, pc * 16 + kb, :], ALU.mult, R=[Pt, mT], W=[Pm])
                        pmat = Pm
                    else:
                        if diag:
                            if c0 > 0:
                                MS("pool", Pt.ap[:, 0:c0], 0.0, W=[Pt])
                            TT("pool", Pt.ap[:, c0:c0 + 32], Pt.ap[:, c0:c0 + 32], bmask.ap, ALU.mult, R=[Pt, bmask], W=[Pt])
                        pmat = Pt
                    pend.append((h, pc, kb, pmat, vp, (pc == 0 and kb == 0), (pc == g and kb == 15)))
                    if len(pend) > min(4, nS - 1):
                        issue_pv(pend.pop(0))
                    yield 1
                while pend:
                    issue_pv(pend.pop(0))

            def b2_units():
                for sb in range(4):
                    nch = 4 * g + sb + 1
                    Lsb = 512 * nch
                    kit = kits[0]
                    for ch in range(nch):
                        if ch % 4 == 0:
                            pc = ch // 4
                            DMA("sp", kit.ap[0:64, :], ki_s[:, pc * 2048:(pc + 1) * 2048], R=["ki_s"], W=[kit])
                        for h in range(4):
                            pb = ps[h]
                            MM(pb.ap, [(qiT.ap[0:64, h, sb * 128:(sb + 1) * 128], kit.ap[0:64, (ch % 4) * 512:(ch % 4 + 1) * 512])],
                               R=[qiT, kit], W=[pb])
                        for h in range(4):
                            pb = ps[h]
                            rt = rts[h % 2]
                            ACT(rt.ap, pb.ap, AF.Relu, R=[pb, absw], W=[rt], scale=absw.ap[:, 4 * sb + h:4 * sb + h + 1])
                            scs = sc.ap[:, ch * 512:(ch + 1) * 512]
                            sg = sgnw.ap[:, 4 * sb + h:4 * sb + h + 1]
                            if h == 0:
                                TS("dve", scs, rt.ap, sg, ALU.mult, R=[rt, sgnw], W=[sc])
                            else:
                                STT(scs, rt.ap, sg, scs, ALU.mult, ALU.add, R=[rt, sgnw, sc], W=[sc])
                        if ch == nch - 1:
                            TT("dve", sc.ap[:, ch * 512:(ch + 1) * 512], sc.ap[:, ch * 512:(ch + 1) * 512], cbias.ap, ALU.add,
                               R=[sc, cbias], W=[sc])
                        yield 3.2
                    if debug and g == 0 and sb == 3:
                        DMA("sp", dbg["sc"][:, 0:Lsb], sc.ap[:, 0:Lsb], R=[sc], W=["dbg4"])
                    MS("dve", bis_lo.ap, -RNG, W=[bis_lo])
                    for it in range(NIT):
                        step = RNG / (2.0 ** it)
                        TS("dve", bis_mid.ap, bis_lo.ap, step, ALU.add, R=[bis_lo], W=[bis_mid])
                        TS("dve", msk.ap[:, 0:Lsb], sc.ap[:, 0:Lsb], bis_mid.ap[:, 0:1], ALU.is_ge, None, ALU.add,
                           R=[sc, bis_mid], W=[msk, bis_cnt], accum=bis_cnt.ap[:, 0:1])
                        TS("dve", bis_ind.ap, bis_cnt.ap, TOPK, ALU.is_ge, step, ALU.mult, R=[bis_cnt], W=[bis_ind])
                        TT("dve", bis_lo.ap, bis_lo.ap, bis_ind.ap, ALU.add, R=[bis_lo, bis_ind], W=[bis_lo])
                        yield 0.5 + Lsb * 1.05e-3
                    TS("dve", bis_ind.ap, bis_lo.ap, -RNG, ALU.is_le, -1000.0, ALU.mult, R=[bis_lo], W=[bis_ind])
                    TT("dve", bis_lo.ap, bis_lo.ap, bis_ind.ap, ALU.add, R=[bis_lo, bis_ind], W=[bis_lo])
                    TS("dve", msk.ap[:, 0:Lsb], sc.ap[:, 0:Lsb], bis_lo.ap[:, 0:1], ALU.is_ge, R=[sc, bis_lo], W=[msk])
                    if debug and g == 0:
                        DMA("sp", dbg["lo"][sb], bis_lo.ap, R=[bis_lo], W=["dbg5"])
                    yield Lsb * 0.6e-3
                    for k4 in range(Lsb // 512):
                        pt = ps[k4 % 2]
                        ptb = pt.ap.bitcast(BF16)
                        for i in range(4):
                            kb = k4 * 4 + i
                            S.op("pe", (lambda o_, i_: (lambda e: e.transpose(out=o_, in_=i_, identity=ident.ap)))(
                                ptb[:, i * 128:(i + 1) * 128], msk.ap[:, kb * 128:(kb + 1) * 128]),
                                _keys([msk, ident]), _keys([pt]))
                        ACT(mT.ap[:, k4 * 4:(k4 + 1) * 4, sb * 128:(sb + 1) * 128],
                            ptb[:, 0:512].rearrange("p (a b) -> p a b", b=128), AF.Copy, R=[pt], W=[mT])
                        yield 0.7
                    if Lsb // 128 < nkb:
                        MS("pool", mT.ap[:, Lsb // 128:nkb, sb * 128:(sb + 1) * 128], 0, W=[mT])

            fox = attn_steps(1, f_kTps, f_vps, f_Pts, None, f_recs, [ps[4], ps[5]], [ps[6], ps[7]])
            n_fox = 128 * (g + 1)
            units = list()
            tot_est = 0.0
            for sb in range(4):
                nch = 4 * g + sb + 1
                Lsb = 512 * nch
                tot_est += nch * 3.2 + NIT * (0.5 + Lsb * 1.05e-3) + Lsb * 0.6e-3 + (Lsb // 512) * 0.7
            rate = n_fox / (0.9 * tot_est)
            acc = 0.0
            fox_done = False
            for wgt in b2_units():
                acc += wgt * rate
                while acc >= 1.0 and not fox_done:
                    acc -= 1.0
                    try:
                        next(fox)
                    except StopIteration:
                        fox_done = True
            if not fox_done:
                for _ in fox:
                    pass
            if debug and g == 0:
                DMA("sp", dbg["mT"][:, 0:nkb, :], mT.ap[:, 0:nkb, :], R=[mT], W=["dbg6"])
            if stop_after == "B2":
                break

            S.barrier()
            AR.reset(GPERS)
            kTps = [AR.alloc([2048], BF16, "kTp") for _ in range(3)]
            vps = [AR.alloc([16, 128], BF16, "vp") for _ in range(3)]
            Pts = [AR.alloc([512], BF16, "Pt") for _ in range(6)]
            Pms = [AR.alloc([512], BF16, "Pm") for _ in range(6)]
            recs = [AR.alloc([512], F32, "rec") for _ in range(2)]
            for kt_ in kTps:
                MS("pool", kt_.ap[64:128, :], 0.0, W=[kt_])
            for _ in attn_steps(0, kTps, vps, Pts, Pms, recs, [ps[0], ps[1], ps[2], ps[3], ps[6], ps[7]], [ps[4], ps[5]]):
                pass
            if debug and g == 0:
                DMA("sp", dbg["yTa"][:, :, :], yTa.ap, R=[yTa], W=["dbg7"])
                DMA("sp", dbg["yTf"][:, :, :], yTf.ap, R=[yTf], W=["dbg8"])
            if stop_after == "B3":
                break

            S.barrier()
            AR.reset(GPERS)
            wqps = [AR.alloc([8, 512], BF16, "wqp") for _ in range(2)]
            wbrs = [AR.alloc([8, 128], BF16, "wbr") for _ in range(2)]
            wobs = [AR.alloc([8, 512], BF16, "wob") for _ in range(2)]
            ygs = [AR.alloc([8, 512], BF16, "yg") for _ in range(2)]
            mrg = AR.alloc([8, 512], BF16, "mrg")
            gtmp = AR.alloc([512], F32, "gtmp")
            gbf = AR.alloc([512], BF16, "gbf")
            gms4 = [AR.alloc([512], F32, "gm") for _ in range(2)]
            e1s = [AR.alloc([512], F32, "e1") for _ in range(1)]
            e2s = [AR.alloc([512], F32, "e2") for _ in range(1)]
            xot = AR.alloc([D], F32, "xot")
            xnew = AR.alloc([D], F32, "xnew")
            wq_i = [0]
            for br in range(2):
                wb = load_wq(4 + br)
                yT = yTa if br == 0 else yTf
                for cc in range(4):
                    pb = ps[cc % 2]
                    qproj(pb, wb, cc * 128)
                    TT("dve", gtmp.ap, pb.ap, rbq.ap, ALU.mult, R=[pb, rbq], W=[gtmp])
                    for hh in range(2):
                        h = 2 * cc + hh
                        ACT(gbf.ap[0:64, :], gtmp.ap[64 * hh:64 * hh + 64, :], AF.Silu, R=[gtmp], W=[gbf])
                        TT("pool", ygs[br].ap[0:64, h, :], yT.ap[0:64, h, :], gbf.ap[0:64, :], ALU.mult, R=[yT, gbf], W=[ygs[br]])
            wbr_n = [0]
            wqm = {}
            for hf in range(2):
                DMA("sp", wobs[hf].ap, wo_s[:, :, hf * 512:(hf + 1) * 512], R=["wo_s"], W=[wobs[hf]])
            for dc in range(8):
                pus = []
                par = dc % 2
                gms = [gms4[0], gms4[1]]
                e1, e2 = e1s[0], e2s[0]
                for br in range(2):
                    wsrc = wbd_s if br == 0 else wbf_s
                    wbr = wbrs[wbr_n[0] % 2]
                    wbr_n[0] += 1
                    DMA("sp", wbr.ap[0:64, :, :], wsrc[:, :, dc * 128:(dc + 1) * 128], R=["wbd_s", "wbf_s"], W=[wbr])
                    pu = ps[4 * par + br]
                    MM(pu.ap, [(wbr.ap[0:64, h, :], ygs[br].ap[0:64, h, :]) for h in range(8)], R=[wbr, ygs[br]], W=[pu])
                    pus.append(pu)
                    mcol = br * 1024 + dc * 128
                    piece = 6 + mcol // 512
                    if wqm.get(br, (None, None))[0] != piece:
                        wqm[br] = (piece, load_wq(piece))
                    wbm = wqm[br][1]
                    pm = ps[4 * par + 2 + br]
                    qproj(pm, wbm, mcol % 512)
                    TT("dve", gms[br].ap, pm.ap, rbq.ap, ALU.mult, R=[pm, rbq], W=[gms[br]])
                    ACT(gms[br].ap, gms[br].ap, AF.Sigmoid, R=[gms[br], cv], W=[gms[br]], bias=BMRG(br * 8 + dc))
                TT("dve", e1.ap, pus[0].ap, gms[0].ap, ALU.mult, R=[pus[0], gms[0]], W=[e1])
                TT("dve", e2.ap, pus[1].ap, gms[1].ap, ALU.mult, R=[pus[1], gms[1]], W=[e2])
                TT("pool", mrg.ap[:, dc, :], e1.ap, e2.ap, ALU.add, R=[e1, e2], W=[mrg])
            for sb in range(4):
                r0 = q0 + sb * 128
                DMA("sp", xot.ap, xo[r0:r0 + 128, :], W=[xot])
                for hf in range(2):
                    wob = wobs[hf]
                    po = ps[(2 * sb + hf) % 8]
                    MM(po.ap, [(mrg.ap[:, dc, sb * 128:(sb + 1) * 128], wob.ap[:, dc, :]) for dc in range(8)], R=[mrg, wob], W=[po])
                    TT("dve", xnew.ap[:, hf * 512:(hf + 1) * 512], po.ap, xot.ap[:, hf * 512:(hf + 1) * 512], ALU.add,
                       R=[po, xot], W=[xnew])
                ACT(xot.ap, xnew.ap, AF.Square, R=[xnew, xot], W=[xot, fin_ss], accum=fin_ss.ap[:, 0:1])
                TS("dve", fin_r.ap, fin_ss.ap, 1.0 / D, ALU.mult, EPS, ALU.add, R=[fin_ss], W=[fin_r])
                ACT(fin_r.ap, fin_r.ap, AF.Sqrt, R=[fin_r], W=[fin_r])
                S.op("dve", lambda e: e.reciprocal(out=fin_r.ap, in_=fin_r.ap), _keys([fin_r]), _keys([fin_r]))
                STT(xnew.ap, xnew.ap, fin_r.ap[:, 0:1], fg_bc.ap, ALU.mult, ALU.mult, R=[xnew, fin_r, fg_bc], W=[xnew])
                DMA("sp", y[r0:r0 + 128, :], xnew.ap, R=[xnew], W=["y"])

        fw = [o for o in S.dlast if o is not None]
        with nc.Block() as block:
            S.emit(block, final_waits=fw)
    return nc


def _rot_perm():
    p = np.arange(64)
    p[0:8] = np.arange(8, 16)
    p[8:16] = np.arange(0, 8)
    return p


def _fm(w):
    n = w.shape[1]
    return np.ascontiguousarray(w.reshape(8, 128, n).transpose(1, 0, 2))


def prep_inputs(x, positions, norm_gain, w_in, b_forget, b_merge, w_branch_dsa, w_branch_fox, w_out, final_gain):
    x = np.asarray(x, np.float32)
    positions = np.asarray(positions, np.int32)
    W = np.asarray(w_in, np.float32)[0]
    o = 0
    cols = {}
    for name, n in (("aq", 512), ("ak", 512), ("av", 512), ("ag", 512), ("iq", 256), ("ik", 64), ("iw", 4),
                    ("fq", 512), ("fk", 512), ("fv", 512), ("fg", 512), ("fl", 8), ("mg", 2048)):
        cols[name] = W[:, o:o + n]
        o += n
    perm = _rot_perm()

    def rot(w, nh):
        idx = np.concatenate([h * 64 + perm for h in range(nh)])
        return w[:, idx]

    z32 = np.zeros((D, 32), np.float32)
    z24 = np.zeros((D, 24), np.float32)
    z64 = np.zeros((D, 64), np.float32)
    wk = np.concatenate([cols["fk"], cols["ak"], rot(cols["ak"], 8),
                         cols["ik"], z32, cols["fl"], z24, rot(cols["ik"], 1), z64,
                         cols["fv"], cols["av"]], axis=1)
    assert wk.shape[1] == NKC
    wq = np.concatenate([cols["fq"], cols["aq"], rot(cols["aq"], 8), cols["iq"], rot(cols["iq"], 4),
                         cols["ag"], cols["fg"], cols["mg"]], axis=1)
    assert wq.shape[1] == NQC
    wk_d, wq_d, wiw_d = _fm(wk), _fm(wq), _fm(np.ascontiguousarray(cols["iw"]))
    wbd = np.ascontiguousarray(np.asarray(w_branch_dsa, np.float32)[0].reshape(8, 64, D).transpose(1, 0, 2))
    wbf = np.ascontiguousarray(np.asarray(w_branch_fox, np.float32)[0].reshape(8, 64, D).transpose(1, 0, 2))
    wo = _fm(np.asarray(w_out, np.float32)[0])
    cvec = np.zeros((128, 64), np.float32)
    cvec[:, 0:8] = np.asarray(norm_gain, np.float32)[0].reshape(8, 128).T
    half = 8
    inv_freq = (500000.0 ** (-np.arange(half, dtype=np.float32) * 2.0 / 16.0)).astype(np.float32)
    for p in range(128):
        r = p % 64
        if r < 8:
            cvec[p, 8] = inv_freq[r]
            cvec[p, 9] = -1.0
        elif r < 16:
            cvec[p, 8] = inv_freq[r - 8]
            cvec[p, 9] = 1.0
    cvec[96:104, 10] = np.asarray(b_forget, np.float32)[0]
    cvec[:, 16:32] = np.asarray(b_merge, np.float32)[0].reshape(16, 128).T
    fgain = np.asarray(final_gain, np.float32).reshape(1, D)
    identf = np.eye(128, dtype=np.float32)
    in_maps = []
    xT_b = [np.ascontiguousarray(x[b].T.reshape(8, 128, S_LEN).transpose(1, 0, 2)) for b in range(x.shape[0])]
    for c in range(8):
        b, j = divmod(c, 4)
        p_idx = np.arange(128)[:, None]
        m_idx = np.arange(32)[None, :]
        bmaskf = (p_idx <= 4 * m_idx + j).astype(np.float32)
        s_idx = np.arange(512)[None, :]
        cbiasf = np.where(s_idx <= 4 * p_idx + j, 0.0, NEGB).astype(np.float32)
        in_maps.append({
            "xT": xT_b[b],
            "xTo": np.ascontiguousarray(xT_b[b][:, :, j::4]),
            "xo": np.ascontiguousarray(x[b, j::4, :]),
            "pos_all": np.ascontiguousarray(positions[b][None, :]),
            "pos_own": np.ascontiguousarray(positions[b][None, j::4]),
            "wk": wk_d, "wq": wq_d, "wiw": wiw_d, "wbd": wbd, "wbf": wbf, "wo": wo,
            "cvec": cvec, "fgain": fgain, "identf": identf, "bmaskf": bmaskf, "cbiasf": cbiasf,
        })
    return in_maps


_NC_CACHE = {}


def kernel(x, positions, norm_gain, w_in, b_forget, b_merge, w_branch_dsa, w_branch_fox, w_out, final_gain):
    in_maps = prep_inputs(x, positions, norm_gain, w_in, b_forget, b_merge, w_branch_dsa, w_branch_fox, w_out, final_gain)
    if "nc" not in _NC_CACHE:
        _NC_CACHE["nc"] = build()
    nc = _NC_CACHE["nc"]
    res = run_bass_kernel_spmd(nc, in_maps, core_ids=list(range(8)))
    out = np.empty((2, S_LEN, D), np.float32)
    for c in range(8):
        b, j = divmod(c, 4)
        out[b, j::4, :] = res.results[c]["y"]
    return out
```

```python
import math
import contextlib
import numpy as np
import concourse.bass as bass
import concourse.mybir as mybir
from concourse.bass_utils import run_bass_kernel_spmd

F32 = mybir.dt.float32
BF16 = mybir.dt.bfloat16
I32 = mybir.dt.int32
U8 = mybir.dt.uint8
AF = mybir.ActivationFunctionType
ALU = mybir.AluOpType

D = 1024
S_LEN = 8192
NQ = 2048
GQ = 512
NG = NQ // GQ
NKC = 2816
NQC = 5120
EPS = 1e-6
NIT = 16
RNG = 8.0
TOPK = 256.0
NEGB = -30000.0
MAGIC = 12582912.0
C1 = 6.28125
C2 = 2.0 * math.pi - 6.28125
ARENA = 164864
GPERS = 81920


class _Op:
    __slots__ = ("eng", "fn", "deps", "ticket", "has_dep", "dma", "sem", "target", "prev")

    def __init__(self, eng, fn, dma):
        self.eng = eng
        self.fn = fn
        self.deps = []
        self.ticket = None
        self.has_dep = False
        self.dma = dma
        self.sem = None
        self.target = None
        self.prev = None


class Sched:
    ENGS = ("pe", "act", "dve", "pool", "sp")

    def __init__(self, nc, n_dma_sems=14):
        self.nc = nc
        self.ops = {e: [] for e in self.ENGS}
        self.last_w = {}
        self.readers = {}
        self.nd = n_dma_sems
        self.rr = 0
        self.rr_sw = 0
        self.n_sw = 4
        self.dlast = [None] * n_dma_sems
        self.dcount = [0] * n_dma_sems
        self.bar_deps = []
        self.bar_pending = set()
        self.last_compute = {e: None for e in self.ENGS}

    def barrier(self):
        deps = [o for o in self.last_compute.values() if o is not None]
        deps += [o for o in self.dlast if o is not None]
        self.bar_deps = deps
        self.bar_pending = set(self.ENGS)
        self.last_w = {}
        self.readers = {}

    def op(self, eng, fn, reads=(), writes=(), dma=False):
        o = _Op(eng, fn, dma)
        deps = []
        if eng in self.bar_pending:
            deps.extend(self.bar_deps)
            self.bar_pending.discard(eng)
        for r in reads:
            w = self.last_w.get(r)
            if w is not None:
                deps.append(w)
        for w_ in writes:
            w = self.last_w.get(w_)
            if w is not None:
                deps.append(w)
            deps.extend(self.readers.get(w_, ()))
        seen = set()
        for d in deps:
            if d is o or id(d) in seen:
                continue
            seen.add(id(d))
            if d.eng == "pe" and eng == "pe" and not d.dma and not dma:
                continue
            o.deps.append(d)
            d.has_dep = True
        for r in reads:
            self.readers.setdefault(r, []).append(o)
        for w_ in writes:
            self.last_w[w_] = o
            self.readers[w_] = []
        if dma:
            if eng == "pool":
                k = self.rr_sw
                self.rr_sw = (self.rr_sw + 1) % self.n_sw
            else:
                k = self.n_sw + self.rr
                self.rr = (self.rr + 1) % (self.nd - self.n_sw)
            o.sem = k
            o.prev = self.dlast[k]
            self.dcount[k] += 16
            o.target = self.dcount[k]
            self.dlast[k] = o
        else:
            self.last_compute[eng] = o
        self.ops[eng].append(o)
        return o

    def alloc_sems(self, st):
        nc = self.nc
        self.esem = {e: st.enter_context(nc.semaphore("s_" + e)) for e in self.ENGS}
        self.dsem = [st.enter_context(nc.semaphore("d_%d" % i)) for i in range(self.nd)]

    def emit(self, block, final_waits=()):
        esem, dsem = self.esem, self.dsem
        for o in final_waits:
            o.has_dep = True
        for e in self.ENGS:
            c = 0
            for o in self.ops[e]:
                if (not o.dma) and o.has_dep:
                    c += 1
                    o.ticket = c

        def run(e, engobj, extra=None):
            waited = {}

            def wait_for(d):
                if d.dma:
                    key, val, sem = ("d", d.sem), d.target, dsem[d.sem]
                else:
                    key, val, sem = ("e", d.eng), d.ticket, esem[d.eng]
                if waited.get(key, 0) >= val:
                    return
                engobj.wait_ge(sem, val)
                waited[key] = val

            for o in self.ops[e]:
                for d in o.deps:
                    wait_for(d)
                if o.dma and o.prev is not None:
                    wait_for(o.prev)
                ins = o.fn(engobj)
                if o.dma:
                    ins.then_inc(dsem[o.sem], 16)
                elif o.has_dep:
                    ins.then_inc(esem[e], 1)
            if extra:
                for d in extra:
                    wait_for(d)

        @block.tensor
        def _(eng):
            run("pe", eng)

        @block.scalar
        def _(eng):
            run("act", eng)

        @block.vector
        def _(eng):
            run("dve", eng)

        @block.gpsimd
        def _(eng):
            run("pool", eng)

        @block.sync
        def _(eng):
            run("sp", eng, extra=list(final_waits))


class Buf:
    _n = 0

    def __init__(self, ap, name=None):
        Buf._n += 1
        self.ap = ap
        self.key = "%s#%d" % (name or "b", Buf._n)


def _keys(xs):
    out = []
    for x in xs:
        out.append(x.key if isinstance(x, Buf) else x)
    return out


_DT_SIZE = {F32: 4, BF16: 2, I32: 4, U8: 1}


class Arena:
    def __init__(self, t, nbytes):
        self.t = t
        self.n = nbytes
        self.off = 0

    def reset(self, off=0):
        self.off = off

    def alloc(self, shape, dt, name=None):
        n = 1
        for s in shape:
            n *= s
        nb = n * _DT_SIZE[dt]
        nb_al = (nb + 63) // 64 * 64
        assert self.off + nb_al <= self.n, ("arena overflow", name, self.off, nb_al, self.n)
        ap = self.t[:, self.off:self.off + nb].bitcast(dt)
        self.off += nb_al
        if len(shape) == 2:
            ap = ap.rearrange("p (a b) -> p a b", a=shape[0], b=shape[1])
        elif len(shape) == 3:
            ap = ap.rearrange("p (a b c) -> p a b c", a=shape[0], b=shape[1], c=shape[2])
        return Buf(ap, name)


def build(debug=False, stop_after=None):
    nc = bass.Bass("TRN2", target_bir_lowering=False)

    def din(name, shape, dt=F32):
        return nc.dram_tensor(name, list(shape), dt, kind="ExternalInput").ap()

    dbg_kind = "ExternalOutput" if debug else "Internal"

    def dscr(name, shape, dt):
        return nc.dram_tensor(name, list(shape), dt, kind=dbg_kind).ap()

    xT = din("xT", [128, 8, S_LEN])
    xTo = din("xTo", [128, 8, NQ])
    xo = din("xo", [NQ, D])
    pos_all = din("pos_all", [1, S_LEN], I32)
    pos_own = din("pos_own", [1, NQ], I32)
    wk = din("wk", [128, 8, NKC])
    wq = din("wq", [128, 8, NQC])
    wiw = din("wiw", [128, 8, 4])
    wbd = din("wbd", [64, 8, D])
    wbf = din("wbf", [64, 8, D])
    wo = din("wo", [128, 8, D])
    cvec = din("cvec", [128, 64])
    fgain = din("fgain", [1, D])
    identf = din("identf", [128, 128])
    bmaskf = din("bmaskf", [128, 32])
    cbiasf = din("cbiasf", [128, 512])
    y = nc.dram_tensor("y", [NQ, D], F32, kind="ExternalOutput").ap()

    kf_s = dscr("kf_s", [8, 68, S_LEN], BF16)
    ka_s = dscr("ka_s", [8, 64, S_LEN], BF16)
    ki_s = dscr("ki_s", [64, S_LEN], BF16)
    vf_s = dscr("vf_s", [8, 128, 64, 128], BF16)
    va_s = dscr("va_s", [8, 128, 64, 128], BF16)
    wq_s = nc.dram_tensor("wq_s", [128, 8, NQC], BF16, kind="Internal").ap()
    wbd_s = nc.dram_tensor("wbd_s", [64, 8, D], BF16, kind="Internal").ap()
    wbf_s = nc.dram_tensor("wbf_s", [64, 8, D], BF16, kind="Internal").ap()
    wo_s = nc.dram_tensor("wo_s", [128, 8, D], BF16, kind="Internal").ap()
    dbg = {}
    if debug:
        dbg["qfT"] = nc.dram_tensor("d_qfT", [128, 8, 512], BF16, kind="ExternalOutput").ap()
        dbg["qaT"] = nc.dram_tensor("d_qaT", [128, 8, 512], BF16, kind="ExternalOutput").ap()
        dbg["qiT"] = nc.dram_tensor("d_qiT", [128, 4, 512], BF16, kind="ExternalOutput").ap()
        dbg["sc"] = nc.dram_tensor("d_sc", [128, 8192], F32, kind="ExternalOutput").ap()
        dbg["lo"] = nc.dram_tensor("d_lo", [4, 128, 1], F32, kind="ExternalOutput").ap()
        dbg["mT"] = nc.dram_tensor("d_mT", [128, 64, 512], U8, kind="ExternalOutput").ap()
        dbg["yTa"] = nc.dram_tensor("d_yTa", [128, 8, 512], BF16, kind="ExternalOutput").ap()
        dbg["yTf"] = nc.dram_tensor("d_yTf", [128, 8, 512], BF16, kind="ExternalOutput").ap()

    st = contextlib.ExitStack()
    with st:
        def T(name, shape, dt):
            return Buf(st.enter_context(nc.sbuf_tensor(name, list(shape), dt))[:], name)

        arena_t = st.enter_context(nc.sbuf_tensor("arena", [128, ARENA], U8))
        AR = Arena(arena_t, ARENA)
        ps = [Buf(st.enter_context(nc.psum_tensor("ps%d" % i, [128, 512], F32))[:], "ps%d" % i) for i in range(8)]

        S = Sched(nc)
        S.alloc_sems(st)

        def DMA(q, out, in_, R=(), W=()):
            return S.op(q, lambda e: e.dma_start(out=out, in_=in_), _keys(R), _keys(W), dma=True)

        def TT(eng, out, in0, in1, op, R=(), W=()):
            return S.op(eng, lambda e: e.tensor_tensor(out=out, in0=in0, in1=in1, op=op), _keys(R), _keys(W))

        def TS(eng, out, in0, s1, op0, s2=None, op1=None, R=(), W=(), accum=None):
            def f(e):
                kw = dict(out=out, in0=in0, scalar1=s1, scalar2=s2, op0=op0)
                if op1 is not None:
                    kw["op1"] = op1
                if accum is not None:
                    kw["accum_out"] = accum
                return e.tensor_scalar(**kw)
            return S.op(eng, f, _keys(R), _keys(W))

        def STT(out, in0, scalar, in1, op0, op1, R=(), W=()):
            return S.op("dve", lambda e: e.scalar_tensor_tensor(out=out, in0=in0, scalar=scalar, in1=in1, op0=op0, op1=op1),
                        _keys(R), _keys(W))

        def ACT(out, in_, func, R=(), W=(), bias=None, scale=None, accum=None):
            def f(e):
                kw = dict(out=out, in_=in_, func=func)
                if bias is not None:
                    kw["bias"] = bias
                if scale is not None:
                    kw["scale"] = scale
                if accum is not None:
                    kw["accum_out"] = accum
                return e.activation(**kw)
            return S.op("act", f, _keys(R), _keys(W))

        def CP(eng, out, in_, R=(), W=()):
            return S.op(eng, lambda e: e.tensor_copy(out=out, in_=in_), _keys(R), _keys(W))

        def MS(eng, ap, val, W=()):
            return S.op(eng, lambda e: e.memset(ap, val), (), _keys(W))

        def MM(out, pairs, R=(), W=()):
            def f(e):
                ins = None
                n = len(pairs)
                for i, (l, r) in enumerate(pairs):
                    ins = e.matmul(out, lhsT=l, rhs=r, start=(i == 0), stop=(i == n - 1))
                return ins
            return S.op("pe", f, _keys(R), _keys(W))

        ones_bf = T("ones_bf", [128, 512], BF16)
        onesf = T("onesf", [128, 512], F32)
        ident = T("ident", [128, 128], BF16)
        bmask = T("bmask", [128, 32], BF16)
        cbias = T("cbias", [128, 512], F32)
        cv = T("cv", [128, 64], F32)
        nbf = T("nbf", [128, 1], F32)
        halfpi = T("halfpi", [128, 1], F32)
        wiwb = T("wiwb", [128, 8, 4], BF16)
        fg_bc = T("fg_bc", [128, D], F32)
        rcol_q = T("rcol_q", [128, 4], F32)
        absw = T("absw", [128, 16], F32)
        sgnw = T("sgnw", [128, 16], F32)
        w4 = T("w4", [128, 16], F32)
        rcol_as = [T("rcol_a0", [128, 4], F32), T("rcol_a1", [128, 4], F32)]
        bis_lo = T("bis_lo", [128, 1], F32)
        bis_mid = T("bis_mid", [128, 1], F32)
        bis_cnt = T("bis_cnt", [128, 1], F32)
        bis_ind = T("bis_ind", [128, 1], F32)
        bis_nb = T("bis_nb", [128, 1], F32)
        bis_ca = T("bis_ca", [128, 1], F32)
        bis_tot = T("bis_tot", [128, 1], F32)
        fin_ss = T("fin_ss", [128, 1], F32)
        fin_r = T("fin_r", [128, 1], F32)

        GAIN = lambda c: cv.ap[:, c:c + 1]
        INVF = cv.ap[:, 8:9]
        SGNS = cv.ap[:, 9:10]
        BMRG = lambda c: cv.ap[:, 16 + c:17 + c]

        MS("dve", ones_bf.ap, 1.0, W=[ones_bf])
        MS("dve", onesf.ap, 1.0, W=[onesf])
        MS("dve", halfpi.ap, math.pi / 2, W=[halfpi])
        DMA("sp", cv.ap, cvec[:, :], W=[cv])
        DMA("sp", cbias.ap, cbiasf[:, :], W=[cbias])
        DMA("pool", ident.ap, identf[:, :], W=[ident])
        DMA("pool", bmask.ap, bmaskf[:, :], W=[bmask])
        DMA("pool", wiwb.ap, wiw[:, :, :], W=[wiwb])
        DMA("sp", fg_bc.ap, fgain[0:1, :].to_broadcast([128, D]), W=[fg_bc])
        TS("dve", nbf.ap, cv.ap[:, 10:11], -1.0, ALU.mult, R=[cv], W=[nbf])

        def load_x_dma(src, t0, xf):
            DMA("sp", xf.ap, src[:, :, t0:t0 + 512], W=[xf])

        def load_x_prep(xf, xb, xsq):
            ACT(xsq.ap, xf.ap, AF.Square, R=[xf], W=[xsq])
            for c in range(8):
                ACT(xb.ap[:, c, :], xf.ap[:, c, :], AF.Copy, R=[xf, cv], W=[xb], scale=GAIN(c))

        def load_x_group(src, t0, xf, xb, xsq):
            load_x_dma(src, t0, xf)
            load_x_prep(xf, xb, xsq)

        def rstd_gen(xsq, rbc, rcol, pA, pB):
            MM(pA.ap, [(ones_bf.ap[:, 0:128], xsq.ap[:, c, :]) for c in range(8)], R=[xsq, ones_bf], W=[pA])
            yield
            for tb in range(4):
                MM(pB.ap[:, tb:tb + 1], [(xsq.ap[:, c, tb * 128:(tb + 1) * 128], ones_bf.ap[:, 0:1]) for c in range(8)],
                   R=[xsq, ones_bf], W=[pB])
            yield
            TS("dve", rcol.ap, pB.ap[:, 0:4], 1.0 / D, ALU.mult, EPS, ALU.add, R=[pB], W=[rcol])
            TS("dve", rbc.ap, pA.ap, 1.0 / D, ALU.mult, EPS, ALU.add, R=[pA], W=[rbc])
            yield
            ACT(rcol.ap, rcol.ap, AF.Sqrt, R=[rcol], W=[rcol])
            ACT(rbc.ap, rbc.ap, AF.Sqrt, R=[rbc], W=[rbc])
            yield
            S.op("dve", lambda e: e.reciprocal(out=rcol.ap, in_=rcol.ap), _keys([rcol]), _keys([rcol]))
            yield
            S.op("dve", lambda e: e.reciprocal(out=rbc.ap, in_=rbc.ap), _keys([rbc]), _keys([rbc]))
            yield

        def rstd_from(xsq, xb_unused, rbc, rcol, pA, pB, scale_extra=1.0):
            for _ in rstd_gen(xsq, rbc, rcol, pA, pB):
                pass

        def rope_gen(possrc, t0, posi, ang, kk, sn, cs, rbc, scale_extra):
            DMA("sp", posi.ap, possrc[0:1, t0:t0 + 512].to_broadcast([128, 512]), W=[posi])
            CP("dve", ang.ap, posi.ap, R=[posi], W=[ang])
            yield
            TS("dve", ang.ap, ang.ap, INVF, ALU.mult, R=[ang, cv], W=[ang])
            yield
            TS("dve", kk.ap, ang.ap, 1.0 / (2.0 * math.pi), ALU.mult, MAGIC, ALU.add, R=[ang], W=[kk])
            yield
            TS("dve", kk.ap, kk.ap, -MAGIC, ALU.add, R=[kk], W=[kk])
            yield
            STT(ang.ap, kk.ap, -C1, ang.ap, ALU.mult, ALU.add, R=[kk, ang], W=[ang])
            yield
            STT(ang.ap, kk.ap, -C2, ang.ap, ALU.mult, ALU.add, R=[kk, ang], W=[ang])
            yield
            TS("dve", ang.ap, ang.ap, math.pi, ALU.min, -math.pi, ALU.max, R=[ang], W=[ang])
            yield
            STT(kk.ap, ang.ap, -1.0, ang.ap, ALU.mult, ALU.max, R=[ang], W=[kk])
            ACT(sn.ap, ang.ap, AF.Sin, R=[ang], W=[sn])
            ACT(cs.ap, kk.ap, AF.Sin, R=[kk, halfpi], W=[cs], bias=halfpi.ap[:, 0:1], scale=-1.0)
            yield
            STT(sn.ap, sn.ap, SGNS, rbc.ap, ALU.mult, ALU.mult, R=[sn, cv, rbc], W=[sn])
            yield
            if scale_extra != 1.0:
                TS("dve", sn.ap, sn.ap, scale_extra, ALU.mult, R=[sn], W=[sn])
                STT(cs.ap, cs.ap, scale_extra, rbc.ap, ALU.mult, ALU.mult, R=[cs, rbc], W=[cs])
            else:
                TT("dve", cs.ap, cs.ap, rbc.ap, ALU.mult, R=[cs, rbc], W=[cs])
            yield

        def rope_tables(possrc, t0, posi, ang, kk, sn, cs, rbc, scale_extra):
            for _ in rope_gen(possrc, t0, posi, ang, kk, sn, cs, rbc, scale_extra):
                pass

        AR.reset(0)
        wkb = AR.alloc([8, NKC], BF16, "wkb")
        xfs = [AR.alloc([8, 512], F32, "xf") for _ in range(2)]
        xbs = [AR.alloc([8, 512], BF16, "xb") for _ in range(2)]
        xsq1 = AR.alloc([8, 512], BF16, "xsq")
        posi = AR.alloc([512], I32, "posi")
        ang = AR.alloc([512], F32, "ang")
        kk = AR.alloc([512], F32, "kk")
        sns = [AR.alloc([512], F32, "sn") for _ in range(2)]
        css = [AR.alloc([512], F32, "cs") for _ in range(2)]
        rbcs = [AR.alloc([512], F32, "rbc") for _ in range(2)]
        t1s = [AR.alloc([512], F32, "t1") for _ in range(2)]
        t2s = [AR.alloc([512], F32, "t2") for _ in range(2)]
        ksts = [AR.alloc([512], BF16, "kst") for _ in range(2)]
        mf = AR.alloc([512], F32, "mf")
        kist = AR.alloc([512], BF16, "kist")
        ee = AR.alloc([512], F32, "ee")
        ncums = [AR.alloc([512], F32, "ncum") for _ in range(2)]
        r1s = AR.alloc([512], F32, "r1s")
        aug = AR.alloc([3, 512], BF16, "aug")
        vsts = [AR.alloc([8, 4, 128], BF16, "vst") for _ in range(2)]

        for i in range(4):
            DMA("pool", wkb.ap[:, :, i * 704:(i + 1) * 704], wk[:, :, i * 704:(i + 1) * 704], W=[wkb])
        for v in vsts:
            MS("pool", v.ap, 1.0, W=[v])
        n_tg = S_LEN // 512
        if stop_after == "A1":
            n_tg = 1
        if stop_after == "A0":
            n_tg = 0
        kst_i = 0
        def chain_gen(tgn):
            k = tgn % 2
            for _ in rstd_gen(xsq1, rbcs[k], rcol_as[k], ps[0], ps[1]):
                yield
            for _ in rope_gen(pos_all, tgn * 512, posi, ang, kk, sns[k], css[k], rbcs[k], 1.0):
                yield

        if n_tg > 0:
            load_x_dma(xT, 0, xfs[0])
            load_x_prep(xfs[0], xbs[0], xsq1)
            for _ in chain_gen(0):
                pass
        for tg in range(n_tg):
            t0 = tg * 512
            xb = xbs[tg % 2]
            rbc = rbcs[tg % 2]
            rcol_a = rcol_as[tg % 2]
            sn = sns[tg % 2]
            cs = css[tg % 2]
            if tg + 1 < n_tg:
                load_x_dma(xT, t0 + 512, xfs[(tg + 1) % 2])
            if tg == 1 or (n_tg == 1 and tg == 0):
                for i in range(NQC // 512):
                    DMA("pool", wq_s[:, :, i * 512:(i + 1) * 512], wq[:, :, i * 512:(i + 1) * 512], W=["wq_s"])
                for i in range(2):
                    DMA("pool", wbd_s[:, :, i * 512:(i + 1) * 512], wbd[:, :, i * 512:(i + 1) * 512], W=["wbd_s"])
                    DMA("pool", wbf_s[:, :, i * 512:(i + 1) * 512], wbf[:, :, i * 512:(i + 1) * 512], W=["wbf_s"])
                    DMA("pool", wo_s[:, :, i * 512:(i + 1) * 512], wo[:, :, i * 512:(i + 1) * 512], W=["wo_s"])


            def proj_chunk(pbuf, n0):
                MM(pbuf.ap, [(wkb.ap[:, c, n0:n0 + 128], xb.ap[:, c, :]) for c in range(8)], R=[wkb, xb], W=[pbuf])

            for cc in range(4):
                pb = ps[2 + (cc % 2)]
                proj_chunk(pb, cc * 128)
                kst = ksts[kst_i % 2]
                kst_i += 1
                TT("dve", kst.ap, pb.ap, rbc.ap, ALU.mult, R=[pb, rbc], W=[kst])
                DMA("sp", kf_s[2 * cc, 0:64, t0:t0 + 512], kst.ap[0:64, :], R=[kst], W=["kf_s"])
                DMA("sp", kf_s[2 * cc + 1, 0:64, t0:t0 + 512], kst.ap[64:128, :], R=[kst], W=["kf_s"])
            DMA("sp", kf_s[:, 64, t0:t0 + 512], ones_bf.ap[0:8, :], R=[ones_bf], W=["kf_s"])
            for br in range(2):
                vst = vsts[br]
                dst = vf_s if br == 0 else va_s
                n0 = 1792 + br * 512
                for tb in range(4):
                    pv = ps[6 + (tb % 2)]
                    MM(pv.ap, [(xb.ap[:, c, tb * 128:(tb + 1) * 128], wkb.ap[:, c, n0:n0 + 512]) for c in range(8)],
                       R=[xb, wkb], W=[pv])
                    ACT(vst.ap[:, :, tb, 0:64], pv.ap.rearrange("p (h c) -> p h c", c=64), AF.Copy,
                        R=[pv, rcol_a], W=[vst], scale=rcol_a.ap[:, tb:tb + 1])
                for h in range(8):
                    DMA("sp", dst[h, :, tg * 4:(tg + 1) * 4, :], vst.ap[:, h, :, :], R=[vst], W=["v_s%d" % br])

            ahead = None
            if tg + 1 < n_tg:
                load_x_prep(xfs[(tg + 1) % 2], xbs[(tg + 1) % 2], xsq1)
                ahead = chain_gen(tg + 1)

            def adv(n):
                if ahead is not None:
                    for _ in range(n):
                        try:
                            next(ahead)
                        except StopIteration:
                            break

            for cc in range(5):
                pa, pb = (ps[4], ps[5]) if cc % 2 == 0 else (ps[2], ps[3])
                if cc < 4:
                    proj_chunk(pa, 512 + cc * 128)
                    proj_chunk(pb, 1024 + cc * 128)
                else:
                    proj_chunk(pa, 1536)
                    proj_chunk(pb, 1664)
                adv(2)
                t1 = t1s[cc % 2]
                t2 = t2s[cc % 2]
                TT("dve", t1.ap, pa.ap, cs.ap, ALU.mult, R=[pa, cs], W=[t1])
                adv(1)
                TT("dve", t2.ap, pb.ap, sn.ap, ALU.mult, R=[pb, sn], W=[t2])
                adv(1)
                if cc < 4:
                    kst = ksts[kst_i % 2]
                    kst_i += 1
                    TT("pool", kst.ap, t1.ap, t2.ap, ALU.add, R=[t1, t2], W=[kst])
                    DMA("sp", ka_s[2 * cc, :, t0:t0 + 512], kst.ap[0:64, :], R=[kst], W=["ka_s"])
                    DMA("sp", ka_s[2 * cc + 1, :, t0:t0 + 512], kst.ap[64:128, :], R=[kst], W=["ka_s"])
                else:
                    TT("pool", mf.ap, t1.ap, t2.ap, ALU.add, R=[t1, t2], W=[mf])
                    CP("pool", kist.ap[0:64, :], mf.ap[0:64, :], R=[mf], W=[kist])
                    DMA("sp", ki_s[:, t0:t0 + 512], kist.ap[0:64, :], R=[kist], W=["ki_s"])
                    ACT(ee.ap[96:104, :], mf.ap[96:104, :], AF.Exp, R=[mf, nbf], W=[ee], bias=nbf.ap[96:104, 0:1], scale=-1.0)
                    ACT(ee.ap[96:104, :], ee.ap[96:104, :], AF.Ln, R=[ee], W=[ee], bias=onesf.ap[96:104, 0:1], scale=1.0)
                    nc_cur = ncums[tg % 2]
                    nc_prev = ncums[(tg + 1) % 2]
                    init = 0.0 if tg == 0 else nc_prev.ap[96:104, 511:512]
                    S.op("dve", (lambda o_, d1_, i_: (lambda e: e.tensor_tensor_scan(
                        out=o_, data0=onesf.ap[96:104, :], data1=d1_, initial=i_, op0=ALU.mult, op1=ALU.add)))(
                        nc_cur.ap[96:104, :], ee.ap[96:104, :], init),
                        _keys([ee, onesf, nc_prev]), _keys([nc_cur]))
                    CP("dve", aug.ap[96:104, 0, :], nc_cur.ap[96:104, :], R=[nc_cur], W=[aug])
                    TT("dve", r1s.ap[96:104, :], nc_cur.ap[96:104, :], aug.ap[96:104, 0, :], ALU.subtract, R=[nc_cur, aug], W=[r1s])
                    CP("dve", aug.ap[96:104, 1, :], r1s.ap[96:104, :], R=[r1s], W=[aug])
                    TT("dve", r1s.ap[96:104, :], r1s.ap[96:104, :], aug.ap[96:104, 1, :], ALU.subtract, R=[r1s, aug], W=[r1s])
                    CP("dve", aug.ap[96:104, 2, :], r1s.ap[96:104, :], R=[r1s], W=[aug])
                    DMA("sp", kf_s[:, 65:68, t0:t0 + 512], aug.ap[96:104, :, :], R=[aug], W=["kf_s"])
            adv(1000)
        n_groups = NG
        if stop_after in ("A", "A1", "A0"):
            n_groups = 0
        elif stop_after is not None and stop_after.startswith("B"):
            n_groups = 1
        for g in range(n_groups):
            S.barrier()
            q0 = g * GQ
            win0 = 2048 * g
            L = 2048 * (g + 1)
            nkb = L // 128
            AR.reset(0)
            qfT = AR.alloc([8, 512], BF16, "qfT")
            qaT = AR.alloc([8, 512], BF16, "qaT")
            qiT = AR.alloc([4, 512], BF16, "qiT")
            xqb = AR.alloc([8, 512], BF16, "xqb")
            rbq = AR.alloc([512], F32, "rbq")
            mT = AR.alloc([64, 512], U8, "mT")
            yTa = AR.alloc([8, 512], BF16, "yTa")
            yTf = AR.alloc([8, 512], BF16, "yTf")
            assert AR.off <= GPERS, AR.off

            AR.reset(GPERS)
            xqf = AR.alloc([8, 512], F32, "xqf")
            xsqq = AR.alloc([8, 512], BF16, "xsqq")
            wqps = [AR.alloc([8, 512], BF16, "wqp") for _ in range(2)]
            posi = AR.alloc([512], I32, "posi")
            ang = AR.alloc([512], F32, "ang")
            kk = AR.alloc([512], F32, "kk")
            sn = AR.alloc([512], F32, "sn")
            cs = AR.alloc([512], F32, "cs")
            rb8 = AR.alloc([512], F32, "rb8")
            t1s = [AR.alloc([512], F32, "t1") for _ in range(2)]
            t2s = [AR.alloc([512], F32, "t2") for _ in range(2)]
            craws = [AR.alloc([2048], BF16, "craw") for _ in range(2)]

            load_x_group(xTo, q0, xqf, xqb, xsqq)
            rstd_from(xsqq, xqb, rbq, rcol_q, ps[0], ps[1])
            TS("dve", rb8.ap, rbq.ap, 0.125, ALU.mult, R=[rbq], W=[rb8])
            qchain = rope_gen(pos_own, q0, posi, ang, kk, sn, cs, rbq, 0.125)

            def qadv(n):
                for _ in range(n):
                    try:
                        next(qchain)
                    except StopIteration:
                        break

            wq_i = [0]

            def load_wq(piece):
                wb = wqps[wq_i[0] % 2]
                wq_i[0] += 1
                DMA("sp", wb.ap, wq_s[:, :, piece * 512:(piece + 1) * 512], R=["wq_s"], W=[wb])
                return wb

            def qproj(pbuf, wb, n0):
                MM(pbuf.ap, [(wb.ap[:, c, n0:n0 + 128], xqb.ap[:, c, :]) for c in range(8)], R=[wb, xqb], W=[pbuf])

            MS("pool", qaT.ap[64:128, :, :], 0.0, W=[qaT])
            MS("pool", qfT.ap[64:68, :, :], 1.0, W=[qfT])
            wb = load_wq(0)
            for cc in range(4):
                pb = ps[2 + cc]
                qproj(pb, wb, cc * 128)
            for cc in range(4):
                pb = ps[2 + cc]
                TT("dve", qfT.ap[0:64, 2 * cc, :], pb.ap[0:64, :], rb8.ap[0:64, :], ALU.mult, R=[pb, rb8], W=[qfT])
                qadv(2)
                TT("dve", qfT.ap[0:64, 2 * cc + 1, :], pb.ap[64:128, :], rb8.ap[64:128, :], ALU.mult, R=[pb, rb8], W=[qfT])
                qadv(2)
            qadv(1000)
            for h in range(8):
                cr = craws[h % 2]
                DMA("sp", cr.ap[64:65, :], kf_s[h, 65:66, win0:win0 + 2048], R=["kf_s"], W=[cr])
                TS("dve", qfT.ap[64:65, h, :], cr.ap[64:65, :].rearrange("p (i r) -> p i r", r=4)[:, :, 0], -1.0, ALU.mult,
                   R=[cr], W=[qfT])
            wb1 = load_wq(1)
            wb2 = load_wq(2)
            for cc in range(4):
                pa, pb = ps[4 + 2 * (cc % 2)], ps[5 + 2 * (cc % 2)]
                qproj(pa, wb1, cc * 128)
                qproj(pb, wb2, cc * 128)
                t1 = t1s[cc % 2]
                t2 = t2s[cc % 2]
                TT("dve", t1.ap, pa.ap, cs.ap, ALU.mult, R=[pa, cs], W=[t1])
                TT("dve", t2.ap, pb.ap, sn.ap, ALU.mult, R=[pb, sn], W=[t2])
                TT("pool", qaT.ap[0:64, 2 * cc, :], t1.ap[0:64, :], t2.ap[0:64, :], ALU.add, R=[t1, t2], W=[qaT])
                TT("pool", qaT.ap[0:64, 2 * cc + 1, :], t1.ap[64:128, :], t2.ap[64:128, :], ALU.add, R=[t1, t2], W=[qaT])
            wb = load_wq(3)
            for cc in range(2):
                pa, pb = ps[4 + 2 * (cc % 2)], ps[5 + 2 * (cc % 2)]
                qproj(pa, wb, cc * 128)
                qproj(pb, wb, 256 + cc * 128)
                t1 = t1s[cc % 2]
                t2 = t2s[cc % 2]
                TT("dve", t1.ap, pa.ap, cs.ap, ALU.mult, R=[pa, cs], W=[t1])
                TT("dve", t2.ap, pb.ap, sn.ap, ALU.mult, R=[pb, sn], W=[t2])
                TT("pool", qiT.ap[0:64, 2 * cc, :], t1.ap[0:64, :], t2.ap[0:64, :], ALU.add, R=[t1, t2], W=[qiT])
                TT("pool", qiT.ap[0:64, 2 * cc + 1, :], t1.ap[64:128, :], t2.ap[64:128, :], ALU.add, R=[t1, t2], W=[qiT])
            for sb in range(4):
                MM(ps[1].ap[:, 8 + 4 * sb:12 + 4 * sb],
                   [(xqb.ap[:, c, sb * 128:(sb + 1) * 128], wiwb.ap[:, c, :]) for c in range(8)], R=[xqb, wiwb], W=[ps[1]])
                TS("dve", w4.ap[:, 4 * sb:4 * sb + 4], ps[1].ap[:, 8 + 4 * sb:12 + 4 * sb], rcol_q.ap[:, sb:sb + 1], ALU.mult,
                   0.5, ALU.mult, R=[ps[1], rcol_q], W=[w4])
            STT(absw.ap, w4.ap, -1.0, w4.ap, ALU.mult, ALU.max, R=[w4], W=[absw])
            TS("dve", sgnw.ap, w4.ap, 0.0, ALU.is_ge, 2.0, ALU.mult, R=[w4], W=[sgnw])
            TS("dve", sgnw.ap, sgnw.ap, -1.0, ALU.add, R=[sgnw], W=[sgnw])
            if debug and g == 0:
                DMA("sp", dbg["qfT"][:, :, :], qfT.ap, R=[qfT], W=["dbg1"])
                DMA("sp", dbg["qaT"][:, :, :], qaT.ap, R=[qaT], W=["dbg2"])
                DMA("sp", dbg["qiT"][:, :, :], qiT.ap, R=[qiT], W=["dbg3"])
            if stop_after == "B1":
                break

            S.barrier()
            AR.reset(GPERS)
            sc = AR.alloc([8192], F32, "sc")
            msk = AR.alloc([8192], BF16, "msk")
            rts = [AR.alloc([512], F32, "rt") for _ in range(2)]
            kits = [AR.alloc([2048], BF16, "kit") for _ in range(1)]
            f_kTps = [AR.alloc([2048], BF16, "kTp") for _ in range(2)]
            f_vps = [AR.alloc([16, 128], BF16, "vp") for _ in range(2)]
            f_Pts = [AR.alloc([512], BF16, "Pt") for _ in range(4)]
            f_recs = [AR.alloc([512], F32, "rec") for _ in range(1)]

            def attn_steps(br, kTps, vps, Pts, Pms, recs, psS, psO):
                tiles = [(h, pc, kb) for h in range(8) for pc in range(g + 1) for kb in range(16)]
                pend = []
                nS = len(psS)
                cur = {}
                piece_n = 0

                def issue_pv(item):
                    (h, pc, kb, pmat, vp, first, last) = item
                    po = psO[h % len(psO)]
                    S.op("pe", (lambda o_, l_, r_, f_, s_: (lambda e: e.matmul(o_, lhsT=l_, rhs=r_, start=f_, stop=s_)))(
                        po.ap, vp.ap[:, kb, :], pmat.ap, first, last), _keys([vp, pmat]), _keys([po]))
                    if last:
                        yT = yTa if br == 0 else yTf
                        rec = recs[h % len(recs)]
                        S.op("dve", (lambda o_, i_: (lambda e: e.reciprocal(out=o_, in_=i_)))(rec.ap[0:64, :], po.ap[64:128, :]),
                             _keys([po]), _keys([rec]))
                        TT("dve", yT.ap[0:64, h, :], po.ap[0:64, :], rec.ap[0:64, :], ALU.mult, R=[po, rec], W=[yT])

                for ti, (h, pc, kb) in enumerate(tiles):
                    if kb == 0:
                        kTp = kTps[piece_n % len(kTps)]
                        vp = vps[piece_n % len(vps)]
                        piece_n += 1
                        if br == 0:
                            DMA("sp", kTp.ap[0:64, :], ka_s[h, :, pc * 2048:(pc + 1) * 2048], R=["ka_s"], W=[kTp])
                            DMA("sp", vp.ap, va_s[h, :, pc * 16:(pc + 1) * 16, :], R=["v_s1"], W=[vp])
                        else:
                            DMA("sp", kTp.ap[0:68, :], kf_s[h, :, pc * 2048:(pc + 1) * 2048], R=["kf_s"], W=[kTp])
                            DMA("sp", vp.ap, vf_s[h, :, pc * 16:(pc + 1) * 16, :], R=["v_s0"], W=[vp])
                        cur["k"], cur["v"] = kTp, vp
                    kTp, vp = cur["k"], cur["v"]
                    pS = psS[ti % nS]
                    Pt = Pts[ti % len(Pts)]
                    diag = (br == 1 and pc == g)
                    c0 = 32 * kb if diag else 0
                    if br == 0:
                        MM(pS.ap[:, c0:512], [(kTp.ap[:, kb * 128:(kb + 1) * 128], qaT.ap[:, h, c0:512])], R=[kTp, qaT], W=[pS])
                    else:
                        MM(pS.ap[:, c0:512], [(kTp.ap[0:68, kb * 128:(kb + 1) * 128], qfT.ap[0:68, h, c0:512])], R=[kTp, qfT], W=[pS])
                    ACT(Pt.ap[:, c0:512], pS.ap[:, c0:512], AF.Exp, R=[pS], W=[Pt])
                    if br == 0:
                        Pm = Pms[ti % len(Pms)]
                        TT("pool" if ti % 2 == 1 else "dve", Pm.ap, Pt.ap, mT.ap[:, pc * 16 + kb, :], ALU.mult, R=[Pt, mT], W=[Pm])
                        pmat = Pm
                    else:
                        if diag:
                            if c0 > 0:
                                MS("pool", Pt.ap[:, 0:c0], 0.0, W=[Pt])
                            TT("pool", Pt.ap[:, c0:c0 + 32], Pt.ap[:, c0:c0 + 32], bmask.ap, ALU.mult, R=[Pt, bmask], W=[Pt])
                        pmat = Pt
                    pend.append((h, pc, kb, pmat, vp, (pc == 0 and kb == 0), (pc == g and kb == 15)))
                    if len(pend) > min(4, nS - 1):
                        issue_pv(pend.pop(0))
                    yield 1
                while pend:
                    issue_pv(pend.pop(0))

            def b2_units():
                for sb in range(4):
                    nch = 4 * g + sb + 1
                    Lsb = 512 * nch
                    kit = kits[0]
                    for ch in range(nch):
                        if ch % 4 == 0:
                            pc = ch // 4
                            DMA("sp", kit.ap[0:64, :], ki_s[:, pc * 2048:(pc + 1) * 2048], R=["ki_s"], W=[kit])
                        for h in range(4):
                            pb = ps[h]
                            MM(pb.ap, [(qiT.ap[0:64, h, sb * 128:(sb + 1) * 128], kit.ap[0:64, (ch % 4) * 512:(ch % 4 + 1) * 512])],
                               R=[qiT, kit], W=[pb])
                        for h in range(4):
                            pb = ps[h]
                            rt = rts[h % 2]
                            ACT(rt.ap, pb.ap, AF.Relu, R=[pb, absw], W=[rt], scale=absw.ap[:, 4 * sb + h:4 * sb + h + 1])
                            scs = sc.ap[:, ch * 512:(ch + 1) * 512]
                            sg = sgnw.ap[:, 4 * sb + h:4 * sb + h + 1]
                            if h == 0:
                                TS("dve", scs, rt.ap, sg, ALU.mult, R=[rt, sgnw], W=[sc])
                            else:
                                STT(scs, rt.ap, sg, scs, ALU.mult, ALU.add, R=[rt, sgnw, sc], W=[sc])
                        if ch == nch - 1:
                            TT("dve", sc.ap[:, ch * 512:(ch + 1) * 512], sc.ap[:, ch * 512:(ch + 1) * 512], cbias.ap, ALU.add,
                               R=[sc, cbias], W=[sc])
                        yield 3.2
                    if debug and g == 0 and sb == 3:
                        DMA("sp", dbg["sc"][:, 0:Lsb], sc.ap[:, 0:Lsb], R=[sc], W=["dbg4"])
                    MS("dve", bis_lo.ap, -RNG, W=[bis_lo])
                    La = 512 * int(0.4 * nch) if nch >= 3 else 0
                    Ld = Lsb - La
                    for it in range(NIT):
                        step = RNG / (2.0 ** it)
                        TS("dve", bis_mid.ap, bis_lo.ap, step, ALU.add, R=[bis_lo], W=[bis_mid])
                        if La > 0:
                            TS("dve", bis_nb.ap, bis_mid.ap, -1.0, ALU.mult, 2.0 ** -20, ALU.add, R=[bis_mid], W=[bis_nb])
                        TS("dve", msk.ap[:, 0:Ld], sc.ap[:, 0:Ld], bis_mid.ap[:, 0:1], ALU.is_ge, None, ALU.add,
                           R=[sc, bis_mid], W=[msk, bis_cnt], accum=bis_cnt.ap[:, 0:1])
                        if La > 0:
                            ACT(msk.ap[:, Ld:Lsb], sc.ap[:, Ld:Lsb], AF.Sign, R=[sc, bis_nb], W=["mskA", bis_ca],
                                bias=bis_nb.ap[:, 0:1], scale=1.0, accum=bis_ca.ap[:, 0:1])
                            STT(bis_tot.ap, bis_ca.ap, 0.5, bis_cnt.ap, ALU.mult, ALU.add, R=[bis_ca, bis_cnt], W=[bis_tot])
                            TS("dve", bis_ind.ap, bis_tot.ap, TOPK - La / 2.0, ALU.is_ge, step, ALU.mult, R=[bis_tot], W=[bis_ind])
                        else:
                            TS("dve", bis_ind.ap, bis_cnt.ap, TOPK, ALU.is_ge, step, ALU.mult, R=[bis_cnt], W=[bis_ind])
                        TT("dve", bis_lo.ap, bis_lo.ap, bis_ind.ap, ALU.add, R=[bis_lo, bis_ind], W=[bis_lo])
                        yield 0.7 + Ld * 1.05e-3
                    TS("dve", bis_ind.ap, bis_lo.ap, -RNG, ALU.is_le, -1000.0, ALU.mult, R=[bis_lo], W=[bis_ind])
                    TT("dve", bis_lo.ap, bis_lo.ap, bis_ind.ap, ALU.add, R=[bis_lo, bis_ind], W=[bis_lo])
                    TS("dve", msk.ap[:, 0:Lsb], sc.ap[:, 0:Lsb], bis_lo.ap[:, 0:1], ALU.is_ge, R=[sc, bis_lo], W=[msk, "mskA"])
                    if debug and g == 0:
                        DMA("sp", dbg["lo"][sb], bis_lo.ap, R=[bis_lo], W=["dbg5"])
                    yield Lsb * 0.6e-3
                    for k4 in range(Lsb // 512):
                        pt = ps[k4 % 2]
                        ptb = pt.ap.bitcast(BF16)
                        for i in range(4):
                            kb = k4 * 4 + i
                            S.op("pe", (lambda o_, i_: (lambda e: e.transpose(out=o_, in_=i_, identity=ident.ap)))(
                                ptb[:, i * 128:(i + 1) * 128], msk.ap[:, kb * 128:(kb + 1) * 128]),
                                _keys([msk, ident]), _keys([pt]))
                        ACT(mT.ap[:, k4 * 4:(k4 + 1) * 4, sb * 128:(sb + 1) * 128],
                            ptb[:, 0:512].rearrange("p (a b) -> p a b", b=128), AF.Copy, R=[pt], W=[mT])
                        yield 0.7
                    if Lsb // 128 < nkb:
                        MS("pool", mT.ap[:, Lsb // 128:nkb, sb * 128:(sb + 1) * 128], 0, W=[mT])

            fox = attn_steps(1, f_kTps, f_vps, f_Pts, None, f_recs, [ps[4], ps[5]], [ps[6], ps[7]])
            n_fox = 128 * (g + 1)
            units = list()
            tot_est = 0.0
            for sb in range(4):
                nch = 4 * g + sb + 1
                Lsb = 512 * nch
                La_ = 512 * int(0.4 * nch) if nch >= 3 else 0
                tot_est += nch * 3.2 + NIT * (0.7 + (Lsb - La_) * 1.05e-3) + Lsb * 0.6e-3 + (Lsb // 512) * 0.7
            rate = n_fox / (0.9 * tot_est)
            acc = 0.0
            fox_done = False
            for wgt in b2_units():
                acc += wgt * rate
                while acc >= 1.0 and not fox_done:
                    acc -= 1.0
                    try:
                        next(fox)
                    except StopIteration:
                        fox_done = True
            if not fox_done:
                for _ in fox:
                    pass
            if debug and g == 0:
                DMA("sp", dbg["mT"][:, 0:nkb, :], mT.ap[:, 0:nkb, :], R=[mT], W=["dbg6"])
            if stop_after == "B2":
                break

            S.barrier()
            AR.reset(GPERS)
            kTps = [AR.alloc([2048], BF16, "kTp") for _ in range(3)]
            vps = [AR.alloc([16, 128], BF16, "vp") for _ in range(3)]
            Pts = [AR.alloc([512], BF16, "Pt") for _ in range(6)]
            Pms = [AR.alloc([512], BF16, "Pm") for _ in range(6)]
            recs = [AR.alloc([512], F32, "rec") for _ in range(2)]
            for kt_ in kTps:
                MS("pool", kt_.ap[64:128, :], 0.0, W=[kt_])
            for _ in attn_steps(0, kTps, vps, Pts, Pms, recs, [ps[0], ps[1], ps[2], ps[3], ps[6], ps[7]], [ps[4], ps[5]]):
                pass
            if debug and g == 0:
                DMA("sp", dbg["yTa"][:, :, :], yTa.ap, R=[yTa], W=["dbg7"])
                DMA("sp", dbg["yTf"][:, :, :], yTf.ap, R=[yTf], W=["dbg8"])
            if stop_after == "B3":
                break

            S.barrier()
            AR.reset(GPERS)
            wqps = [AR.alloc([8, 512], BF16, "wqp") for _ in range(2)]
            wbrs = [AR.alloc([8, 128], BF16, "wbr") for _ in range(2)]
            wobs = [AR.alloc([8, 512], BF16, "wob") for _ in range(2)]
            ygs = [AR.alloc([8, 512], BF16, "yg") for _ in range(2)]
            mrg = AR.alloc([8, 512], BF16, "mrg")
            gtmp = AR.alloc([512], F32, "gtmp")
            gbf = AR.alloc([512], BF16, "gbf")
            gms4 = [AR.alloc([512], F32, "gm") for _ in range(2)]
            e1s = [AR.alloc([512], F32, "e1") for _ in range(1)]
            e2s = [AR.alloc([512], F32, "e2") for _ in range(1)]
            xot = AR.alloc([D], F32, "xot")
            xnew = AR.alloc([D], F32, "xnew")
            wq_i = [0]
            for br in range(2):
                wb = load_wq(4 + br)
                yT = yTa if br == 0 else yTf
                for cc in range(4):
                    pb = ps[cc % 2]
                    qproj(pb, wb, cc * 128)
                    TT("dve", gtmp.ap, pb.ap, rbq.ap, ALU.mult, R=[pb, rbq], W=[gtmp])
                    for hh in range(2):
                        h = 2 * cc + hh
                        ACT(gbf.ap[0:64, :], gtmp.ap[64 * hh:64 * hh + 64, :], AF.Silu, R=[gtmp], W=[gbf])
                        TT("pool", ygs[br].ap[0:64, h, :], yT.ap[0:64, h, :], gbf.ap[0:64, :], ALU.mult, R=[yT, gbf], W=[ygs[br]])
            wbr_n = [0]
            wqm = {}
            for hf in range(2):
                DMA("sp", wobs[hf].ap, wo_s[:, :, hf * 512:(hf + 1) * 512], R=["wo_s"], W=[wobs[hf]])
            for dc in range(8):
                pus = []
                par = dc % 2
                gms = [gms4[0], gms4[1]]
                e1, e2 = e1s[0], e2s[0]
                for br in range(2):
                    wsrc = wbd_s if br == 0 else wbf_s
                    wbr = wbrs[wbr_n[0] % 2]
                    wbr_n[0] += 1
                    DMA("sp", wbr.ap[0:64, :, :], wsrc[:, :, dc * 128:(dc + 1) * 128], R=["wbd_s", "wbf_s"], W=[wbr])
                    pu = ps[4 * par + br]
                    MM(pu.ap, [(wbr.ap[0:64, h, :], ygs[br].ap[0:64, h, :]) for h in range(8)], R=[wbr, ygs[br]], W=[pu])
                    pus.append(pu)
                    mcol = br * 1024 + dc * 128
                    piece = 6 + mcol // 512
                    if wqm.get(br, (None, None))[0] != piece:
                        wqm[br] = (piece, load_wq(piece))
                    wbm = wqm[br][1]
                    pm = ps[4 * par + 2 + br]
                    qproj(pm, wbm, mcol % 512)
                    TT("dve", gms[br].ap, pm.ap, rbq.ap, ALU.mult, R=[pm, rbq], W=[gms[br]])
                    ACT(gms[br].ap, gms[br].ap, AF.Sigmoid, R=[gms[br], cv], W=[gms[br]], bias=BMRG(br * 8 + dc))
                TT("dve", e1.ap, pus[0].ap, gms[0].ap, ALU.mult, R=[pus[0], gms[0]], W=[e1])
                TT("dve", e2.ap, pus[1].ap, gms[1].ap, ALU.mult, R=[pus[1], gms[1]], W=[e2])
                TT("pool", mrg.ap[:, dc, :], e1.ap, e2.ap, ALU.add, R=[e1, e2], W=[mrg])
            for sb in range(4):
                r0 = q0 + sb * 128
                DMA("sp", xot.ap, xo[r0:r0 + 128, :], W=[xot])
                for hf in range(2):
                    wob = wobs[hf]
                    po = ps[(2 * sb + hf) % 8]
                    MM(po.ap, [(mrg.ap[:, dc, sb * 128:(sb + 1) * 128], wob.ap[:, dc, :]) for dc in range(8)], R=[mrg, wob], W=[po])
                    TT("dve", xnew.ap[:, hf * 512:(hf + 1) * 512], po.ap, xot.ap[:, hf * 512:(hf + 1) * 512], ALU.add,
                       R=[po, xot], W=[xnew])
                ACT(xot.ap, xnew.ap, AF.Square, R=[xnew, xot], W=[xot, fin_ss], accum=fin_ss.ap[:, 0:1])
                TS("dve", fin_r.ap, fin_ss.ap, 1.0 / D, ALU.mult, EPS, ALU.add, R=[fin_ss], W=[fin_r])
                ACT(fin_r.ap, fin_r.ap, AF.Sqrt, R=[fin_r], W=[fin_r])
                S.op("dve", lambda e: e.reciprocal(out=fin_r.ap, in_=fin_r.ap), _keys([fin_r]), _keys([fin_r]))
                STT(xnew.ap, xnew.ap, fin_r.ap[:, 0:1], fg_bc.ap, ALU.mult, ALU.mult, R=[xnew, fin_r, fg_bc], W=[xnew])
                DMA("sp", y[r0:r0 + 128, :], xnew.ap, R=[xnew], W=["y"])

        fw = [o for o in S.dlast if o is not None]
        with nc.Block() as block:
            S.emit(block, final_waits=fw)
    return nc


def _rot_perm():
    p = np.arange(64)
    p[0:8] = np.arange(8, 16)
    p[8:16] = np.arange(0, 8)
    return p


def _fm(w):
    n = w.shape[1]
    return np.ascontiguousarray(w.reshape(8, 128, n).transpose(1, 0, 2))


def prep_inputs(x, positions, norm_gain, w_in, b_forget, b_merge, w_branch_dsa, w_branch_fox, w_out, final_gain):
    x = np.asarray(x, np.float32)
    positions = np.asarray(positions, np.int32)
    W = np.asarray(w_in, np.float32)[0]
    o = 0
    cols = {}
    for name, n in (("aq", 512), ("ak", 512), ("av", 512), ("ag", 512), ("iq", 256), ("ik", 64), ("iw", 4),
                    ("fq", 512), ("fk", 512), ("fv", 512), ("fg", 512), ("fl", 8), ("mg", 2048)):
        cols[name] = W[:, o:o + n]
        o += n
    perm = _rot_perm()

    def rot(w, nh):
        idx = np.concatenate([h * 64 + perm for h in range(nh)])
        return w[:, idx]

    z32 = np.zeros((D, 32), np.float32)
    z24 = np.zeros((D, 24), np.float32)
    z64 = np.zeros((D, 64), np.float32)
    wk = np.concatenate([cols["fk"], cols["ak"], rot(cols["ak"], 8),
                         cols["ik"], z32, cols["fl"], z24, rot(cols["ik"], 1), z64,
                         cols["fv"], cols["av"]], axis=1)
    assert wk.shape[1] == NKC
    wq = np.concatenate([cols["fq"], cols["aq"], rot(cols["aq"], 8), cols["iq"], rot(cols["iq"], 4),
                         cols["ag"], cols["fg"], cols["mg"]], axis=1)
    assert wq.shape[1] == NQC
    wk_d, wq_d, wiw_d = _fm(wk), _fm(wq), _fm(np.ascontiguousarray(cols["iw"]))
    wbd = np.ascontiguousarray(np.asarray(w_branch_dsa, np.float32)[0].reshape(8, 64, D).transpose(1, 0, 2))
    wbf = np.ascontiguousarray(np.asarray(w_branch_fox, np.float32)[0].reshape(8, 64, D).transpose(1, 0, 2))
    wo = _fm(np.asarray(w_out, np.float32)[0])
    cvec = np.zeros((128, 64), np.float32)
    cvec[:, 0:8] = np.asarray(norm_gain, np.float32)[0].reshape(8, 128).T
    half = 8
    inv_freq = (500000.0 ** (-np.arange(half, dtype=np.float32) * 2.0 / 16.0)).astype(np.float32)
    for p in range(128):
        r = p % 64
        if r < 8:
            cvec[p, 8] = inv_freq[r]
            cvec[p, 9] = -1.0
        elif r < 16:
            cvec[p, 8] = inv_freq[r - 8]
            cvec[p, 9] = 1.0
    cvec[96:104, 10] = np.asarray(b_forget, np.float32)[0]
    cvec[:, 16:32] = np.asarray(b_merge, np.float32)[0].reshape(16, 128).T
    fgain = np.asarray(final_gain, np.float32).reshape(1, D)
    identf = np.eye(128, dtype=np.float32)
    in_maps = []
    xT_b = [np.ascontiguousarray(x[b].T.reshape(8, 128, S_LEN).transpose(1, 0, 2)) for b in range(x.shape[0])]
    for c in range(8):
        b, j = divmod(c, 4)
        p_idx = np.arange(128)[:, None]
        m_idx = np.arange(32)[None, :]
        bmaskf = (p_idx <= 4 * m_idx + j).astype(np.float32)
        s_idx = np.arange(512)[None, :]
        cbiasf = np.where(s_idx <= 4 * p_idx + j, 0.0, NEGB).astype(np.float32)
        in_maps.append({
            "xT": xT_b[b],
            "xTo": np.ascontiguousarray(xT_b[b][:, :, j::4]),
            "xo": np.ascontiguousarray(x[b, j::4, :]),
            "pos_all": np.ascontiguousarray(positions[b][None, :]),
            "pos_own": np.ascontiguousarray(positions[b][None, j::4]),
            "wk": wk_d, "wq": wq_d, "wiw": wiw_d, "wbd": wbd, "wbf": wbf, "wo": wo,
            "cvec": cvec, "fgain": fgain, "identf": identf, "bmaskf": bmaskf, "cbiasf": cbiasf,
        })
    return in_maps


_NC_CACHE = {}


def kernel(x, positions, norm_gain, w_in, b_forget, b_merge, w_branch_dsa, w_branch_fox, w_out, final_gain):
    in_maps = prep_inputs(x, positions, norm_gain, w_in, b_forget, b_merge, w_branch_dsa, w_branch_fox, w_out, final_gain)
    if "nc" not in _NC_CACHE:
        _NC_CACHE["nc"] = build()
    nc = _NC_CACHE["nc"]
    res = run_bass_kernel_spmd(nc, in_maps, core_ids=list(range(8)))
    out = np.empty((2, S_LEN, D), np.float32)
    for c in range(8):
        b, j = divmod(c, 4)
        out[b, j::4, :] = res.results[c]["y"]
    return out
```

```python
import math
import contextlib
import numpy as np
import concourse.bass as bass
import concourse.mybir as mybir
from concourse.bass_utils import run_bass_kernel_spmd

F32 = mybir.dt.float32
BF16 = mybir.dt.bfloat16
I32 = mybir.dt.int32
U8 = mybir.dt.uint8
AF = mybir.ActivationFunctionType
ALU = mybir.AluOpType

D = 1024
S_LEN = 8192
NQ = 2048
GQ = 512
NG = NQ // GQ
NKC = 2816
NQC = 5120
EPS = 1e-6
NIT = 16
RNG = 8.0
TOPK = 256.0
NEGB = -30000.0
MAGIC = 12582912.0
C1 = 6.28125
C2 = 2.0 * math.pi - 6.28125
ARENA = 164864
GPERS = 81920


class _Op:
    __slots__ = ("eng", "fn", "deps", "ticket", "has_dep", "dma", "sem", "target", "prev")

    def __init__(self, eng, fn, dma):
        self.eng = eng
        self.fn = fn
        self.deps = []
        self.ticket = None
        self.has_dep = False
        self.dma = dma
        self.sem = None
        self.target = None
        self.prev = None


class Sched:
    ENGS = ("pe", "act", "dve", "pool", "sp")

    def __init__(self, nc, n_dma_sems=14):
        self.nc = nc
        self.ops = {e: [] for e in self.ENGS}
        self.last_w = {}
        self.readers = {}
        self.nd = n_dma_sems
        self.rr = 0
        self.rr_sw = 0
        self.n_sw = 4
        self.dlast = [None] * n_dma_sems
        self.dcount = [0] * n_dma_sems
        self.bar_deps = []
        self.bar_pending = set()
        self.last_compute = {e: None for e in self.ENGS}

    def barrier(self):
        deps = [o for o in self.last_compute.values() if o is not None]
        deps += [o for o in self.dlast if o is not None]
        self.bar_deps = deps
        self.bar_pending = set(self.ENGS)
        self.last_w = {}
        self.readers = {}

    def op(self, eng, fn, reads=(), writes=(), dma=False):
        o = _Op(eng, fn, dma)
        deps = []
        if eng in self.bar_pending:
            deps.extend(self.bar_deps)
            self.bar_pending.discard(eng)
        for r in reads:
            w = self.last_w.get(r)
            if w is not None:
                deps.append(w)
        for w_ in writes:
            w = self.last_w.get(w_)
            if w is not None:
                deps.append(w)
            deps.extend(self.readers.get(w_, ()))
        seen = set()
        for d in deps:
            if d is o or id(d) in seen:
                continue
            seen.add(id(d))
            if d.eng == "pe" and eng == "pe" and not d.dma and not dma:
                continue
            o.deps.append(d)
            d.has_dep = True
        for r in reads:
            self.readers.setdefault(r, []).append(o)
        for w_ in writes:
            self.last_w[w_] = o
            self.readers[w_] = []
        if dma:
            if eng == "pool":
                k = self.rr_sw
                self.rr_sw = (self.rr_sw + 1) % self.n_sw
            else:
                k = self.n_sw + self.rr
                self.rr = (self.rr + 1) % (self.nd - self.n_sw)
            o.sem = k
            o.prev = self.dlast[k]
            self.dcount[k] += 16
            o.target = self.dcount[k]
            self.dlast[k] = o
        else:
            self.last_compute[eng] = o
        self.ops[eng].append(o)
        return o

    def alloc_sems(self, st):
        nc = self.nc
        self.esem = {e: st.enter_context(nc.semaphore("s_" + e)) for e in self.ENGS}
        self.dsem = [st.enter_context(nc.semaphore("d_%d" % i)) for i in range(self.nd)]

    def emit(self, block, final_waits=()):
        esem, dsem = self.esem, self.dsem
        for o in final_waits:
            o.has_dep = True
        for e in self.ENGS:
            c = 0
            for o in self.ops[e]:
                if (not o.dma) and o.has_dep:
                    c += 1
                    o.ticket = c

        def run(e, engobj, extra=None):
            waited = {}

            def wait_for(d):
                if d.dma:
                    key, val, sem = ("d", d.sem), d.target, dsem[d.sem]
                else:
                    key, val, sem = ("e", d.eng), d.ticket, esem[d.eng]
                if waited.get(key, 0) >= val:
                    return
                engobj.wait_ge(sem, val)
                waited[key] = val

            for o in self.ops[e]:
                for d in o.deps:
                    wait_for(d)
                if o.dma and o.prev is not None:
                    wait_for(o.prev)
                ins = o.fn(engobj)
                if o.dma:
                    ins.then_inc(dsem[o.sem], 16)
                elif o.has_dep:
                    ins.then_inc(esem[e], 1)
            if extra:
                for d in extra:
                    wait_for(d)

        @block.tensor
        def _(eng):
            run("pe", eng)

        @block.scalar
        def _(eng):
            run("act", eng)

        @block.vector
        def _(eng):
            run("dve", eng)

        @block.gpsimd
        def _(eng):
            run("pool", eng)

        @block.sync
        def _(eng):
            run("sp", eng, extra=list(final_waits))


class Buf:
    _n = 0

    def __init__(self, ap, name=None):
        Buf._n += 1
        self.ap = ap
        self.key = "%s#%d" % (name or "b", Buf._n)


def _keys(xs):
    out = []
    for x in xs:
        out.append(x.key if isinstance(x, Buf) else x)
    return out


_DT_SIZE = {F32: 4, BF16: 2, I32: 4, U8: 1}


class Arena:
    def __init__(self, t, nbytes):
        self.t = t
        self.n = nbytes
        self.off = 0

    def reset(self, off=0):
        self.off = off

    def alloc(self, shape, dt, name=None):
        n = 1
        for s in shape:
            n *= s
        nb = n * _DT_SIZE[dt]
        nb_al = (nb + 63) // 64 * 64
        assert self.off + nb_al <= self.n, ("arena overflow", name, self.off, nb_al, self.n)
        ap = self.t[:, self.off:self.off + nb].bitcast(dt)
        self.off += nb_al
        if len(shape) == 2:
            ap = ap.rearrange("p (a b) -> p a b", a=shape[0], b=shape[1])
        elif len(shape) == 3:
            ap = ap.rearrange("p (a b c) -> p a b c", a=shape[0], b=shape[1], c=shape[2])
        return Buf(ap, name)


def build(debug=False, stop_after=None):
    nc = bass.Bass("TRN2", target_bir_lowering=False)

    def din(name, shape, dt=F32):
        return nc.dram_tensor(name, list(shape), dt, kind="ExternalInput").ap()

    dbg_kind = "ExternalOutput" if debug else "Internal"

    def dscr(name, shape, dt):
        return nc.dram_tensor(name, list(shape), dt, kind=dbg_kind).ap()

    xT = din("xT", [128, 8, S_LEN])
    xTo = din("xTo", [128, 8, NQ])
    xo = din("xo", [NQ, D])
    pos_all = din("pos_all", [1, S_LEN], I32)
    pos_own = din("pos_own", [1, NQ], I32)
    wk = din("wk", [128, 8, NKC])
    wq = din("wq", [128, 8, NQC])
    wiw = din("wiw", [128, 8, 4])
    wbd = din("wbd", [64, 8, D])
    wbf = din("wbf", [64, 8, D])
    wo = din("wo", [128, 8, D])
    cvec = din("cvec", [128, 64])
    fgain = din("fgain", [1, D])
    identf = din("identf", [128, 128])
    bmaskf = din("bmaskf", [128, 32])
    cbiasf = din("cbiasf", [128, 512])
    y = nc.dram_tensor("y", [NQ, D], F32, kind="ExternalOutput").ap()

    kf_s = dscr("kf_s", [8, 68, S_LEN], BF16)
    ka_s = dscr("ka_s", [8, 64, S_LEN], BF16)
    ki_s = dscr("ki_s", [64, S_LEN], BF16)
    vf_s = dscr("vf_s", [8, 128, 64, 128], BF16)
    va_s = dscr("va_s", [8, 128, 64, 128], BF16)
    wq_s = nc.dram_tensor("wq_s", [128, 8, NQC], BF16, kind="Internal").ap()
    wbd_s = nc.dram_tensor("wbd_s", [64, 8, D], BF16, kind="Internal").ap()
    wbf_s = nc.dram_tensor("wbf_s", [64, 8, D], BF16, kind="Internal").ap()
    wo_s = nc.dram_tensor("wo_s", [128, 8, D], BF16, kind="Internal").ap()
    dbg = {}
    if debug:
        dbg["qfT"] = nc.dram_tensor("d_qfT", [128, 8, 512], BF16, kind="ExternalOutput").ap()
        dbg["qaT"] = nc.dram_tensor("d_qaT", [128, 8, 512], BF16, kind="ExternalOutput").ap()
        dbg["qiT"] = nc.dram_tensor("d_qiT", [128, 4, 512], BF16, kind="ExternalOutput").ap()
        dbg["sc"] = nc.dram_tensor("d_sc", [128, 8192], F32, kind="ExternalOutput").ap()
        dbg["lo"] = nc.dram_tensor("d_lo", [4, 128, 1], F32, kind="ExternalOutput").ap()
        dbg["mT"] = nc.dram_tensor("d_mT", [128, 64, 512], U8, kind="ExternalOutput").ap()
        dbg["yTa"] = nc.dram_tensor("d_yTa", [128, 8, 512], BF16, kind="ExternalOutput").ap()
        dbg["yTf"] = nc.dram_tensor("d_yTf", [128, 8, 512], BF16, kind="ExternalOutput").ap()

    st = contextlib.ExitStack()
    with st:
        def T(name, shape, dt):
            return Buf(st.enter_context(nc.sbuf_tensor(name, list(shape), dt))[:], name)

        arena_t = st.enter_context(nc.sbuf_tensor("arena", [128, ARENA], U8))
        AR = Arena(arena_t, ARENA)
        ps = [Buf(st.enter_context(nc.psum_tensor("ps%d" % i, [128, 512], F32))[:], "ps%d" % i) for i in range(8)]

        S = Sched(nc)
        S.alloc_sems(st)

        def DMA(q, out, in_, R=(), W=()):
            return S.op(q, lambda e: e.dma_start(out=out, in_=in_), _keys(R), _keys(W), dma=True)

        def TT(eng, out, in0, in1, op, R=(), W=()):
            return S.op(eng, lambda e: e.tensor_tensor(out=out, in0=in0, in1=in1, op=op), _keys(R), _keys(W))

        def TS(eng, out, in0, s1, op0, s2=None, op1=None, R=(), W=(), accum=None):
            def f(e):
                kw = dict(out=out, in0=in0, scalar1=s1, scalar2=s2, op0=op0)
                if op1 is not None:
                    kw["op1"] = op1
                if accum is not None:
                    kw["accum_out"] = accum
                return e.tensor_scalar(**kw)
            return S.op(eng, f, _keys(R), _keys(W))

        def STT(out, in0, scalar, in1, op0, op1, R=(), W=()):
            return S.op("dve", lambda e: e.scalar_tensor_tensor(out=out, in0=in0, scalar=scalar, in1=in1, op0=op0, op1=op1),
                        _keys(R), _keys(W))

        def ACT(out, in_, func, R=(), W=(), bias=None, scale=None, accum=None):
            def f(e):
                kw = dict(out=out, in_=in_, func=func)
                if bias is not None:
                    kw["bias"] = bias
                if scale is not None:
                    kw["scale"] = scale
                if accum is not None:
                    kw["accum_out"] = accum
                return e.activation(**kw)
            return S.op("act", f, _keys(R), _keys(W))

        def CP(eng, out, in_, R=(), W=()):
            return S.op(eng, lambda e: e.tensor_copy(out=out, in_=in_), _keys(R), _keys(W))

        def MS(eng, ap, val, W=()):
            return S.op(eng, lambda e: e.memset(ap, val), (), _keys(W))

        def MM(out, pairs, R=(), W=()):
            def f(e):
                ins = None
                n = len(pairs)
                for i, (l, r) in enumerate(pairs):
                    ins = e.matmul(out, lhsT=l, rhs=r, start=(i == 0), stop=(i == n - 1))
                return ins
            return S.op("pe", f, _keys(R), _keys(W))

        ones_bf = T("ones_bf", [128, 512], BF16)
        onesf = T("onesf", [128, 512], F32)
        ident = T("ident", [128, 128], BF16)
        bmask = T("bmask", [128, 32], BF16)
        cbias = T("cbias", [128, 512], F32)
        cv = T("cv", [128, 64], F32)
        nbf = T("nbf", [128, 1], F32)
        halfpi = T("halfpi", [128, 1], F32)
        wiwb = T("wiwb", [128, 8, 4], BF16)
        fg_bc = T("fg_bc", [128, D], F32)
        rcol_q = T("rcol_q", [128, 4], F32)
        absw = T("absw", [128, 16], F32)
        sgnw = T("sgnw", [128, 16], F32)
        w4 = T("w4", [128, 16], F32)
        rcol_as = [T("rcol_a0", [128, 4], F32), T("rcol_a1", [128, 4], F32)]
        bis_lo = T("bis_lo", [128, 1], F32)
        bis_mid = T("bis_mid", [128, 1], F32)
        bis_cnt = T("bis_cnt", [128, 1], F32)
        bis_ind = T("bis_ind", [128, 1], F32)
        bis_nb = T("bis_nb", [128, 1], F32)
        bis_ca = T("bis_ca", [128, 1], F32)
        bis_tot = T("bis_tot", [128, 1], F32)
        fin_ss = T("fin_ss", [128, 1], F32)
        fin_r = T("fin_r", [128, 1], F32)

        GAIN = lambda c: cv.ap[:, c:c + 1]
        INVF = cv.ap[:, 8:9]
        SGNS = cv.ap[:, 9:10]
        BMRG = lambda c: cv.ap[:, 16 + c:17 + c]

        MS("dve", ones_bf.ap, 1.0, W=[ones_bf])
        MS("dve", onesf.ap, 1.0, W=[onesf])
        MS("dve", halfpi.ap, math.pi / 2, W=[halfpi])
        DMA("sp", cv.ap, cvec[:, :], W=[cv])
        DMA("sp", cbias.ap, cbiasf[:, :], W=[cbias])
        DMA("pool", ident.ap, identf[:, :], W=[ident])
        DMA("pool", bmask.ap, bmaskf[:, :], W=[bmask])
        DMA("pool", wiwb.ap, wiw[:, :, :], W=[wiwb])
        DMA("sp", fg_bc.ap, fgain[0:1, :].to_broadcast([128, D]), W=[fg_bc])
        TS("dve", nbf.ap, cv.ap[:, 10:11], -1.0, ALU.mult, R=[cv], W=[nbf])

        def load_x_dma(src, t0, xf):
            DMA("sp", xf.ap, src[:, :, t0:t0 + 512], W=[xf])

        def load_x_prep(xf, xb, xsq):
            ACT(xsq.ap, xf.ap, AF.Square, R=[xf], W=[xsq])
            for c in range(8):
                ACT(xb.ap[:, c, :], xf.ap[:, c, :], AF.Copy, R=[xf, cv], W=[xb], scale=GAIN(c))

        def load_x_group(src, t0, xf, xb, xsq):
            load_x_dma(src, t0, xf)
            load_x_prep(xf, xb, xsq)

        def rstd_gen(xsq, rbc, rcol, pA, pB):
            MM(pA.ap, [(ones_bf.ap[:, 0:128], xsq.ap[:, c, :]) for c in range(8)], R=[xsq, ones_bf], W=[pA])
            yield
            for tb in range(4):
                MM(pB.ap[:, tb:tb + 1], [(xsq.ap[:, c, tb * 128:(tb + 1) * 128], ones_bf.ap[:, 0:1]) for c in range(8)],
                   R=[xsq, ones_bf], W=[pB])
            yield
            TS("dve", rcol.ap, pB.ap[:, 0:4], 1.0 / D, ALU.mult, EPS, ALU.add, R=[pB], W=[rcol])
            TS("dve", rbc.ap, pA.ap, 1.0 / D, ALU.mult, EPS, ALU.add, R=[pA], W=[rbc])
            yield
            ACT(rcol.ap, rcol.ap, AF.Sqrt, R=[rcol], W=[rcol])
            ACT(rbc.ap, rbc.ap, AF.Sqrt, R=[rbc], W=[rbc])
            yield
            S.op("dve", lambda e: e.reciprocal(out=rcol.ap, in_=rcol.ap), _keys([rcol]), _keys([rcol]))
            yield
            S.op("dve", lambda e: e.reciprocal(out=rbc.ap, in_=rbc.ap), _keys([rbc]), _keys([rbc]))
            yield

        def rstd_from(xsq, xb_unused, rbc, rcol, pA, pB, scale_extra=1.0):
            for _ in rstd_gen(xsq, rbc, rcol, pA, pB):
                pass

        def rope_gen(possrc, t0, posi, ang, kk, sn, cs, rbc, scale_extra):
            DMA("sp", posi.ap, possrc[0:1, t0:t0 + 512].to_broadcast([128, 512]), W=[posi])
            CP("dve", ang.ap, posi.ap, R=[posi], W=[ang])
            yield
            TS("dve", ang.ap, ang.ap, INVF, ALU.mult, R=[ang, cv], W=[ang])
            yield
            TS("dve", kk.ap, ang.ap, 1.0 / (2.0 * math.pi), ALU.mult, MAGIC, ALU.add, R=[ang], W=[kk])
            yield
            TS("dve", kk.ap, kk.ap, -MAGIC, ALU.add, R=[kk], W=[kk])
            yield
            STT(ang.ap, kk.ap, -C1, ang.ap, ALU.mult, ALU.add, R=[kk, ang], W=[ang])
            yield
            STT(ang.ap, kk.ap, -C2, ang.ap, ALU.mult, ALU.add, R=[kk, ang], W=[ang])
            yield
            TS("dve", ang.ap, ang.ap, math.pi, ALU.min, -math.pi, ALU.max, R=[ang], W=[ang])
            yield
            STT(kk.ap, ang.ap, -1.0, ang.ap, ALU.mult, ALU.max, R=[ang], W=[kk])
            ACT(sn.ap, ang.ap, AF.Sin, R=[ang], W=[sn])
            ACT(cs.ap, kk.ap, AF.Sin, R=[kk, halfpi], W=[cs], bias=halfpi.ap[:, 0:1], scale=-1.0)
            yield
            STT(sn.ap, sn.ap, SGNS, rbc.ap, ALU.mult, ALU.mult, R=[sn, cv, rbc], W=[sn])
            yield
            if scale_extra != 1.0:
                TS("dve", sn.ap, sn.ap, scale_extra, ALU.mult, R=[sn], W=[sn])
                STT(cs.ap, cs.ap, scale_extra, rbc.ap, ALU.mult, ALU.mult, R=[cs, rbc], W=[cs])
            else:
                TT("dve", cs.ap, cs.ap, rbc.ap, ALU.mult, R=[cs, rbc], W=[cs])
            yield

        def rope_tables(possrc, t0, posi, ang, kk, sn, cs, rbc, scale_extra):
            for _ in rope_gen(possrc, t0, posi, ang, kk, sn, cs, rbc, scale_extra):
                pass

        AR.reset(0)
        wkb = AR.alloc([8, NKC], BF16, "wkb")
        xfs = [AR.alloc([8, 512], F32, "xf") for _ in range(2)]
        xbs = [AR.alloc([8, 512], BF16, "xb") for _ in range(2)]
        xsq1 = AR.alloc([8, 512], BF16, "xsq")
        posi = AR.alloc([512], I32, "posi")
        ang = AR.alloc([512], F32, "ang")
        kk = AR.alloc([512], F32, "kk")
        sns = [AR.alloc([512], F32, "sn") for _ in range(2)]
        css = [AR.alloc([512], F32, "cs") for _ in range(2)]
        rbcs = [AR.alloc([512], F32, "rbc") for _ in range(2)]
        t1s = [AR.alloc([512], F32, "t1") for _ in range(2)]
        t2s = [AR.alloc([512], F32, "t2") for _ in range(2)]
        ksts = [AR.alloc([512], BF16, "kst") for _ in range(2)]
        mf = AR.alloc([512], F32, "mf")
        kist = AR.alloc([512], BF16, "kist")
        ee = AR.alloc([512], F32, "ee")
        ncums = [AR.alloc([512], F32, "ncum") for _ in range(2)]
        r1s = AR.alloc([512], F32, "r1s")
        aug = AR.alloc([3, 512], BF16, "aug")
        vsts = [AR.alloc([8, 4, 128], BF16, "vst") for _ in range(2)]

        for i in range(4):
            DMA("pool", wkb.ap[:, :, i * 704:(i + 1) * 704], wk[:, :, i * 704:(i + 1) * 704], W=[wkb])
        for v in vsts:
            MS("pool", v.ap, 1.0, W=[v])
        n_tg = S_LEN // 512
        if stop_after == "A1":
            n_tg = 1
        if stop_after == "A0":
            n_tg = 0
        kst_i = 0
        def chain_gen(tgn):
            k = tgn % 2
            for _ in rstd_gen(xsq1, rbcs[k], rcol_as[k], ps[0], ps[1]):
                yield
            for _ in rope_gen(pos_all, tgn * 512, posi, ang, kk, sns[k], css[k], rbcs[k], 1.0):
                yield

        if n_tg > 0:
            load_x_dma(xT, 0, xfs[0])
            load_x_prep(xfs[0], xbs[0], xsq1)
            for _ in chain_gen(0):
                pass
        for tg in range(n_tg):
            t0 = tg * 512
            xb = xbs[tg % 2]
            rbc = rbcs[tg % 2]
            rcol_a = rcol_as[tg % 2]
            sn = sns[tg % 2]
            cs = css[tg % 2]
            if tg + 1 < n_tg:
                load_x_dma(xT, t0 + 512, xfs[(tg + 1) % 2])
            if tg == 1 or (n_tg == 1 and tg == 0):
                for i in range(NQC // 512):
                    DMA("pool", wq_s[:, :, i * 512:(i + 1) * 512], wq[:, :, i * 512:(i + 1) * 512], W=["wq_s"])
                for i in range(2):
                    DMA("pool", wbd_s[:, :, i * 512:(i + 1) * 512], wbd[:, :, i * 512:(i + 1) * 512], W=["wbd_s"])
                    DMA("pool", wbf_s[:, :, i * 512:(i + 1) * 512], wbf[:, :, i * 512:(i + 1) * 512], W=["wbf_s"])
                    DMA("pool", wo_s[:, :, i * 512:(i + 1) * 512], wo[:, :, i * 512:(i + 1) * 512], W=["wo_s"])


            def proj_chunk(pbuf, n0):
                MM(pbuf.ap, [(wkb.ap[:, c, n0:n0 + 128], xb.ap[:, c, :]) for c in range(8)], R=[wkb, xb], W=[pbuf])

            for cc in range(4):
                pb = ps[2 + (cc % 2)]
                proj_chunk(pb, cc * 128)
                kst = ksts[kst_i % 2]
                kst_i += 1
                TT("dve", kst.ap, pb.ap, rbc.ap, ALU.mult, R=[pb, rbc], W=[kst])
                DMA("sp", kf_s[2 * cc, 0:64, t0:t0 + 512], kst.ap[0:64, :], R=[kst], W=["kf_s"])
                DMA("sp", kf_s[2 * cc + 1, 0:64, t0:t0 + 512], kst.ap[64:128, :], R=[kst], W=["kf_s"])
            DMA("sp", kf_s[:, 64, t0:t0 + 512], ones_bf.ap[0:8, :], R=[ones_bf], W=["kf_s"])
            for br in range(2):
                vst = vsts[br]
                dst = vf_s if br == 0 else va_s
                n0 = 1792 + br * 512
                for tb in range(4):
                    pv = ps[6 + (tb % 2)]
                    MM(pv.ap, [(xb.ap[:, c, tb * 128:(tb + 1) * 128], wkb.ap[:, c, n0:n0 + 512]) for c in range(8)],
                       R=[xb, wkb], W=[pv])
                    ACT(vst.ap[:, :, tb, 0:64], pv.ap.rearrange("p (h c) -> p h c", c=64), AF.Copy,
                        R=[pv, rcol_a], W=[vst], scale=rcol_a.ap[:, tb:tb + 1])
                for h in range(8):
                    DMA("sp", dst[h, :, tg * 4:(tg + 1) * 4, :], vst.ap[:, h, :, :], R=[vst], W=["v_s%d" % br])

            ahead = None
            if tg + 1 < n_tg:
                load_x_prep(xfs[(tg + 1) % 2], xbs[(tg + 1) % 2], xsq1)
                ahead = chain_gen(tg + 1)

            def adv(n):
                if ahead is not None:
                    for _ in range(n):
                        try:
                            next(ahead)
                        except StopIteration:
                            break

            for cc in range(5):
                pa, pb = (ps[4], ps[5]) if cc % 2 == 0 else (ps[2], ps[3])
                if cc < 4:
                    proj_chunk(pa, 512 + cc * 128)
                    proj_chunk(pb, 1024 + cc * 128)
                else:
                    proj_chunk(pa, 1536)
                    proj_chunk(pb, 1664)
                adv(2)
                t1 = t1s[cc % 2]
                t2 = t2s[cc % 2]
                TT("dve", t1.ap, pa.ap, cs.ap, ALU.mult, R=[pa, cs], W=[t1])
                adv(1)
                TT("dve", t2.ap, pb.ap, sn.ap, ALU.mult, R=[pb, sn], W=[t2])
                adv(1)
                if cc < 4:
                    kst = ksts[kst_i % 2]
                    kst_i += 1
                    TT("pool", kst.ap, t1.ap, t2.ap, ALU.add, R=[t1, t2], W=[kst])
                    DMA("sp", ka_s[2 * cc, :, t0:t0 + 512], kst.ap[0:64, :], R=[kst], W=["ka_s"])
                    DMA("sp", ka_s[2 * cc + 1, :, t0:t0 + 512], kst.ap[64:128, :], R=[kst], W=["ka_s"])
                else:
                    TT("pool", mf.ap, t1.ap, t2.ap, ALU.add, R=[t1, t2], W=[mf])
                    CP("pool", kist.ap[0:64, :], mf.ap[0:64, :], R=[mf], W=[kist])
                    DMA("sp", ki_s[:, t0:t0 + 512], kist.ap[0:64, :], R=[kist], W=["ki_s"])
                    ACT(ee.ap[96:104, :], mf.ap[96:104, :], AF.Exp, R=[mf, nbf], W=[ee], bias=nbf.ap[96:104, 0:1], scale=-1.0)
                    ACT(ee.ap[96:104, :], ee.ap[96:104, :], AF.Ln, R=[ee], W=[ee], bias=onesf.ap[96:104, 0:1], scale=1.0)
                    nc_cur = ncums[tg % 2]
                    nc_prev = ncums[(tg + 1) % 2]
                    init = 0.0 if tg == 0 else nc_prev.ap[96:104, 511:512]
                    S.op("dve", (lambda o_, d1_, i_: (lambda e: e.tensor_tensor_scan(
                        out=o_, data0=onesf.ap[96:104, :], data1=d1_, initial=i_, op0=ALU.mult, op1=ALU.add)))(
                        nc_cur.ap[96:104, :], ee.ap[96:104, :], init),
                        _keys([ee, onesf, nc_prev]), _keys([nc_cur]))
                    CP("dve", aug.ap[96:104, 0, :], nc_cur.ap[96:104, :], R=[nc_cur], W=[aug])
                    TT("dve", r1s.ap[96:104, :], nc_cur.ap[96:104, :], aug.ap[96:104, 0, :], ALU.subtract, R=[nc_cur, aug], W=[r1s])
                    CP("dve", aug.ap[96:104, 1, :], r1s.ap[96:104, :], R=[r1s], W=[aug])
                    TT("dve", r1s.ap[96:104, :], r1s.ap[96:104, :], aug.ap[96:104, 1, :], ALU.subtract, R=[r1s, aug], W=[r1s])
                    CP("dve", aug.ap[96:104, 2, :], r1s.ap[96:104, :], R=[r1s], W=[aug])
                    DMA("sp", kf_s[:, 65:68, t0:t0 + 512], aug.ap[96:104, :, :], R=[aug], W=["kf_s"])
            adv(1000)
        n_groups = NG
        if stop_after in ("A", "A1", "A0"):
            n_groups = 0
        elif stop_after is not None and stop_after.startswith("B"):
            n_groups = 1
        for g in range(n_groups):
            S.barrier()
            q0 = g * GQ
            win0 = 2048 * g
            L = 2048 * (g + 1)
            nkb = L // 128
            AR.reset(0)
            qfT = AR.alloc([8, 512], BF16, "qfT")
            qaT = AR.alloc([8, 512], BF16, "qaT")
            qiT = AR.alloc([4, 512], BF16, "qiT")
            xqb = AR.alloc([8, 512], BF16, "xqb")
            rbq = AR.alloc([512], F32, "rbq")
            mT = AR.alloc([64, 512], U8, "mT")
            yTa = AR.alloc([8, 512], BF16, "yTa")
            yTf = AR.alloc([8, 512], BF16, "yTf")
            assert AR.off <= GPERS, AR.off

            AR.reset(GPERS)
            xqf = AR.alloc([8, 512], F32, "xqf")
            xsqq = AR.alloc([8, 512], BF16, "xsqq")
            wqps = [AR.alloc([8, 512], BF16, "wqp") for _ in range(2)]
            posi = AR.alloc([512], I32, "posi")
            ang = AR.alloc([512], F32, "ang")
            kk = AR.alloc([512], F32, "kk")
            sn = AR.alloc([512], F32, "sn")
            cs = AR.alloc([512], F32, "cs")
            rb8 = AR.alloc([512], F32, "rb8")
            t1s = [AR.alloc([512], F32, "t1") for _ in range(2)]
            t2s = [AR.alloc([512], F32, "t2") for _ in range(2)]
            craws = [AR.alloc([2048], BF16, "craw") for _ in range(2)]

            load_x_group(xTo, q0, xqf, xqb, xsqq)
            rstd_from(xsqq, xqb, rbq, rcol_q, ps[0], ps[1])
            TS("dve", rb8.ap, rbq.ap, 0.125, ALU.mult, R=[rbq], W=[rb8])
            qchain = rope_gen(pos_own, q0, posi, ang, kk, sn, cs, rbq, 0.125)

            def qadv(n):
                for _ in range(n):
                    try:
                        next(qchain)
                    except StopIteration:
                        break

            wq_i = [0]

            def load_wq(piece):
                wb = wqps[wq_i[0] % 2]
                wq_i[0] += 1
                DMA("sp", wb.ap, wq_s[:, :, piece * 512:(piece + 1) * 512], R=["wq_s"], W=[wb])
                return wb

            def qproj(pbuf, wb, n0):
                MM(pbuf.ap, [(wb.ap[:, c, n0:n0 + 128], xqb.ap[:, c, :]) for c in range(8)], R=[wb, xqb], W=[pbuf])

            MS("pool", qaT.ap[64:128, :, :], 0.0, W=[qaT])
            MS("pool", qfT.ap[64:128, :, :], 0.0, W=[qfT])
            MS("pool", qfT.ap[64:68, :, :], 1.0, W=[qfT])
            MS("pool", qiT.ap[64:128, :, :], 0.0, W=[qiT])
            wb = load_wq(0)
            for cc in range(4):
                pb = ps[2 + cc]
                qproj(pb, wb, cc * 128)
            for cc in range(4):
                pb = ps[2 + cc]
                TT("dve", qfT.ap[0:64, 2 * cc, :], pb.ap[0:64, :], rb8.ap[0:64, :], ALU.mult, R=[pb, rb8], W=[qfT])
                qadv(2)
                TT("dve", qfT.ap[0:64, 2 * cc + 1, :], pb.ap[64:128, :], rb8.ap[64:128, :], ALU.mult, R=[pb, rb8], W=[qfT])
                qadv(2)
            qadv(1000)
            for h in range(8):
                cr = craws[h % 2]
                DMA("sp", cr.ap[64:65, :], kf_s[h, 65:66, win0:win0 + 2048], R=["kf_s"], W=[cr])
                TS("dve", qfT.ap[64:65, h, :], cr.ap[64:65, :].rearrange("p (i r) -> p i r", r=4)[:, :, 0], -1.0, ALU.mult,
                   R=[cr], W=[qfT])
            wb1 = load_wq(1)
            wb2 = load_wq(2)
            for cc in range(4):
                pa, pb = ps[4 + 2 * (cc % 2)], ps[5 + 2 * (cc % 2)]
                qproj(pa, wb1, cc * 128)
                qproj(pb, wb2, cc * 128)
                t1 = t1s[cc % 2]
                t2 = t2s[cc % 2]
                TT("dve", t1.ap, pa.ap, cs.ap, ALU.mult, R=[pa, cs], W=[t1])
                TT("dve", t2.ap, pb.ap, sn.ap, ALU.mult, R=[pb, sn], W=[t2])
                TT("pool", qaT.ap[0:64, 2 * cc, :], t1.ap[0:64, :], t2.ap[0:64, :], ALU.add, R=[t1, t2], W=[qaT])
                TT("pool", qaT.ap[0:64, 2 * cc + 1, :], t1.ap[64:128, :], t2.ap[64:128, :], ALU.add, R=[t1, t2], W=[qaT])
            wb = load_wq(3)
            for cc in range(2):
                pa, pb = ps[4 + 2 * (cc % 2)], ps[5 + 2 * (cc % 2)]
                qproj(pa, wb, cc * 128)
                qproj(pb, wb, 256 + cc * 128)
                t1 = t1s[cc % 2]
                t2 = t2s[cc % 2]
                TT("dve", t1.ap, pa.ap, cs.ap, ALU.mult, R=[pa, cs], W=[t1])
                TT("dve", t2.ap, pb.ap, sn.ap, ALU.mult, R=[pb, sn], W=[t2])
                TT("pool", qiT.ap[0:64, 2 * cc, :], t1.ap[0:64, :], t2.ap[0:64, :], ALU.add, R=[t1, t2], W=[qiT])
                TT("pool", qiT.ap[0:64, 2 * cc + 1, :], t1.ap[64:128, :], t2.ap[64:128, :], ALU.add, R=[t1, t2], W=[qiT])
            for sb in range(4):
                MM(ps[1].ap[:, 8 + 4 * sb:12 + 4 * sb],
                   [(xqb.ap[:, c, sb * 128:(sb + 1) * 128], wiwb.ap[:, c, :]) for c in range(8)], R=[xqb, wiwb], W=[ps[1]])
                TS("dve", w4.ap[:, 4 * sb:4 * sb + 4], ps[1].ap[:, 8 + 4 * sb:12 + 4 * sb], rcol_q.ap[:, sb:sb + 1], ALU.mult,
                   0.5, ALU.mult, R=[ps[1], rcol_q], W=[w4])
            STT(absw.ap, w4.ap, -1.0, w4.ap, ALU.mult, ALU.max, R=[w4], W=[absw])
            TS("dve", sgnw.ap, w4.ap, 0.0, ALU.is_ge, 2.0, ALU.mult, R=[w4], W=[sgnw])
            TS("dve", sgnw.ap, sgnw.ap, -1.0, ALU.add, R=[sgnw], W=[sgnw])
            if debug and g == 0:
                DMA("sp", dbg["qfT"][:, :, :], qfT.ap, R=[qfT], W=["dbg1"])
                DMA("sp", dbg["qaT"][:, :, :], qaT.ap, R=[qaT], W=["dbg2"])
                DMA("sp", dbg["qiT"][:, :, :], qiT.ap, R=[qiT], W=["dbg3"])
            if stop_after == "B1":
                break

            S.barrier()
            AR.reset(GPERS)
            sc = AR.alloc([8192], F32, "sc")
            msk = AR.alloc([8192], BF16, "msk")
            rts = [AR.alloc([512], F32, "rt") for _ in range(2)]
            kits = [AR.alloc([2048], BF16, "kit") for _ in range(1)]
            f_kTps = [AR.alloc([2048], BF16, "kTp") for _ in range(2)]
            f_vps = [AR.alloc([16, 128], BF16, "vp") for _ in range(2)]
            f_Pts = [AR.alloc([512], BF16, "Pt") for _ in range(4)]
            f_recs = [AR.alloc([512], F32, "rec") for _ in range(1)]

            for kt_ in f_kTps + kits:
                MS("pool", kt_.ap[64:128, :], 0.0, W=[kt_])

            def attn_steps(br, kTps, vps, Pts, Pms, recs, psS, psO):
                tiles = [(h, pc, kb) for h in range(8) for pc in range(g + 1) for kb in range(16)]
                pend = []
                nS = len(psS)
                cur = {}
                piece_n = 0

                def issue_pv(item):
                    (h, pc, kb, pmat, vp, first, last) = item
                    po = psO[h % len(psO)]
                    S.op("pe", (lambda o_, l_, r_, f_, s_: (lambda e: e.matmul(o_, lhsT=l_, rhs=r_, start=f_, stop=s_)))(
                        po.ap, vp.ap[:, kb, :], pmat.ap, first, last), _keys([vp, pmat]), _keys([po]))
                    if last:
                        yT = yTa if br == 0 else yTf
                        rec = recs[h % len(recs)]
                        S.op("dve", (lambda o_, i_: (lambda e: e.reciprocal(out=o_, in_=i_)))(rec.ap[0:64, :], po.ap[64:128, :]),
                             _keys([po]), _keys([rec]))
                        TT("dve", yT.ap[0:64, h, :], po.ap[0:64, :], rec.ap[0:64, :], ALU.mult, R=[po, rec], W=[yT])

                for ti, (h, pc, kb) in enumerate(tiles):
                    if kb == 0:
                        kTp = kTps[piece_n % len(kTps)]
                        vp = vps[piece_n % len(vps)]
                        piece_n += 1
                        if br == 0:
                            DMA("sp", kTp.ap[0:64, :], ka_s[h, :, pc * 2048:(pc + 1) * 2048], R=["ka_s"], W=[kTp])
                            DMA("sp", vp.ap, va_s[h, :, pc * 16:(pc + 1) * 16, :], R=["v_s1"], W=[vp])
                        else:
                            DMA("sp", kTp.ap[0:68, :], kf_s[h, :, pc * 2048:(pc + 1) * 2048], R=["kf_s"], W=[kTp])
                            DMA("sp", vp.ap, vf_s[h, :, pc * 16:(pc + 1) * 16, :], R=["v_s0"], W=[vp])
                        cur["k"], cur["v"] = kTp, vp
                    kTp, vp = cur["k"], cur["v"]
                    pS = psS[ti % nS]
                    Pt = Pts[ti % len(Pts)]
                    diag = (br == 1 and pc == g)
                    c0 = 32 * kb if diag else 0
                    if br == 0:
                        MM(pS.ap[:, c0:512], [(kTp.ap[:, kb * 128:(kb + 1) * 128], qaT.ap[:, h, c0:512])], R=[kTp, qaT], W=[pS])
                    else:
                        MM(pS.ap[:, c0:512], [(kTp.ap[:, kb * 128:(kb + 1) * 128], qfT.ap[:, h, c0:512])], R=[kTp, qfT], W=[pS])
                    ACT(Pt.ap[:, c0:512], pS.ap[:, c0:512], AF.Exp, R=[pS], W=[Pt])
                    if br == 0:
                        Pm = Pms[ti % len(Pms)]
                        TT("pool" if ti % 2 == 1 else "dve", Pm.ap, Pt.ap, mT.ap[:, pc * 16 + kb, :], ALU.mult, R=[Pt, mT], W=[Pm])
                        pmat = Pm
                    else:
                        if diag:
                            if c0 > 0:
                                MS("pool", Pt.ap[:, 0:c0], 0.0, W=[Pt])
                            TT("pool", Pt.ap[:, c0:c0 + 32], Pt.ap[:, c0:c0 + 32], bmask.ap, ALU.mult, R=[Pt, bmask], W=[Pt])
                        pmat = Pt
                    pend.append((h, pc, kb, pmat, vp, (pc == 0 and kb == 0), (pc == g and kb == 15)))
                    if len(pend) > min(4, nS - 1):
                        issue_pv(pend.pop(0))
                    yield 1
                while pend:
                    issue_pv(pend.pop(0))

            def b2_units():
                for sb in range(4):
                    nch = 4 * g + sb + 1
                    Lsb = 512 * nch
                    kit = kits[0]
                    for ch in range(nch):
                        if ch % 4 == 0:
                            pc = ch // 4
                            DMA("sp", kit.ap[0:64, :], ki_s[:, pc * 2048:(pc + 1) * 2048], R=["ki_s"], W=[kit])
                        for h in range(4):
                            pb = ps[h]
                            MM(pb.ap, [(qiT.ap[:, h, sb * 128:(sb + 1) * 128], kit.ap[:, (ch % 4) * 512:(ch % 4 + 1) * 512])],
                               R=[qiT, kit], W=[pb])
                        for h in range(4):
                            pb = ps[h]
                            rt = rts[h % 2]
                            ACT(rt.ap, pb.ap, AF.Relu, R=[pb, absw], W=[rt], scale=absw.ap[:, 4 * sb + h:4 * sb + h + 1])
                            scs = sc.ap[:, ch * 512:(ch + 1) * 512]
                            sg = sgnw.ap[:, 4 * sb + h:4 * sb + h + 1]
                            if h == 0:
                                TS("dve", scs, rt.ap, sg, ALU.mult, R=[rt, sgnw], W=[sc])
                            else:
                                STT(scs, rt.ap, sg, scs, ALU.mult, ALU.add, R=[rt, sgnw, sc], W=[sc])
                        if ch == nch - 1:
                            TT("dve", sc.ap[:, ch * 512:(ch + 1) * 512], sc.ap[:, ch * 512:(ch + 1) * 512], cbias.ap, ALU.add,
                               R=[sc, cbias], W=[sc])
                        yield 3.2
                    if debug and g == 0 and sb == 3:
                        DMA("sp", dbg["sc"][:, 0:Lsb], sc.ap[:, 0:Lsb], R=[sc], W=["dbg4"])
                    MS("dve", bis_lo.ap, -RNG, W=[bis_lo])
                    La = 512 * int(0.4 * nch) if nch >= 3 else 0
                    Ld = Lsb - La
                    for it in range(NIT):
                        step = RNG / (2.0 ** it)
                        TS("dve", bis_mid.ap, bis_lo.ap, step, ALU.add, R=[bis_lo], W=[bis_mid])
                        if La > 0:
                            TS("dve", bis_nb.ap, bis_mid.ap, -1.0, ALU.mult, 2.0 ** -20, ALU.add, R=[bis_mid], W=[bis_nb])
                        TS("dve", msk.ap[:, 0:Ld], sc.ap[:, 0:Ld], bis_mid.ap[:, 0:1], ALU.is_ge, None, ALU.add,
                           R=[sc, bis_mid], W=[msk, bis_cnt], accum=bis_cnt.ap[:, 0:1])
                        if La > 0:
                            ACT(msk.ap[:, Ld:Lsb], sc.ap[:, Ld:Lsb], AF.Sign, R=[sc, bis_nb], W=["mskA", bis_ca],
                                bias=bis_nb.ap[:, 0:1], scale=1.0, accum=bis_ca.ap[:, 0:1])
                            STT(bis_tot.ap, bis_ca.ap, 0.5, bis_cnt.ap, ALU.mult, ALU.add, R=[bis_ca, bis_cnt], W=[bis_tot])
                            TS("dve", bis_ind.ap, bis_tot.ap, TOPK - La / 2.0, ALU.is_ge, step, ALU.mult, R=[bis_tot], W=[bis_ind])
                        else:
                            TS("dve", bis_ind.ap, bis_cnt.ap, TOPK, ALU.is_ge, step, ALU.mult, R=[bis_cnt], W=[bis_ind])
                        TT("dve", bis_lo.ap, bis_lo.ap, bis_ind.ap, ALU.add, R=[bis_lo, bis_ind], W=[bis_lo])
                        yield 0.7 + Ld * 1.05e-3
                    TS("dve", bis_ind.ap, bis_lo.ap, -RNG, ALU.is_le, -1000.0, ALU.mult, R=[bis_lo], W=[bis_ind])
                    TT("dve", bis_lo.ap, bis_lo.ap, bis_ind.ap, ALU.add, R=[bis_lo, bis_ind], W=[bis_lo])
                    TS("dve", msk.ap[:, 0:Lsb], sc.ap[:, 0:Lsb], bis_lo.ap[:, 0:1], ALU.is_ge, R=[sc, bis_lo], W=[msk, "mskA"])
                    if debug and g == 0:
                        DMA("sp", dbg["lo"][sb], bis_lo.ap, R=[bis_lo], W=["dbg5"])
                    yield Lsb * 0.6e-3
                    for k4 in range(Lsb // 512):
                        pt = ps[k4 % 2]
                        ptb = pt.ap.bitcast(BF16)
                        for i in range(4):
                            kb = k4 * 4 + i
                            S.op("pe", (lambda o_, i_: (lambda e: e.transpose(out=o_, in_=i_, identity=ident.ap)))(
                                ptb[:, i * 128:(i + 1) * 128], msk.ap[:, kb * 128:(kb + 1) * 128]),
                                _keys([msk, ident]), _keys([pt]))
                        ACT(mT.ap[:, k4 * 4:(k4 + 1) * 4, sb * 128:(sb + 1) * 128],
                            ptb[:, 0:512].rearrange("p (a b) -> p a b", b=128), AF.Copy, R=[pt], W=[mT])
                        yield 0.7
                    if Lsb // 128 < nkb:
                        MS("pool", mT.ap[:, Lsb // 128:nkb, sb * 128:(sb + 1) * 128], 0, W=[mT])

            fox = attn_steps(1, f_kTps, f_vps, f_Pts, None, f_recs, [ps[4], ps[5]], [ps[6], ps[7]])
            n_fox = 128 * (g + 1)
            units = list()
            tot_est = 0.0
            for sb in range(4):
                nch = 4 * g + sb + 1
                Lsb = 512 * nch
                La_ = 512 * int(0.4 * nch) if nch >= 3 else 0
                tot_est += nch * 3.2 + NIT * (0.7 + (Lsb - La_) * 1.05e-3) + Lsb * 0.6e-3 + (Lsb // 512) * 0.7
            rate = n_fox / (0.9 * tot_est)
            acc = 0.0
            fox_done = False
            for wgt in b2_units():
                acc += wgt * rate
                while acc >= 1.0 and not fox_done:
                    acc -= 1.0
                    try:
                        next(fox)
                    except StopIteration:
                        fox_done = True
            if not fox_done:
                for _ in fox:
                    pass
            if debug and g == 0:
                DMA("sp", dbg["mT"][:, 0:nkb, :], mT.ap[:, 0:nkb, :], R=[mT], W=["dbg6"])
            if stop_after == "B2":
                break

            S.barrier()
            AR.reset(GPERS)
            kTps = [AR.alloc([2048], BF16, "kTp") for _ in range(3)]
            vps = [AR.alloc([16, 128], BF16, "vp") for _ in range(3)]
            Pts = [AR.alloc([512], BF16, "Pt") for _ in range(6)]
            Pms = [AR.alloc([512], BF16, "Pm") for _ in range(6)]
            recs = [AR.alloc([512], F32, "rec") for _ in range(2)]
            for kt_ in kTps:
                MS("pool", kt_.ap[64:128, :], 0.0, W=[kt_])
            for _ in attn_steps(0, kTps, vps, Pts, Pms, recs, [ps[0], ps[1], ps[2], ps[3], ps[6], ps[7]], [ps[4], ps[5]]):
                pass
            if debug and g == 0:
                DMA("sp", dbg["yTa"][:, :, :], yTa.ap, R=[yTa], W=["dbg7"])
                DMA("sp", dbg["yTf"][:, :, :], yTf.ap, R=[yTf], W=["dbg8"])
            if stop_after == "B3":
                break

            S.barrier()
            AR.reset(GPERS)
            wqps = [AR.alloc([8, 512], BF16, "wqp") for _ in range(2)]
            wbrs = [AR.alloc([8, 128], BF16, "wbr") for _ in range(2)]
            wobs = [AR.alloc([8, 512], BF16, "wob") for _ in range(2)]
            ygs = [AR.alloc([8, 512], BF16, "yg") for _ in range(2)]
            mrg = AR.alloc([8, 512], BF16, "mrg")
            gtmp = AR.alloc([512], F32, "gtmp")
            gbf = AR.alloc([512], BF16, "gbf")
            gms4 = [AR.alloc([512], F32, "gm") for _ in range(2)]
            e1s = [AR.alloc([512], F32, "e1") for _ in range(1)]
            e2s = [AR.alloc([512], F32, "e2") for _ in range(1)]
            xot = AR.alloc([D], F32, "xot")
            xnew = AR.alloc([D], F32, "xnew")
            wq_i = [0]
            for br in range(2):
                wb = load_wq(4 + br)
                yT = yTa if br == 0 else yTf
                for cc in range(4):
                    pb = ps[cc % 2]
                    qproj(pb, wb, cc * 128)
                    TT("dve", gtmp.ap, pb.ap, rbq.ap, ALU.mult, R=[pb, rbq], W=[gtmp])
                    for hh in range(2):
                        h = 2 * cc + hh
                        ACT(gbf.ap[0:64, :], gtmp.ap[64 * hh:64 * hh + 64, :], AF.Silu, R=[gtmp], W=[gbf])
                        TT("pool", ygs[br].ap[0:64, h, :], yT.ap[0:64, h, :], gbf.ap[0:64, :], ALU.mult, R=[yT, gbf], W=[ygs[br]])
            wbr_n = [0]
            wqm = {}
            for hf in range(2):
                DMA("sp", wobs[hf].ap, wo_s[:, :, hf * 512:(hf + 1) * 512], R=["wo_s"], W=[wobs[hf]])
            for dc in range(8):
                pus = []
                par = dc % 2
                gms = [gms4[0], gms4[1]]
                e1, e2 = e1s[0], e2s[0]
                for br in range(2):
                    wsrc = wbd_s if br == 0 else wbf_s
                    wbr = wbrs[wbr_n[0] % 2]
                    wbr_n[0] += 1
                    DMA("sp", wbr.ap[0:64, :, :], wsrc[:, :, dc * 128:(dc + 1) * 128], R=["wbd_s", "wbf_s"], W=[wbr])
                    pu = ps[4 * par + br]
                    MM(pu.ap, [(wbr.ap[0:64, h, :], ygs[br].ap[0:64, h, :]) for h in range(8)], R=[wbr, ygs[br]], W=[pu])
                    pus.append(pu)
                    mcol = br * 1024 + dc * 128
                    piece = 6 + mcol // 512
                    if wqm.get(br, (None, None))[0] != piece:
                        wqm[br] = (piece, load_wq(piece))
                    wbm = wqm[br][1]
                    pm = ps[4 * par + 2 + br]
                    qproj(pm, wbm, mcol % 512)
                    TT("dve", gms[br].ap, pm.ap, rbq.ap, ALU.mult, R=[pm, rbq], W=[gms[br]])
                    ACT(gms[br].ap, gms[br].ap, AF.Sigmoid, R=[gms[br], cv], W=[gms[br]], bias=BMRG(br * 8 + dc))
                TT("dve", e1.ap, pus[0].ap, gms[0].ap, ALU.mult, R=[pus[0], gms[0]], W=[e1])
                TT("dve", e2.ap, pus[1].ap, gms[1].ap, ALU.mult, R=[pus[1], gms[1]], W=[e2])
                TT("pool", mrg.ap[:, dc, :], e1.ap, e2.ap, ALU.add, R=[e1, e2], W=[mrg])
            for sb in range(4):
                r0 = q0 + sb * 128
                DMA("sp", xot.ap, xo[r0:r0 + 128, :], W=[xot])
                for hf in range(2):
                    wob = wobs[hf]
                    po = ps[(2 * sb + hf) % 8]
                    MM(po.ap, [(mrg.ap[:, dc, sb * 128:(sb + 1) * 128], wob.ap[:, dc, :]) for dc in range(8)], R=[mrg, wob], W=[po])
                    TT("dve", xnew.ap[:, hf * 512:(hf + 1) * 512], po.ap, xot.ap[:, hf * 512:(hf + 1) * 512], ALU.add,
                       R=[po, xot], W=[xnew])
                ACT(xot.ap, xnew.ap, AF.Square, R=[xnew, xot], W=[xot, fin_ss], accum=fin_ss.ap[:, 0:1])
                TS("dve", fin_r.ap, fin_ss.ap, 1.0 / D, ALU.mult, EPS, ALU.add, R=[fin_ss], W=[fin_r])
                ACT(fin_r.ap, fin_r.ap, AF.Sqrt, R=[fin_r], W=[fin_r])
                S.op("dve", lambda e: e.reciprocal(out=fin_r.ap, in_=fin_r.ap), _keys([fin_r]), _keys([fin_r]))
                STT(xnew.ap, xnew.ap, fin_r.ap[:, 0:1], fg_bc.ap, ALU.mult, ALU.mult, R=[xnew, fin_r, fg_bc], W=[xnew])
                DMA("sp", y[r0:r0 + 128, :], xnew.ap, R=[xnew], W=["y"])

        fw = [o for o in S.dlast if o is not None]
        with nc.Block() as block:
            S.emit(block, final_waits=fw)
    return nc


def _rot_perm():
    p = np.arange(64)
    p[0:8] = np.arange(8, 16)
    p[8:16] = np.arange(0, 8)
    return p


def _fm(w):
    n = w.shape[1]
    return np.ascontiguousarray(w.reshape(8, 128, n).transpose(1, 0, 2))


def prep_inputs(x, positions, norm_gain, w_in, b_forget, b_merge, w_branch_dsa, w_branch_fox, w_out, final_gain):
    x = np.asarray(x, np.float32)
    positions = np.asarray(positions, np.int32)
    W = np.asarray(w_in, np.float32)[0]
    o = 0
    cols = {}
    for name, n in (("aq", 512), ("ak", 512), ("av", 512), ("ag", 512), ("iq", 256), ("ik", 64), ("iw", 4),
                    ("fq", 512), ("fk", 512), ("fv", 512), ("fg", 512), ("fl", 8), ("mg", 2048)):
        cols[name] = W[:, o:o + n]
        o += n
    perm = _rot_perm()

    def rot(w, nh):
        idx = np.concatenate([h * 64 + perm for h in range(nh)])
        return w[:, idx]

    z32 = np.zeros((D, 32), np.float32)
    z24 = np.zeros((D, 24), np.float32)
    z64 = np.zeros((D, 64), np.float32)
    wk = np.concatenate([cols["fk"], cols["ak"], rot(cols["ak"], 8),
                         cols["ik"], z32, cols["fl"], z24, rot(cols["ik"], 1), z64,
                         cols["fv"], cols["av"]], axis=1)
    assert wk.shape[1] == NKC
    wq = np.concatenate([cols["fq"], cols["aq"], rot(cols["aq"], 8), cols["iq"], rot(cols["iq"], 4),
                         cols["ag"], cols["fg"], cols["mg"]], axis=1)
    assert wq.shape[1] == NQC
    wk_d, wq_d, wiw_d = _fm(wk), _fm(wq), _fm(np.ascontiguousarray(cols["iw"]))
    wbd = np.ascontiguousarray(np.asarray(w_branch_dsa, np.float32)[0].reshape(8, 64, D).transpose(1, 0, 2))
    wbf = np.ascontiguousarray(np.asarray(w_branch_fox, np.float32)[0].reshape(8, 64, D).transpose(1, 0, 2))
    wo = _fm(np.asarray(w_out, np.float32)[0])
    cvec = np.zeros((128, 64), np.float32)
    cvec[:, 0:8] = np.asarray(norm_gain, np.float32)[0].reshape(8, 128).T
    half = 8
    inv_freq = (500000.0 ** (-np.arange(half, dtype=np.float32) * 2.0 / 16.0)).astype(np.float32)
    for p in range(128):
        r = p % 64
        if r < 8:
            cvec[p, 8] = inv_freq[r]
            cvec[p, 9] = -1.0
        elif r < 16:
            cvec[p, 8] = inv_freq[r - 8]
            cvec[p, 9] = 1.0
    cvec[96:104, 10] = np.asarray(b_forget, np.float32)[0]
    cvec[:, 16:32] = np.asarray(b_merge, np.float32)[0].reshape(16, 128).T
    fgain = np.asarray(final_gain, np.float32).reshape(1, D)
    identf = np.eye(128, dtype=np.float32)
    in_maps = []
    xT_b = [np.ascontiguousarray(x[b].T.reshape(8, 128, S_LEN).transpose(1, 0, 2)) for b in range(x.shape[0])]
    for c in range(8):
        b, j = divmod(c, 4)
        p_idx = np.arange(128)[:, None]
        m_idx = np.arange(32)[None, :]
        bmaskf = (p_idx <= 4 * m_idx + j).astype(np.float32)
        s_idx = np.arange(512)[None, :]
        cbiasf = np.where(s_idx <= 4 * p_idx + j, 0.0, NEGB).astype(np.float32)
        in_maps.append({
            "xT": xT_b[b],
            "xTo": np.ascontiguousarray(xT_b[b][:, :, j::4]),
            "xo": np.ascontiguousarray(x[b, j::4, :]),
            "pos_all": np.ascontiguousarray(positions[b][None, :]),
            "pos_own": np.ascontiguousarray(positions[b][None, j::4]),
            "wk": wk_d, "wq": wq_d, "wiw": wiw_d, "wbd": wbd, "wbf": wbf, "wo": wo,
            "cvec": cvec, "fgain": fgain, "identf": identf, "bmaskf": bmaskf, "cbiasf": cbiasf,
        })
    return in_maps


_NC_CACHE = {}


def kernel(x, positions, norm_gain, w_in, b_forget, b_merge, w_branch_dsa, w_branch_fox, w_out, final_gain):
    in_maps = prep_inputs(x, positions, norm_gain, w_in, b_forget, b_merge, w_branch_dsa, w_branch_fox, w_out, final_gain)
    if "nc" not in _NC_CACHE:
        _NC_CACHE["nc"] = build()
    nc = _NC_CACHE["nc"]
    res = run_bass_kernel_spmd(nc, in_maps, core_ids=list(range(8)))
    out = np.empty((2, S_LEN, D), np.float32)
    for c in range(8):
        b, j = divmod(c, 4)
        out[b, j::4, :] = res.results[c]["y"]
    return out
```

```python
import math
import contextlib
import numpy as np
import concourse.bass as bass
import concourse.mybir as mybir
from concourse.bass_utils import run_bass_kernel_spmd

F32 = mybir.dt.float32
BF16 = mybir.dt.bfloat16
I32 = mybir.dt.int32
U8 = mybir.dt.uint8
AF = mybir.ActivationFunctionType
ALU = mybir.AluOpType

D = 1024
S_LEN = 8192
NQ = 2048
GQ = 512
NG = NQ // GQ
NKC = 2816
NQC = 5120
EPS = 1e-6
NIT = 16
RNG = 8.0
TOPK = 256.0
NEGB = -30000.0
MAGIC = 12582912.0
C1 = 6.28125
C2 = 2.0 * math.pi - 6.28125
ARENA = 164864
GPERS = 81920


class _Op:
    __slots__ = ("eng", "fn", "deps", "ticket", "has_dep", "dma", "sem", "target", "prev")

    def __init__(self, eng, fn, dma):
        self.eng = eng
        self.fn = fn
        self.deps = []
        self.ticket = None
        self.has_dep = False
        self.dma = dma
        self.sem = None
        self.target = None
        self.prev = None


class Sched:
    ENGS = ("pe", "act", "dve", "pool", "sp")

    def __init__(self, nc, n_dma_sems=14):
        self.nc = nc
        self.ops = {e: [] for e in self.ENGS}
        self.last_w = {}
        self.readers = {}
        self.nd = n_dma_sems
        self.rr = 0
        self.rr_sw = 0
        self.n_sw = 4
        self.dlast = [None] * n_dma_sems
        self.dcount = [0] * n_dma_sems
        self.bar_deps = []
        self.bar_pending = set()
        self.last_compute = {e: None for e in self.ENGS}

    def barrier(self):
        deps = [o for o in self.last_compute.values() if o is not None]
        deps += [o for o in self.dlast if o is not None]
        self.bar_deps = deps
        self.bar_pending = set(self.ENGS)
        self.last_w = {}
        self.readers = {}

    def op(self, eng, fn, reads=(), writes=(), dma=False):
        o = _Op(eng, fn, dma)
        deps = []
        if eng in self.bar_pending:
            deps.extend(self.bar_deps)
            self.bar_pending.discard(eng)
        for r in reads:
            w = self.last_w.get(r)
            if w is not None:
                deps.append(w)
        for w_ in writes:
            w = self.last_w.get(w_)
            if w is not None:
                deps.append(w)
            deps.extend(self.readers.get(w_, ()))
        seen = set()
        for d in deps:
            if d is o or id(d) in seen:
                continue
            seen.add(id(d))
            if d.eng == "pe" and eng == "pe" and not d.dma and not dma:
                continue
            o.deps.append(d)
            d.has_dep = True
        for r in reads:
            self.readers.setdefault(r, []).append(o)
        for w_ in writes:
            self.last_w[w_] = o
            self.readers[w_] = []
        if dma:
            if eng == "pool":
                k = self.rr_sw
                self.rr_sw = (self.rr_sw + 1) % self.n_sw
            else:
                k = self.n_sw + self.rr
                self.rr = (self.rr + 1) % (self.nd - self.n_sw)
            o.sem = k
            o.prev = self.dlast[k]
            self.dcount[k] += 16
            o.target = self.dcount[k]
            self.dlast[k] = o
        else:
            self.last_compute[eng] = o
        self.ops[eng].append(o)
        return o

    def alloc_sems(self, st):
        nc = self.nc
        self.esem = {e: st.enter_context(nc.semaphore("s_" + e)) for e in self.ENGS}
        self.dsem = [st.enter_context(nc.semaphore("d_%d" % i)) for i in range(self.nd)]

    def emit(self, block, final_waits=()):
        esem, dsem = self.esem, self.dsem
        for o in final_waits:
            o.has_dep = True
        for e in self.ENGS:
            c = 0
            for o in self.ops[e]:
                if (not o.dma) and o.has_dep:
                    c += 1
                    o.ticket = c

        def run(e, engobj, extra=None):
            waited = {}

            def wait_for(d):
                if d.dma:
                    key, val, sem = ("d", d.sem), d.target, dsem[d.sem]
                else:
                    key, val, sem = ("e", d.eng), d.ticket, esem[d.eng]
                if waited.get(key, 0) >= val:
                    return
                engobj.wait_ge(sem, val)
                waited[key] = val

            for o in self.ops[e]:
                for d in o.deps:
                    wait_for(d)
                if o.dma and o.prev is not None:
                    wait_for(o.prev)
                ins = o.fn(engobj)
                if o.dma:
                    ins.then_inc(dsem[o.sem], 16)
                elif o.has_dep:
                    ins.then_inc(esem[e], 1)
            if extra:
                for d in extra:
                    wait_for(d)

        @block.tensor
        def _(eng):
            run("pe", eng)

        @block.scalar
        def _(eng):
            run("act", eng)

        @block.vector
        def _(eng):
            run("dve", eng)

        @block.gpsimd
        def _(eng):
            run("pool", eng)

        @block.sync
        def _(eng):
            run("sp", eng, extra=list(final_waits))


class Buf:
    _n = 0

    def __init__(self, ap, name=None):
        Buf._n += 1
        self.ap = ap
        self.key = "%s#%d" % (name or "b", Buf._n)


def _keys(xs):
    out = []
    for x in xs:
        out.append(x.key if isinstance(x, Buf) else x)
    return out


_DT_SIZE = {F32: 4, BF16: 2, I32: 4, U8: 1}


class Arena:
    def __init__(self, t, nbytes):
        self.t = t
        self.n = nbytes
        self.off = 0

    def reset(self, off=0):
        self.off = off

    def alloc(self, shape, dt, name=None):
        n = 1
        for s in shape:
            n *= s
        nb = n * _DT_SIZE[dt]
        nb_al = (nb + 63) // 64 * 64
        assert self.off + nb_al <= self.n, ("arena overflow", name, self.off, nb_al, self.n)
        ap = self.t[:, self.off:self.off + nb].bitcast(dt)
        self.off += nb_al
        if len(shape) == 2:
            ap = ap.rearrange("p (a b) -> p a b", a=shape[0], b=shape[1])
        elif len(shape) == 3:
            ap = ap.rearrange("p (a b c) -> p a b c", a=shape[0], b=shape[1], c=shape[2])
        return Buf(ap, name)


def build(debug=False, stop_after=None):
    nc = bass.Bass("TRN2", target_bir_lowering=False)

    def din(name, shape, dt=F32):
        return nc.dram_tensor(name, list(shape), dt, kind="ExternalInput").ap()

    dbg_kind = "ExternalOutput" if debug else "Internal"

    def dscr(name, shape, dt):
        return nc.dram_tensor(name, list(shape), dt, kind=dbg_kind).ap()

    xT = din("xT", [128, 8, S_LEN])
    xTo = din("xTo", [128, 8, NQ])
    xo = din("xo", [NQ, D])
    pos_all = din("pos_all", [1, S_LEN], I32)
    pos_own = din("pos_own", [1, NQ], I32)
    wk = din("wk", [128, 8, NKC])
    wq = din("wq", [128, 8, NQC])
    wiw = din("wiw", [128, 8, 4])
    wbd = din("wbd", [64, 8, D])
    wbf = din("wbf", [64, 8, D])
    wo = din("wo", [128, 8, D])
    cvec = din("cvec", [128, 64])
    fgain = din("fgain", [1, D])
    identf = din("identf", [128, 128])
    bmaskf = din("bmaskf", [128, 32])
    cbiasf = din("cbiasf", [128, 512])
    y = nc.dram_tensor("y", [NQ, D], F32, kind="ExternalOutput").ap()

    kf_s = dscr("kf_s", [8, 68, S_LEN], BF16)
    ka_s = dscr("ka_s", [8, 64, S_LEN], BF16)
    ki_s = dscr("ki_s", [64, S_LEN], BF16)
    vf_s = dscr("vf_s", [8, 128, 64, 128], BF16)
    va_s = dscr("va_s", [8, 128, 64, 128], BF16)
    wq_s = nc.dram_tensor("wq_s", [128, 8, NQC], BF16, kind="Internal").ap()
    wbd_s = nc.dram_tensor("wbd_s", [64, 8, D], BF16, kind="Internal").ap()
    wbf_s = nc.dram_tensor("wbf_s", [64, 8, D], BF16, kind="Internal").ap()
    wo_s = nc.dram_tensor("wo_s", [128, 8, D], BF16, kind="Internal").ap()
    dbg = {}
    if debug:
        dbg["qfT"] = nc.dram_tensor("d_qfT", [128, 8, 512], BF16, kind="ExternalOutput").ap()
        dbg["qaT"] = nc.dram_tensor("d_qaT", [128, 8, 512], BF16, kind="ExternalOutput").ap()
        dbg["qiT"] = nc.dram_tensor("d_qiT", [128, 4, 512], BF16, kind="ExternalOutput").ap()
        dbg["sc"] = nc.dram_tensor("d_sc", [128, 8192], F32, kind="ExternalOutput").ap()
        dbg["lo"] = nc.dram_tensor("d_lo", [4, 128, 1], F32, kind="ExternalOutput").ap()
        dbg["mT"] = nc.dram_tensor("d_mT", [128, 64, 512], U8, kind="ExternalOutput").ap()
        dbg["yTa"] = nc.dram_tensor("d_yTa", [128, 8, 512], BF16, kind="ExternalOutput").ap()
        dbg["yTf"] = nc.dram_tensor("d_yTf", [128, 8, 512], BF16, kind="ExternalOutput").ap()

    st = contextlib.ExitStack()
    with st:
        def T(name, shape, dt):
            return Buf(st.enter_context(nc.sbuf_tensor(name, list(shape), dt))[:], name)

        arena_t = st.enter_context(nc.sbuf_tensor("arena", [128, ARENA], U8))
        AR = Arena(arena_t, ARENA)
        ps = [Buf(st.enter_context(nc.psum_tensor("ps%d" % i, [128, 512], F32))[:], "ps%d" % i) for i in range(8)]

        S = Sched(nc)
        S.alloc_sems(st)

        def DMA(q, out, in_, R=(), W=()):
            return S.op(q, lambda e: e.dma_start(out=out, in_=in_), _keys(R), _keys(W), dma=True)

        def TT(eng, out, in0, in1, op, R=(), W=()):
            return S.op(eng, lambda e: e.tensor_tensor(out=out, in0=in0, in1=in1, op=op), _keys(R), _keys(W))

        def TS(eng, out, in0, s1, op0, s2=None, op1=None, R=(), W=(), accum=None):
            def f(e):
                kw = dict(out=out, in0=in0, scalar1=s1, scalar2=s2, op0=op0)
                if op1 is not None:
                    kw["op1"] = op1
                if accum is not None:
                    kw["accum_out"] = accum
                return e.tensor_scalar(**kw)
            return S.op(eng, f, _keys(R), _keys(W))

        def STT(out, in0, scalar, in1, op0, op1, R=(), W=()):
            return S.op("dve", lambda e: e.scalar_tensor_tensor(out=out, in0=in0, scalar=scalar, in1=in1, op0=op0, op1=op1),
                        _keys(R), _keys(W))

        def ACT(out, in_, func, R=(), W=(), bias=None, scale=None, accum=None):
            def f(e):
                kw = dict(out=out, in_=in_, func=func)
                if bias is not None:
                    kw["bias"] = bias
                if scale is not None:
                    kw["scale"] = scale
                if accum is not None:
                    kw["accum_out"] = accum
                return e.activation(**kw)
            return S.op("act", f, _keys(R), _keys(W))

        def CP(eng, out, in_, R=(), W=()):
            return S.op(eng, lambda e: e.tensor_copy(out=out, in_=in_), _keys(R), _keys(W))

        def MS(eng, ap, val, W=()):
            return S.op(eng, lambda e: e.memset(ap, val), (), _keys(W))

        def MM(out, pairs, R=(), W=()):
            def f(e):
                ins = None
                n = len(pairs)
                for i, (l, r) in enumerate(pairs):
                    ins = e.matmul(out, lhsT=l, rhs=r, start=(i == 0), stop=(i == n - 1))
                return ins
            return S.op("pe", f, _keys(R), _keys(W))

        ones_bf = T("ones_bf", [128, 512], BF16)
        onesf = T("onesf", [128, 512], F32)
        ident = T("ident", [128, 128], BF16)
        bmask = T("bmask", [128, 32], BF16)
        cbias = T("cbias", [128, 512], F32)
        cv = T("cv", [128, 64], F32)
        nbf = T("nbf", [128, 1], F32)
        halfpi = T("halfpi", [128, 1], F32)
        wiwb = T("wiwb", [128, 8, 4], BF16)
        fg_bc = T("fg_bc", [128, D], F32)
        rcol_q = T("rcol_q", [128, 4], F32)
        absw = T("absw", [128, 16], F32)
        sgnw = T("sgnw", [128, 16], F32)
        w4 = T("w4", [128, 16], F32)
        rcol_as = [T("rcol_a0", [128, 4], F32), T("rcol_a1", [128, 4], F32)]
        bis_lo = T("bis_lo", [128, 1], F32)
        bis_mid = T("bis_mid", [128, 1], F32)
        bis_cnt = T("bis_cnt", [128, 1], F32)
        bis_ind = T("bis_ind", [128, 1], F32)
        bis_nb = T("bis_nb", [128, 1], F32)
        bis_ca = T("bis_ca", [128, 1], F32)
        bis_tot = T("bis_tot", [128, 1], F32)
        fin_ss = T("fin_ss", [128, 1], F32)
        fin_r = T("fin_r", [128, 1], F32)

        GAIN = lambda c: cv.ap[:, c:c + 1]
        INVF = cv.ap[:, 8:9]
        SGNS = cv.ap[:, 9:10]
        BMRG = lambda c: cv.ap[:, 16 + c:17 + c]

        MS("dve", ones_bf.ap, 1.0, W=[ones_bf])
        MS("dve", onesf.ap, 1.0, W=[onesf])
        MS("dve", halfpi.ap, math.pi / 2, W=[halfpi])
        DMA("sp", cv.ap, cvec[:, :], W=[cv])
        DMA("sp", cbias.ap, cbiasf[:, :], W=[cbias])
        DMA("pool", ident.ap, identf[:, :], W=[ident])
        DMA("pool", bmask.ap, bmaskf[:, :], W=[bmask])
        DMA("pool", wiwb.ap, wiw[:, :, :], W=[wiwb])
        DMA("sp", fg_bc.ap, fgain[0:1, :].to_broadcast([128, D]), W=[fg_bc])
        TS("dve", nbf.ap, cv.ap[:, 10:11], -1.0, ALU.mult, R=[cv], W=[nbf])

        def load_x_dma(src, t0, xf):
            DMA("sp", xf.ap, src[:, :, t0:t0 + 512], W=[xf])

        def load_x_prep(xf, xb, xsq):
            ACT(xsq.ap, xf.ap, AF.Square, R=[xf], W=[xsq])
            for c in range(8):
                ACT(xb.ap[:, c, :], xf.ap[:, c, :], AF.Copy, R=[xf, cv], W=[xb], scale=GAIN(c))

        def load_x_group(src, t0, xf, xb, xsq):
            load_x_dma(src, t0, xf)
            load_x_prep(xf, xb, xsq)

        def rstd_gen(xsq, rbc, rcol, pA, pB):
            MM(pA.ap, [(ones_bf.ap[:, 0:128], xsq.ap[:, c, :]) for c in range(8)], R=[xsq, ones_bf], W=[pA])
            yield
            for tb in range(4):
                MM(pB.ap[:, tb:tb + 1], [(xsq.ap[:, c, tb * 128:(tb + 1) * 128], ones_bf.ap[:, 0:1]) for c in range(8)],
                   R=[xsq, ones_bf], W=[pB])
            yield
            TS("dve", rcol.ap, pB.ap[:, 0:4], 1.0 / D, ALU.mult, EPS, ALU.add, R=[pB], W=[rcol])
            TS("dve", rbc.ap, pA.ap, 1.0 / D, ALU.mult, EPS, ALU.add, R=[pA], W=[rbc])
            yield
            ACT(rcol.ap, rcol.ap, AF.Sqrt, R=[rcol], W=[rcol])
            ACT(rbc.ap, rbc.ap, AF.Sqrt, R=[rbc], W=[rbc])
            yield
            S.op("dve", lambda e: e.reciprocal(out=rcol.ap, in_=rcol.ap), _keys([rcol]), _keys([rcol]))
            yield
            S.op("dve", lambda e: e.reciprocal(out=rbc.ap, in_=rbc.ap), _keys([rbc]), _keys([rbc]))
            yield

        def rstd_from(xsq, xb_unused, rbc, rcol, pA, pB, scale_extra=1.0):
            for _ in rstd_gen(xsq, rbc, rcol, pA, pB):
                pass

        def rope_gen(possrc, t0, posi, ang, kk, sn, cs, rbc, scale_extra):
            DMA("sp", posi.ap, possrc[0:1, t0:t0 + 512].to_broadcast([128, 512]), W=[posi])
            CP("dve", ang.ap, posi.ap, R=[posi], W=[ang])
            yield
            TS("dve", ang.ap, ang.ap, INVF, ALU.mult, R=[ang, cv], W=[ang])
            yield
            TS("dve", kk.ap, ang.ap, 1.0 / (2.0 * math.pi), ALU.mult, MAGIC, ALU.add, R=[ang], W=[kk])
            yield
            TS("dve", kk.ap, kk.ap, -MAGIC, ALU.add, R=[kk], W=[kk])
            yield
            STT(ang.ap, kk.ap, -C1, ang.ap, ALU.mult, ALU.add, R=[kk, ang], W=[ang])
            yield
            STT(ang.ap, kk.ap, -C2, ang.ap, ALU.mult, ALU.add, R=[kk, ang], W=[ang])
            yield
            TS("dve", ang.ap, ang.ap, math.pi, ALU.min, -math.pi, ALU.max, R=[ang], W=[ang])
            yield
            STT(kk.ap, ang.ap, -1.0, ang.ap, ALU.mult, ALU.max, R=[ang], W=[kk])
            ACT(sn.ap, ang.ap, AF.Sin, R=[ang], W=[sn])
            ACT(cs.ap, kk.ap, AF.Sin, R=[kk, halfpi], W=[cs], bias=halfpi.ap[:, 0:1], scale=-1.0)
            yield
            STT(sn.ap, sn.ap, SGNS, rbc.ap, ALU.mult, ALU.mult, R=[sn, cv, rbc], W=[sn])
            yield
            if scale_extra != 1.0:
                TS("dve", sn.ap, sn.ap, scale_extra, ALU.mult, R=[sn], W=[sn])
                STT(cs.ap, cs.ap, scale_extra, rbc.ap, ALU.mult, ALU.mult, R=[cs, rbc], W=[cs])
            else:
                TT("dve", cs.ap, cs.ap, rbc.ap, ALU.mult, R=[cs, rbc], W=[cs])
            yield

        def rope_tables(possrc, t0, posi, ang, kk, sn, cs, rbc, scale_extra):
            for _ in rope_gen(possrc, t0, posi, ang, kk, sn, cs, rbc, scale_extra):
                pass

        AR.reset(0)
        wkb = AR.alloc([8, NKC], BF16, "wkb")
        xfs = [AR.alloc([8, 512], F32, "xf") for _ in range(2)]
        xbs = [AR.alloc([8, 512], BF16, "xb") for _ in range(2)]
        xsq1 = AR.alloc([8, 512], BF16, "xsq")
        posi = AR.alloc([512], I32, "posi")
        ang = AR.alloc([512], F32, "ang")
        kk = AR.alloc([512], F32, "kk")
        sns = [AR.alloc([512], F32, "sn") for _ in range(2)]
        css = [AR.alloc([512], F32, "cs") for _ in range(2)]
        rbcs = [AR.alloc([512], F32, "rbc") for _ in range(2)]
        t1s = [AR.alloc([512], F32, "t1") for _ in range(2)]
        t2s = [AR.alloc([512], F32, "t2") for _ in range(2)]
        ksts = [AR.alloc([512], BF16, "kst") for _ in range(2)]
        mf = AR.alloc([512], F32, "mf")
        kist = AR.alloc([512], BF16, "kist")
        ee = AR.alloc([512], F32, "ee")
        ncums = [AR.alloc([512], F32, "ncum") for _ in range(2)]
        r1s = AR.alloc([512], F32, "r1s")
        aug = AR.alloc([3, 512], BF16, "aug")
        vsts = [AR.alloc([8, 4, 128], BF16, "vst") for _ in range(2)]

        for i in range(4):
            DMA("pool", wkb.ap[:, :, i * 704:(i + 1) * 704], wk[:, :, i * 704:(i + 1) * 704], W=[wkb])
        for v in vsts:
            MS("pool", v.ap, 1.0, W=[v])
        n_tg = S_LEN // 512
        if stop_after == "A1":
            n_tg = 1
        if stop_after == "A0":
            n_tg = 0
        kst_i = 0
        def chain_gen(tgn):
            k = tgn % 2
            for _ in rstd_gen(xsq1, rbcs[k], rcol_as[k], ps[0], ps[1]):
                yield
            for _ in rope_gen(pos_all, tgn * 512, posi, ang, kk, sns[k], css[k], rbcs[k], 1.0):
                yield

        if n_tg > 0:
            load_x_dma(xT, 0, xfs[0])
            load_x_prep(xfs[0], xbs[0], xsq1)
            for _ in chain_gen(0):
                pass
        for tg in range(n_tg):
            t0 = tg * 512
            xb = xbs[tg % 2]
            rbc = rbcs[tg % 2]
            rcol_a = rcol_as[tg % 2]
            sn = sns[tg % 2]
            cs = css[tg % 2]
            if tg + 1 < n_tg:
                load_x_dma(xT, t0 + 512, xfs[(tg + 1) % 2])
            if tg == 1 or (n_tg == 1 and tg == 0):
                for i in range(NQC // 512):
                    DMA("pool", wq_s[:, :, i * 512:(i + 1) * 512], wq[:, :, i * 512:(i + 1) * 512], W=["wq_s"])
                for i in range(2):
                    DMA("pool", wbd_s[:, :, i * 512:(i + 1) * 512], wbd[:, :, i * 512:(i + 1) * 512], W=["wbd_s"])
                    DMA("pool", wbf_s[:, :, i * 512:(i + 1) * 512], wbf[:, :, i * 512:(i + 1) * 512], W=["wbf_s"])
                    DMA("pool", wo_s[:, :, i * 512:(i + 1) * 512], wo[:, :, i * 512:(i + 1) * 512], W=["wo_s"])


            def proj_chunk(pbuf, n0):
                MM(pbuf.ap, [(wkb.ap[:, c, n0:n0 + 128], xb.ap[:, c, :]) for c in range(8)], R=[wkb, xb], W=[pbuf])

            for cc in range(4):
                pb = ps[2 + (cc % 2)]
                proj_chunk(pb, cc * 128)
                kst = ksts[kst_i % 2]
                kst_i += 1
                TT("dve", kst.ap, pb.ap, rbc.ap, ALU.mult, R=[pb, rbc], W=[kst])
                DMA("sp", kf_s[2 * cc, 0:64, t0:t0 + 512], kst.ap[0:64, :], R=[kst], W=["kf_s"])
                DMA("sp", kf_s[2 * cc + 1, 0:64, t0:t0 + 512], kst.ap[64:128, :], R=[kst], W=["kf_s"])
            DMA("sp", kf_s[:, 64, t0:t0 + 512], ones_bf.ap[0:8, :], R=[ones_bf], W=["kf_s"])
            for br in range(2):
                vst = vsts[br]
                dst = vf_s if br == 0 else va_s
                n0 = 1792 + br * 512
                for tb in range(4):
                    pv = ps[6 + (tb % 2)]
                    MM(pv.ap, [(xb.ap[:, c, tb * 128:(tb + 1) * 128], wkb.ap[:, c, n0:n0 + 512]) for c in range(8)],
                       R=[xb, wkb], W=[pv])
                    ACT(vst.ap[:, :, tb, 0:64], pv.ap.rearrange("p (h c) -> p h c", c=64), AF.Copy,
                        R=[pv, rcol_a], W=[vst], scale=rcol_a.ap[:, tb:tb + 1])
                for h in range(8):
                    DMA("sp", dst[h, :, tg * 4:(tg + 1) * 4, :], vst.ap[:, h, :, :], R=[vst], W=["v_s%d" % br])

            ahead = None
            if tg + 1 < n_tg:
                load_x_prep(xfs[(tg + 1) % 2], xbs[(tg + 1) % 2], xsq1)
                ahead = chain_gen(tg + 1)

            def adv(n):
                if ahead is not None:
                    for _ in range(n):
                        try:
                            next(ahead)
                        except StopIteration:
                            break

            for cc in range(5):
                pa, pb = (ps[4], ps[5]) if cc % 2 == 0 else (ps[2], ps[3])
                if cc < 4:
                    proj_chunk(pa, 512 + cc * 128)
                    proj_chunk(pb, 1024 + cc * 128)
                else:
                    proj_chunk(pa, 1536)
                    proj_chunk(pb, 1664)
                adv(2)
                t1 = t1s[cc % 2]
                t2 = t2s[cc % 2]
                TT("dve", t1.ap, pa.ap, cs.ap, ALU.mult, R=[pa, cs], W=[t1])
                adv(1)
                TT("dve", t2.ap, pb.ap, sn.ap, ALU.mult, R=[pb, sn], W=[t2])
                adv(1)
                if cc < 4:
                    kst = ksts[kst_i % 2]
                    kst_i += 1
                    TT("pool", kst.ap, t1.ap, t2.ap, ALU.add, R=[t1, t2], W=[kst])
                    DMA("sp", ka_s[2 * cc, :, t0:t0 + 512], kst.ap[0:64, :], R=[kst], W=["ka_s"])
                    DMA("sp", ka_s[2 * cc + 1, :, t0:t0 + 512], kst.ap[64:128, :], R=[kst], W=["ka_s"])
                else:
                    TT("pool", mf.ap, t1.ap, t2.ap, ALU.add, R=[t1, t2], W=[mf])
                    CP("pool", kist.ap[0:64, :], mf.ap[0:64, :], R=[mf], W=[kist])
                    DMA("sp", ki_s[:, t0:t0 + 512], kist.ap[0:64, :], R=[kist], W=["ki_s"])
                    ACT(ee.ap[96:104, :], mf.ap[96:104, :], AF.Exp, R=[mf, nbf], W=[ee], bias=nbf.ap[96:104, 0:1], scale=-1.0)
                    ACT(ee.ap[96:104, :], ee.ap[96:104, :], AF.Ln, R=[ee], W=[ee], bias=onesf.ap[96:104, 0:1], scale=1.0)
                    nc_cur = ncums[tg % 2]
                    nc_prev = ncums[(tg + 1) % 2]
                    init = 0.0 if tg == 0 else nc_prev.ap[96:104, 511:512]
                    S.op("dve", (lambda o_, d1_, i_: (lambda e: e.tensor_tensor_scan(
                        out=o_, data0=onesf.ap[96:104, :], data1=d1_, initial=i_, op0=ALU.mult, op1=ALU.add)))(
                        nc_cur.ap[96:104, :], ee.ap[96:104, :], init),
                        _keys([ee, onesf, nc_prev]), _keys([nc_cur]))
                    CP("dve", aug.ap[96:104, 0, :], nc_cur.ap[96:104, :], R=[nc_cur], W=[aug])
                    TT("dve", r1s.ap[96:104, :], nc_cur.ap[96:104, :], aug.ap[96:104, 0, :], ALU.subtract, R=[nc_cur, aug], W=[r1s])
                    CP("dve", aug.ap[96:104, 1, :], r1s.ap[96:104, :], R=[r1s], W=[aug])
                    TT("dve", r1s.ap[96:104, :], r1s.ap[96:104, :], aug.ap[96:104, 1, :], ALU.subtract, R=[r1s, aug], W=[r1s])
                    CP("dve", aug.ap[96:104, 2, :], r1s.ap[96:104, :], R=[r1s], W=[aug])
                    DMA("sp", kf_s[:, 65:68, t0:t0 + 512], aug.ap[96:104, :, :], R=[aug], W=["kf_s"])
            adv(1000)
        n_groups = NG
        if stop_after in ("A", "A1", "A0"):
            n_groups = 0
        elif stop_after is not None and stop_after.startswith("B"):
            n_groups = 1
        for g in range(n_groups):
            S.barrier()
            q0 = g * GQ
            win0 = 2048 * g
            L = 2048 * (g + 1)
            nkb = L // 128
            AR.reset(0)
            qfT = AR.alloc([8, 512], BF16, "qfT")
            qaT = AR.alloc([8, 512], BF16, "qaT")
            qiT = AR.alloc([4, 512], BF16, "qiT")
            xqb = AR.alloc([8, 512], BF16, "xqb")
            rbq = AR.alloc([512], F32, "rbq")
            mT = AR.alloc([64, 512], U8, "mT")
            yTa = AR.alloc([8, 512], BF16, "yTa")
            yTf = AR.alloc([8, 512], BF16, "yTf")
            assert AR.off <= GPERS, AR.off

            AR.reset(GPERS)
            xqf = AR.alloc([8, 512], F32, "xqf")
            xsqq = AR.alloc([8, 512], BF16, "xsqq")
            wqps = [AR.alloc([8, 512], BF16, "wqp") for _ in range(2)]
            posi = AR.alloc([512], I32, "posi")
            ang = AR.alloc([512], F32, "ang")
            kk = AR.alloc([512], F32, "kk")
            sn = AR.alloc([512], F32, "sn")
            cs = AR.alloc([512], F32, "cs")
            rb8 = AR.alloc([512], F32, "rb8")
            t1s = [AR.alloc([512], F32, "t1") for _ in range(2)]
            t2s = [AR.alloc([512], F32, "t2") for _ in range(2)]
            craws = [AR.alloc([2048], BF16, "craw") for _ in range(2)]

            load_x_group(xTo, q0, xqf, xqb, xsqq)
            rstd_from(xsqq, xqb, rbq, rcol_q, ps[0], ps[1])
            TS("dve", rb8.ap, rbq.ap, 0.125, ALU.mult, R=[rbq], W=[rb8])
            qchain = rope_gen(pos_own, q0, posi, ang, kk, sn, cs, rbq, 0.125)

            def qadv(n):
                for _ in range(n):
                    try:
                        next(qchain)
                    except StopIteration:
                        break

            wq_i = [0]

            def load_wq(piece):
                wb = wqps[wq_i[0] % 2]
                wq_i[0] += 1
                DMA("sp", wb.ap, wq_s[:, :, piece * 512:(piece + 1) * 512], R=["wq_s"], W=[wb])
                return wb

            def qproj(pbuf, wb, n0):
                MM(pbuf.ap, [(wb.ap[:, c, n0:n0 + 128], xqb.ap[:, c, :]) for c in range(8)], R=[wb, xqb], W=[pbuf])

            MS("pool", qaT.ap[64:128, :, :], 0.0, W=[qaT])
            MS("pool", qfT.ap[64:128, :, :], 0.0, W=[qfT])
            MS("pool", qfT.ap[64:68, :, :], 1.0, W=[qfT])
            MS("pool", qiT.ap[64:128, :, :], 0.0, W=[qiT])
            wb = load_wq(0)
            for cc in range(4):
                pb = ps[2 + cc]
                qproj(pb, wb, cc * 128)
            for cc in range(4):
                pb = ps[2 + cc]
                TT("dve", qfT.ap[0:64, 2 * cc, :], pb.ap[0:64, :], rb8.ap[0:64, :], ALU.mult, R=[pb, rb8], W=[qfT])
                qadv(2)
                TT("dve", qfT.ap[0:64, 2 * cc + 1, :], pb.ap[64:128, :], rb8.ap[64:128, :], ALU.mult, R=[pb, rb8], W=[qfT])
                qadv(2)
            qadv(1000)
            for h in range(8):
                cr = craws[h % 2]
                DMA("sp", cr.ap[64:65, :], kf_s[h, 65:66, win0:win0 + 2048], R=["kf_s"], W=[cr])
                TS("dve", qfT.ap[64:65, h, :], cr.ap[64:65, :].rearrange("p (i r) -> p i r", r=4)[:, :, 0], -1.0, ALU.mult,
                   R=[cr], W=[qfT])
            wb1 = load_wq(1)
            wb2 = load_wq(2)
            for cc in range(4):
                pa, pb = ps[4 + 2 * (cc % 2)], ps[5 + 2 * (cc % 2)]
                qproj(pa, wb1, cc * 128)
                qproj(pb, wb2, cc * 128)
                t1 = t1s[cc % 2]
                t2 = t2s[cc % 2]
                TT("dve", t1.ap, pa.ap, cs.ap, ALU.mult, R=[pa, cs], W=[t1])
                TT("dve", t2.ap, pb.ap, sn.ap, ALU.mult, R=[pb, sn], W=[t2])
                TT("pool", qaT.ap[0:64, 2 * cc, :], t1.ap[0:64, :], t2.ap[0:64, :], ALU.add, R=[t1, t2], W=[qaT])
                TT("pool", qaT.ap[0:64, 2 * cc + 1, :], t1.ap[64:128, :], t2.ap[64:128, :], ALU.add, R=[t1, t2], W=[qaT])
            wb = load_wq(3)
            for cc in range(2):
                pa, pb = ps[4 + 2 * (cc % 2)], ps[5 + 2 * (cc % 2)]
                qproj(pa, wb, cc * 128)
                qproj(pb, wb, 256 + cc * 128)
                t1 = t1s[cc % 2]
                t2 = t2s[cc % 2]
                TT("dve", t1.ap, pa.ap, cs.ap, ALU.mult, R=[pa, cs], W=[t1])
                TT("dve", t2.ap, pb.ap, sn.ap, ALU.mult, R=[pb, sn], W=[t2])
                TT("pool", qiT.ap[0:64, 2 * cc, :], t1.ap[0:64, :], t2.ap[0:64, :], ALU.add, R=[t1, t2], W=[qiT])
                TT("pool", qiT.ap[0:64, 2 * cc + 1, :], t1.ap[64:128, :], t2.ap[64:128, :], ALU.add, R=[t1, t2], W=[qiT])
            for sb in range(4):
                MM(ps[1].ap[:, 8 + 4 * sb:12 + 4 * sb],
                   [(xqb.ap[:, c, sb * 128:(sb + 1) * 128], wiwb.ap[:, c, :]) for c in range(8)], R=[xqb, wiwb], W=[ps[1]])
                TS("dve", w4.ap[:, 4 * sb:4 * sb + 4], ps[1].ap[:, 8 + 4 * sb:12 + 4 * sb], rcol_q.ap[:, sb:sb + 1], ALU.mult,
                   0.5, ALU.mult, R=[ps[1], rcol_q], W=[w4])
            STT(absw.ap, w4.ap, -1.0, w4.ap, ALU.mult, ALU.max, R=[w4], W=[absw])
            TS("dve", sgnw.ap, w4.ap, 0.0, ALU.is_ge, 2.0, ALU.mult, R=[w4], W=[sgnw])
            TS("dve", sgnw.ap, sgnw.ap, -1.0, ALU.add, R=[sgnw], W=[sgnw])
            if debug and g == 0:
                DMA("sp", dbg["qfT"][:, :, :], qfT.ap, R=[qfT], W=["dbg1"])
                DMA("sp", dbg["qaT"][:, :, :], qaT.ap, R=[qaT], W=["dbg2"])
                DMA("sp", dbg["qiT"][:, :, :], qiT.ap, R=[qiT], W=["dbg3"])
            if stop_after == "B1":
                break

            S.barrier()
            AR.reset(GPERS)
            sc = AR.alloc([8192], F32, "sc")
            msk = AR.alloc([8192], BF16, "msk")
            rts = [AR.alloc([512], F32, "rt") for _ in range(2)]
            kits = [AR.alloc([2048], BF16, "kit") for _ in range(1)]
            f_kTps = [AR.alloc([2048], BF16, "kTp") for _ in range(2)]
            f_vps = [AR.alloc([16, 128], BF16, "vp") for _ in range(2)]
            f_Pts = [AR.alloc([512], BF16, "Pt") for _ in range(4)]
            f_recs = [AR.alloc([512], F32, "rec") for _ in range(1)]

            for kt_ in f_kTps + kits:
                MS("pool", kt_.ap[64:128, :], 0.0, W=[kt_])

            def attn_steps(br, kTps, vps, Pts, Pms, recs, psS, psO):
                tiles = [(h, pc, kb) for h in range(8) for pc in range(g + 1) for kb in range(16)]
                pend = []
                nS = len(psS)
                cur = {}
                piece_n = 0

                def issue_pv(item):
                    (h, pc, kb, pmat, vp, first, last, pc0) = item
                    po = psO[h % len(psO)]
                    S.op("pe", (lambda o_, l_, r_, f_, s_: (lambda e: e.matmul(o_, lhsT=l_, rhs=r_, start=f_, stop=s_)))(
                        po.ap[:, pc0:512], vp.ap[:, kb, :], pmat.ap[:, pc0:512], first, last), _keys([vp, pmat]), _keys([po]))
                    if last:
                        yT = yTa if br == 0 else yTf
                        rec = recs[h % len(recs)]
                        S.op("dve", (lambda o_, i_: (lambda e: e.reciprocal(out=o_, in_=i_)))(rec.ap[0:64, :], po.ap[64:128, :]),
                             _keys([po]), _keys([rec]))
                        TT("dve", yT.ap[0:64, h, :], po.ap[0:64, :], rec.ap[0:64, :], ALU.mult, R=[po, rec], W=[yT])

                for ti, (h, pc, kb) in enumerate(tiles):
                    if kb == 0:
                        kTp = kTps[piece_n % len(kTps)]
                        vp = vps[piece_n % len(vps)]
                        piece_n += 1
                        if br == 0:
                            DMA("sp", kTp.ap[0:64, :], ka_s[h, :, pc * 2048:(pc + 1) * 2048], R=["ka_s"], W=[kTp])
                            DMA("sp", vp.ap, va_s[h, :, pc * 16:(pc + 1) * 16, :], R=["v_s1"], W=[vp])
                        else:
                            DMA("sp", kTp.ap[0:68, :], kf_s[h, :, pc * 2048:(pc + 1) * 2048], R=["kf_s"], W=[kTp])
                            DMA("sp", vp.ap, vf_s[h, :, pc * 16:(pc + 1) * 16, :], R=["v_s0"], W=[vp])
                        cur["k"], cur["v"] = kTp, vp
                    kTp, vp = cur["k"], cur["v"]
                    pS = psS[ti % nS]
                    Pt = Pts[ti % len(Pts)]
                    diag = (br == 1 and pc == g)
                    c0 = 32 * kb if diag else 0
                    if br == 0:
                        MM(pS.ap[:, c0:512], [(kTp.ap[:, kb * 128:(kb + 1) * 128], qaT.ap[:, h, c0:512])], R=[kTp, qaT], W=[pS])
                    else:
                        MM(pS.ap[:, c0:512], [(kTp.ap[:, kb * 128:(kb + 1) * 128], qfT.ap[:, h, c0:512])], R=[kTp, qfT], W=[pS])
                    ACT(Pt.ap[:, c0:512], pS.ap[:, c0:512], AF.Exp, R=[pS], W=[Pt])
                    if br == 0:
                        Pm = Pms[ti % len(Pms)]
                        TT("pool" if ti % 2 == 1 else "dve", Pm.ap, Pt.ap, mT.ap[:, pc * 16 + kb, :], ALU.mult, R=[Pt, mT], W=[Pm])
                        pmat = Pm
                    else:
                        if diag:
                            TT("pool", Pt.ap[:, c0:c0 + 32], Pt.ap[:, c0:c0 + 32], bmask.ap, ALU.mult, R=[Pt, bmask], W=[Pt])
                        pmat = Pt
                    pend.append((h, pc, kb, pmat, vp, (pc == 0 and kb == 0), (pc == g and kb == 15), c0))
                    if len(pend) > min(4, nS - 1):
                        issue_pv(pend.pop(0))
                    yield 1
                while pend:
                    issue_pv(pend.pop(0))

            def b2_units():
                for sb in range(4):
                    nch = 4 * g + sb + 1
                    Lsb = 512 * nch
                    kit = kits[0]
                    for ch in range(nch):
                        if ch % 4 == 0:
                            pc = ch // 4
                            DMA("sp", kit.ap[0:64, :], ki_s[:, pc * 2048:(pc + 1) * 2048], R=["ki_s"], W=[kit])
                        for h in range(4):
                            pb = ps[h]
                            MM(pb.ap, [(qiT.ap[:, h, sb * 128:(sb + 1) * 128], kit.ap[:, (ch % 4) * 512:(ch % 4 + 1) * 512])],
                               R=[qiT, kit], W=[pb])
                        for h in range(4):
                            pb = ps[h]
                            rt = rts[h % 2]
                            ACT(rt.ap, pb.ap, AF.Relu, R=[pb, absw], W=[rt], scale=absw.ap[:, 4 * sb + h:4 * sb + h + 1])
                            scs = sc.ap[:, ch * 512:(ch + 1) * 512]
                            sg = sgnw.ap[:, 4 * sb + h:4 * sb + h + 1]
                            if h == 0:
                                TS("dve", scs, rt.ap, sg, ALU.mult, R=[rt, sgnw], W=[sc])
                            else:
                                STT(scs, rt.ap, sg, scs, ALU.mult, ALU.add, R=[rt, sgnw, sc], W=[sc])
                        if ch == nch - 1:
                            TT("dve", sc.ap[:, ch * 512:(ch + 1) * 512], sc.ap[:, ch * 512:(ch + 1) * 512], cbias.ap, ALU.add,
                               R=[sc, cbias], W=[sc])
                        yield 3.2
                    if debug and g == 0 and sb == 3:
                        DMA("sp", dbg["sc"][:, 0:Lsb], sc.ap[:, 0:Lsb], R=[sc], W=["dbg4"])
                    MS("dve", bis_lo.ap, -RNG, W=[bis_lo])
                    La = 512 * int(0.4 * nch) if nch >= 3 else 0
                    Ld = Lsb - La
                    for it in range(NIT):
                        step = RNG / (2.0 ** it)
                        TS("dve", bis_mid.ap, bis_lo.ap, step, ALU.add, R=[bis_lo], W=[bis_mid])
                        if La > 0:
                            TS("dve", bis_nb.ap, bis_mid.ap, -1.0, ALU.mult, 2.0 ** -20, ALU.add, R=[bis_mid], W=[bis_nb])
                        TS("dve", msk.ap[:, 0:Ld], sc.ap[:, 0:Ld], bis_mid.ap[:, 0:1], ALU.is_ge, None, ALU.add,
                           R=[sc, bis_mid], W=[msk, bis_cnt], accum=bis_cnt.ap[:, 0:1])
                        if La > 0:
                            ACT(msk.ap[:, Ld:Lsb], sc.ap[:, Ld:Lsb], AF.Sign, R=[sc, bis_nb], W=["mskA", bis_ca],
                                bias=bis_nb.ap[:, 0:1], scale=1.0, accum=bis_ca.ap[:, 0:1])
                            STT(bis_tot.ap, bis_ca.ap, 0.5, bis_cnt.ap, ALU.mult, ALU.add, R=[bis_ca, bis_cnt], W=[bis_tot])
                            TS("dve", bis_ind.ap, bis_tot.ap, TOPK - La / 2.0, ALU.is_ge, step, ALU.mult, R=[bis_tot], W=[bis_ind])
                        else:
                            TS("dve", bis_ind.ap, bis_cnt.ap, TOPK, ALU.is_ge, step, ALU.mult, R=[bis_cnt], W=[bis_ind])
                        TT("dve", bis_lo.ap, bis_lo.ap, bis_ind.ap, ALU.add, R=[bis_lo, bis_ind], W=[bis_lo])
                        yield 0.7 + Ld * 1.05e-3
                    TS("dve", bis_ind.ap, bis_lo.ap, -RNG, ALU.is_le, -1000.0, ALU.mult, R=[bis_lo], W=[bis_ind])
                    TT("dve", bis_lo.ap, bis_lo.ap, bis_ind.ap, ALU.add, R=[bis_lo, bis_ind], W=[bis_lo])
                    TS("dve", msk.ap[:, 0:Lsb], sc.ap[:, 0:Lsb], bis_lo.ap[:, 0:1], ALU.is_ge, R=[sc, bis_lo], W=[msk, "mskA"])
                    if debug and g == 0:
                        DMA("sp", dbg["lo"][sb], bis_lo.ap, R=[bis_lo], W=["dbg5"])
                    yield Lsb * 0.6e-3
                    for k4 in range(Lsb // 512):
                        pt = ps[k4 % 2]
                        ptb = pt.ap.bitcast(BF16)
                        for i in range(4):
                            kb = k4 * 4 + i
                            S.op("pe", (lambda o_, i_: (lambda e: e.transpose(out=o_, in_=i_, identity=ident.ap)))(
                                ptb[:, i * 128:(i + 1) * 128], msk.ap[:, kb * 128:(kb + 1) * 128]),
                                _keys([msk, ident]), _keys([pt]))
                        ACT(mT.ap[:, k4 * 4:(k4 + 1) * 4, sb * 128:(sb + 1) * 128],
                            ptb[:, 0:512].rearrange("p (a b) -> p a b", b=128), AF.Copy, R=[pt], W=[mT])
                        yield 0.7
                    if Lsb // 128 < nkb:
                        MS("pool", mT.ap[:, Lsb // 128:nkb, sb * 128:(sb + 1) * 128], 0, W=[mT])

            fox = attn_steps(1, f_kTps, f_vps, f_Pts, None, f_recs, [ps[4], ps[5]], [ps[6], ps[7]])
            n_fox = 128 * (g + 1)
            units = list()
            tot_est = 0.0
            for sb in range(4):
                nch = 4 * g + sb + 1
                Lsb = 512 * nch
                La_ = 512 * int(0.4 * nch) if nch >= 3 else 0
                tot_est += nch * 3.2 + NIT * (0.7 + (Lsb - La_) * 1.05e-3) + Lsb * 0.6e-3 + (Lsb // 512) * 0.7
            rate = n_fox / (0.9 * tot_est)
            acc = 0.0
            fox_done = False
            for wgt in b2_units():
                acc += wgt * rate
                while acc >= 1.0 and not fox_done:
                    acc -= 1.0
                    try:
                        next(fox)
                    except StopIteration:
                        fox_done = True
            if not fox_done:
                for _ in fox:
                    pass
            if debug and g == 0:
                DMA("sp", dbg["mT"][:, 0:nkb, :], mT.ap[:, 0:nkb, :], R=[mT], W=["dbg6"])
            if stop_after == "B2":
                break

            S.barrier()
            AR.reset(GPERS)
            kTps = [AR.alloc([2048], BF16, "kTp") for _ in range(3)]
            vps = [AR.alloc([16, 128], BF16, "vp") for _ in range(3)]
            Pts = [AR.alloc([512], BF16, "Pt") for _ in range(6)]
            Pms = [AR.alloc([512], BF16, "Pm") for _ in range(6)]
            recs = [AR.alloc([512], F32, "rec") for _ in range(2)]
            for kt_ in kTps:
                MS("pool", kt_.ap[64:128, :], 0.0, W=[kt_])
            for _ in attn_steps(0, kTps, vps, Pts, Pms, recs, [ps[0], ps[1], ps[2], ps[3], ps[6], ps[7]], [ps[4], ps[5]]):
                pass
            if debug and g == 0:
                DMA("sp", dbg["yTa"][:, :, :], yTa.ap, R=[yTa], W=["dbg7"])
                DMA("sp", dbg["yTf"][:, :, :], yTf.ap, R=[yTf], W=["dbg8"])
            if stop_after == "B3":
                break

            S.barrier()
            AR.reset(GPERS)
            wqps = [AR.alloc([8, 512], BF16, "wqp") for _ in range(2)]
            wbrs = [AR.alloc([8, 128], BF16, "wbr") for _ in range(2)]
            wobs = [AR.alloc([8, 512], BF16, "wob") for _ in range(2)]
            ygs = [AR.alloc([8, 512], BF16, "yg") for _ in range(2)]
            mrg = AR.alloc([8, 512], BF16, "mrg")
            gtmp = AR.alloc([512], F32, "gtmp")
            gbf = AR.alloc([512], BF16, "gbf")
            gms4 = [AR.alloc([512], F32, "gm") for _ in range(2)]
            e1s = [AR.alloc([512], F32, "e1") for _ in range(1)]
            e2s = [AR.alloc([512], F32, "e2") for _ in range(1)]
            xot = AR.alloc([D], F32, "xot")
            xnew = AR.alloc([D], F32, "xnew")
            wq_i = [0]
            for br in range(2):
                wb = load_wq(4 + br)
                yT = yTa if br == 0 else yTf
                for cc in range(4):
                    pb = ps[cc % 2]
                    qproj(pb, wb, cc * 128)
                    TT("dve", gtmp.ap, pb.ap, rbq.ap, ALU.mult, R=[pb, rbq], W=[gtmp])
                    for hh in range(2):
                        h = 2 * cc + hh
                        ACT(gbf.ap[0:64, :], gtmp.ap[64 * hh:64 * hh + 64, :], AF.Silu, R=[gtmp], W=[gbf])
                        TT("pool", ygs[br].ap[0:64, h, :], yT.ap[0:64, h, :], gbf.ap[0:64, :], ALU.mult, R=[yT, gbf], W=[ygs[br]])
            wbr_n = [0]
            wqm = {}
            for hf in range(2):
                DMA("sp", wobs[hf].ap, wo_s[:, :, hf * 512:(hf + 1) * 512], R=["wo_s"], W=[wobs[hf]])
            for dc in range(8):
                pus = []
                par = dc % 2
                gms = [gms4[0], gms4[1]]
                e1, e2 = e1s[0], e2s[0]
                for br in range(2):
                    wsrc = wbd_s if br == 0 else wbf_s
                    wbr = wbrs[wbr_n[0] % 2]
                    wbr_n[0] += 1
                    DMA("sp", wbr.ap[0:64, :, :], wsrc[:, :, dc * 128:(dc + 1) * 128], R=["wbd_s", "wbf_s"], W=[wbr])
                    pu = ps[4 * par + br]
                    MM(pu.ap, [(wbr.ap[0:64, h, :], ygs[br].ap[0:64, h, :]) for h in range(8)], R=[wbr, ygs[br]], W=[pu])
                    pus.append(pu)
                    mcol = br * 1024 + dc * 128
                    piece = 6 + mcol // 512
                    if wqm.get(br, (None, None))[0] != piece:
                        wqm[br] = (piece, load_wq(piece))
                    wbm = wqm[br][1]
                    pm = ps[4 * par + 2 + br]
                    qproj(pm, wbm, mcol % 512)
                    TT("dve", gms[br].ap, pm.ap, rbq.ap, ALU.mult, R=[pm, rbq], W=[gms[br]])
                    ACT(gms[br].ap, gms[br].ap, AF.Sigmoid, R=[gms[br], cv], W=[gms[br]], bias=BMRG(br * 8 + dc))
                TT("dve", e1.ap, pus[0].ap, gms[0].ap, ALU.mult, R=[pus[0], gms[0]], W=[e1])
                TT("dve", e2.ap, pus[1].ap, gms[1].ap, ALU.mult, R=[pus[1], gms[1]], W=[e2])
                TT("pool", mrg.ap[:, dc, :], e1.ap, e2.ap, ALU.add, R=[e1, e2], W=[mrg])
            for sb in range(4):
                r0 = q0 + sb * 128
                DMA("sp", xot.ap, xo[r0:r0 + 128, :], W=[xot])
                for hf in range(2):
                    wob = wobs[hf]
                    po = ps[(2 * sb + hf) % 8]
                    MM(po.ap, [(mrg.ap[:, dc, sb * 128:(sb + 1) * 128], wob.ap[:, dc, :]) for dc in range(8)], R=[mrg, wob], W=[po])
                    TT("dve", xnew.ap[:, hf * 512:(hf + 1) * 512], po.ap, xot.ap[:, hf * 512:(hf + 1) * 512], ALU.add,
                       R=[po, xot], W=[xnew])
                ACT(xot.ap, xnew.ap, AF.Square, R=[xnew, xot], W=[xot, fin_ss], accum=fin_ss.ap[:, 0:1])
                TS("dve", fin_r.ap, fin_ss.ap, 1.0 / D, ALU.mult, EPS, ALU.add, R=[fin_ss], W=[fin_r])
                ACT(fin_r.ap, fin_r.ap, AF.Sqrt, R=[fin_r], W=[fin_r])
                S.op("dve", lambda e: e.reciprocal(out=fin_r.ap, in_=fin_r.ap), _keys([fin_r]), _keys([fin_r]))
                STT(xnew.ap, xnew.ap, fin_r.ap[:, 0:1], fg_bc.ap, ALU.mult, ALU.mult, R=[xnew, fin_r, fg_bc], W=[xnew])
                DMA("sp", y[r0:r0 + 128, :], xnew.ap, R=[xnew], W=["y"])

        fw = [o for o in S.dlast if o is not None]
        with nc.Block() as block:
            S.emit(block, final_waits=fw)
    return nc


def _rot_perm():
    p = np.arange(64)
    p[0:8] = np.arange(8, 16)
    p[8:16] = np.arange(0, 8)
    return p


def _fm(w):
    n = w.shape[1]
    return np.ascontiguousarray(w.reshape(8, 128, n).transpose(1, 0, 2))


def prep_inputs(x, positions, norm_gain, w_in, b_forget, b_merge, w_branch_dsa, w_branch_fox, w_out, final_gain):
    x = np.asarray(x, np.float32)
    positions = np.asarray(positions, np.int32)
    W = np.asarray(w_in, np.float32)[0]
    o = 0
    cols = {}
    for name, n in (("aq", 512), ("ak", 512), ("av", 512), ("ag", 512), ("iq", 256), ("ik", 64), ("iw", 4),
                    ("fq", 512), ("fk", 512), ("fv", 512), ("fg", 512), ("fl", 8), ("mg", 2048)):
        cols[name] = W[:, o:o + n]
        o += n
    perm = _rot_perm()

    def rot(w, nh):
        idx = np.concatenate([h * 64 + perm for h in range(nh)])
        return w[:, idx]

    z32 = np.zeros((D, 32), np.float32)
    z24 = np.zeros((D, 24), np.float32)
    z64 = np.zeros((D, 64), np.float32)
    wk = np.concatenate([cols["fk"], cols["ak"], rot(cols["ak"], 8),
                         cols["ik"], z32, cols["fl"], z24, rot(cols["ik"], 1), z64,
                         cols["fv"], cols["av"]], axis=1)
    assert wk.shape[1] == NKC
    wq = np.concatenate([cols["fq"], cols["aq"], rot(cols["aq"], 8), cols["iq"], rot(cols["iq"], 4),
                         cols["ag"], cols["fg"], cols["mg"]], axis=1)
    assert wq.shape[1] == NQC
    wk_d, wq_d, wiw_d = _fm(wk), _fm(wq), _fm(np.ascontiguousarray(cols["iw"]))
    wbd = np.ascontiguousarray(np.asarray(w_branch_dsa, np.float32)[0].reshape(8, 64, D).transpose(1, 0, 2))
    wbf = np.ascontiguousarray(np.asarray(w_branch_fox, np.float32)[0].reshape(8, 64, D).transpose(1, 0, 2))
    wo = _fm(np.asarray(w_out, np.float32)[0])
    cvec = np.zeros((128, 64), np.float32)
    cvec[:, 0:8] = np.asarray(norm_gain, np.float32)[0].reshape(8, 128).T
    half = 8
    inv_freq = (500000.0 ** (-np.arange(half, dtype=np.float32) * 2.0 / 16.0)).astype(np.float32)
    for p in range(128):
        r = p % 64
        if r < 8:
            cvec[p, 8] = inv_freq[r]
            cvec[p, 9] = -1.0
        elif r < 16:
            cvec[p, 8] = inv_freq[r - 8]
            cvec[p, 9] = 1.0
    cvec[96:104, 10] = np.asarray(b_forget, np.float32)[0]
    cvec[:, 16:32] = np.asarray(b_merge, np.float32)[0].reshape(16, 128).T
    fgain = np.asarray(final_gain, np.float32).reshape(1, D)
    identf = np.eye(128, dtype=np.float32)
    in_maps = []
    xT_b = [np.ascontiguousarray(x[b].T.reshape(8, 128, S_LEN).transpose(1, 0, 2)) for b in range(x.shape[0])]
    for c in range(8):
        b, j = divmod(c, 4)
        p_idx = np.arange(128)[:, None]
        m_idx = np.arange(32)[None, :]
        bmaskf = (p_idx <= 4 * m_idx + j).astype(np.float32)
        s_idx = np.arange(512)[None, :]
        cbiasf = np.where(s_idx <= 4 * p_idx + j, 0.0, NEGB).astype(np.float32)
        in_maps.append({
            "xT": xT_b[b],
            "xTo": np.ascontiguousarray(xT_b[b][:, :, j::4]),
            "xo": np.ascontiguousarray(x[b, j::4, :]),
            "pos_all": np.ascontiguousarray(positions[b][None, :]),
            "pos_own": np.ascontiguousarray(positions[b][None, j::4]),
            "wk": wk_d, "wq": wq_d, "wiw": wiw_d, "wbd": wbd, "wbf": wbf, "wo": wo,
            "cvec": cvec, "fgain": fgain, "identf": identf, "bmaskf": bmaskf, "cbiasf": cbiasf,
        })
    return in_maps


_NC_CACHE = {}


def kernel(x, positions, norm_gain, w_in, b_forget, b_merge, w_branch_dsa, w_branch_fox, w_out, final_gain):
    in_maps = prep_inputs(x, positions, norm_gain, w_in, b_forget, b_merge, w_branch_dsa, w_branch_fox, w_out, final_gain)
    if "nc" not in _NC_CACHE:
        _NC_CACHE["nc"] = build()
    nc = _NC_CACHE["nc"]
    res = run_bass_kernel_spmd(nc, in_maps, core_ids=list(range(8)))
    out = np.empty((2, S_LEN, D), np.float32)
    for c in range(8):
        b, j = divmod(c, 4)
        out[b, j::4, :] = res.results[c]["y"]
    return out
```

```python
import math
import contextlib
import numpy as np
import concourse.bass as bass
import concourse.mybir as mybir
from concourse.bass_utils import run_bass_kernel_spmd

F32 = mybir.dt.float32
BF16 = mybir.dt.bfloat16
I32 = mybir.dt.int32
U8 = mybir.dt.uint8
AF = mybir.ActivationFunctionType
ALU = mybir.AluOpType

D = 1024
S_LEN = 8192
NQ = 2048
GQ = 512
NG = NQ // GQ
NKC = 2816
NQC = 5120
EPS = 1e-6
NIT = 16
RNG = 8.0
TOPK = 256.0
NEGB = -30000.0
MAGIC = 12582912.0
C1 = 6.28125
C2 = 2.0 * math.pi - 6.28125
ARENA = 164864
GPERS = 81920


class _Op:
    __slots__ = ("eng", "fn", "deps", "ticket", "has_dep", "dma", "sem", "target", "prev")

    def __init__(self, eng, fn, dma):
        self.eng = eng
        self.fn = fn
        self.deps = []
        self.ticket = None
        self.has_dep = False
        self.dma = dma
        self.sem = None
        self.target = None
        self.prev = None


class Sched:
    ENGS = ("pe", "act", "dve", "pool", "sp")

    def __init__(self, nc, n_dma_sems=14):
        self.nc = nc
        self.ops = {e: [] for e in self.ENGS}
        self.last_w = {}
        self.readers = {}
        self.nd = n_dma_sems
        self.rr = 0
        self.rr_sw = 0
        self.n_sw = 4
        self.dlast = [None] * n_dma_sems
        self.dcount = [0] * n_dma_sems
        self.bar_deps = []
        self.bar_pending = set()
        self.last_compute = {e: None for e in self.ENGS}

    def barrier(self):
        deps = [o for o in self.last_compute.values() if o is not None]
        deps += [o for o in self.dlast if o is not None]
        self.bar_deps = deps
        self.bar_pending = set(self.ENGS)
        self.last_w = {}
        self.readers = {}

    def op(self, eng, fn, reads=(), writes=(), dma=False):
        o = _Op(eng, fn, dma)
        deps = []
        if eng in self.bar_pending:
            deps.extend(self.bar_deps)
            self.bar_pending.discard(eng)
        for r in reads:
            w = self.last_w.get(r)
            if w is not None:
                deps.append(w)
        for w_ in writes:
            w = self.last_w.get(w_)
            if w is not None:
                deps.append(w)
            deps.extend(self.readers.get(w_, ()))
        seen = set()
        for d in deps:
            if d is o or id(d) in seen:
                continue
            seen.add(id(d))
            if d.eng == "pe" and eng == "pe" and not d.dma and not dma:
                continue
            o.deps.append(d)
            d.has_dep = True
        for r in reads:
            self.readers.setdefault(r, []).append(o)
        for w_ in writes:
            self.last_w[w_] = o
            self.readers[w_] = []
        if dma:
            if eng == "pool":
                k = self.rr_sw
                self.rr_sw = (self.rr_sw + 1) % self.n_sw
            else:
                k = self.n_sw + self.rr
                self.rr = (self.rr + 1) % (self.nd - self.n_sw)
            o.sem = k
            o.prev = self.dlast[k]
            self.dcount[k] += 16
            o.target = self.dcount[k]
            self.dlast[k] = o
        else:
            self.last_compute[eng] = o
        self.ops[eng].append(o)
        return o

    def alloc_sems(self, st):
        nc = self.nc
        self.esem = {e: st.enter_context(nc.semaphore("s_" + e)) for e in self.ENGS}
        self.dsem = [st.enter_context(nc.semaphore("d_%d" % i)) for i in range(self.nd)]

    def emit(self, block, final_waits=()):
        esem, dsem = self.esem, self.dsem
        for o in final_waits:
            o.has_dep = True
        for e in self.ENGS:
            c = 0
            for o in self.ops[e]:
                if (not o.dma) and o.has_dep:
                    c += 1
                    o.ticket = c

        def run(e, engobj, extra=None):
            waited = {}

            def wait_for(d):
                if d.dma:
                    key, val, sem = ("d", d.sem), d.target, dsem[d.sem]
                else:
                    key, val, sem = ("e", d.eng), d.ticket, esem[d.eng]
                if waited.get(key, 0) >= val:
                    return
                engobj.wait_ge(sem, val)
                waited[key] = val

            for o in self.ops[e]:
                for d in o.deps:
                    wait_for(d)
                if o.dma and o.prev is not None:
                    wait_for(o.prev)
                ins = o.fn(engobj)
                if o.dma:
                    ins.then_inc(dsem[o.sem], 16)
                elif o.has_dep:
                    ins.then_inc(esem[e], 1)
            if extra:
                for d in extra:
                    wait_for(d)

        @block.tensor
        def _(eng):
            run("pe", eng)

        @block.scalar
        def _(eng):
            run("act", eng)

        @block.vector
        def _(eng):
            run("dve", eng)

        @block.gpsimd
        def _(eng):
            run("pool", eng)

        @block.sync
        def _(eng):
            run("sp", eng, extra=list(final_waits))


class Buf:
    _n = 0

    def __init__(self, ap, name=None):
        Buf._n += 1
        self.ap = ap
        self.key = "%s#%d" % (name or "b", Buf._n)


def _keys(xs):
    out = []
    for x in xs:
        out.append(x.key if isinstance(x, Buf) else x)
    return out


_DT_SIZE = {F32: 4, BF16: 2, I32: 4, U8: 1}


class Arena:
    def __init__(self, t, nbytes):
        self.t = t
        self.n = nbytes
        self.off = 0

    def reset(self, off=0):
        self.off = off

    def alloc(self, shape, dt, name=None):
        n = 1
        for s in shape:
            n *= s
        nb = n * _DT_SIZE[dt]
        nb_al = (nb + 63) // 64 * 64
        assert self.off + nb_al <= self.n, ("arena overflow", name, self.off, nb_al, self.n)
        ap = self.t[:, self.off:self.off + nb].bitcast(dt)
        self.off += nb_al
        if len(shape) == 2:
            ap = ap.rearrange("p (a b) -> p a b", a=shape[0], b=shape[1])
        elif len(shape) == 3:
            ap = ap.rearrange("p (a b c) -> p a b c", a=shape[0], b=shape[1], c=shape[2])
        return Buf(ap, name)


def build(debug=False, stop_after=None):
    nc = bass.Bass("TRN2", target_bir_lowering=False)

    def din(name, shape, dt=F32):
        return nc.dram_tensor(name, list(shape), dt, kind="ExternalInput").ap()

    dbg_kind = "ExternalOutput" if debug else "Internal"

    def dscr(name, shape, dt):
        return nc.dram_tensor(name, list(shape), dt, kind=dbg_kind).ap()

    xT = din("xT", [128, 8, S_LEN])
    xTo = din("xTo", [128, 8, NQ])
    xo = din("xo", [NQ, D])
    pos_all = din("pos_all", [1, S_LEN], I32)
    pos_own = din("pos_own", [1, NQ], I32)
    wk = din("wk", [128, 8, NKC])
    wq = din("wq", [128, 8, NQC])
    wiw = din("wiw", [128, 8, 4])
    wbd = din("wbd", [64, 8, D])
    wbf = din("wbf", [64, 8, D])
    wo = din("wo", [128, 8, D])
    cvec = din("cvec", [128, 64])
    fgain = din("fgain", [1, D])
    identf = din("identf", [128, 128])
    bmaskf = din("bmaskf", [128, 32])
    cbiasf = din("cbiasf", [128, 512])
    y = nc.dram_tensor("y", [NQ, D], F32, kind="ExternalOutput").ap()

    kf_s = dscr("kf_s", [8, 68, S_LEN], BF16)
    ka_s = dscr("ka_s", [8, 64, S_LEN], BF16)
    ki_s = dscr("ki_s", [64, S_LEN], BF16)
    vf_s = dscr("vf_s", [8, 128, 64, 128], BF16)
    va_s = dscr("va_s", [8, 128, 64, 128], BF16)
    wq_s = nc.dram_tensor("wq_s", [128, 8, NQC], BF16, kind="Internal").ap()
    wbd_s = nc.dram_tensor("wbd_s", [64, 8, D], BF16, kind="Internal").ap()
    wbf_s = nc.dram_tensor("wbf_s", [64, 8, D], BF16, kind="Internal").ap()
    wo_s = nc.dram_tensor("wo_s", [128, 8, D], BF16, kind="Internal").ap()
    dbg = {}
    if debug:
        dbg["qfT"] = nc.dram_tensor("d_qfT", [128, 8, 512], BF16, kind="ExternalOutput").ap()
        dbg["qaT"] = nc.dram_tensor("d_qaT", [128, 8, 512], BF16, kind="ExternalOutput").ap()
        dbg["qiT"] = nc.dram_tensor("d_qiT", [128, 4, 512], BF16, kind="ExternalOutput").ap()
        dbg["sc"] = nc.dram_tensor("d_sc", [128, 8192], F32, kind="ExternalOutput").ap()
        dbg["lo"] = nc.dram_tensor("d_lo", [4, 128, 1], F32, kind="ExternalOutput").ap()
        dbg["mT"] = nc.dram_tensor("d_mT", [128, 64, 512], U8, kind="ExternalOutput").ap()
        dbg["yTa"] = nc.dram_tensor("d_yTa", [128, 8, 512], BF16, kind="ExternalOutput").ap()
        dbg["yTf"] = nc.dram_tensor("d_yTf", [128, 8, 512], BF16, kind="ExternalOutput").ap()

    st = contextlib.ExitStack()
    with st:
        def T(name, shape, dt):
            return Buf(st.enter_context(nc.sbuf_tensor(name, list(shape), dt))[:], name)

        arena_t = st.enter_context(nc.sbuf_tensor("arena", [128, ARENA], U8))
        AR = Arena(arena_t, ARENA)
        ps = [Buf(st.enter_context(nc.psum_tensor("ps%d" % i, [128, 512], F32))[:], "ps%d" % i) for i in range(8)]

        S = Sched(nc)
        S.alloc_sems(st)

        def DMA(q, out, in_, R=(), W=()):
            return S.op(q, lambda e: e.dma_start(out=out, in_=in_), _keys(R), _keys(W), dma=True)

        def TT(eng, out, in0, in1, op, R=(), W=()):
            return S.op(eng, lambda e: e.tensor_tensor(out=out, in0=in0, in1=in1, op=op), _keys(R), _keys(W))

        def TS(eng, out, in0, s1, op0, s2=None, op1=None, R=(), W=(), accum=None):
            def f(e):
                kw = dict(out=out, in0=in0, scalar1=s1, scalar2=s2, op0=op0)
                if op1 is not None:
                    kw["op1"] = op1
                if accum is not None:
                    kw["accum_out"] = accum
                return e.tensor_scalar(**kw)
            return S.op(eng, f, _keys(R), _keys(W))

        def STT(out, in0, scalar, in1, op0, op1, R=(), W=()):
            return S.op("dve", lambda e: e.scalar_tensor_tensor(out=out, in0=in0, scalar=scalar, in1=in1, op0=op0, op1=op1),
                        _keys(R), _keys(W))

        def ACT(out, in_, func, R=(), W=(), bias=None, scale=None, accum=None):
            def f(e):
                kw = dict(out=out, in_=in_, func=func)
                if bias is not None:
                    kw["bias"] = bias
                if scale is not None:
                    kw["scale"] = scale
                if accum is not None:
                    kw["accum_out"] = accum
                return e.activation(**kw)
            return S.op("act", f, _keys(R), _keys(W))

        def CP(eng, out, in_, R=(), W=()):
            return S.op(eng, lambda e: e.tensor_copy(out=out, in_=in_), _keys(R), _keys(W))

        def MS(eng, ap, val, W=()):
            return S.op(eng, lambda e: e.memset(ap, val), (), _keys(W))

        def MM(out, pairs, R=(), W=()):
            def f(e):
                ins = None
                n = len(pairs)
                for i, (l, r) in enumerate(pairs):
                    ins = e.matmul(out, lhsT=l, rhs=r, start=(i == 0), stop=(i == n - 1))
                return ins
            return S.op("pe", f, _keys(R), _keys(W))

        ones_bf = T("ones_bf", [128, 512], BF16)
        onesf = T("onesf", [128, 512], F32)
        ident = T("ident", [128, 128], BF16)
        bmask = T("bmask", [128, 32], BF16)
        cbias = T("cbias", [128, 512], F32)
        cv = T("cv", [128, 64], F32)
        nbf = T("nbf", [128, 1], F32)
        halfpi = T("halfpi", [128, 1], F32)
        wiwb = T("wiwb", [128, 8, 4], BF16)
        fg_bc = T("fg_bc", [128, D], F32)
        rcol_q = T("rcol_q", [128, 4], F32)
        absw = T("absw", [128, 16], F32)
        sgnw = T("sgnw", [128, 16], F32)
        w4 = T("w4", [128, 16], F32)
        rcol_as = [T("rcol_a0", [128, 4], F32), T("rcol_a1", [128, 4], F32)]
        bis_lo = T("bis_lo", [128, 1], F32)
        bis_mid = T("bis_mid", [128, 1], F32)
        bis_cnt = T("bis_cnt", [128, 1], F32)
        bis_ind = T("bis_ind", [128, 1], F32)
        bis_nb = T("bis_nb", [128, 1], F32)
        bis_ca = T("bis_ca", [128, 1], F32)
        bis_tot = T("bis_tot", [128, 1], F32)
        fin_ss = T("fin_ss", [128, 1], F32)
        fin_r = T("fin_r", [128, 1], F32)

        GAIN = lambda c: cv.ap[:, c:c + 1]
        INVF = cv.ap[:, 8:9]
        SGNS = cv.ap[:, 9:10]
        BMRG = lambda c: cv.ap[:, 16 + c:17 + c]

        MS("dve", ones_bf.ap, 1.0, W=[ones_bf])
        MS("dve", onesf.ap, 1.0, W=[onesf])
        MS("dve", halfpi.ap, math.pi / 2, W=[halfpi])
        DMA("sp", cv.ap, cvec[:, :], W=[cv])
        DMA("sp", cbias.ap, cbiasf[:, :], W=[cbias])
        DMA("pool", ident.ap, identf[:, :], W=[ident])
        DMA("pool", bmask.ap, bmaskf[:, :], W=[bmask])
        DMA("pool", wiwb.ap, wiw[:, :, :], W=[wiwb])
        DMA("sp", fg_bc.ap, fgain[0:1, :].to_broadcast([128, D]), W=[fg_bc])
        TS("dve", nbf.ap, cv.ap[:, 10:11], -1.0, ALU.mult, R=[cv], W=[nbf])

        def load_x_dma(src, t0, xf):
            DMA("sp", xf.ap, src[:, :, t0:t0 + 512], W=[xf])

        def load_x_prep(xf, xb, xsq):
            ACT(xsq.ap, xf.ap, AF.Square, R=[xf], W=[xsq])
            for c in range(8):
                ACT(xb.ap[:, c, :], xf.ap[:, c, :], AF.Copy, R=[xf, cv], W=[xb], scale=GAIN(c))

        def load_x_group(src, t0, xf, xb, xsq):
            load_x_dma(src, t0, xf)
            load_x_prep(xf, xb, xsq)

        def rstd_gen(xsq, rbc, rcol, pA, pB):
            MM(pA.ap, [(ones_bf.ap[:, 0:128], xsq.ap[:, c, :]) for c in range(8)], R=[xsq, ones_bf], W=[pA])
            yield
            for tb in range(4):
                MM(pB.ap[:, tb:tb + 1], [(xsq.ap[:, c, tb * 128:(tb + 1) * 128], ones_bf.ap[:, 0:1]) for c in range(8)],
                   R=[xsq, ones_bf], W=[pB])
            yield
            TS("dve", rcol.ap, pB.ap[:, 0:4], 1.0 / D, ALU.mult, EPS, ALU.add, R=[pB], W=[rcol])
            TS("dve", rbc.ap, pA.ap, 1.0 / D, ALU.mult, EPS, ALU.add, R=[pA], W=[rbc])
            yield
            ACT(rcol.ap, rcol.ap, AF.Sqrt, R=[rcol], W=[rcol])
            ACT(rbc.ap, rbc.ap, AF.Sqrt, R=[rbc], W=[rbc])
            yield
            S.op("dve", lambda e: e.reciprocal(out=rcol.ap, in_=rcol.ap), _keys([rcol]), _keys([rcol]))
            yield
            S.op("dve", lambda e: e.reciprocal(out=rbc.ap, in_=rbc.ap), _keys([rbc]), _keys([rbc]))
            yield

        def rstd_from(xsq, xb_unused, rbc, rcol, pA, pB, scale_extra=1.0):
            for _ in rstd_gen(xsq, rbc, rcol, pA, pB):
                pass

        def rope_gen(possrc, t0, posi, ang, kk, sn, cs, rbc, scale_extra):
            DMA("sp", posi.ap, possrc[0:1, t0:t0 + 512].to_broadcast([128, 512]), W=[posi])
            CP("dve", ang.ap, posi.ap, R=[posi], W=[ang])
            yield
            TS("dve", ang.ap, ang.ap, INVF, ALU.mult, R=[ang, cv], W=[ang])
            yield
            TS("dve", kk.ap, ang.ap, 1.0 / (2.0 * math.pi), ALU.mult, MAGIC, ALU.add, R=[ang], W=[kk])
            yield
            TS("dve", kk.ap, kk.ap, -MAGIC, ALU.add, R=[kk], W=[kk])
            yield
            STT(ang.ap, kk.ap, -C1, ang.ap, ALU.mult, ALU.add, R=[kk, ang], W=[ang])
            yield
            STT(ang.ap, kk.ap, -C2, ang.ap, ALU.mult, ALU.add, R=[kk, ang], W=[ang])
            yield
            TS("dve", ang.ap, ang.ap, math.pi, ALU.min, -math.pi, ALU.max, R=[ang], W=[ang])
            yield
            STT(kk.ap, ang.ap, -1.0, ang.ap, ALU.mult, ALU.max, R=[ang], W=[kk])
            ACT(sn.ap, ang.ap, AF.Sin, R=[ang], W=[sn])
            ACT(cs.ap, kk.ap, AF.Sin, R=[kk, halfpi], W=[cs], bias=halfpi.ap[:, 0:1], scale=-1.0)
            yield
            STT(sn.ap, sn.ap, SGNS, rbc.ap, ALU.mult, ALU.mult, R=[sn, cv, rbc], W=[sn])
            yield
            if scale_extra != 1.0:
                TS("dve", sn.ap, sn.ap, scale_extra, ALU.mult, R=[sn], W=[sn])
                STT(cs.ap, cs.ap, scale_extra, rbc.ap, ALU.mult, ALU.mult, R=[cs, rbc], W=[cs])
            else:
                TT("dve", cs.ap, cs.ap, rbc.ap, ALU.mult, R=[cs, rbc], W=[cs])
            yield

        def rope_tables(possrc, t0, posi, ang, kk, sn, cs, rbc, scale_extra):
            for _ in rope_gen(possrc, t0, posi, ang, kk, sn, cs, rbc, scale_extra):
                pass

        AR.reset(0)
        wkb = AR.alloc([8, NKC], BF16, "wkb")
        xfs = [AR.alloc([8, 512], F32, "xf") for _ in range(2)]
        xbs = [AR.alloc([8, 512], BF16, "xb") for _ in range(2)]
        xsq1 = AR.alloc([8, 512], BF16, "xsq")
        posi = AR.alloc([512], I32, "posi")
        ang = AR.alloc([512], F32, "ang")
        kk = AR.alloc([512], F32, "kk")
        sns = [AR.alloc([512], F32, "sn") for _ in range(2)]
        css = [AR.alloc([512], F32, "cs") for _ in range(2)]
        rbcs = [AR.alloc([512], F32, "rbc") for _ in range(2)]
        t1s = [AR.alloc([512], F32, "t1") for _ in range(2)]
        t2s = [AR.alloc([512], F32, "t2") for _ in range(2)]
        ksts = [AR.alloc([512], BF16, "kst") for _ in range(2)]
        mf = AR.alloc([512], F32, "mf")
        kist = AR.alloc([512], BF16, "kist")
        ee = AR.alloc([512], F32, "ee")
        ncums = [AR.alloc([512], F32, "ncum") for _ in range(2)]
        r1s = AR.alloc([512], F32, "r1s")
        aug = AR.alloc([3, 512], BF16, "aug")
        vsts = [AR.alloc([8, 4, 128], BF16, "vst") for _ in range(2)]

        for i in range(4):
            DMA("pool", wkb.ap[:, :, i * 704:(i + 1) * 704], wk[:, :, i * 704:(i + 1) * 704], W=[wkb])
        for v in vsts:
            MS("pool", v.ap, 1.0, W=[v])
        n_tg = S_LEN // 512
        if stop_after == "A1":
            n_tg = 1
        if stop_after == "A0":
            n_tg = 0
        kst_i = 0
        def chain_gen(tgn):
            k = tgn % 2
            for _ in rstd_gen(xsq1, rbcs[k], rcol_as[k], ps[0], ps[1]):
                yield
            for _ in rope_gen(pos_all, tgn * 512, posi, ang, kk, sns[k], css[k], rbcs[k], 1.0):
                yield

        if n_tg > 0:
            load_x_dma(xT, 0, xfs[0])
            load_x_prep(xfs[0], xbs[0], xsq1)
            for _ in chain_gen(0):
                pass
        for tg in range(n_tg):
            t0 = tg * 512
            xb = xbs[tg % 2]
            rbc = rbcs[tg % 2]
            rcol_a = rcol_as[tg % 2]
            sn = sns[tg % 2]
            cs = css[tg % 2]
            if tg + 1 < n_tg:
                load_x_dma(xT, t0 + 512, xfs[(tg + 1) % 2])
            if tg == 1 or (n_tg == 1 and tg == 0):
                for i in range(NQC // 512):
                    DMA("pool", wq_s[:, :, i * 512:(i + 1) * 512], wq[:, :, i * 512:(i + 1) * 512], W=["wq_s"])
                for i in range(2):
                    DMA("pool", wbd_s[:, :, i * 512:(i + 1) * 512], wbd[:, :, i * 512:(i + 1) * 512], W=["wbd_s"])
                    DMA("pool", wbf_s[:, :, i * 512:(i + 1) * 512], wbf[:, :, i * 512:(i + 1) * 512], W=["wbf_s"])
                    DMA("pool", wo_s[:, :, i * 512:(i + 1) * 512], wo[:, :, i * 512:(i + 1) * 512], W=["wo_s"])


            def proj_chunk(pbuf, n0):
                MM(pbuf.ap, [(wkb.ap[:, c, n0:n0 + 128], xb.ap[:, c, :]) for c in range(8)], R=[wkb, xb], W=[pbuf])

            for cc in range(4):
                pb = ps[2 + (cc % 2)]
                proj_chunk(pb, cc * 128)
                kst = ksts[kst_i % 2]
                kst_i += 1
                TT("dve", kst.ap, pb.ap, rbc.ap, ALU.mult, R=[pb, rbc], W=[kst])
                DMA("sp", kf_s[2 * cc, 0:64, t0:t0 + 512], kst.ap[0:64, :], R=[kst], W=["kf_s"])
                DMA("sp", kf_s[2 * cc + 1, 0:64, t0:t0 + 512], kst.ap[64:128, :], R=[kst], W=["kf_s"])
            DMA("sp", kf_s[:, 64, t0:t0 + 512], ones_bf.ap[0:8, :], R=[ones_bf], W=["kf_s"])
            for br in range(2):
                vst = vsts[br]
                dst = vf_s if br == 0 else va_s
                n0 = 1792 + br * 512
                for tb in range(4):
                    pv = ps[6 + (tb % 2)]
                    MM(pv.ap, [(xb.ap[:, c, tb * 128:(tb + 1) * 128], wkb.ap[:, c, n0:n0 + 512]) for c in range(8)],
                       R=[xb, wkb], W=[pv])
                    ACT(vst.ap[:, :, tb, 0:64], pv.ap.rearrange("p (h c) -> p h c", c=64), AF.Copy,
                        R=[pv, rcol_a], W=[vst], scale=rcol_a.ap[:, tb:tb + 1])
                for h in range(8):
                    DMA("sp", dst[h, :, tg * 4:(tg + 1) * 4, :], vst.ap[:, h, :, :], R=[vst], W=["v_s%d" % br])

            ahead = None
            if tg + 1 < n_tg:
                load_x_prep(xfs[(tg + 1) % 2], xbs[(tg + 1) % 2], xsq1)
                ahead = chain_gen(tg + 1)

            def adv(n):
                if ahead is not None:
                    for _ in range(n):
                        try:
                            next(ahead)
                        except StopIteration:
                            break

            for cc in range(5):
                pa, pb = (ps[4], ps[5]) if cc % 2 == 0 else (ps[2], ps[3])
                if cc < 4:
                    proj_chunk(pa, 512 + cc * 128)
                    proj_chunk(pb, 1024 + cc * 128)
                else:
                    proj_chunk(pa, 1536)
                    proj_chunk(pb, 1664)
                adv(2)
                t1 = t1s[cc % 2]
                t2 = t2s[cc % 2]
                TT("dve", t1.ap, pa.ap, cs.ap, ALU.mult, R=[pa, cs], W=[t1])
                adv(1)
                TT("dve", t2.ap, pb.ap, sn.ap, ALU.mult, R=[pb, sn], W=[t2])
                adv(1)
                if cc < 4:
                    kst = ksts[kst_i % 2]
                    kst_i += 1
                    TT("pool", kst.ap, t1.ap, t2.ap, ALU.add, R=[t1, t2], W=[kst])
                    DMA("sp", ka_s[2 * cc, :, t0:t0 + 512], kst.ap[0:64, :], R=[kst], W=["ka_s"])
                    DMA("sp", ka_s[2 * cc + 1, :, t0:t0 + 512], kst.ap[64:128, :], R=[kst], W=["ka_s"])
                else:
                    TT("pool", mf.ap, t1.ap, t2.ap, ALU.add, R=[t1, t2], W=[mf])
                    CP("pool", kist.ap[0:64, :], mf.ap[0:64, :], R=[mf], W=[kist])
                    DMA("sp", ki_s[:, t0:t0 + 512], kist.ap[0:64, :], R=[kist], W=["ki_s"])
                    ACT(ee.ap[96:104, :], mf.ap[96:104, :], AF.Exp, R=[mf, nbf], W=[ee], bias=nbf.ap[96:104, 0:1], scale=-1.0)
                    ACT(ee.ap[96:104, :], ee.ap[96:104, :], AF.Ln, R=[ee], W=[ee], bias=onesf.ap[96:104, 0:1], scale=1.0)
                    nc_cur = ncums[tg % 2]
                    nc_prev = ncums[(tg + 1) % 2]
                    init = 0.0 if tg == 0 else nc_prev.ap[96:104, 511:512]
                    S.op("dve", (lambda o_, d1_, i_: (lambda e: e.tensor_tensor_scan(
                        out=o_, data0=onesf.ap[96:104, :], data1=d1_, initial=i_, op0=ALU.mult, op1=ALU.add)))(
                        nc_cur.ap[96:104, :], ee.ap[96:104, :], init),
                        _keys([ee, onesf, nc_prev]), _keys([nc_cur]))
                    CP("dve", aug.ap[96:104, 0, :], nc_cur.ap[96:104, :], R=[nc_cur], W=[aug])
                    TT("dve", r1s.ap[96:104, :], nc_cur.ap[96:104, :], aug.ap[96:104, 0, :], ALU.subtract, R=[nc_cur, aug], W=[r1s])
                    CP("dve", aug.ap[96:104, 1, :], r1s.ap[96:104, :], R=[r1s], W=[aug])
                    TT("dve", r1s.ap[96:104, :], r1s.ap[96:104, :], aug.ap[96:104, 1, :], ALU.subtract, R=[r1s, aug], W=[r1s])
                    CP("dve", aug.ap[96:104, 2, :], r1s.ap[96:104, :], R=[r1s], W=[aug])
                    DMA("sp", kf_s[:, 65:68, t0:t0 + 512], aug.ap[96:104, :, :], R=[aug], W=["kf_s"])
            adv(1000)
        n_groups = NG
        if stop_after in ("A", "A1", "A0"):
            n_groups = 0
        elif stop_after is not None and stop_after.startswith("B"):
            n_groups = 1
        for g in range(n_groups):
            S.barrier()
            q0 = g * GQ
            win0 = 2048 * g
            L = 2048 * (g + 1)
            nkb = L // 128
            AR.reset(0)
            qfT = AR.alloc([8, 512], BF16, "qfT")
            qaT = AR.alloc([8, 512], BF16, "qaT")
            qiT = AR.alloc([4, 512], BF16, "qiT")
            xqb = AR.alloc([8, 512], BF16, "xqb")
            rbq = AR.alloc([512], F32, "rbq")
            mT = AR.alloc([64, 512], U8, "mT")
            yTa = AR.alloc([8, 512], BF16, "yTa")
            yTf = AR.alloc([8, 512], BF16, "yTf")
            assert AR.off <= GPERS, AR.off

            AR.reset(GPERS)
            xqf = AR.alloc([8, 512], F32, "xqf")
            xsqq = AR.alloc([8, 512], BF16, "xsqq")
            wqps = [AR.alloc([8, 512], BF16, "wqp") for _ in range(2)]
            posi = AR.alloc([512], I32, "posi")
            ang = AR.alloc([512], F32, "ang")
            kk = AR.alloc([512], F32, "kk")
            sn = AR.alloc([512], F32, "sn")
            cs = AR.alloc([512], F32, "cs")
            rb8 = AR.alloc([512], F32, "rb8")
            t1s = [AR.alloc([512], F32, "t1") for _ in range(2)]
            t2s = [AR.alloc([512], F32, "t2") for _ in range(2)]
            craws = [AR.alloc([2048], BF16, "craw") for _ in range(2)]

            load_x_group(xTo, q0, xqf, xqb, xsqq)
            rstd_from(xsqq, xqb, rbq, rcol_q, ps[0], ps[1])
            TS("dve", rb8.ap, rbq.ap, 0.125, ALU.mult, R=[rbq], W=[rb8])
            qchain = rope_gen(pos_own, q0, posi, ang, kk, sn, cs, rbq, 0.125)

            def qadv(n):
                for _ in range(n):
                    try:
                        next(qchain)
                    except StopIteration:
                        break

            wq_i = [0]

            def load_wq(piece):
                wb = wqps[wq_i[0] % 2]
                wq_i[0] += 1
                DMA("sp", wb.ap, wq_s[:, :, piece * 512:(piece + 1) * 512], R=["wq_s"], W=[wb])
                return wb

            def qproj(pbuf, wb, n0):
                MM(pbuf.ap, [(wb.ap[:, c, n0:n0 + 128], xqb.ap[:, c, :]) for c in range(8)], R=[wb, xqb], W=[pbuf])

            MS("pool", qaT.ap[64:128, :, :], 0.0, W=[qaT])
            MS("pool", qfT.ap[64:128, :, :], 0.0, W=[qfT])
            MS("pool", qfT.ap[64:68, :, :], 1.0, W=[qfT])
            MS("pool", qiT.ap[64:128, :, :], 0.0, W=[qiT])
            wb = load_wq(0)
            for cc in range(4):
                pb = ps[2 + cc]
                qproj(pb, wb, cc * 128)
            for cc in range(4):
                pb = ps[2 + cc]
                TT("dve", qfT.ap[0:64, 2 * cc, :], pb.ap[0:64, :], rb8.ap[0:64, :], ALU.mult, R=[pb, rb8], W=[qfT])
                qadv(2)
                TT("dve", qfT.ap[0:64, 2 * cc + 1, :], pb.ap[64:128, :], rb8.ap[64:128, :], ALU.mult, R=[pb, rb8], W=[qfT])
                qadv(2)
            qadv(1000)
            for h in range(8):
                cr = craws[h % 2]
                DMA("sp", cr.ap[64:65, :], kf_s[h, 65:66, win0:win0 + 2048], R=["kf_s"], W=[cr])
                TS("dve", qfT.ap[64:65, h, :], cr.ap[64:65, :].rearrange("p (i r) -> p i r", r=4)[:, :, 0], -1.0, ALU.mult,
                   R=[cr], W=[qfT])
            wb1 = load_wq(1)
            wb2 = load_wq(2)
            for cc in range(4):
                pa, pb = ps[4 + 2 * (cc % 2)], ps[5 + 2 * (cc % 2)]
                qproj(pa, wb1, cc * 128)
                qproj(pb, wb2, cc * 128)
                t1 = t1s[cc % 2]
                t2 = t2s[cc % 2]
                TT("dve", t1.ap, pa.ap, cs.ap, ALU.mult, R=[pa, cs], W=[t1])
                TT("dve", t2.ap, pb.ap, sn.ap, ALU.mult, R=[pb, sn], W=[t2])
                TT("pool", qaT.ap[0:64, 2 * cc, :], t1.ap[0:64, :], t2.ap[0:64, :], ALU.add, R=[t1, t2], W=[qaT])
                TT("pool", qaT.ap[0:64, 2 * cc + 1, :], t1.ap[64:128, :], t2.ap[64:128, :], ALU.add, R=[t1, t2], W=[qaT])
            wb = load_wq(3)
            for cc in range(2):
                pa, pb = ps[4 + 2 * (cc % 2)], ps[5 + 2 * (cc % 2)]
                qproj(pa, wb, cc * 128)
                qproj(pb, wb, 256 + cc * 128)
                t1 = t1s[cc % 2]
                t2 = t2s[cc % 2]
                TT("dve", t1.ap, pa.ap, cs.ap, ALU.mult, R=[pa, cs], W=[t1])
                TT("dve", t2.ap, pb.ap, sn.ap, ALU.mult, R=[pb, sn], W=[t2])
                TT("pool", qiT.ap[0:64, 2 * cc, :], t1.ap[0:64, :], t2.ap[0:64, :], ALU.add, R=[t1, t2], W=[qiT])
                TT("pool", qiT.ap[0:64, 2 * cc + 1, :], t1.ap[64:128, :], t2.ap[64:128, :], ALU.add, R=[t1, t2], W=[qiT])
            for sb in range(4):
                MM(ps[1].ap[:, 8 + 4 * sb:12 + 4 * sb],
                   [(xqb.ap[:, c, sb * 128:(sb + 1) * 128], wiwb.ap[:, c, :]) for c in range(8)], R=[xqb, wiwb], W=[ps[1]])
                TS("dve", w4.ap[:, 4 * sb:4 * sb + 4], ps[1].ap[:, 8 + 4 * sb:12 + 4 * sb], rcol_q.ap[:, sb:sb + 1], ALU.mult,
                   0.5, ALU.mult, R=[ps[1], rcol_q], W=[w4])
            STT(absw.ap, w4.ap, -1.0, w4.ap, ALU.mult, ALU.max, R=[w4], W=[absw])
            TS("dve", sgnw.ap, w4.ap, 0.0, ALU.is_ge, 2.0, ALU.mult, R=[w4], W=[sgnw])
            TS("dve", sgnw.ap, sgnw.ap, -1.0, ALU.add, R=[sgnw], W=[sgnw])
            if debug and g == 0:
                DMA("sp", dbg["qfT"][:, :, :], qfT.ap, R=[qfT], W=["dbg1"])
                DMA("sp", dbg["qaT"][:, :, :], qaT.ap, R=[qaT], W=["dbg2"])
                DMA("sp", dbg["qiT"][:, :, :], qiT.ap, R=[qiT], W=["dbg3"])
            if stop_after == "B1":
                break

            S.barrier()
            AR.reset(GPERS)
            sc = AR.alloc([8192], F32, "sc")
            msk = AR.alloc([8192], BF16, "msk")
            rts = [AR.alloc([512], F32, "rt") for _ in range(2)]
            kits = [AR.alloc([2048], BF16, "kit") for _ in range(1)]
            f_kTps = [AR.alloc([2048], BF16, "kTp") for _ in range(2)]
            f_vps = [AR.alloc([16, 128], BF16, "vp") for _ in range(2)]
            f_Pts = [AR.alloc([512], BF16, "Pt") for _ in range(4)]
            f_recs = [AR.alloc([512], F32, "rec") for _ in range(1)]

            for kt_ in f_kTps + kits:
                MS("pool", kt_.ap[64:128, :], 0.0, W=[kt_])

            def attn_steps(br, kTps, vps, Pts, Pms, recs, psS, psO):
                tiles = [(h, pc, kb) for h in range(8) for pc in range(g + 1) for kb in range(16)]
                pend = []
                nS = len(psS)
                cur = {}
                piece_n = 0

                def issue_pv(item):
                    (h, pc, kb, pmat, vp, first, last, pc0) = item
                    po = psO[h % len(psO)]
                    S.op("pe", (lambda o_, l_, r_, f_, s_: (lambda e: e.matmul(o_, lhsT=l_, rhs=r_, start=f_, stop=s_)))(
                        po.ap[:, pc0:512], vp.ap[:, kb, :], pmat.ap[:, pc0:512], first, last), _keys([vp, pmat]), _keys([po]))
                    if last:
                        yT = yTa if br == 0 else yTf
                        rec = recs[h % len(recs)]
                        S.op("dve", (lambda o_, i_: (lambda e: e.reciprocal(out=o_, in_=i_)))(rec.ap[0:64, :], po.ap[64:128, :]),
                             _keys([po]), _keys([rec]))
                        TT("dve", yT.ap[0:64, h, :], po.ap[0:64, :], rec.ap[0:64, :], ALU.mult, R=[po, rec], W=[yT])

                for ti, (h, pc, kb) in enumerate(tiles):
                    if kb == 0:
                        kTp = kTps[piece_n % len(kTps)]
                        vp = vps[piece_n % len(vps)]
                        piece_n += 1
                        if br == 0:
                            DMA("sp", kTp.ap[0:64, :], ka_s[h, :, pc * 2048:(pc + 1) * 2048], R=["ka_s"], W=[kTp])
                            DMA("sp", vp.ap, va_s[h, :, pc * 16:(pc + 1) * 16, :], R=["v_s1"], W=[vp])
                        else:
                            DMA("sp", kTp.ap[0:68, :], kf_s[h, :, pc * 2048:(pc + 1) * 2048], R=["kf_s"], W=[kTp])
                            DMA("sp", vp.ap, vf_s[h, :, pc * 16:(pc + 1) * 16, :], R=["v_s0"], W=[vp])
                        cur["k"], cur["v"] = kTp, vp
                    kTp, vp = cur["k"], cur["v"]
                    pS = psS[ti % nS]
                    Pt = Pts[ti % len(Pts)]
                    diagc = (pc == g)
                    diag = (br == 1 and diagc)
                    c0 = 32 * kb if diagc else 0
                    if br == 0:
                        MM(pS.ap[:, c0:512], [(kTp.ap[:, kb * 128:(kb + 1) * 128], qaT.ap[:, h, c0:512])], R=[kTp, qaT], W=[pS])
                    else:
                        MM(pS.ap[:, c0:512], [(kTp.ap[:, kb * 128:(kb + 1) * 128], qfT.ap[:, h, c0:512])], R=[kTp, qfT], W=[pS])
                    ACT(Pt.ap[:, c0:512], pS.ap[:, c0:512], AF.Exp, R=[pS], W=[Pt])
                    if br == 0:
                        Pm = Pms[ti % len(Pms)]
                        TT("pool" if ti % 2 == 1 else "dve", Pm.ap[:, c0:512], Pt.ap[:, c0:512], mT.ap[:, pc * 16 + kb, c0:512], ALU.mult,
                           R=[Pt, mT], W=[Pm])
                        pmat = Pm
                    else:
                        if diag:
                            TT("pool", Pt.ap[:, c0:c0 + 32], Pt.ap[:, c0:c0 + 32], bmask.ap, ALU.mult, R=[Pt, bmask], W=[Pt])
                        pmat = Pt
                    pend.append((h, pc, kb, pmat, vp, (pc == 0 and kb == 0), (pc == g and kb == 15), c0))
                    if len(pend) > min(4, nS - 1):
                        issue_pv(pend.pop(0))
                    yield 1
                while pend:
                    issue_pv(pend.pop(0))

            def b2_units():
                for sb in range(4):
                    nch = 4 * g + sb + 1
                    Lsb = 512 * nch
                    kit = kits[0]
                    for ch in range(nch):
                        if ch % 4 == 0:
                            pc = ch // 4
                            DMA("sp", kit.ap[0:64, :], ki_s[:, pc * 2048:(pc + 1) * 2048], R=["ki_s"], W=[kit])
                        for h in range(4):
                            pb = ps[h]
                            MM(pb.ap, [(qiT.ap[:, h, sb * 128:(sb + 1) * 128], kit.ap[:, (ch % 4) * 512:(ch % 4 + 1) * 512])],
                               R=[qiT, kit], W=[pb])
                        for h in range(4):
                            pb = ps[h]
                            rt = rts[h % 2]
                            ACT(rt.ap, pb.ap, AF.Relu, R=[pb, absw], W=[rt], scale=absw.ap[:, 4 * sb + h:4 * sb + h + 1])
                            scs = sc.ap[:, ch * 512:(ch + 1) * 512]
                            sg = sgnw.ap[:, 4 * sb + h:4 * sb + h + 1]
                            if h == 0:
                                TS("dve", scs, rt.ap, sg, ALU.mult, R=[rt, sgnw], W=[sc])
                            else:
                                STT(scs, rt.ap, sg, scs, ALU.mult, ALU.add, R=[rt, sgnw, sc], W=[sc])
                        if ch == nch - 1:
                            TT("dve", sc.ap[:, ch * 512:(ch + 1) * 512], sc.ap[:, ch * 512:(ch + 1) * 512], cbias.ap, ALU.add,
                               R=[sc, cbias], W=[sc])
                        yield 3.2
                    if debug and g == 0 and sb == 3:
                        DMA("sp", dbg["sc"][:, 0:Lsb], sc.ap[:, 0:Lsb], R=[sc], W=["dbg4"])
                    MS("dve", bis_lo.ap, -RNG, W=[bis_lo])
                    La = 512 * int(0.4 * nch) if nch >= 3 else 0
                    Ld = Lsb - La
                    for it in range(NIT):
                        step = RNG / (2.0 ** it)
                        TS("dve", bis_mid.ap, bis_lo.ap, step, ALU.add, R=[bis_lo], W=[bis_mid])
                        if La > 0:
                            TS("dve", bis_nb.ap, bis_mid.ap, -1.0, ALU.mult, 2.0 ** -20, ALU.add, R=[bis_mid], W=[bis_nb])
                        TS("dve", msk.ap[:, 0:Ld], sc.ap[:, 0:Ld], bis_mid.ap[:, 0:1], ALU.is_ge, None, ALU.add,
                           R=[sc, bis_mid], W=[msk, bis_cnt], accum=bis_cnt.ap[:, 0:1])
                        if La > 0:
                            ACT(msk.ap[:, Ld:Lsb], sc.ap[:, Ld:Lsb], AF.Sign, R=[sc, bis_nb], W=["mskA", bis_ca],
                                bias=bis_nb.ap[:, 0:1], scale=1.0, accum=bis_ca.ap[:, 0:1])
                            STT(bis_tot.ap, bis_ca.ap, 0.5, bis_cnt.ap, ALU.mult, ALU.add, R=[bis_ca, bis_cnt], W=[bis_tot])
                            TS("dve", bis_ind.ap, bis_tot.ap, TOPK - La / 2.0, ALU.is_ge, step, ALU.mult, R=[bis_tot], W=[bis_ind])
                        else:
                            TS("dve", bis_ind.ap, bis_cnt.ap, TOPK, ALU.is_ge, step, ALU.mult, R=[bis_cnt], W=[bis_ind])
                        TT("dve", bis_lo.ap, bis_lo.ap, bis_ind.ap, ALU.add, R=[bis_lo, bis_ind], W=[bis_lo])
                        yield 0.7 + Ld * 1.05e-3
                    TS("dve", bis_ind.ap, bis_lo.ap, -RNG, ALU.is_le, -1000.0, ALU.mult, R=[bis_lo], W=[bis_ind])
                    TT("dve", bis_lo.ap, bis_lo.ap, bis_ind.ap, ALU.add, R=[bis_lo, bis_ind], W=[bis_lo])
                    TS("dve", msk.ap[:, 0:Lsb], sc.ap[:, 0:Lsb], bis_lo.ap[:, 0:1], ALU.is_ge, R=[sc, bis_lo], W=[msk, "mskA"])
                    if debug and g == 0:
                        DMA("sp", dbg["lo"][sb], bis_lo.ap, R=[bis_lo], W=["dbg5"])
                    yield Lsb * 0.6e-3
                    for k4 in range(Lsb // 512):
                        pt = ps[k4 % 2]
                        ptb = pt.ap.bitcast(BF16)
                        for i in range(4):
                            kb = k4 * 4 + i
                            S.op("pe", (lambda o_, i_: (lambda e: e.transpose(out=o_, in_=i_, identity=ident.ap)))(
                                ptb[:, i * 128:(i + 1) * 128], msk.ap[:, kb * 128:(kb + 1) * 128]),
                                _keys([msk, ident]), _keys([pt]))
                        ACT(mT.ap[:, k4 * 4:(k4 + 1) * 4, sb * 128:(sb + 1) * 128],
                            ptb[:, 0:512].rearrange("p (a b) -> p a b", b=128), AF.Copy, R=[pt], W=[mT])
                        yield 0.7
                    if Lsb // 128 < nkb:
                        MS("pool", mT.ap[:, Lsb // 128:nkb, sb * 128:(sb + 1) * 128], 0, W=[mT])

            fox = attn_steps(1, f_kTps, f_vps, f_Pts, None, f_recs, [ps[4], ps[5]], [ps[6], ps[7]])
            n_fox = 128 * (g + 1)
            units = list()
            tot_est = 0.0
            for sb in range(4):
                nch = 4 * g + sb + 1
                Lsb = 512 * nch
                La_ = 512 * int(0.4 * nch) if nch >= 3 else 0
                tot_est += nch * 3.2 + NIT * (0.7 + (Lsb - La_) * 1.05e-3) + Lsb * 0.6e-3 + (Lsb // 512) * 0.7
            rate = n_fox / (0.9 * tot_est)
            acc = 0.0
            fox_done = False
            for wgt in b2_units():
                acc += wgt * rate
                while acc >= 1.0 and not fox_done:
                    acc -= 1.0
                    try:
                        next(fox)
                    except StopIteration:
                        fox_done = True
            if not fox_done:
                for _ in fox:
                    pass
            if debug and g == 0:
                DMA("sp", dbg["mT"][:, 0:nkb, :], mT.ap[:, 0:nkb, :], R=[mT], W=["dbg6"])
            if stop_after == "B2":
                break

            S.barrier()
            AR.reset(GPERS)
            kTps = [AR.alloc([2048], BF16, "kTp") for _ in range(3)]
            vps = [AR.alloc([16, 128], BF16, "vp") for _ in range(3)]
            Pts = [AR.alloc([512], BF16, "Pt") for _ in range(6)]
            Pms = [AR.alloc([512], BF16, "Pm") for _ in range(6)]
            recs = [AR.alloc([512], F32, "rec") for _ in range(2)]
            for kt_ in kTps:
                MS("pool", kt_.ap[64:128, :], 0.0, W=[kt_])
            for _ in attn_steps(0, kTps, vps, Pts, Pms, recs, [ps[0], ps[1], ps[2], ps[3], ps[6], ps[7]], [ps[4], ps[5]]):
                pass
            if debug and g == 0:
                DMA("sp", dbg["yTa"][:, :, :], yTa.ap, R=[yTa], W=["dbg7"])
                DMA("sp", dbg["yTf"][:, :, :], yTf.ap, R=[yTf], W=["dbg8"])
            if stop_after == "B3":
                break

            S.barrier()
            AR.reset(GPERS)
            wqps = [AR.alloc([8, 512], BF16, "wqp") for _ in range(2)]
            wbrs = [AR.alloc([8, 128], BF16, "wbr") for _ in range(2)]
            wobs = [AR.alloc([8, 512], BF16, "wob") for _ in range(2)]
            ygs = [AR.alloc([8, 512], BF16, "yg") for _ in range(2)]
            mrg = AR.alloc([8, 512], BF16, "mrg")
            gtmp = AR.alloc([512], F32, "gtmp")
            gbf = AR.alloc([512], BF16, "gbf")
            gms4 = [AR.alloc([512], F32, "gm") for _ in range(2)]
            e1s = [AR.alloc([512], F32, "e1") for _ in range(1)]
            e2s = [AR.alloc([512], F32, "e2") for _ in range(1)]
            xot = AR.alloc([D], F32, "xot")
            xnew = AR.alloc([D], F32, "xnew")
            wq_i = [0]
            for br in range(2):
                wb = load_wq(4 + br)
                yT = yTa if br == 0 else yTf
                for cc in range(4):
                    pb = ps[cc % 2]
                    qproj(pb, wb, cc * 128)
                    TT("dve", gtmp.ap, pb.ap, rbq.ap, ALU.mult, R=[pb, rbq], W=[gtmp])
                    for hh in range(2):
                        h = 2 * cc + hh
                        ACT(gbf.ap[0:64, :], gtmp.ap[64 * hh:64 * hh + 64, :], AF.Silu, R=[gtmp], W=[gbf])
                        TT("pool", ygs[br].ap[0:64, h, :], yT.ap[0:64, h, :], gbf.ap[0:64, :], ALU.mult, R=[yT, gbf], W=[ygs[br]])
            wbr_n = [0]
            wqm = {}
            for hf in range(2):
                DMA("sp", wobs[hf].ap, wo_s[:, :, hf * 512:(hf + 1) * 512], R=["wo_s"], W=[wobs[hf]])
            for dc in range(8):
                pus = []
                par = dc % 2
                gms = [gms4[0], gms4[1]]
                e1, e2 = e1s[0], e2s[0]
                for br in range(2):
                    wsrc = wbd_s if br == 0 else wbf_s
                    wbr = wbrs[wbr_n[0] % 2]
                    wbr_n[0] += 1
                    DMA("sp", wbr.ap[0:64, :, :], wsrc[:, :, dc * 128:(dc + 1) * 128], R=["wbd_s", "wbf_s"], W=[wbr])
                    pu = ps[4 * par + br]
                    MM(pu.ap, [(wbr.ap[0:64, h, :], ygs[br].ap[0:64, h, :]) for h in range(8)], R=[wbr, ygs[br]], W=[pu])
                    pus.append(pu)
                    mcol = br * 1024 + dc * 128
                    piece = 6 + mcol // 512
                    if wqm.get(br, (None, None))[0] != piece:
                        wqm[br] = (piece, load_wq(piece))
                    wbm = wqm[br][1]
                    pm = ps[4 * par + 2 + br]
                    qproj(pm, wbm, mcol % 512)
                    TT("dve", gms[br].ap, pm.ap, rbq.ap, ALU.mult, R=[pm, rbq], W=[gms[br]])
                    ACT(gms[br].ap, gms[br].ap, AF.Sigmoid, R=[gms[br], cv], W=[gms[br]], bias=BMRG(br * 8 + dc))
                TT("dve", e1.ap, pus[0].ap, gms[0].ap, ALU.mult, R=[pus[0], gms[0]], W=[e1])
                TT("dve", e2.ap, pus[1].ap, gms[1].ap, ALU.mult, R=[pus[1], gms[1]], W=[e2])
                TT("pool", mrg.ap[:, dc, :], e1.ap, e2.ap, ALU.add, R=[e1, e2], W=[mrg])
            for sb in range(4):
                r0 = q0 + sb * 128
                DMA("sp", xot.ap, xo[r0:r0 + 128, :], W=[xot])
                for hf in range(2):
                    wob = wobs[hf]
                    po = ps[(2 * sb + hf) % 8]
                    MM(po.ap, [(mrg.ap[:, dc, sb * 128:(sb + 1) * 128], wob.ap[:, dc, :]) for dc in range(8)], R=[mrg, wob], W=[po])
                    TT("dve", xnew.ap[:, hf * 512:(hf + 1) * 512], po.ap, xot.ap[:, hf * 512:(hf + 1) * 512], ALU.add,
                       R=[po, xot], W=[xnew])
                ACT(xot.ap, xnew.ap, AF.Square, R=[xnew, xot], W=[xot, fin_ss], accum=fin_ss.ap[:, 0:1])
                TS("dve", fin_r.ap, fin_ss.ap, 1.0 / D, ALU.mult, EPS, ALU.add, R=[fin_ss], W=[fin_r])
                ACT(fin_r.ap, fin_r.ap, AF.Sqrt, R=[fin_r], W=[fin_r])
                S.op("dve", lambda e: e.reciprocal(out=fin_r.ap, in_=fin_r.ap), _keys([fin_r]), _keys([fin_r]))
                STT(xnew.ap, xnew.ap, fin_r.ap[:, 0:1], fg_bc.ap, ALU.mult, ALU.mult, R=[xnew, fin_r, fg_bc], W=[xnew])
                DMA("sp", y[r0:r0 + 128, :], xnew.ap, R=[xnew], W=["y"])

        fw = [o for o in S.dlast if o is not None]
        with nc.Block() as block:
            S.emit(block, final_waits=fw)
    return nc


def _rot_perm():
    p = np.arange(64)
    p[0:8] = np.arange(8, 16)
    p[8:16] = np.arange(0, 8)
    return p


def _fm(w):
    n = w.shape[1]
    return np.ascontiguousarray(w.reshape(8, 128, n).transpose(1, 0, 2))


def prep_inputs(x, positions, norm_gain, w_in, b_forget, b_merge, w_branch_dsa, w_branch_fox, w_out, final_gain):
    x = np.asarray(x, np.float32)
    positions = np.asarray(positions, np.int32)
    W = np.asarray(w_in, np.float32)[0]
    o = 0
    cols = {}
    for name, n in (("aq", 512), ("ak", 512), ("av", 512), ("ag", 512), ("iq", 256), ("ik", 64), ("iw", 4),
                    ("fq", 512), ("fk", 512), ("fv", 512), ("fg", 512), ("fl", 8), ("mg", 2048)):
        cols[name] = W[:, o:o + n]
        o += n
    perm = _rot_perm()

    def rot(w, nh):
        idx = np.concatenate([h * 64 + perm for h in range(nh)])
        return w[:, idx]

    z32 = np.zeros((D, 32), np.float32)
    z24 = np.zeros((D, 24), np.float32)
    z64 = np.zeros((D, 64), np.float32)
    wk = np.concatenate([cols["fk"], cols["ak"], rot(cols["ak"], 8),
                         cols["ik"], z32, cols["fl"], z24, rot(cols["ik"], 1), z64,
                         cols["fv"], cols["av"]], axis=1)
    assert wk.shape[1] == NKC
    wq = np.concatenate([cols["fq"], cols["aq"], rot(cols["aq"], 8), cols["iq"], rot(cols["iq"], 4),
                         cols["ag"], cols["fg"], cols["mg"]], axis=1)
    assert wq.shape[1] == NQC
    wk_d, wq_d, wiw_d = _fm(wk), _fm(wq), _fm(np.ascontiguousarray(cols["iw"]))
    wbd = np.ascontiguousarray(np.asarray(w_branch_dsa, np.float32)[0].reshape(8, 64, D).transpose(1, 0, 2))
    wbf = np.ascontiguousarray(np.asarray(w_branch_fox, np.float32)[0].reshape(8, 64, D).transpose(1, 0, 2))
    wo = _fm(np.asarray(w_out, np.float32)[0])
    cvec = np.zeros((128, 64), np.float32)
    cvec[:, 0:8] = np.asarray(norm_gain, np.float32)[0].reshape(8, 128).T
    half = 8
    inv_freq = (500000.0 ** (-np.arange(half, dtype=np.float32) * 2.0 / 16.0)).astype(np.float32)
    for p in range(128):
        r = p % 64
        if r < 8:
            cvec[p, 8] = inv_freq[r]
            cvec[p, 9] = -1.0
        elif r < 16:
            cvec[p, 8] = inv_freq[r - 8]
            cvec[p, 9] = 1.0
    cvec[96:104, 10] = np.asarray(b_forget, np.float32)[0]
    cvec[:, 16:32] = np.asarray(b_merge, np.float32)[0].reshape(16, 128).T
    fgain = np.asarray(final_gain, np.float32).reshape(1, D)
    identf = np.eye(128, dtype=np.float32)
    in_maps = []
    xT_b = [np.ascontiguousarray(x[b].T.reshape(8, 128, S_LEN).transpose(1, 0, 2)) for b in range(x.shape[0])]
    for c in range(8):
        b, j = divmod(c, 4)
        p_idx = np.arange(128)[:, None]
        m_idx = np.arange(32)[None, :]
        bmaskf = (p_idx <= 4 * m_idx + j).astype(np.float32)
        s_idx = np.arange(512)[None, :]
        cbiasf = np.where(s_idx <= 4 * p_idx + j, 0.0, NEGB).astype(np.float32)
        in_maps.append({
            "xT": xT_b[b],
            "xTo": np.ascontiguousarray(xT_b[b][:, :, j::4]),
            "xo": np.ascontiguousarray(x[b, j::4, :]),
            "pos_all": np.ascontiguousarray(positions[b][None, :]),
            "pos_own": np.ascontiguousarray(positions[b][None, j::4]),
            "wk": wk_d, "wq": wq_d, "wiw": wiw_d, "wbd": wbd, "wbf": wbf, "wo": wo,
            "cvec": cvec, "fgain": fgain, "identf": identf, "bmaskf": bmaskf, "cbiasf": cbiasf,
        })
    return in_maps


_NC_CACHE = {}


def kernel(x, positions, norm_gain, w_in, b_forget, b_merge, w_branch_dsa, w_branch_fox, w_out, final_gain):
    in_maps = prep_inputs(x, positions, norm_gain, w_in, b_forget, b_merge, w_branch_dsa, w_branch_fox, w_out, final_gain)
    if "nc" not in _NC_CACHE:
        _NC_CACHE["nc"] = build()
    nc = _NC_CACHE["nc"]
    res = run_bass_kernel_spmd(nc, in_maps, core_ids=list(range(8)))
    out = np.empty((2, S_LEN, D), np.float32)
    for c in range(8):
        b, j = divmod(c, 4)
        out[b, j::4, :] = res.results[c]["y"]
    return out
```

```python
import math
import contextlib
import numpy as np
import concourse.bass as bass
import concourse.mybir as mybir
from concourse.bass_utils import run_bass_kernel_spmd

F32 = mybir.dt.float32
BF16 = mybir.dt.bfloat16
I32 = mybir.dt.int32
U8 = mybir.dt.uint8
AF = mybir.ActivationFunctionType
ALU = mybir.AluOpType

D = 1024
S_LEN = 8192
NQ = 2048
GQ = 512
NG = NQ // GQ
NKC = 2816
NQC = 5120
EPS = 1e-6
NIT = 16
RNG = 8.0
TOPK = 256.0
NEGB = -30000.0
MAGIC = 12582912.0
C1 = 6.28125
C2 = 2.0 * math.pi - 6.28125
ARENA = 164864
GPERS = 81920


class _Op:
    __slots__ = ("eng", "fn", "deps", "ticket", "has_dep", "dma", "sem", "target", "prev")

    def __init__(self, eng, fn, dma):
        self.eng = eng
        self.fn = fn
        self.deps = []
        self.ticket = None
        self.has_dep = False
        self.dma = dma
        self.sem = None
        self.target = None
        self.prev = None


class Sched:
    ENGS = ("pe", "act", "dve", "pool", "sp")

    def __init__(self, nc, n_dma_sems=14):
        self.nc = nc
        self.ops = {e: [] for e in self.ENGS}
        self.last_w = {}
        self.readers = {}
        self.nd = n_dma_sems
        self.rr = 0
        self.rr_sw = 0
        self.n_sw = 4
        self.dlast = [None] * n_dma_sems
        self.dcount = [0] * n_dma_sems
        self.bar_deps = []
        self.bar_pending = set()
        self.last_compute = {e: None for e in self.ENGS}

    def barrier(self):
        deps = [o for o in self.last_compute.values() if o is not None]
        deps += [o for o in self.dlast if o is not None]
        self.bar_deps = deps
        self.bar_pending = set(self.ENGS)
        self.last_w = {}
        self.readers = {}

    def op(self, eng, fn, reads=(), writes=(), dma=False):
        o = _Op(eng, fn, dma)
        deps = []
        if eng in self.bar_pending:
            deps.extend(self.bar_deps)
            self.bar_pending.discard(eng)
        for r in reads:
            w = self.last_w.get(r)
            if w is not None:
                deps.append(w)
        for w_ in writes:
            w = self.last_w.get(w_)
            if w is not None:
                deps.append(w)
            deps.extend(self.readers.get(w_, ()))
        seen = set()
        for d in deps:
            if d is o or id(d) in seen:
                continue
            seen.add(id(d))
            if d.eng == "pe" and eng == "pe" and not d.dma and not dma:
                continue
            o.deps.append(d)
            d.has_dep = True
        for r in reads:
            self.readers.setdefault(r, []).append(o)
        for w_ in writes:
            self.last_w[w_] = o
            self.readers[w_] = []
        if dma:
            if eng == "pool":
                k = self.rr_sw
                self.rr_sw = (self.rr_sw + 1) % self.n_sw
            else:
                k = self.n_sw + self.rr
                self.rr = (self.rr + 1) % (self.nd - self.n_sw)
            o.sem = k
            o.prev = self.dlast[k]
            self.dcount[k] += 16
            o.target = self.dcount[k]
            self.dlast[k] = o
        else:
            self.last_compute[eng] = o
        self.ops[eng].append(o)
        return o

    def alloc_sems(self, st):
        nc = self.nc
        self.esem = {e: st.enter_context(nc.semaphore("s_" + e)) for e in self.ENGS}
        self.dsem = [st.enter_context(nc.semaphore("d_%d" % i)) for i in range(self.nd)]

    def emit(self, block, final_waits=()):
        esem, dsem = self.esem, self.dsem
        for o in final_waits:
            o.has_dep = True
        for e in self.ENGS:
            c = 0
            for o in self.ops[e]:
                if (not o.dma) and o.has_dep:
                    c += 1
                    o.ticket = c

        def run(e, engobj, extra=None):
            waited = {}

            def wait_for(d):
                if d.dma:
                    key, val, sem = ("d", d.sem), d.target, dsem[d.sem]
                else:
                    key, val, sem = ("e", d.eng), d.ticket, esem[d.eng]
                if waited.get(key, 0) >= val:
                    return
                engobj.wait_ge(sem, val)
                waited[key] = val

            for o in self.ops[e]:
                for d in o.deps:
                    wait_for(d)
                if o.dma and o.prev is not None:
                    wait_for(o.prev)
                ins = o.fn(engobj)
                if o.dma:
                    ins.then_inc(dsem[o.sem], 16)
                elif o.has_dep:
                    ins.then_inc(esem[e], 1)
            if extra:
                for d in extra:
                    wait_for(d)

        @block.tensor
        def _(eng):
            run("pe", eng)

        @block.scalar
        def _(eng):
            run("act", eng)

        @block.vector
        def _(eng):
            run("dve", eng)

        @block.gpsimd
        def _(eng):
            run("pool", eng)

        @block.sync
        def _(eng):
            run("sp", eng, extra=list(final_waits))


class Buf:
    _n = 0

    def __init__(self, ap, name=None):
        Buf._n += 1
        self.ap = ap
        self.key = "%s#%d" % (name or "b", Buf._n)


def _keys(xs):
    out = []
    for x in xs:
        out.append(x.key if isinstance(x, Buf) else x)
    return out


_DT_SIZE = {F32: 4, BF16: 2, I32: 4, U8: 1}


class Arena:
    def __init__(self, t, nbytes):
        self.t = t
        self.n = nbytes
        self.off = 0

    def reset(self, off=0):
        self.off = off

    def alloc(self, shape, dt, name=None):
        n = 1
        for s in shape:
            n *= s
        nb = n * _DT_SIZE[dt]
        nb_al = (nb + 63) // 64 * 64
        assert self.off + nb_al <= self.n, ("arena overflow", name, self.off, nb_al, self.n)
        ap = self.t[:, self.off:self.off + nb].bitcast(dt)
        self.off += nb_al
        if len(shape) == 2:
            ap = ap.rearrange("p (a b) -> p a b", a=shape[0], b=shape[1])
        elif len(shape) == 3:
            ap = ap.rearrange("p (a b c) -> p a b c", a=shape[0], b=shape[1], c=shape[2])
        return Buf(ap, name)


def build(debug=False, stop_after=None):
    nc = bass.Bass("TRN2", target_bir_lowering=False)

    def din(name, shape, dt=F32):
        return nc.dram_tensor(name, list(shape), dt, kind="ExternalInput").ap()

    dbg_kind = "ExternalOutput" if debug else "Internal"

    def dscr(name, shape, dt):
        return nc.dram_tensor(name, list(shape), dt, kind=dbg_kind).ap()

    xT = din("xT", [128, 8, S_LEN])
    xTo = din("xTo", [128, 8, NQ])
    xo = din("xo", [NQ, D])
    pos_all = din("pos_all", [1, S_LEN], I32)
    pos_own = din("pos_own", [1, NQ], I32)
    wk = din("wk", [128, 8, NKC])
    wq = din("wq", [128, 8, NQC])
    wiw = din("wiw", [128, 8, 4])
    wbd = din("wbd", [64, 8, D])
    wbf = din("wbf", [64, 8, D])
    wo = din("wo", [128, 8, D])
    cvec = din("cvec", [128, 64])
    fgain = din("fgain", [1, D])
    identf = din("identf", [128, 128])
    bmaskf = din("bmaskf", [128, 32])
    cbiasf = din("cbiasf", [128, 512])
    y = nc.dram_tensor("y", [NQ, D], F32, kind="ExternalOutput").ap()

    kf_s = dscr("kf_s", [8, 68, S_LEN], BF16)
    ka_s = dscr("ka_s", [8, 64, S_LEN], BF16)
    ki_s = dscr("ki_s", [64, S_LEN], BF16)
    vf_s = dscr("vf_s", [8, 128, 64, 128], BF16)
    va_s = dscr("va_s", [8, 128, 64, 128], BF16)
    wq_s = nc.dram_tensor("wq_s", [128, 8, NQC], BF16, kind="Internal").ap()
    wbd_s = nc.dram_tensor("wbd_s", [64, 8, D], BF16, kind="Internal").ap()
    wbf_s = nc.dram_tensor("wbf_s", [64, 8, D], BF16, kind="Internal").ap()
    wo_s = nc.dram_tensor("wo_s", [128, 8, D], BF16, kind="Internal").ap()
    dbg = {}
    if debug:
        dbg["qfT"] = nc.dram_tensor("d_qfT", [128, 8, 512], BF16, kind="ExternalOutput").ap()
        dbg["qaT"] = nc.dram_tensor("d_qaT", [128, 8, 512], BF16, kind="ExternalOutput").ap()
        dbg["qiT"] = nc.dram_tensor("d_qiT", [128, 4, 512], BF16, kind="ExternalOutput").ap()
        dbg["sc"] = nc.dram_tensor("d_sc", [128, 8192], F32, kind="ExternalOutput").ap()
        dbg["lo"] = nc.dram_tensor("d_lo", [4, 128, 1], F32, kind="ExternalOutput").ap()
        dbg["mT"] = nc.dram_tensor("d_mT", [128, 64, 512], U8, kind="ExternalOutput").ap()
        dbg["yTa"] = nc.dram_tensor("d_yTa", [128, 8, 512], BF16, kind="ExternalOutput").ap()
        dbg["yTf"] = nc.dram_tensor("d_yTf", [128, 8, 512], BF16, kind="ExternalOutput").ap()

    st = contextlib.ExitStack()
    with st:
        def T(name, shape, dt):
            return Buf(st.enter_context(nc.sbuf_tensor(name, list(shape), dt))[:], name)

        arena_t = st.enter_context(nc.sbuf_tensor("arena", [128, ARENA], U8))
        AR = Arena(arena_t, ARENA)
        ps = [Buf(st.enter_context(nc.psum_tensor("ps%d" % i, [128, 512], F32))[:], "ps%d" % i) for i in range(8)]

        S = Sched(nc)
        S.alloc_sems(st)

        def DMA(q, out, in_, R=(), W=()):
            return S.op(q, lambda e: e.dma_start(out=out, in_=in_), _keys(R), _keys(W), dma=True)

        def TT(eng, out, in0, in1, op, R=(), W=()):
            return S.op(eng, lambda e: e.tensor_tensor(out=out, in0=in0, in1=in1, op=op), _keys(R), _keys(W))

        def TS(eng, out, in0, s1, op0, s2=None, op1=None, R=(), W=(), accum=None):
            def f(e):
                kw = dict(out=out, in0=in0, scalar1=s1, scalar2=s2, op0=op0)
                if op1 is not None:
                    kw["op1"] = op1
                if accum is not None:
                    kw["accum_out"] = accum
                return e.tensor_scalar(**kw)
            return S.op(eng, f, _keys(R), _keys(W))

        def STT(out, in0, scalar, in1, op0, op1, R=(), W=()):
            return S.op("dve", lambda e: e.scalar_tensor_tensor(out=out, in0=in0, scalar=scalar, in1=in1, op0=op0, op1=op1),
                        _keys(R), _keys(W))

        def ACT(out, in_, func, R=(), W=(), bias=None, scale=None, accum=None):
            def f(e):
                kw = dict(out=out, in_=in_, func=func)
                if bias is not None:
                    kw["bias"] = bias
                if scale is not None:
                    kw["scale"] = scale
                if accum is not None:
                    kw["accum_out"] = accum
                return e.activation(**kw)
            return S.op("act", f, _keys(R), _keys(W))

        def CP(eng, out, in_, R=(), W=()):
            return S.op(eng, lambda e: e.tensor_copy(out=out, in_=in_), _keys(R), _keys(W))

        def MS(eng, ap, val, W=()):
            return S.op(eng, lambda e: e.memset(ap, val), (), _keys(W))

        def MM(out, pairs, R=(), W=()):
            def f(e):
                ins = None
                n = len(pairs)
                for i, (l, r) in enumerate(pairs):
                    ins = e.matmul(out, lhsT=l, rhs=r, start=(i == 0), stop=(i == n - 1))
                return ins
            return S.op("pe", f, _keys(R), _keys(W))

        ones_bf = T("ones_bf", [128, 512], BF16)
        onesf = T("onesf", [128, 512], F32)
        ident = T("ident", [128, 128], BF16)
        bmask = T("bmask", [128, 32], BF16)
        cbias = T("cbias", [128, 512], F32)
        cv = T("cv", [128, 64], F32)
        nbf = T("nbf", [128, 1], F32)
        halfpi = T("halfpi", [128, 1], F32)
        wiwb = T("wiwb", [128, 8, 4], BF16)
        fg_bc = T("fg_bc", [128, D], F32)
        rcol_q = T("rcol_q", [128, 4], F32)
        absw = T("absw", [128, 16], F32)
        sgnw = T("sgnw", [128, 16], F32)
        w4 = T("w4", [128, 16], F32)
        rcol_as = [T("rcol_a0", [128, 4], F32), T("rcol_a1", [128, 4], F32)]
        bis_lo = T("bis_lo", [128, 1], F32)
        bis_mid = T("bis_mid", [128, 1], F32)
        bis_cnt = T("bis_cnt", [128, 1], F32)
        bis_ind = T("bis_ind", [128, 1], F32)
        bis_nb = T("bis_nb", [128, 1], F32)
        bis_ca = T("bis_ca", [128, 1], F32)
        bis_tot = T("bis_tot", [128, 1], F32)
        fin_ss = T("fin_ss", [128, 1], F32)
        fin_r = T("fin_r", [128, 1], F32)

        GAIN = lambda c: cv.ap[:, c:c + 1]
        INVF = cv.ap[:, 8:9]
        SGNS = cv.ap[:, 9:10]
        BMRG = lambda c: cv.ap[:, 16 + c:17 + c]

        MS("dve", ones_bf.ap, 1.0, W=[ones_bf])
        MS("dve", onesf.ap, 1.0, W=[onesf])
        MS("dve", halfpi.ap, math.pi / 2, W=[halfpi])
        DMA("sp", cv.ap, cvec[:, :], W=[cv])
        DMA("sp", cbias.ap, cbiasf[:, :], W=[cbias])
        DMA("pool", ident.ap, identf[:, :], W=[ident])
        DMA("pool", bmask.ap, bmaskf[:, :], W=[bmask])
        DMA("pool", wiwb.ap, wiw[:, :, :], W=[wiwb])
        DMA("sp", fg_bc.ap, fgain[0:1, :].to_broadcast([128, D]), W=[fg_bc])
        TS("dve", nbf.ap, cv.ap[:, 10:11], -1.0, ALU.mult, R=[cv], W=[nbf])

        def load_x_dma(src, t0, xf):
            DMA("sp", xf.ap, src[:, :, t0:t0 + 512], W=[xf])

        def load_x_prep(xf, xb, xsq):
            ACT(xsq.ap, xf.ap, AF.Square, R=[xf], W=[xsq])
            for c in range(8):
                ACT(xb.ap[:, c, :], xf.ap[:, c, :], AF.Copy, R=[xf, cv], W=[xb], scale=GAIN(c))

        def load_x_group(src, t0, xf, xb, xsq):
            load_x_dma(src, t0, xf)
            load_x_prep(xf, xb, xsq)

        def rstd_gen(xsq, rbc, rcol, pA, pB):
            MM(pA.ap, [(ones_bf.ap[:, 0:128], xsq.ap[:, c, :]) for c in range(8)], R=[xsq, ones_bf], W=[pA])
            yield
            for tb in range(4):
                MM(pB.ap[:, tb:tb + 1], [(xsq.ap[:, c, tb * 128:(tb + 1) * 128], ones_bf.ap[:, 0:1]) for c in range(8)],
                   R=[xsq, ones_bf], W=[pB])
            yield
            TS("dve", rcol.ap, pB.ap[:, 0:4], 1.0 / D, ALU.mult, EPS, ALU.add, R=[pB], W=[rcol])
            TS("dve", rbc.ap, pA.ap, 1.0 / D, ALU.mult, EPS, ALU.add, R=[pA], W=[rbc])
            yield
            ACT(rcol.ap, rcol.ap, AF.Sqrt, R=[rcol], W=[rcol])
            ACT(rbc.ap, rbc.ap, AF.Sqrt, R=[rbc], W=[rbc])
            yield
            S.op("dve", lambda e: e.reciprocal(out=rcol.ap, in_=rcol.ap), _keys([rcol]), _keys([rcol]))
            yield
            S.op("dve", lambda e: e.reciprocal(out=rbc.ap, in_=rbc.ap), _keys([rbc]), _keys([rbc]))
            yield

        def rstd_from(xsq, xb_unused, rbc, rcol, pA, pB, scale_extra=1.0):
            for _ in rstd_gen(xsq, rbc, rcol, pA, pB):
                pass

        def rope_gen(possrc, t0, posi, ang, kk, sn, cs, rbc, scale_extra):
            DMA("sp", posi.ap, possrc[0:1, t0:t0 + 512].to_broadcast([128, 512]), W=[posi])
            CP("dve", ang.ap, posi.ap, R=[posi], W=[ang])
            yield
            TS("dve", ang.ap, ang.ap, INVF, ALU.mult, R=[ang, cv], W=[ang])
            yield
            TS("dve", kk.ap, ang.ap, 1.0 / (2.0 * math.pi), ALU.mult, MAGIC, ALU.add, R=[ang], W=[kk])
            yield
            TS("dve", kk.ap, kk.ap, -MAGIC, ALU.add, R=[kk], W=[kk])
            yield
            STT(ang.ap, kk.ap, -C1, ang.ap, ALU.mult, ALU.add, R=[kk, ang], W=[ang])
            yield
            STT(ang.ap, kk.ap, -C2, ang.ap, ALU.mult, ALU.add, R=[kk, ang], W=[ang])
            yield
            TS("dve", ang.ap, ang.ap, math.pi, ALU.min, -math.pi, ALU.max, R=[ang], W=[ang])
            yield
            STT(kk.ap, ang.ap, -1.0, ang.ap, ALU.mult, ALU.max, R=[ang], W=[kk])
            ACT(sn.ap, ang.ap, AF.Sin, R=[ang], W=[sn])
            ACT(cs.ap, kk.ap, AF.Sin, R=[kk, halfpi], W=[cs], bias=halfpi.ap[:, 0:1], scale=-1.0)
            yield
            STT(sn.ap, sn.ap, SGNS, rbc.ap, ALU.mult, ALU.mult, R=[sn, cv, rbc], W=[sn])
            yield
            if scale_extra != 1.0:
                TS("dve", sn.ap, sn.ap, scale_extra, ALU.mult, R=[sn], W=[sn])
                STT(cs.ap, cs.ap, scale_extra, rbc.ap, ALU.mult, ALU.mult, R=[cs, rbc], W=[cs])
            else:
                TT("dve", cs.ap, cs.ap, rbc.ap, ALU.mult, R=[cs, rbc], W=[cs])
            yield

        def rope_tables(possrc, t0, posi, ang, kk, sn, cs, rbc, scale_extra):
            for _ in rope_gen(possrc, t0, posi, ang, kk, sn, cs, rbc, scale_extra):
                pass

        AR.reset(0)
        wkb = AR.alloc([8, NKC], BF16, "wkb")
        xfs = [AR.alloc([8, 512], F32, "xf") for _ in range(2)]
        xbs = [AR.alloc([8, 512], BF16, "xb") for _ in range(2)]
        xsq1 = AR.alloc([8, 512], BF16, "xsq")
        posi = AR.alloc([512], I32, "posi")
        ang = AR.alloc([512], F32, "ang")
        kk = AR.alloc([512], F32, "kk")
        sns = [AR.alloc([512], F32, "sn") for _ in range(2)]
        css = [AR.alloc([512], F32, "cs") for _ in range(2)]
        rbcs = [AR.alloc([512], F32, "rbc") for _ in range(2)]
        t1s = [AR.alloc([512], F32, "t1") for _ in range(2)]
        t2s = [AR.alloc([512], F32, "t2") for _ in range(2)]
        ksts = [AR.alloc([512], BF16, "kst") for _ in range(2)]
        mf = AR.alloc([512], F32, "mf")
        kist = AR.alloc([512], BF16, "kist")
        ee = AR.alloc([512], F32, "ee")
        ncums = [AR.alloc([512], F32, "ncum") for _ in range(2)]
        r1s = AR.alloc([512], F32, "r1s")
        aug = AR.alloc([3, 512], BF16, "aug")
        vsts = [AR.alloc([8, 4, 128], BF16, "vst") for _ in range(2)]

        for i in range(4):
            DMA("pool", wkb.ap[:, :, i * 704:(i + 1) * 704], wk[:, :, i * 704:(i + 1) * 704], W=[wkb])
        for v in vsts:
            MS("pool", v.ap, 1.0, W=[v])
        n_tg = S_LEN // 512
        if stop_after == "A1":
            n_tg = 1
        if stop_after == "A0":
            n_tg = 0
        kst_i = 0
        def chain_gen(tgn):
            k = tgn % 2
            for _ in rstd_gen(xsq1, rbcs[k], rcol_as[k], ps[0], ps[1]):
                yield
            for _ in rope_gen(pos_all, tgn * 512, posi, ang, kk, sns[k], css[k], rbcs[k], 1.0):
                yield

        if n_tg > 0:
            load_x_dma(xT, 0, xfs[0])
            load_x_prep(xfs[0], xbs[0], xsq1)
            for _ in chain_gen(0):
                pass
        for tg in range(n_tg):
            t0 = tg * 512
            xb = xbs[tg % 2]
            rbc = rbcs[tg % 2]
            rcol_a = rcol_as[tg % 2]
            sn = sns[tg % 2]
            cs = css[tg % 2]
            if tg + 1 < n_tg:
                load_x_dma(xT, t0 + 512, xfs[(tg + 1) % 2])
            if tg == 1 or (n_tg == 1 and tg == 0):
                for i in range(NQC // 512):
                    DMA("pool", wq_s[:, :, i * 512:(i + 1) * 512], wq[:, :, i * 512:(i + 1) * 512], W=["wq_s"])
                for i in range(2):
                    DMA("pool", wbd_s[:, :, i * 512:(i + 1) * 512], wbd[:, :, i * 512:(i + 1) * 512], W=["wbd_s"])
                    DMA("pool", wbf_s[:, :, i * 512:(i + 1) * 512], wbf[:, :, i * 512:(i + 1) * 512], W=["wbf_s"])
                    DMA("pool", wo_s[:, :, i * 512:(i + 1) * 512], wo[:, :, i * 512:(i + 1) * 512], W=["wo_s"])


            def proj_chunk(pbuf, n0):
                MM(pbuf.ap, [(wkb.ap[:, c, n0:n0 + 128], xb.ap[:, c, :]) for c in range(8)], R=[wkb, xb], W=[pbuf])

            for cc in range(4):
                pb = ps[2 + (cc % 2)]
                proj_chunk(pb, cc * 128)
                kst = ksts[kst_i % 2]
                kst_i += 1
                TT("dve", kst.ap, pb.ap, rbc.ap, ALU.mult, R=[pb, rbc], W=[kst])
                DMA("sp", kf_s[2 * cc, 0:64, t0:t0 + 512], kst.ap[0:64, :], R=[kst], W=["kf_s"])
                DMA("sp", kf_s[2 * cc + 1, 0:64, t0:t0 + 512], kst.ap[64:128, :], R=[kst], W=["kf_s"])
            DMA("sp", kf_s[:, 64, t0:t0 + 512], ones_bf.ap[0:8, :], R=[ones_bf], W=["kf_s"])
            for br in range(2):
                vst = vsts[br]
                dst = vf_s if br == 0 else va_s
                n0 = 1792 + br * 512
                for tb in range(4):
                    pv = ps[6 + (tb % 2)]
                    MM(pv.ap, [(xb.ap[:, c, tb * 128:(tb + 1) * 128], wkb.ap[:, c, n0:n0 + 512]) for c in range(8)],
                       R=[xb, wkb], W=[pv])
                    ACT(vst.ap[:, :, tb, 0:64], pv.ap.rearrange("p (h c) -> p h c", c=64), AF.Copy,
                        R=[pv, rcol_a], W=[vst], scale=rcol_a.ap[:, tb:tb + 1])
                for h in range(8):
                    DMA("sp", dst[h, :, tg * 4:(tg + 1) * 4, :], vst.ap[:, h, :, :], R=[vst], W=["v_s%d" % br])

            ahead = None
            if tg + 1 < n_tg:
                load_x_prep(xfs[(tg + 1) % 2], xbs[(tg + 1) % 2], xsq1)
                ahead = chain_gen(tg + 1)

            def adv(n):
                if ahead is not None:
                    for _ in range(n):
                        try:
                            next(ahead)
                        except StopIteration:
                            break

            for cc in range(5):
                pa, pb = (ps[4], ps[5]) if cc % 2 == 0 else (ps[2], ps[3])
                if cc < 4:
                    proj_chunk(pa, 512 + cc * 128)
                    proj_chunk(pb, 1024 + cc * 128)
                else:
                    proj_chunk(pa, 1536)
                    proj_chunk(pb, 1664)
                adv(2)
                t1 = t1s[cc % 2]
                t2 = t2s[cc % 2]
                TT("dve", t1.ap, pa.ap, cs.ap, ALU.mult, R=[pa, cs], W=[t1])
                adv(1)
                TT("dve", t2.ap, pb.ap, sn.ap, ALU.mult, R=[pb, sn], W=[t2])
                adv(1)
                if cc < 4:
                    kst = ksts[kst_i % 2]
                    kst_i += 1
                    TT("pool", kst.ap, t1.ap, t2.ap, ALU.add, R=[t1, t2], W=[kst])
                    DMA("sp", ka_s[2 * cc, :, t0:t0 + 512], kst.ap[0:64, :], R=[kst], W=["ka_s"])
                    DMA("sp", ka_s[2 * cc + 1, :, t0:t0 + 512], kst.ap[64:128, :], R=[kst], W=["ka_s"])
                else:
                    TT("pool", mf.ap, t1.ap, t2.ap, ALU.add, R=[t1, t2], W=[mf])
                    CP("pool", kist.ap[0:64, :], mf.ap[0:64, :], R=[mf], W=[kist])
                    DMA("sp", ki_s[:, t0:t0 + 512], kist.ap[0:64, :], R=[kist], W=["ki_s"])
                    ACT(ee.ap[96:104, :], mf.ap[96:104, :], AF.Exp, R=[mf, nbf], W=[ee], bias=nbf.ap[96:104, 0:1], scale=-1.0)
                    ACT(ee.ap[96:104, :], ee.ap[96:104, :], AF.Ln, R=[ee], W=[ee], bias=onesf.ap[96:104, 0:1], scale=1.0)
                    nc_cur = ncums[tg % 2]
                    nc_prev = ncums[(tg + 1) % 2]
                    init = 0.0 if tg == 0 else nc_prev.ap[96:104, 511:512]
                    S.op("dve", (lambda o_, d1_, i_: (lambda e: e.tensor_tensor_scan(
                        out=o_, data0=onesf.ap[96:104, :], data1=d1_, initial=i_, op0=ALU.mult, op1=ALU.add)))(
                        nc_cur.ap[96:104, :], ee.ap[96:104, :], init),
                        _keys([ee, onesf, nc_prev]), _keys([nc_cur]))
                    CP("dve", aug.ap[96:104, 0, :], nc_cur.ap[96:104, :], R=[nc_cur], W=[aug])
                    TT("dve", r1s.ap[96:104, :], nc_cur.ap[96:104, :], aug.ap[96:104, 0, :], ALU.subtract, R=[nc_cur, aug], W=[r1s])
                    CP("dve", aug.ap[96:104, 1, :], r1s.ap[96:104, :], R=[r1s], W=[aug])
                    TT("dve", r1s.ap[96:104, :], r1s.ap[96:104, :], aug.ap[96:104, 1, :], ALU.subtract, R=[r1s, aug], W=[r1s])
                    CP("dve", aug.ap[96:104, 2, :], r1s.ap[96:104, :], R=[r1s], W=[aug])
                    DMA("sp", kf_s[:, 65:68, t0:t0 + 512], aug.ap[96:104, :, :], R=[aug], W=["kf_s"])
            adv(1000)
        n_groups = NG
        if stop_after in ("A", "A1", "A0"):
            n_groups = 0
        elif stop_after is not None and stop_after.startswith("B"):
            n_groups = 1
        for g in range(n_groups):
            S.barrier()
            q0 = g * GQ
            win0 = 2048 * g
            L = 2048 * (g + 1)
            nkb = L // 128
            AR.reset(0)
            qfT = AR.alloc([8, 512], BF16, "qfT")
            qaT = AR.alloc([8, 512], BF16, "qaT")
            qiT = AR.alloc([4, 512], BF16, "qiT")
            xqb = AR.alloc([8, 512], BF16, "xqb")
            rbq = AR.alloc([512], F32, "rbq")
            mT = AR.alloc([64, 512], U8, "mT")
            yTa = AR.alloc([8, 512], BF16, "yTa")
            yTf = AR.alloc([8, 512], BF16, "yTf")
            assert AR.off <= GPERS, AR.off

            AR.reset(GPERS)
            xqf = AR.alloc([8, 512], F32, "xqf")
            xsqq = AR.alloc([8, 512], BF16, "xsqq")
            wqps = [AR.alloc([8, 512], BF16, "wqp") for _ in range(2)]
            posi = AR.alloc([512], I32, "posi")
            ang = AR.alloc([512], F32, "ang")
            kk = AR.alloc([512], F32, "kk")
            sn = AR.alloc([512], F32, "sn")
            cs = AR.alloc([512], F32, "cs")
            rb8 = AR.alloc([512], F32, "rb8")
            t1s = [AR.alloc([512], F32, "t1") for _ in range(2)]
            t2s = [AR.alloc([512], F32, "t2") for _ in range(2)]
            craws = [AR.alloc([2048], BF16, "craw") for _ in range(2)]

            load_x_group(xTo, q0, xqf, xqb, xsqq)
            rstd_from(xsqq, xqb, rbq, rcol_q, ps[0], ps[1])
            TS("dve", rb8.ap, rbq.ap, 0.125, ALU.mult, R=[rbq], W=[rb8])
            qchain = rope_gen(pos_own, q0, posi, ang, kk, sn, cs, rbq, 0.125)

            def qadv(n):
                for _ in range(n):
                    try:
                        next(qchain)
                    except StopIteration:
                        break

            wq_i = [0]

            def load_wq(piece):
                wb = wqps[wq_i[0] % 2]
                wq_i[0] += 1
                DMA("sp", wb.ap, wq_s[:, :, piece * 512:(piece + 1) * 512], R=["wq_s"], W=[wb])
                return wb

            def qproj(pbuf, wb, n0):
                MM(pbuf.ap, [(wb.ap[:, c, n0:n0 + 128], xqb.ap[:, c, :]) for c in range(8)], R=[wb, xqb], W=[pbuf])

            MS("pool", qaT.ap[64:128, :, :], 0.0, W=[qaT])
            MS("pool", qfT.ap[64:128, :, :], 0.0, W=[qfT])
            MS("pool", qfT.ap[64:68, :, :], 1.0, W=[qfT])
            MS("pool", qiT.ap[64:128, :, :], 0.0, W=[qiT])
            wb = load_wq(0)
            for cc in range(4):
                pb = ps[2 + cc]
                qproj(pb, wb, cc * 128)
            for cc in range(4):
                pb = ps[2 + cc]
                TT("dve", qfT.ap[0:64, 2 * cc, :], pb.ap[0:64, :], rb8.ap[0:64, :], ALU.mult, R=[pb, rb8], W=[qfT])
                qadv(2)
                TT("dve", qfT.ap[0:64, 2 * cc + 1, :], pb.ap[64:128, :], rb8.ap[64:128, :], ALU.mult, R=[pb, rb8], W=[qfT])
                qadv(2)
            qadv(1000)
            for h in range(8):
                cr = craws[h % 2]
                DMA("sp", cr.ap[64:65, :], kf_s[h, 65:66, win0:win0 + 2048], R=["kf_s"], W=[cr])
                TS("dve", qfT.ap[64:65, h, :], cr.ap[64:65, :].rearrange("p (i r) -> p i r", r=4)[:, :, 0], -1.0, ALU.mult,
                   R=[cr], W=[qfT])
            wb1 = load_wq(1)
            wb2 = load_wq(2)
            for cc in range(4):
                pa, pb = ps[4 + 2 * (cc % 2)], ps[5 + 2 * (cc % 2)]
                qproj(pa, wb1, cc * 128)
                qproj(pb, wb2, cc * 128)
                t1 = t1s[cc % 2]
                t2 = t2s[cc % 2]
                TT("dve", t1.ap, pa.ap, cs.ap, ALU.mult, R=[pa, cs], W=[t1])
                TT("dve", t2.ap, pb.ap, sn.ap, ALU.mult, R=[pb, sn], W=[t2])
                TT("pool", qaT.ap[0:64, 2 * cc, :], t1.ap[0:64, :], t2.ap[0:64, :], ALU.add, R=[t1, t2], W=[qaT])
                TT("pool", qaT.ap[0:64, 2 * cc + 1, :], t1.ap[64:128, :], t2.ap[64:128, :], ALU.add, R=[t1, t2], W=[qaT])
            wb = load_wq(3)
            for cc in range(2):
                pa, pb = ps[4 + 2 * (cc % 2)], ps[5 + 2 * (cc % 2)]
                qproj(pa, wb, cc * 128)
                qproj(pb, wb, 256 + cc * 128)
                t1 = t1s[cc % 2]
                t2 = t2s[cc % 2]
                TT("dve", t1.ap, pa.ap, cs.ap, ALU.mult, R=[pa, cs], W=[t1])
                TT("dve", t2.ap, pb.ap, sn.ap, ALU.mult, R=[pb, sn], W=[t2])
                TT("pool", qiT.ap[0:64, 2 * cc, :], t1.ap[0:64, :], t2.ap[0:64, :], ALU.add, R=[t1, t2], W=[qiT])
                TT("pool", qiT.ap[0:64, 2 * cc + 1, :], t1.ap[64:128, :], t2.ap[64:128, :], ALU.add, R=[t1, t2], W=[qiT])
            for sb in range(4):
                MM(ps[1].ap[:, 8 + 4 * sb:12 + 4 * sb],
                   [(xqb.ap[:, c, sb * 128:(sb + 1) * 128], wiwb.ap[:, c, :]) for c in range(8)], R=[xqb, wiwb], W=[ps[1]])
                TS("dve", w4.ap[:, 4 * sb:4 * sb + 4], ps[1].ap[:, 8 + 4 * sb:12 + 4 * sb], rcol_q.ap[:, sb:sb + 1], ALU.mult,
                   0.5, ALU.mult, R=[ps[1], rcol_q], W=[w4])
            STT(absw.ap, w4.ap, -1.0, w4.ap, ALU.mult, ALU.max, R=[w4], W=[absw])
            TS("dve", sgnw.ap, w4.ap, 0.0, ALU.is_ge, 2.0, ALU.mult, R=[w4], W=[sgnw])
            TS("dve", sgnw.ap, sgnw.ap, -1.0, ALU.add, R=[sgnw], W=[sgnw])
            if debug and g == 0:
                DMA("sp", dbg["qfT"][:, :, :], qfT.ap, R=[qfT], W=["dbg1"])
                DMA("sp", dbg["qaT"][:, :, :], qaT.ap, R=[qaT], W=["dbg2"])
                DMA("sp", dbg["qiT"][:, :, :], qiT.ap, R=[qiT], W=["dbg3"])
            if stop_after == "B1":
                break

            S.barrier()
            AR.reset(GPERS)
            sc = AR.alloc([8192], F32, "sc")
            msk = AR.alloc([8192], BF16, "msk")
            rts = [AR.alloc([512], F32, "rt") for _ in range(2)]
            kits = [AR.alloc([2048], BF16, "kit") for _ in range(1)]
            f_kTps = [AR.alloc([2048], BF16, "kTp") for _ in range(2)]
            f_vps = [AR.alloc([16, 128], BF16, "vp") for _ in range(2)]
            f_Pts = [AR.alloc([512], BF16, "Pt") for _ in range(4)]
            f_recs = [AR.alloc([512], F32, "rec") for _ in range(1)]

            for kt_ in f_kTps + kits:
                MS("pool", kt_.ap[64:128, :], 0.0, W=[kt_])

            def attn_steps(br, kTps, vps, Pts, Pms, recs, psS, psO):
                tiles = [(h, pc, kb) for h in range(8) for pc in range(g + 1) for kb in range(16)]
                pend = []
                nS = len(psS)
                cur = {}
                piece_n = 0

                def issue_pv(item):
                    (h, pc, kb, pmat, vp, first, last, pc0) = item
                    po = psO[h % len(psO)]
                    S.op("pe", (lambda o_, l_, r_, f_, s_: (lambda e: e.matmul(o_, lhsT=l_, rhs=r_, start=f_, stop=s_)))(
                        po.ap[:, pc0:512], vp.ap[:, kb, :], pmat.ap[:, pc0:512], first, last), _keys([vp, pmat]), _keys([po]))
                    if last:
                        yT = yTa if br == 0 else yTf
                        rec = recs[h % len(recs)]
                        S.op("dve", (lambda o_, i_: (lambda e: e.reciprocal(out=o_, in_=i_)))(rec.ap[0:64, :], po.ap[64:128, :]),
                             _keys([po]), _keys([rec]))
                        TT("dve", yT.ap[0:64, h, :], po.ap[0:64, :], rec.ap[0:64, :], ALU.mult, R=[po, rec], W=[yT])

                for ti, (h, pc, kb) in enumerate(tiles):
                    if kb == 0:
                        kTp = kTps[piece_n % len(kTps)]
                        vp = vps[piece_n % len(vps)]
                        piece_n += 1
                        if br == 0:
                            DMA("sp", kTp.ap[0:64, :], ka_s[h, :, pc * 2048:(pc + 1) * 2048], R=["ka_s"], W=[kTp])
                            DMA("sp", vp.ap, va_s[h, :, pc * 16:(pc + 1) * 16, :], R=["v_s1"], W=[vp])
                        else:
                            DMA("sp", kTp.ap[0:68, :], kf_s[h, :, pc * 2048:(pc + 1) * 2048], R=["kf_s"], W=[kTp])
                            DMA("sp", vp.ap, vf_s[h, :, pc * 16:(pc + 1) * 16, :], R=["v_s0"], W=[vp])
                        cur["k"], cur["v"] = kTp, vp
                    kTp, vp = cur["k"], cur["v"]
                    pS = psS[ti % nS]
                    Pt = Pts[ti % len(Pts)]
                    diagc = (pc == g)
                    diag = (br == 1 and diagc)
                    c0 = 32 * kb if diagc else 0
                    if br == 0:
                        MM(pS.ap[:, c0:512], [(kTp.ap[:, kb * 128:(kb + 1) * 128], qaT.ap[:, h, c0:512])], R=[kTp, qaT], W=[pS])
                    else:
                        MM(pS.ap[:, c0:512], [(kTp.ap[:, kb * 128:(kb + 1) * 128], qfT.ap[:, h, c0:512])], R=[kTp, qfT], W=[pS])
                    ACT(Pt.ap[:, c0:512], pS.ap[:, c0:512], AF.Exp, R=[pS], W=[Pt])
                    if br == 0:
                        Pm = Pms[ti % len(Pms)]
                        TT("pool" if ti % 2 == 1 else "dve", Pm.ap[:, c0:512], Pt.ap[:, c0:512], mT.ap[:, pc * 16 + kb, c0:512], ALU.mult,
                           R=[Pt, mT], W=[Pm])
                        pmat = Pm
                    else:
                        if diag:
                            TT("pool", Pt.ap[:, c0:c0 + 32], Pt.ap[:, c0:c0 + 32], bmask.ap, ALU.mult, R=[Pt, bmask], W=[Pt])
                        pmat = Pt
                    pend.append((h, pc, kb, pmat, vp, (pc == 0 and kb == 0), (pc == g and kb == 15), c0))
                    if len(pend) > min(4, nS - 1):
                        issue_pv(pend.pop(0))
                    yield 1
                while pend:
                    issue_pv(pend.pop(0))

            def _b2_evac(h, sb, ch):
                pb = ps[h % 2]
                rt = rts[h % 2]
                ACT(rt.ap, pb.ap, AF.Relu, R=[pb, absw], W=[rt], scale=absw.ap[:, 4 * sb + h:4 * sb + h + 1])
                scs = sc.ap[:, ch * 512:(ch + 1) * 512]
                sg = sgnw.ap[:, 4 * sb + h:4 * sb + h + 1]
                if h == 0:
                    TS("dve", scs, rt.ap, sg, ALU.mult, R=[rt, sgnw], W=[sc])
                else:
                    STT(scs, rt.ap, sg, scs, ALU.mult, ALU.add, R=[rt, sgnw, sc], W=[sc])

            def b2_units():
                for sb in range(4):
                    nch = 4 * g + sb + 1
                    Lsb = 512 * nch
                    kit = kits[0]
                    for ch in range(nch):
                        if ch % 4 == 0:
                            pc = ch // 4
                            DMA("sp", kit.ap[0:64, :], ki_s[:, pc * 2048:(pc + 1) * 2048], R=["ki_s"], W=[kit])
                        for h in range(4):
                            pb = ps[h % 2]
                            MM(pb.ap, [(qiT.ap[:, h, sb * 128:(sb + 1) * 128], kit.ap[:, (ch % 4) * 512:(ch % 4 + 1) * 512])],
                               R=[qiT, kit], W=[pb])
                            if h % 2 == 0:
                                continue
                            for hh in (h - 1, h):
                                _b2_evac(hh, sb, ch)
                        for h in range(0):
                            pb = ps[h]
                            rt = rts[h % 2]
                            ACT(rt.ap, pb.ap, AF.Relu, R=[pb, absw], W=[rt], scale=absw.ap[:, 4 * sb + h:4 * sb + h + 1])
                            scs = sc.ap[:, ch * 512:(ch + 1) * 512]
                            sg = sgnw.ap[:, 4 * sb + h:4 * sb + h + 1]
                            if h == 0:
                                TS("dve", scs, rt.ap, sg, ALU.mult, R=[rt, sgnw], W=[sc])
                            else:
                                STT(scs, rt.ap, sg, scs, ALU.mult, ALU.add, R=[rt, sgnw, sc], W=[sc])
                        if ch == nch - 1:
                            TT("dve", sc.ap[:, ch * 512:(ch + 1) * 512], sc.ap[:, ch * 512:(ch + 1) * 512], cbias.ap, ALU.add,
                               R=[sc, cbias], W=[sc])
                        yield 3.2
                    if debug and g == 0 and sb == 3:
                        DMA("sp", dbg["sc"][:, 0:Lsb], sc.ap[:, 0:Lsb], R=[sc], W=["dbg4"])
                    MS("dve", bis_lo.ap, -RNG, W=[bis_lo])
                    La = 512 * int(0.4 * nch) if nch >= 3 else 0
                    Ld = Lsb - La
                    for it in range(NIT):
                        step = RNG / (2.0 ** it)
                        TS("dve", bis_mid.ap, bis_lo.ap, step, ALU.add, R=[bis_lo], W=[bis_mid])
                        if La > 0:
                            TS("dve", bis_nb.ap, bis_mid.ap, -1.0, ALU.mult, 2.0 ** -20, ALU.add, R=[bis_mid], W=[bis_nb])
                        TS("dve", msk.ap[:, 0:Ld], sc.ap[:, 0:Ld], bis_mid.ap[:, 0:1], ALU.is_ge, None, ALU.add,
                           R=[sc, bis_mid], W=[msk, bis_cnt], accum=bis_cnt.ap[:, 0:1])
                        if La > 0:
                            ACT(msk.ap[:, Ld:Lsb], sc.ap[:, Ld:Lsb], AF.Sign, R=[sc, bis_nb], W=["mskA", bis_ca],
                                bias=bis_nb.ap[:, 0:1], scale=1.0, accum=bis_ca.ap[:, 0:1])
                            STT(bis_tot.ap, bis_ca.ap, 0.5, bis_cnt.ap, ALU.mult, ALU.add, R=[bis_ca, bis_cnt], W=[bis_tot])
                            TS("dve", bis_ind.ap, bis_tot.ap, TOPK - La / 2.0, ALU.is_ge, step, ALU.mult, R=[bis_tot], W=[bis_ind])
                        else:
                            TS("dve", bis_ind.ap, bis_cnt.ap, TOPK, ALU.is_ge, step, ALU.mult, R=[bis_cnt], W=[bis_ind])
                        TT("dve", bis_lo.ap, bis_lo.ap, bis_ind.ap, ALU.add, R=[bis_lo, bis_ind], W=[bis_lo])
                        yield 0.7 + Ld * 1.05e-3
                    TS("dve", bis_ind.ap, bis_lo.ap, -RNG, ALU.is_le, -1000.0, ALU.mult, R=[bis_lo], W=[bis_ind])
                    TT("dve", bis_lo.ap, bis_lo.ap, bis_ind.ap, ALU.add, R=[bis_lo, bis_ind], W=[bis_lo])
                    TS("dve", msk.ap[:, 0:Lsb], sc.ap[:, 0:Lsb], bis_lo.ap[:, 0:1], ALU.is_ge, R=[sc, bis_lo], W=[msk, "mskA"])
                    if debug and g == 0:
                        DMA("sp", dbg["lo"][sb], bis_lo.ap, R=[bis_lo], W=["dbg5"])
                    yield Lsb * 0.6e-3
                    for k4 in range(Lsb // 512):
                        pt = ps[k4 % 2]
                        ptb = pt.ap.bitcast(BF16)
                        for i in range(4):
                            kb = k4 * 4 + i
                            S.op("pe", (lambda o_, i_: (lambda e: e.transpose(out=o_, in_=i_, identity=ident.ap)))(
                                ptb[:, i * 128:(i + 1) * 128], msk.ap[:, kb * 128:(kb + 1) * 128]),
                                _keys([msk, ident]), _keys([pt]))
                        ACT(mT.ap[:, k4 * 4:(k4 + 1) * 4, sb * 128:(sb + 1) * 128],
                            ptb[:, 0:512].rearrange("p (a b) -> p a b", b=128), AF.Copy, R=[pt], W=[mT])
                        yield 0.7
                    if Lsb // 128 < nkb:
                        MS("pool", mT.ap[:, Lsb // 128:nkb, sb * 128:(sb + 1) * 128], 0, W=[mT])

            fox = attn_steps(1, f_kTps, f_vps, f_Pts, None, f_recs, [ps[2], ps[3], ps[4], ps[5]], [ps[6], ps[7]])
            n_fox = 128 * (g + 1)
            units = list()
            tot_est = 0.0
            for sb in range(4):
                nch = 4 * g + sb + 1
                Lsb = 512 * nch
                La_ = 512 * int(0.4 * nch) if nch >= 3 else 0
                tot_est += nch * 3.2 + NIT * (0.7 + (Lsb - La_) * 1.05e-3) + Lsb * 0.6e-3 + (Lsb // 512) * 0.7
            rate = n_fox / (0.9 * tot_est)
            acc = 0.0
            fox_done = False
            for wgt in b2_units():
                acc += wgt * rate
                while acc >= 1.0 and not fox_done:
                    acc -= 1.0
                    try:
                        next(fox)
                    except StopIteration:
                        fox_done = True
            if not fox_done:
                for _ in fox:
                    pass
            if debug and g == 0:
                DMA("sp", dbg["mT"][:, 0:nkb, :], mT.ap[:, 0:nkb, :], R=[mT], W=["dbg6"])
            if stop_after == "B2":
                break

            S.barrier()
            AR.reset(GPERS)
            kTps = [AR.alloc([2048], BF16, "kTp") for _ in range(3)]
            vps = [AR.alloc([16, 128], BF16, "vp") for _ in range(3)]
            Pts = [AR.alloc([512], BF16, "Pt") for _ in range(6)]
            Pms = [AR.alloc([512], BF16, "Pm") for _ in range(6)]
            recs = [AR.alloc([512], F32, "rec") for _ in range(2)]
            for kt_ in kTps:
                MS("pool", kt_.ap[64:128, :], 0.0, W=[kt_])
            for _ in attn_steps(0, kTps, vps, Pts, Pms, recs, [ps[0], ps[1], ps[2], ps[3], ps[6], ps[7]], [ps[4], ps[5]]):
                pass
            if debug and g == 0:
                DMA("sp", dbg["yTa"][:, :, :], yTa.ap, R=[yTa], W=["dbg7"])
                DMA("sp", dbg["yTf"][:, :, :], yTf.ap, R=[yTf], W=["dbg8"])
            if stop_after == "B3":
                break

            S.barrier()
            AR.reset(GPERS)
            wqps = [AR.alloc([8, 512], BF16, "wqp") for _ in range(2)]
            wbrs = [AR.alloc([8, 128], BF16, "wbr") for _ in range(2)]
            wobs = [AR.alloc([8, 512], BF16, "wob") for _ in range(2)]
            ygs = [AR.alloc([8, 512], BF16, "yg") for _ in range(2)]
            mrg = AR.alloc([8, 512], BF16, "mrg")
            gtmp = AR.alloc([512], F32, "gtmp")
            gbf = AR.alloc([512], BF16, "gbf")
            gms4 = [AR.alloc([512], F32, "gm") for _ in range(2)]
            e1s = [AR.alloc([512], F32, "e1") for _ in range(1)]
            e2s = [AR.alloc([512], F32, "e2") for _ in range(1)]
            xot = AR.alloc([D], F32, "xot")
            xnew = AR.alloc([D], F32, "xnew")
            wq_i = [0]
            for br in range(2):
                wb = load_wq(4 + br)
                yT = yTa if br == 0 else yTf
                for cc in range(4):
                    pb = ps[cc % 2]
                    qproj(pb, wb, cc * 128)
                    TT("dve", gtmp.ap, pb.ap, rbq.ap, ALU.mult, R=[pb, rbq], W=[gtmp])
                    for hh in range(2):
                        h = 2 * cc + hh
                        ACT(gbf.ap[0:64, :], gtmp.ap[64 * hh:64 * hh + 64, :], AF.Silu, R=[gtmp], W=[gbf])
                        TT("pool", ygs[br].ap[0:64, h, :], yT.ap[0:64, h, :], gbf.ap[0:64, :], ALU.mult, R=[yT, gbf], W=[ygs[br]])
            wbr_n = [0]
            wqm = {}
            for hf in range(2):
                DMA("sp", wobs[hf].ap, wo_s[:, :, hf * 512:(hf + 1) * 512], R=["wo_s"], W=[wobs[hf]])
            for dc in range(8):
                pus = []
                par = dc % 2
                gms = [gms4[0], gms4[1]]
                e1, e2 = e1s[0], e2s[0]
                for br in range(2):
                    wsrc = wbd_s if br == 0 else wbf_s
                    wbr = wbrs[wbr_n[0] % 2]
                    wbr_n[0] += 1
                    DMA("sp", wbr.ap[0:64, :, :], wsrc[:, :, dc * 128:(dc + 1) * 128], R=["wbd_s", "wbf_s"], W=[wbr])
                    pu = ps[4 * par + br]
                    MM(pu.ap, [(wbr.ap[0:64, h, :], ygs[br].ap[0:64, h, :]) for h in range(8)], R=[wbr, ygs[br]], W=[pu])
                    pus.append(pu)
                    mcol = br * 1024 + dc * 128
                    piece = 6 + mcol // 512
                    if wqm.get(br, (None, None))[0] != piece:
                        wqm[br] = (piece, load_wq(piece))
                    wbm = wqm[br][1]
                    pm = ps[4 * par + 2 + br]
                    qproj(pm, wbm, mcol % 512)
                    TT("dve", gms[br].ap, pm.ap, rbq.ap, ALU.mult, R=[pm, rbq], W=[gms[br]])
                    ACT(gms[br].ap, gms[br].ap, AF.Sigmoid, R=[gms[br], cv], W=[gms[br]], bias=BMRG(br * 8 + dc))
                TT("dve", e1.ap, pus[0].ap, gms[0].ap, ALU.mult, R=[pus[0], gms[0]], W=[e1])
                TT("dve", e2.ap, pus[1].ap, gms[1].ap, ALU.mult, R=[pus[1], gms[1]], W=[e2])
                TT("pool", mrg.ap[:, dc, :], e1.ap, e2.ap, ALU.add, R=[e1, e2], W=[mrg])
            for sb in range(4):
                r0 = q0 + sb * 128
                DMA("sp", xot.ap, xo[r0:r0 + 128, :], W=[xot])
                for hf in range(2):
                    wob = wobs[hf]
                    po = ps[(2 * sb + hf) % 8]
                    MM(po.ap, [(mrg.ap[:, dc, sb * 128:(sb + 1) * 128], wob.ap[:, dc, :]) for dc in range(8)], R=[mrg, wob], W=[po])
                    TT("dve", xnew.ap[:, hf * 512:(hf + 1) * 512], po.ap, xot.ap[:, hf * 512:(hf + 1) * 512], ALU.add,
                       R=[po, xot], W=[xnew])
                ACT(xot.ap, xnew.ap, AF.Square, R=[xnew, xot], W=[xot, fin_ss], accum=fin_ss.ap[:, 0:1])
                TS("dve", fin_r.ap, fin_ss.ap, 1.0 / D, ALU.mult, EPS, ALU.add, R=[fin_ss], W=[fin_r])
                ACT(fin_r.ap, fin_r.ap, AF.Sqrt, R=[fin_r], W=[fin_r])
                S.op("dve", lambda e: e.reciprocal(out=fin_r.ap, in_=fin_r.ap), _keys([fin_r]), _keys([fin_r]))
                STT(xnew.ap, xnew.ap, fin_r.ap[:, 0:1], fg_bc.ap, ALU.mult, ALU.mult, R=[xnew, fin_r, fg_bc], W=[xnew])
                DMA("sp", y[r0:r0 + 128, :], xnew.ap, R=[xnew], W=["y"])

        fw = [o for o in S.dlast if o is not None]
        with nc.Block() as block:
            S.emit(block, final_waits=fw)
    return nc


def _rot_perm():
    p = np.arange(64)
    p[0:8] = np.arange(8, 16)
    p[8:16] = np.arange(0, 8)
    return p


def _fm(w):
    n = w.shape[1]
    return np.ascontiguousarray(w.reshape(8, 128, n).transpose(1, 0, 2))


def prep_inputs(x, positions, norm_gain, w_in, b_forget, b_merge, w_branch_dsa, w_branch_fox, w_out, final_gain):
    x = np.asarray(x, np.float32)
    positions = np.asarray(positions, np.int32)
    W = np.asarray(w_in, np.float32)[0]
    o = 0
    cols = {}
    for name, n in (("aq", 512), ("ak", 512), ("av", 512), ("ag", 512), ("iq", 256), ("ik", 64), ("iw", 4),
                    ("fq", 512), ("fk", 512), ("fv", 512), ("fg", 512), ("fl", 8), ("mg", 2048)):
        cols[name] = W[:, o:o + n]
        o += n
    perm = _rot_perm()

    def rot(w, nh):
        idx = np.concatenate([h * 64 + perm for h in range(nh)])
        return w[:, idx]

    z32 = np.zeros((D, 32), np.float32)
    z24 = np.zeros((D, 24), np.float32)
    z64 = np.zeros((D, 64), np.float32)
    wk = np.concatenate([cols["fk"], cols["ak"], rot(cols["ak"], 8),
                         cols["ik"], z32, cols["fl"], z24, rot(cols["ik"], 1), z64,
                         cols["fv"], cols["av"]], axis=1)
    assert wk.shape[1] == NKC
    wq = np.concatenate([cols["fq"], cols["aq"], rot(cols["aq"], 8), cols["iq"], rot(cols["iq"], 4),
                         cols["ag"], cols["fg"], cols["mg"]], axis=1)
    assert wq.shape[1] == NQC
    wk_d, wq_d, wiw_d = _fm(wk), _fm(wq), _fm(np.ascontiguousarray(cols["iw"]))
    wbd = np.ascontiguousarray(np.asarray(w_branch_dsa, np.float32)[0].reshape(8, 64, D).transpose(1, 0, 2))
    wbf = np.ascontiguousarray(np.asarray(w_branch_fox, np.float32)[0].reshape(8, 64, D).transpose(1, 0, 2))
    wo = _fm(np.asarray(w_out, np.float32)[0])
    cvec = np.zeros((128, 64), np.float32)
    cvec[:, 0:8] = np.asarray(norm_gain, np.float32)[0].reshape(8, 128).T
    half = 8
    inv_freq = (500000.0 ** (-np.arange(half, dtype=np.float32) * 2.0 / 16.0)).astype(np.float32)
    for p in range(128):
        r = p % 64
        if r < 8:
            cvec[p, 8] = inv_freq[r]
            cvec[p, 9] = -1.0
        elif r < 16:
            cvec[p, 8] = inv_freq[r - 8]
            cvec[p, 9] = 1.0
    cvec[96:104, 10] = np.asarray(b_forget, np.float32)[0]
    cvec[:, 16:32] = np.asarray(b_merge, np.float32)[0].reshape(16, 128).T
    fgain = np.asarray(final_gain, np.float32).reshape(1, D)
    identf = np.eye(128, dtype=np.float32)
    in_maps = []
    xT_b = [np.ascontiguousarray(x[b].T.reshape(8, 128, S_LEN).transpose(1, 0, 2)) for b in range(x.shape[0])]
    for c in range(8):
        b, j = divmod(c, 4)
        p_idx = np.arange(128)[:, None]
        m_idx = np.arange(32)[None, :]
        bmaskf = (p_idx <= 4 * m_idx + j).astype(np.float32)
        s_idx = np.arange(512)[None, :]
        cbiasf = np.where(s_idx <= 4 * p_idx + j, 0.0, NEGB).astype(np.float32)
        in_maps.append({
            "xT": xT_b[b],
            "xTo": np.ascontiguousarray(xT_b[b][:, :, j::4]),
            "xo": np.ascontiguousarray(x[b, j::4, :]),
            "pos_all": np.ascontiguousarray(positions[b][None, :]),
            "pos_own": np.ascontiguousarray(positions[b][None, j::4]),
            "wk": wk_d, "wq": wq_d, "wiw": wiw_d, "wbd": wbd, "wbf": wbf, "wo": wo,
            "cvec": cvec, "fgain": fgain, "identf": identf, "bmaskf": bmaskf, "cbiasf": cbiasf,
        })
    return in_maps


_NC_CACHE = {}


def kernel(x, positions, norm_gain, w_in, b_forget, b_merge, w_branch_dsa, w_branch_fox, w_out, final_gain):
    in_maps = prep_inputs(x, positions, norm_gain, w_in, b_forget, b_merge, w_branch_dsa, w_branch_fox, w_out, final_gain)
    if "nc" not in _NC_CACHE:
        _NC_CACHE["nc"] = build()
    nc = _NC_CACHE["nc"]
    res = run_bass_kernel_spmd(nc, in_maps, core_ids=list(range(8)))
    out = np.empty((2, S_LEN, D), np.float32)
    for c in range(8):
        b, j = divmod(c, 4)
        out[b, j::4, :] = res.results[c]["y"]
    return out
```
